# Optimizing a Trainium2 kernel written in Bass

```python
import math
import jax, jax.numpy as jnp
from jax import lax
import numpy as np

D_MODEL = 1024
BATCH = 4
SEQ = 4096
DEPTH = 2

MIX_WIDTH = D_MODEL
RWKV_WIDTH = MIX_WIDTH // 2
HEAD_SIZE = 64
RWKV_HEADS = RWKV_WIDTH // HEAD_SIZE
DECAY_LORA = 64
AAA_LORA = 64
MV_LORA = 32
GATE_LORA = 128
RWKV_COLS = 3 * RWKV_WIDTH + DECAY_LORA + AAA_LORA + GATE_LORA
POOL_WIDTH = MIX_WIDTH - RWKV_WIDTH
POOL_WINDOWS = (2, 4, 8, 16)
POOL_GROUP = POOL_WIDTH // len(POOL_WINDOWS)
IN_COLS = RWKV_COLS + POOL_WIDTH
GN_EPS = 64e-5
NORM_EPS = 1e-6
N_KEYS = 128
N_EXPERTS = N_KEYS * N_KEYS
PEER_HEADS = 8
PEER_QDIM = 256
PEER_HALF = PEER_QDIM // 2
PEER_TOPK = 16
TOK_BLOCK = 128

kernel_name = "hybrid_rwkv7_pool_peer_adaln"


def rmsnorm(x, g):
    x32 = x.astype(jnp.float32)
    y = x32 * lax.rsqrt(jnp.mean(x32 * x32, axis=-1, keepdims=True) + NORM_EPS)
    return (y * g.astype(jnp.float32)).astype(x.dtype)


def modulate(h, shift, scale):
    return h * (1 + scale[:, None, :]) + shift[:, None, :]


def token_shift(z, mu):
    prev = jnp.pad(z, ((0, 0), (1, 0), (0, 0)))[:, :-1]
    return z + (prev - z) * mu


def rwkv7_step(state, inp):
    r_t, w_t, k_t, v_t, kk_t, a_t = inp
    sa = jnp.einsum('bhvk,bhk->bhv', state, -kk_t)
    state = (state * w_t[:, :, None, :]
             + sa[..., None] * (kk_t * a_t)[:, :, None, :]
             + v_t[..., None] * k_t[:, :, None, :])
    y = jnp.einsum('bhvk,bhk->bhv', state, r_t)
    return state, y


def rwkv7_mix(r, k, v, wd, ad, gd, w0, w_up, a0, a_up, g_up, k_k, k_a, r_k, lnx_g, lnx_b):
    B, S, _ = r.shape
    f32 = jnp.float32
    w = -jax.nn.softplus(-(w0 + jnp.tanh(wd) @ w_up)) - 0.5
    decay = jnp.exp(-jnp.exp(w.astype(f32)))
    a = jax.nn.sigmoid(a0 + ad @ a_up)
    g = jax.nn.sigmoid(gd) @ g_up

    def heads(t):
        return t.reshape(B, S, RWKV_HEADS, HEAD_SIZE).astype(f32)

    kk = heads(k * k_k)
    kk = kk / jnp.maximum(jnp.sqrt(jnp.sum(kk * kk, axis=-1, keepdims=True)), 1e-12)
    k = k * (1 + (a - 1) * k_a)
    rh, kh, vh, ah, wh = heads(r), heads(k), heads(v), heads(a), heads(decay)
    tm = lambda t: jnp.moveaxis(t, 1, 0)
    s0 = jnp.zeros((B, RWKV_HEADS, HEAD_SIZE, HEAD_SIZE), f32)
    _, ys = lax.scan(rwkv7_step, s0, (tm(rh), tm(wh), tm(kh), tm(vh), tm(kk), tm(ah)))
    y = jnp.moveaxis(ys, 0, 1)
    mean = jnp.mean(y, axis=-1, keepdims=True)
    var = jnp.mean(jnp.square(y - mean), axis=-1, keepdims=True)
    yn = ((y - mean) * lax.rsqrt(var + GN_EPS)).reshape(B, S, RWKV_WIDTH)
    yn = yn * lnx_g.astype(f32) + lnx_b.astype(f32)
    bonus = jnp.sum(rh * kh * r_k.astype(f32), axis=-1, keepdims=True) * vh
    out = (yn + bonus.reshape(B, S, RWKV_WIDTH)) * g.astype(f32)
    return out.astype(r.dtype)


def multiscale_pool(p, pool_w, pool_scale):
    B, S, _ = p.shape
    f32 = jnp.float32
    p32 = p.astype(f32)
    cs = jnp.cumsum(p32, axis=1)
    pos = jnp.arange(1, S + 1, dtype=f32)
    groups = []
    for j, win in enumerate(POOL_WINDOWS):
        sl = slice(j * POOL_GROUP, (j + 1) * POOL_GROUP)
        csj = cs[..., sl]
        lag = jnp.pad(csj, ((0, 0), (win, 0), (0, 0)))[:, :S]
        mean = (csj - lag) / jnp.minimum(pos, float(win))[None, :, None]
        groups.append(mean - p32[..., sl])
    d = jnp.stack(groups, axis=2)
    out = jnp.einsum('bsgc,gcd->bsgd', d, pool_w.astype(f32)).reshape(B, S, POOL_WIDTH)
    return (out * pool_scale.astype(f32)).astype(p.dtype)


def peer_ffn(h, peer_q, peer_keys, peer_u, peer_v):
    B, S, D = h.shape
    q = (h @ peer_q).reshape(B, S, PEER_HEADS, 2, PEER_HALF)
    scores = jnp.einsum('bshpd,hpkd->bshpk', q, peer_keys)
    s, i = lax.top_k(scores, PEER_TOPK)
    cand_s = (s[..., 0, :, None] + s[..., 1, None, :]).reshape(B, S, PEER_HEADS, PEER_TOPK * PEER_TOPK)
    cand_i = (i[..., 0, :, None] * N_KEYS + i[..., 1, None, :]).reshape(B, S, PEER_HEADS, PEER_TOPK * PEER_TOPK)
    top_s, sel = lax.top_k(cand_s, PEER_TOPK)
    idx = jnp.take_along_axis(cand_i, sel, axis=-1)
    gate = jax.nn.softmax(top_s.astype(jnp.float32), axis=-1)
    T = B * S
    E = PEER_HEADS * PEER_TOPK
    nb = T // TOK_BLOCK
    hb = h.reshape(nb, TOK_BLOCK, D)
    ib = idx.reshape(nb, TOK_BLOCK, E)
    gb = gate.astype(h.dtype).reshape(nb, TOK_BLOCK, E)

    def block(args):
        hx, ix, gx = args
        u_sel = jnp.take(peer_u, ix, axis=0)
        v_sel = jnp.take(peer_v, ix, axis=0)
        act = jax.nn.gelu(jnp.einsum('td,ted->te', hx, u_sel), approximate=False)
        return jnp.einsum('te,ted->td', gx * act, v_sel)

    out = lax.map(block, (hb, ib, gb))
    return out.reshape(B, S, D)


def setup_inputs(seed: int = 0) -> dict:
    key = jax.random.key(seed)
    ks = jax.random.split(key, 32)
    nrm = lambda k, shape, s: jax.random.normal(k, shape, jnp.float32) * s
    L, Lr = DEPTH, DEPTH - 1
    return {
        "x": nrm(ks[0], (BATCH, SEQ, D_MODEL), 1.0),
        "c": nrm(ks[1], (BATCH, D_MODEL), 1.0),
        "ada_w": nrm(ks[2], (L, D_MODEL, 6 * D_MODEL), 0.5 * D_MODEL ** -0.5),
        "ada_b": nrm(ks[3], (L, 6 * D_MODEL), 0.01),
        "ln1_g": 1.0 + nrm(ks[4], (L, D_MODEL), 0.01),
        "w_in": nrm(ks[5], (L, D_MODEL, IN_COLS), D_MODEL ** -0.5),
        "mu_shift": jax.random.uniform(ks[6], (L, RWKV_COLS), jnp.float32),
        "w0": nrm(ks[7], (L, RWKV_WIDTH), 0.5),
        "w_up": nrm(ks[8], (L, DECAY_LORA, RWKV_WIDTH), 0.1 * DECAY_LORA ** -0.5),
        "a0": nrm(ks[9], (L, RWKV_WIDTH), 0.1),
        "a_up": nrm(ks[10], (L, AAA_LORA, RWKV_WIDTH), 0.1 * AAA_LORA ** -0.5),
        "g_up": nrm(ks[11], (L, GATE_LORA, RWKV_WIDTH), GATE_LORA ** -0.5),
        "vres_down": nrm(ks[12], (Lr, D_MODEL, MV_LORA), D_MODEL ** -0.5),
        "vres_mu": jax.random.uniform(ks[13], (Lr, MV_LORA), jnp.float32),
        "vres_v0": 1.0 + nrm(ks[14], (Lr, RWKV_WIDTH), 0.1),
        "vres_up": nrm(ks[15], (Lr, MV_LORA, RWKV_WIDTH), 0.1 * MV_LORA ** -0.5),
        "k_k": 0.85 + nrm(ks[16], (L, RWKV_WIDTH), 0.02),
        "k_a": 1.0 + nrm(ks[17], (L, RWKV_WIDTH), 0.02),
        "r_k": -0.04 + nrm(ks[18], (L, RWKV_HEADS, HEAD_SIZE), 0.02),
        "lnx_g": 1.0 + nrm(ks[19], (L, RWKV_WIDTH), 0.01),
        "lnx_b": nrm(ks[20], (L, RWKV_WIDTH), 0.01),
        "pool_w": nrm(ks[21], (L, len(POOL_WINDOWS), POOL_GROUP, POOL_GROUP), POOL_GROUP ** -0.5),
        "pool_scale": 1.0 + nrm(ks[22], (L, POOL_WIDTH), 0.01),
        "w_out": nrm(ks[23], (L, MIX_WIDTH, D_MODEL), MIX_WIDTH ** -0.5),
        "ln2_g": 1.0 + nrm(ks[24], (L, D_MODEL), 0.01),
        "peer_q": nrm(ks[25], (L, D_MODEL, PEER_HEADS * PEER_QDIM), D_MODEL ** -0.5),
        "peer_keys": nrm(ks[26], (L, PEER_HEADS, 2, N_KEYS, PEER_HALF), PEER_HALF ** -0.5),
        "peer_u": nrm(ks[27], (L, N_EXPERTS, D_MODEL), D_MODEL ** -0.5),
        "peer_v": nrm(ks[28], (L, N_EXPERTS, D_MODEL), PEER_HEADS ** -0.5),
        "lnf_g": 1.0 + nrm(ks[29], (D_MODEL,), 0.01),
    }


def reference(x, c, ada_w, ada_b, ln1_g, w_in, mu_shift, w0, w_up, a0, a_up, g_up,
              vres_down, vres_mu, vres_v0, vres_up, k_k, k_a, r_k, lnx_g, lnx_b,
              pool_w, pool_scale, w_out, ln2_g, peer_q, peer_keys, peer_u, peer_v, lnf_g):
    split_pts = [RWKV_WIDTH, 2 * RWKV_WIDTH, 3 * RWKV_WIDTH,
                 3 * RWKV_WIDTH + DECAY_LORA, 3 * RWKV_WIDTH + DECAY_LORA + AAA_LORA]
    c_act = jax.nn.silu(c)
    v_first = None
    for l in range(DEPTH):
        mod = c_act @ ada_w[l] + ada_b[l]
        sh1, sc1, g1, sh2, sc2, g2 = jnp.split(mod, 6, axis=-1)

        h = modulate(rmsnorm(x, ln1_g[l]), sh1, sc1)
        w_full = w_in[l] if l == 0 else jnp.concatenate([w_in[l], vres_down[l - 1]], axis=1)
        proj = h @ w_full
        rw = token_shift(proj[..., :RWKV_COLS], mu_shift[l])
        r, k, v, wd, ad, gd = jnp.split(rw, split_pts, axis=-1)
        pool_in = proj[..., RWKV_COLS:RWKV_COLS + POOL_WIDTH]
        if l == 0:
            v_first = v
        else:
            vd = token_shift(proj[..., RWKV_COLS + POOL_WIDTH:], vres_mu[l - 1])
            v = v + (v_first - v) * jax.nn.sigmoid(vres_v0[l - 1] + vd @ vres_up[l - 1])
        y_rwkv = rwkv7_mix(r, k, v, wd, ad, gd, w0[l], w_up[l], a0[l], a_up[l], g_up[l],
                           k_k[l], k_a[l], r_k[l], lnx_g[l], lnx_b[l])
        y_pool = multiscale_pool(pool_in, pool_w[l], pool_scale[l])
        mix = jnp.concatenate([y_rwkv, y_pool], axis=-1) @ w_out[l]
        x = x + g1[:, None, :] * mix

        h2 = modulate(rmsnorm(x, ln2_g[l]), sh2, sc2)
        x = x + g2[:, None, :] * peer_ffn(h2, peer_q[l], peer_keys[l], peer_u[l], peer_v[l])
    return rmsnorm(x, lnf_g)
```

```python
from contextlib import ExitStack
import numpy as np
import ml_dtypes
import concourse.bass as bass
import concourse.mybir as mybir
from concourse.bass_utils import run_bass_kernel_spmd

F32 = mybir.dt.float32
BF16 = mybir.dt.bfloat16
AF = mybir.ActivationFunctionType
ALU = mybir.AluOpType

D = 1024
SEQ = 4096
NB = 4
NCORE = 8
TOK = 2048
NORM_EPS = 1e-6
GN_EPS = 64e-5
CH = 64


class Ctx:
    NDMA = 12

    def __init__(self, nc):
        self.nc = nc
        self.stack = ExitStack()
        self.E = dict(pe=nc.tensor, act=nc.scalar, dve=nc.vector, pool=nc.gpsimd, sp=nc.sync)
        self.sem = {}
        for e in ("pe", "act", "dve", "pool"):
            self.sem[e] = self.stack.enter_context(nc.semaphore("c_" + e))
        self.cnt = {e: 0 for e in self.sem}
        self.seen = {e: {} for e in self.E}
        self.dsem = {}
        self.dval = {}
        self.dnext = {}
        for q in ("sp", "pool", "act"):
            self.dsem[q] = [self.stack.enter_context(nc.semaphore("d_%s%d" % (q, i))) for i in range(self.NDMA)]
            self.dval[q] = [0] * self.NDMA
            self.dnext[q] = 0
        self.W = {}
        self.R = {}
        self.n_ins = 0
        self.nwait = {}
        self.ccsem = self.stack.enter_context(nc.semaphore("cc_sem"))
        self.ccval = 0

    scope = None
    pfx = ""

    def begin_phase(self, pfx):
        self.scope = ExitStack()
        self.pfx = pfx

    def end_phase(self):
        self.barrier()
        self.scope.close()
        self.scope = None
        self.W = {}
        self.R = {}

    def barrier(self):
        for eng in self.E:
            for e2 in self.cnt:
                if eng == "pe" and e2 == "pe":
                    continue
                self._wait(eng, e2, self.cnt[e2])
            for q in self.dsem:
                for i in range(self.NDMA):
                    self._wait(eng, (q, i), self.dval[q][i])

    def sb(self, name, shape, dt=F32):
        st = self.scope if self.scope is not None else self.stack
        return st.enter_context(self.nc.sbuf_tensor(self.pfx + name, list(shape), dt))

    def ps(self, name, shape, dt=F32):
        st = self.scope if self.scope is not None else self.stack
        return st.enter_context(self.nc.psum_tensor(self.pfx + name, list(shape), dt))

    def _semh(self, semid):
        if semid == "cc":
            return self.ccsem
        if isinstance(semid, str):
            return self.sem[semid]
        return self.dsem[semid[0]][semid[1]]

    def _wait(self, eng, semid, val):
        if val <= 0:
            return
        if self.seen[eng].get(semid, 0) >= val:
            return
        self.E[eng].wait_ge(self._semh(semid), val)
        self.seen[eng][semid] = val
        self.n_ins += 1
        self.nwait[eng] = self.nwait.get(eng, 0) + 1

    def _deps(self, r, w):
        deps = {}
        for k in r:
            for s, v in self.W.get(k, {}).items():
                deps[s] = max(deps.get(s, 0), v)
        for k in w:
            for s, v in self.W.get(k, {}).items():
                deps[s] = max(deps.get(s, 0), v)
            for s, v in self.R.get(k, {}).items():
                deps[s] = max(deps.get(s, 0), v)
        return deps

    def _record(self, semid, val, r, w):
        for k in w:
            self.W[k] = {semid: val}
            self.R[k] = {}
        for k in r:
            self.R.setdefault(k, {})[semid] = val

    def op(self, eng, fn, r=(), w=()):
        deps = self._deps(r, w)
        for s, v in deps.items():
            if eng == "pe" and s == "pe":
                continue
            self._wait(eng, s, v)
        ins = fn(self.E[eng])
        self.cnt[eng] += 1
        ins.then_inc(self.sem[eng], 1)
        self.n_ins += 1
        self._record(eng, self.cnt[eng], r, w)
        return ins

    def dma(self, q, out, in_, r=(), w=(), **kw):
        i = self.dnext[q]
        self.dnext[q] = (i + 1) % self.NDMA
        semid = (q, i)
        self._wait(q, semid, self.dval[q][i])
        deps = self._deps(r, w)
        for s, v in deps.items():
            self._wait(q, s, v)
        ins = self.E[q].dma_start(out=out, in_=in_, **kw)
        self.dval[q][i] += 16
        ins.then_inc(self.dsem[q][i], 16)
        self.n_ins += 1
        self._record(semid, self.dval[q][i], r, w)
        return ins

    def collective(self, fn):
        self.barrier()
        ins = fn(self.nc.gpsimd)
        self.ccval += 1
        ins.then_inc(self.ccsem)
        for eng in self.E:
            self._wait(eng, "cc", self.ccval)

    def finish(self):
        global LAST_CNT
        LAST_CNT = (dict(self.cnt), {q: list(v) for q, v in self.dval.items()}, self.n_ins, dict(self.nwait))
        for q in self.dsem:
            for i in range(self.NDMA):
                self._wait("sp", (q, i), self.dval[q][i])
        for e in self.cnt:
            self._wait("sp", e, self.cnt[e])
        self.stack.close()


def _fm(vec, n=None):
    v = np.ascontiguousarray(np.asarray(vec, dtype=np.float32).reshape(-1, 128).T)
    return v


def emit_mod_fm(c, adaw_dram, col0, ncolchunks, silu_c, adab_fm_sb, adab_col0, out_sb, ps_tile, tag):
    nc = c.nc
    wv = adaw_dram.rearrange("(dc p) j -> p dc j", p=128)
    cap = c.adaw_sb.shape[2] // 128
    for half in range(0, ncolchunks, cap):
        nch = min(cap, ncolchunks - half)
        wt = c.adaw_sb
        c.dma("sp", wt[:, :, 0:nch * 128], wv[:, :, col0 + half * 128: col0 + (half + nch) * 128],
              w=["adaw"])
        for jc in range(nch):
            for dc in range(8):
                c.op("pe", lambda e, jc=jc, dc=dc: e.matmul(
                    ps_tile[:, half + jc: half + jc + 1], wt[:, dc, jc * 128:(jc + 1) * 128],
                    silu_c[:, dc:dc + 1], start=(dc == 0), stop=(dc == 7)),
                    r=["adaw", "silu_c"], w=[tag + "_ps"])
    c.op("dve", lambda e: e.tensor_tensor(out=out_sb, in0=ps_tile[:, 0:ncolchunks],
                                           in1=adab_fm_sb[:, adab_col0: adab_col0 + ncolchunks], op=ALU.add),
         r=[tag + "_ps", "adab"], w=[tag])


DBG_STAGE = 99
DBG_PHASES = None
DBG_COLL = True
DBG_SKIP0 = False
LAST_CNT = None
DBG_NST = None


def emit_L1(c, A):
    ncol = A["ncol"]
    njc = (ncol + 127) // 128
    x_d, c_d, adaw_d, adab_d, g_d, id_d, out_d = (A["x"], A["c_fm"], A["ada_w"], A["ada_b_fm"], A["ln_g_fm"],
                                                   A["ident"], A["out"])
    c.adaw_sb = c.sb("adaw", [128, 8, 1024], F32)
    c_sb = c.sb("c_sb", [128, 8]); sig_c = c.sb("sig_c", [128, 8]); silu_c = c.sb("silu_c", [128, 8])
    adab = c.sb("adab", [128, 48]); lng = c.sb("lng", [128, 8])
    modT = c.sb("modT", [128, 16]); effs = c.sb("effs", [128, 8])
    identf = c.sb("identf", [128, 128]); identb = c.sb("identb", [128, 128], BF16)
    wbf = c.sb("wbf", [128, 8, njc * 128], BF16)
    hT = c.sb("hT", [128, 8, TOK], BF16)
    xt = [c.sb("xt%d" % i, [128, D]) for i in range(2)]
    junk = c.sb("junk", [128, D], BF16)
    xn = [c.sb("xn%d" % i, [128, D], BF16) for i in range(2)]
    ss = c.sb("ss", [128, 32]); rstd = c.sb("rstd", [128, 32])
    stg = [c.sb("stg%d" % i, [128, TOK]) for i in range(2)]
    mod_ps = c.ps("mod_ps", [128, 512])
    tp_ps = [c.ps("tp_ps%d" % i, [128, 1024], BF16) for i in range(2)]
    mm_ps = [c.ps("mm_ps%d" % i, [128, 512]) for i in range(4)]

    c.dma("sp", c_sb[:], c_d, w=["c_sb"])
    c.dma("sp", adab[:], adab_d, w=["adab"])
    c.dma("sp", lng[:], g_d, w=["lng"])
    c.dma("sp", identf[:], id_d, w=["identf"])
    c.op("dve", lambda e: e.tensor_copy(identb[:], identf[:]), r=["identf"], w=["identb"])
    if njc * 128 != ncol:
        c.op("pool", lambda e: e.memset(wbf[:, :, ncol:njc * 128], 0.0), w=["wbf"])
    for (w_ap, col0, ncols) in A["w_parts"]:
        wv = w_ap.rearrange("(dc p) j -> p dc j", p=128)
        for dc in range(8):
            c.dma("pool", wbf[:, dc, col0:col0 + ncols], wv[:, dc, :], w=["wbf"])
    c.op("act", lambda e: e.activation(out=sig_c[:], in_=c_sb[:], func=AF.Sigmoid), r=["c_sb"], w=["sig_c"])
    c.op("dve", lambda e: e.tensor_tensor(out=silu_c[:], in0=c_sb[:], in1=sig_c[:], op=ALU.mult),
         r=["c_sb", "sig_c"], w=["silu_c"])
    emit_mod_fm(c, adaw_d, 0, 16, silu_c, adab, 0, modT[:, 0:16], mod_ps, "modT")
    c.op("dve", lambda e: e.scalar_tensor_tensor(out=effs[:], in0=modT[:, 8:16], scalar=1.0, in1=lng[:],
                                                  op0=ALU.add, op1=ALU.mult), r=["modT", "lng"], w=["effs"])
    c.op("dve", lambda e: e.memset(ss[:], 0.0), w=["ss%d" % i for i in range(TOK // 128)])
    epsb = c.sb("epsb", [128, 1])
    c.op("dve", lambda e: e.memset(epsb[:], NORM_EPS), w=["epsb"])
    ntile = TOK // 128
    for i in range(ntile):
        s = i % 2
        c.dma("sp", xt[s][:], (A["x_tile"](i) if "x_tile" in A else x_d[i * 128:(i + 1) * 128, :]), w=["xt%d" % s])
        c.op("act", lambda e, s=s, i=i: e.activation(out=junk[:], in_=xt[s][:], func=AF.Square,
                                                     accum_out=ss[:, i:i + 1]),
             r=["xt%d" % s], w=["junk", "ss%d" % i])
        c.op("act", lambda e, i=i: e.activation(out=rstd[:, i:i + 1], in_=ss[:, i:i + 1], func=AF.Sqrt,
                                                bias=epsb[:, 0:1], scale=1.0 / D),
             r=["ss%d" % i, "epsb"], w=["rstd%d" % i])
        c.op("dve", lambda e, i=i: e.reciprocal(out=rstd[:, i:i + 1], in_=rstd[:, i:i + 1]),
             r=["rstd%d" % i], w=["rstd%d" % i])
        c.op("dve", lambda e, s=s, i=i: e.tensor_scalar(out=xn[s][:], in0=xt[s][:], scalar1=rstd[:, i:i + 1],
                                                        scalar2=None, op0=ALU.mult),
             r=["xt%d" % s, "rstd%d" % i], w=["xn%d" % s])
        for dc in range(8):
            c.op("pe", lambda e, s=s, dc=dc: e.transpose(tp_ps[s][:, dc * 128:(dc + 1) * 128],
                                                         xn[s][:, dc * 128:(dc + 1) * 128], identb[:]),
                 r=["xn%d" % s, "identb"], w=["tp%d" % s])
        for dc in range(8):
            if s == 0:
                c.op("act", lambda e, s=s, dc=dc, i=i: e.activation(
                    out=hT[:, dc, i * 128:(i + 1) * 128], in_=tp_ps[s][:, dc * 128:(dc + 1) * 128],
                    func=AF.Identity, bias=modT[:, dc:dc + 1], scale=effs[:, dc:dc + 1]),
                    r=["tp%d" % s, "modT", "effs"], w=["hT_%d_%d_%d" % (dc, i // 4, s)])
            else:
                c.op("dve", lambda e, s=s, dc=dc, i=i: e.tensor_scalar(
                    out=hT[:, dc, i * 128:(i + 1) * 128], in0=tp_ps[s][:, dc * 128:(dc + 1) * 128],
                    scalar1=effs[:, dc:dc + 1], scalar2=modT[:, dc:dc + 1], op0=ALU.mult, op1=ALU.add),
                    r=["tp%d" % s, "modT", "effs"], w=["hT_%d_%d_%d" % (dc, i // 4, s)])
    k = 0
    for jc in range(njc):
        st = stg[jc % 2]
        for tb in range(TOK // 512):
            pt = mm_ps[k % 4]; pk = "mm%d" % (k % 4); k += 1
            for dc in range(8):
                c.op("pe", lambda e, pt=pt, dc=dc, jc=jc, tb=tb: e.matmul(
                    pt[:], wbf[:, dc, jc * 128:(jc + 1) * 128], hT[:, dc, tb * 512:(tb + 1) * 512],
                    start=(dc == 0), stop=(dc == 7)),
                    r=["wbf", "hT_%d_%d_0" % (dc, tb), "hT_%d_%d_1" % (dc, tb)], w=[pk])
            eng = "act" if tb % 2 == 0 else "dve"
            if eng == "act":
                c.op("act", lambda e, pt=pt, st=st, tb=tb: e.copy(out=st[:, tb * 512:(tb + 1) * 512], in_=pt[:]),
                     r=[pk], w=["stg%d_%d" % (jc % 2, tb)])
            else:
                c.op("dve", lambda e, pt=pt, st=st, tb=tb: e.tensor_copy(st[:, tb * 512:(tb + 1) * 512], pt[:]),
                     r=[pk], w=["stg%d_%d" % (jc % 2, tb)])
        c.dma("sp", out_d[jc * 128:(jc + 1) * 128, :], st[:],
              r=["stg%d_%d" % (jc % 2, tb) for tb in range(4)])


def build_L1(ncol):
    nc = bass.Bass("TRN2", target_bir_lowering=False)
    njc = (ncol + 127) // 128
    di = lambda name, shape: nc.dram_tensor(name, list(shape), F32, kind="ExternalInput").ap()
    A = dict(x=di("x", [TOK, D]), c_fm=di("c_fm", [128, 8]), ada_w=di("ada_w", [D, 2 * D]),
             ada_b_fm=di("ada_b_fm", [128, 48]), ln_g_fm=di("ln_g_fm", [128, 8]), ident=di("ident", [128, 128]),
             ncol=ncol)
    A["w_parts"] = [(di("w_full", [D, ncol]), 0, ncol)]
    A["out"] = nc.dram_tensor("projT", [njc * 128, TOK], F32, kind="ExternalOutput").ap()
    c = Ctx(nc)
    c.begin_phase("")
    emit_L1(c, A)
    c.end_phase()
    c.finish()
    return nc


class _Stop(Exception):
    pass


def dbg(stage):
    if DBG_STAGE == stage:
        raise _Stop()


TB = 512
NBLK = SEQ // TB
CPB = TB // CH
C0 = float(np.exp(-0.5))
POOL_WINDOWS = (2, 4, 8, 16)


def l2_consts():
    s_idx = np.arange(64)[:, None]; t_idx = np.arange(64)[None, :]
    su = (t_idx > s_idx).astype(np.float32); iu = (t_idx >= s_idx).astype(np.float32)
    maskA = np.tile(np.concatenate([su, iu], axis=1), (1, 4))
    maskC = np.tile((t_idx < s_idx).astype(np.float32), (1, 4))
    identrep = np.tile(np.eye(64, dtype=np.float32), (1, 4))
    scanmask = np.ones((128, TB), np.float32); scanmask[:, ::CH] = 0.0
    blockones = np.kron(np.eye(2, dtype=np.float32), np.ones((64, 64), np.float32))
    return dict(maskA=maskA, maskC=maskC, identrep=identrep, scanmask=scanmask, blockones=blockones,
                ident=np.eye(128, dtype=np.float32))


def emit_L2(c, A, layer1):
    nc = c.nc
    rT, kT, vT, waT, gdT, poolT = A["rT"], A["kT"], A["vT"], A["waT"], A["gdT"], A["poolT"]
    if layer1:
        vdT, vfT, vup_d = A["vdT"], A["vfT"], A["vup"]
    pvec_d, wup_d, aup_d, gup_d, poolw_d = A["pvec"], A["wup"], A["aup"], A["gup"], A["poolw"]
    maskA_d, maskC_d, identrep_d = A["maskA"], A["maskC"], A["identrep"]
    scanmask_d, bo_d, id_d, invdiv_d, psel_d = A["scanmask"], A["blockones"], A["ident"], A["invdiv"], A["psel"]
    yrw, ypl = A["yrw"], A["ypl"]
    if not layer1:
        vout = A["vout"]
    sb = c.sb
    pvec = sb("pvec_sb", [128, 32]); omk = sb("omk", [128, 2])
    wup = sb("wup_sb", [128, 256], BF16); aup = sb("aup_sb", [128, 256], BF16); gup = sb("gup_sb", [128, 256], BF16)
    vup = sb("vup_sb", [32, 256], BF16); poolw = sb("poolw_sb", [128, 2, 128], BF16)
    maskA = sb("maskA_sb", [64, 512]); maskC = sb("maskC_sb", [64, 256]); identrep = sb("identrep_sb", [64, 256])
    scanmask = sb("scanmask_sb", [128, TB]); bo = sb("bo_sb", [128, 128]); identf = sb("identf", [128, 128])
    identb = sb("identb", [128, 128], BF16); invdiv = sb("invdiv_sb", [128, 2, TB])
    epsk = sb("epsk", [128, 1]); gneps = sb("gneps", [128, 1]); psel = sb("psel_sb", [128, 2, 4])
    Xwa = sb("Xwa", [128, TB + 1]); Xg = sb("Xg", [128, TB + 1]); Xvd = sb("Xvd", [32, TB + 1])
    dwa = sb("dwa", [128, TB]); swa = sb("swa", [128, TB]); th = sb("th", [64, TB], BF16); adb = sb("adb", [128, TB], BF16)
    dg = sb("dg", [128, TB]); sgd = sb("sgd", [128, TB]); sg = sb("sg", [128, TB], BF16)
    dvd = sb("dvd", [32, TB]); vdb = sb("vdb", [32, TB], BF16)
    P2 = range(2)
    Xr = [sb("Xr_sh", [128, TB + 1])] * 2; Xk = [sb("Xk_sh", [128, TB + 1])] * 2
    Xv = [sb("Xv_sh", [128, TB + 1])] * 2; Xvf = [sb("Xvf_sh", [128, TB])] * 2
    tmpd = [sb("tmpd_sh", [128, TB])] * 2
    r_s = [sb("r_s%d" % i, [128, TB]) for i in P2]; k_s = [sb("k_s%d" % i, [128, TB]) for i in P2]
    v_s = [sb("v_s%d" % i, [128, TB]) for i in P2]
    sigw = [sb("sigw%d" % i, [128, TB]) for i in P2]; cum = [sb("cum%d" % i, [128, TB]) for i in P2]
    cumx = [sb("cumx_sh", [128, TB])] * 2
    G = [sb("G%d" % i, [128, TB]) for i in P2]; Ginv = [sb("Ginv%d" % i, [128, TB]) for i in P2]
    Gex = [sb("Gex%d" % i, [128, TB]) for i in P2]
    a_ = [sb("a_%d" % i, [128, TB]) for i in P2]; gg = [sb("gg%d" % i, [128, TB]) for i in P2]
    vsig = [sb("vsig_sh", [128, TB])] * 2
    kkraw = [sb("kkraw_sh", [128, TB])] * 2; sq = [sb("sq_sh", [128, TB])] * 2
    rn = [sb("rn_sh", [128, TB])] * 2; kk = [sb("kk%d" % i, [128, TB]) for i in P2]
    fac = [sb("fac_sh", [128, TB])] * 2; kmod = [sb("kmod%d" % i, [128, TB]) for i in P2]
    rk2 = [sb("rk2_sh", [128, TB])] * 2; bonus = [sb("bonus%d" % i, [128, TB]) for i in P2]
    t1 = [sb("t1_sh", [128, TB])] * 2
    ARt = [sb("ARt%d" % i, [128, CPB * 128], BF16) for i in P2]
    Bt = [sb("Bt%d" % i, [128, TB], BF16) for i in P2]; Kt = [sb("Kt%d" % i, [128, TB], BF16) for i in P2]
    Vb = [sb("Vb%d" % i, [128, TB], BF16) for i in P2]
    yraw = [sb("yraw%d" % i, [128, TB]) for i in P2]; yc = [sb("yc%d" % i, [128, TB]) for i in P2]
    ysq = [sb("ysq_sh", [128, TB])] * 2; yrs = [sb("yrs_sh", [128, TB])] * 2
    yo = [sb("yo%d" % i, [128, TB]) for i in P2]
    M0b = [[sb("M0b%d_%d" % (i, j), [64, 64], BF16) for j in range(2)] for i in range(4)]
    ARo = [sb("ARo%d" % i, [64, CPB * 128], BF16) for i in P2]
    Bo = [sb("Bo%d" % i, [64, TB], BF16) for i in P2]; Ko = [sb("Ko%d" % i, [64, TB], BF16) for i in P2]
    GCs = [sb("GCs%d" % i, [128, CPB]) for i in P2]; GCo = [sb("GCo%d" % i, [64, CPB]) for i in P2]
    tokmaj = [sb("tokmaj%d" % j, [64, 768], BF16) for j in range(2)]
    SA = [sb("SA%d" % j, [64, 512], BF16) for j in range(2)]; SB_ = [sb("SB%d" % j, [64, 512], BF16) for j in range(2)]
    SC = [sb("SC%d" % j, [64, 256], BF16) for j in range(2)]
    TT = [sb("TT%d" % j, [64, 256], BF16) for j in range(2)]; PP = [sb("PP%d" % j, [64, 512], BF16) for j in range(2)]
    W1 = sb("W1", [64, 256], BF16); U = [sb("U%d" % j, [64, 256], BF16) for j in range(2)]
    Xp = [sb("Xp%d" % i, [128, TB + 15]) for i in P2]
    plv = [[sb("plv%d_%d" % (i, k), [128, TB + 15]) for k in range(4)] for i in P2]
    pacc = [sb("pacc%d" % i, [128, TB]) for i in P2]
    pd = [sb("pd%d" % i, [128, TB], BF16) for i in P2]; pm = [sb("pm%d" % i, [128, TB]) for i in P2]
    po = [sb("po%d" % i, [128, TB]) for i in P2]
    bk = {n: c.ps("bk_" + n, [128, 512]) for n in ("A", "B", "C", "D", "E", "FG", "HI")}
    trp = c.ps("bk_trp", [128, 1024], BF16)

    ld = lambda dst, src, key: c.dma("sp", dst, src, w=[key])
    ld(pvec[:], pvec_d, "pvec"); ld(maskA[:], maskA_d, "maskA"); ld(maskC[:], maskC_d, "maskC")
    ld(identrep[:], identrep_d, "identrep"); ld(scanmask[:], scanmask_d, "scanmask"); ld(bo[:], bo_d, "bo")
    ld(identf[:], id_d, "identf"); ld(invdiv[:], invdiv_d, "invdiv"); ld(psel[:], psel_d, "psel")
    c.dma("pool", wup[0:64, :], wup_d, w=["wup"]); c.dma("pool", aup[64:128, :], aup_d, w=["aup"])
    c.dma("pool", gup[:], gup_d, w=["gup"]); c.dma("pool", poolw[:], poolw_d.rearrange("g c d -> c g d"), w=["poolw"])
    if layer1:
        c.dma("pool", vup[0:32, :], vup_d, w=["vup"])
    c.op("dve", lambda e: e.tensor_copy(identb[:], identf[:]), r=["identf"], w=["identb"])
    c.op("dve", lambda e: e.memset(epsk[:], 1e-24), w=["epsk"])
    c.op("dve", lambda e: e.memset(gneps[:], GN_EPS), w=["gneps"])
    c.op("dve", lambda e: e.tensor_scalar(out=omk[:], in0=pvec[:, 12:14], scalar1=-1.0, scalar2=1.0,
                                          op0=ALU.mult, op1=ALU.add), r=["pvec"], w=["omk"])
    for i in range(4):
        for j in range(2):
            c.op("dve", lambda e, i=i, j=j: e.memset(M0b[i][j][:], 0.0), w=["M0b%d_%d" % (i, j)])
    pv = lambda col, hp=0: pvec[:, col + hp: col + hp + 1]

    def tshift(eng, X, d, out, mu, n, kX, kd, kout):
        c.op(eng, lambda e: e.tensor_tensor(out=d, in0=X[0:n, 0:TB], in1=X[0:n, 1:TB + 1], op=ALU.subtract),
             r=[kX], w=[kd])
        if eng == "dve":
            c.op(eng, lambda e: e.scalar_tensor_tensor(out=out, in0=d, scalar=mu, in1=X[0:n, 1:TB + 1],
                                                       op0=ALU.mult, op1=ALU.add), r=[kX, kd, "pvec"], w=[kout])
        else:
            c.op(eng, lambda e: e.tensor_scalar(out=d, in0=d, scalar1=mu, scalar2=None, op0=ALU.mult),
                 r=[kd, "pvec"], w=[kd])
            c.op(eng, lambda e: e.tensor_tensor(out=out, in0=d, in1=X[0:n, 1:TB + 1], op=ALU.add),
                 r=[kX, kd], w=[kout])

    def load_halo(X, src, rows, b, n, key, halo=1):
        t0 = b * TB
        if b == 0:
            c.op("pool", lambda e: e.memset(X[0:n, 0:halo], 0.0), w=[key])
            c.dma("sp", X[0:n, halo:halo + TB], src[rows, 0:TB], w=[key])
        else:
            c.dma("sp", X[0:n, :], src[rows, t0 - halo:t0 + TB], w=[key])

    chunk_counter = [0]

    def body():
        for b in range(NBLK):
            t0 = b * TB
            load_halo(Xwa, waT, slice(0, 128), b, 128, "Xwa")
            load_halo(Xg, gdT, slice(0, 128), b, 128, "Xg")
            tshift("pool", Xwa, dwa[:], swa[:], pv(25), 128, "Xwa", "dwa", "swa")
            c.op("act", lambda e: e.activation(out=th[:], in_=swa[0:64, :], func=AF.Tanh), r=["swa"], w=["th"])
            c.op("pool", lambda e: e.tensor_copy(adb[64:128, :], swa[64:128, :]), r=["swa"], w=["adb"])
            tshift("pool", Xg, dg[:], sgd[:], pv(24), 128, "Xg", "dg", "sgd")
            c.op("act", lambda e: e.activation(out=sg[:], in_=sgd[:], func=AF.Sigmoid), r=["sgd"], w=["sg"])
            if layer1:
                load_halo(Xvd, vdT, slice(0, 32), b, 32, "Xvd")
                tshift("pool", Xvd, dvd[:], vdb[:], pvec[0:32, 26:27], 32, "Xvd", "dvd", "vdb")
            dbg(1)
            for hp in P2:
                rows = slice(hp * 128, (hp + 1) * 128)
                H = str(hp)
                load_halo(Xr[hp], rT, rows, b, 128, "Xr")
                load_halo(Xk[hp], kT, rows, b, 128, "Xk")
                load_halo(Xv[hp], vT, rows, b, 128, "Xv")
                tshift("dve", Xr[hp], tmpd[hp][:], r_s[hp][:], pv(0, hp), 128, "Xr", "tmpd", "r_s" + H)
                tshift("dve", Xk[hp], tmpd[hp][:], k_s[hp][:], pv(2, hp), 128, "Xk", "tmpd", "k_s" + H)
                tshift("dve", Xv[hp], tmpd[hp][:], v_s[hp][:], pv(4, hp), 128, "Xv", "tmpd", "v_s" + H)
                cols = slice(hp * 128, (hp + 1) * 128)
                c.op("pe", lambda e: e.matmul(bk["A"][:], wup[0:64, cols], th[:], start=True, stop=True),
                     r=["wup", "th"], w=["bkA"])
                c.op("act", lambda e: e.activation(out=sigw[hp][:], in_=bk["A"][:], func=AF.Sigmoid, bias=pv(6, hp)),
                     r=["bkA", "pvec"], w=["sigw" + H])
                c.op("pe", lambda e: e.matmul(bk["B"][:], aup[64:128, cols], adb[64:128, :], start=True, stop=True),
                     r=["aup", "adb"], w=["bkB"])
                c.op("act", lambda e: e.activation(out=a_[hp][:], in_=bk["B"][:], func=AF.Sigmoid, bias=pv(8, hp)),
                     r=["bkB", "pvec"], w=["a_" + H])
                c.op("pe", lambda e: e.matmul(bk["C"][:], gup[:, cols], sg[:], start=True, stop=True),
                     r=["gup", "sg"], w=["bkC"])
                c.op("act", lambda e: e.copy(out=gg[hp][:], in_=bk["C"][:]), r=["bkC"], w=["gg" + H])
                if layer1:
                    c.dma("sp", Xvf[hp][:], vfT[rows, t0:t0 + TB], w=["Xvf"])
                    c.op("pe", lambda e: e.matmul(bk["D"][:], vup[0:32, cols], vdb[0:32, :], start=True, stop=True),
                         r=["vup", "vdb"], w=["bkD"])
                    c.op("act", lambda e: e.activation(out=vsig[hp][:], in_=bk["D"][:], func=AF.Sigmoid, bias=pv(27, hp)),
                         r=["bkD", "pvec"], w=["vsig"])
                    c.op("dve", lambda e: e.tensor_tensor(out=tmpd[hp][:], in0=Xvf[hp][:], in1=v_s[hp][:], op=ALU.subtract),
                         r=["Xvf", "v_s" + H], w=["tmpd"])
                    c.op("dve", lambda e: e.tensor_tensor(out=tmpd[hp][:], in0=tmpd[hp][:], in1=vsig[hp][:], op=ALU.mult),
                         r=["vsig", "tmpd"], w=["tmpd"])
                    c.op("dve", lambda e: e.tensor_tensor(out=v_s[hp][:], in0=v_s[hp][:], in1=tmpd[hp][:], op=ALU.add),
                         r=["v_s" + H, "tmpd"], w=["v_s" + H])
                else:
                    c.dma("sp", vout[rows, t0:t0 + TB], v_s[hp][:], r=["v_s" + H])
                c.op("dve", lambda e: e.tensor_scalar(out=kkraw[hp][:], in0=k_s[hp][:], scalar1=pv(10, hp), scalar2=None,
                                                      op0=ALU.mult), r=["k_s" + H, "pvec"], w=["kkraw"])
                c.op("pool", lambda e: e.tensor_tensor(out=sq[hp][:], in0=kkraw[hp][:], in1=kkraw[hp][:], op=ALU.mult),
                     r=["kkraw"], w=["sq"])
                c.op("pe", lambda e: e.matmul(bk["E"][:], bo[:], sq[hp][:], start=True, stop=True),
                     r=["bo", "sq"], w=["bkE"])
                c.op("act", lambda e: e.activation(out=rn[hp][:], in_=bk["E"][:], func=AF.Sqrt, bias=epsk[:, 0:1]),
                     r=["bkE", "epsk"], w=["rn"])
                c.op("dve", lambda e: e.reciprocal(out=rn[hp][:], in_=rn[hp][:]), r=["rn"], w=["rn"])
                c.op("dve", lambda e: e.tensor_tensor(out=kk[hp][:], in0=kkraw[hp][:], in1=rn[hp][:], op=ALU.mult),
                     r=["kkraw", "rn"], w=["kk" + H])
                c.op("pool", lambda e: e.tensor_scalar(out=fac[hp][:], in0=a_[hp][:], scalar1=pv(12, hp),
                                                       scalar2=omk[:, hp:hp + 1], op0=ALU.mult, op1=ALU.add),
                     r=["a_" + H, "pvec", "omk"], w=["fac"])
                c.op("pool", lambda e: e.tensor_tensor(out=kmod[hp][:], in0=k_s[hp][:], in1=fac[hp][:], op=ALU.mult),
                     r=["k_s" + H, "fac"], w=["kmod" + H])
                c.op("pool", lambda e: e.tensor_scalar(out=rk2[hp][:], in0=r_s[hp][:], scalar1=pv(16, hp), scalar2=None,
                                                       op0=ALU.mult), r=["r_s" + H, "pvec"], w=["rk2"])
                c.op("pool", lambda e: e.tensor_tensor(out=rk2[hp][:], in0=rk2[hp][:], in1=kmod[hp][:], op=ALU.mult),
                     r=["rk2", "kmod" + H], w=["rk2"])
                c.op("pe", lambda e: e.matmul(bk["FG"][:], bo[:], rk2[hp][:], start=True, stop=True),
                     r=["bo", "rk2"], w=["bkFG"])
                c.op("dve", lambda e: e.tensor_tensor(out=bonus[hp][:], in0=bk["FG"][:], in1=v_s[hp][:], op=ALU.mult),
                     r=["bkFG", "v_s" + H], w=["bonus" + H])
                c.op("dve", lambda e: e.tensor_tensor_scan(out=cum[hp][:], data0=scanmask[:], data1=sigw[hp][:],
                                                           initial=0.0, op0=ALU.mult, op1=ALU.add),
                     r=["scanmask", "sigw" + H], w=["cum" + H])
                c.op("pool", lambda e: e.tensor_tensor(out=cumx[hp][:], in0=cum[hp][:], in1=sigw[hp][:], op=ALU.subtract),
                     r=["cum" + H, "sigw" + H], w=["cumx"])
                c.op("act", lambda e: e.activation(out=G[hp][:], in_=cum[hp][:], func=AF.Exp, scale=-C0),
                     r=["cum" + H], w=["G" + H])
                c.op("act", lambda e: e.activation(out=Ginv[hp][:], in_=cum[hp][:], func=AF.Exp, scale=C0),
                     r=["cum" + H], w=["Ginv" + H])
                c.op("act", lambda e: e.activation(out=Gex[hp][:], in_=cumx[hp][:], func=AF.Exp, scale=-C0),
                     r=["cumx"], w=["Gex" + H])
                AR3 = ARt[hp][:].rearrange("p (c two t) -> p c two t", two=2, t=CH)
                v3 = lambda ap: ap.rearrange("p (c t) -> p c t", t=CH)
                c.op("dve", lambda e: e.tensor_tensor(out=AR3[:, :, 1, :], in0=v3(r_s[hp][:]), in1=v3(G[hp][:]), op=ALU.mult),
                     r=["r_s" + H, "G" + H], w=["ARt" + H])
                c.op("dve", lambda e: e.scalar_tensor_tensor(out=AR3[:, :, 0, :], in0=v3(kk[hp][:]), scalar=-1.0,
                                                             in1=v3(Gex[hp][:]), op0=ALU.mult, op1=ALU.mult),
                     r=["kk" + H, "Gex" + H], w=["ARt" + H])
                c.op("pool", lambda e: e.tensor_tensor(out=t1[hp][:], in0=kk[hp][:], in1=a_[hp][:], op=ALU.mult),
                     r=["kk" + H, "a_" + H], w=["t1"])
                c.op("dve", lambda e: e.tensor_tensor(out=Bt[hp][:], in0=t1[hp][:], in1=Ginv[hp][:], op=ALU.mult),
                     r=["t1", "Ginv" + H], w=["Bt" + H])
                c.op("pool", lambda e: e.tensor_tensor(out=Kt[hp][:], in0=kmod[hp][:], in1=Ginv[hp][:], op=ALU.mult),
                     r=["kmod" + H, "Ginv" + H], w=["Kt" + H])
                c.op("pool", lambda e: e.tensor_copy(Vb[hp][:], v_s[hp][:]), r=["v_s" + H], w=["Vb" + H])
                c.op("pool", lambda e: e.tensor_copy(GCs[hp][:], G[hp][:].rearrange("p (c t) -> p c t", t=CH)[:, :, CH - 1]),
                     r=["G" + H], w=["GCs" + H])
                c.dma("sp", ARo[hp][:], ARt[hp][64:128, :], r=["ARt" + H], w=["ARo" + H])
                c.dma("sp", Bo[hp][:], Bt[hp][64:128, :], r=["Bt" + H], w=["Bo" + H])
                c.dma("sp", Ko[hp][:], Kt[hp][64:128, :], r=["Kt" + H], w=["Ko" + H])
                c.dma("sp", GCo[hp][:], GCs[hp][64:128, :], r=["GCs" + H], w=["GCo" + H])

            dbg(2)
            for gi in P2:
                Gk = str(gi)
                load_halo(Xp[gi], poolT, slice(gi * 128, (gi + 1) * 128), b, 128, "Xp" + Gk, halo=15)
                cur = Xp[gi]; curk = "Xp" + Gk
                for lv in range(4):
                    sh = 1 << lv
                    dst = plv[gi][lv]; dk = "plv%d_%d" % (gi, lv)
                    c.op("pool", lambda e: e.tensor_tensor(out=dst[:, sh:15 + TB], in0=cur[:, sh:15 + TB],
                                                           in1=cur[:, 0:15 + TB - sh], op=ALU.add), r=[curk], w=[dk])
                    cur, curk = dst, dk
                c.op("dve", lambda e: e.tensor_scalar(out=pacc[gi][:], in0=plv[gi][0][:, 15:15 + TB],
                                                      scalar1=psel[:, gi, 0:1], scalar2=None, op0=ALU.mult),
                     r=["plv%d_0" % gi, "psel"], w=["pacc" + Gk])
                for lv in range(1, 4):
                    c.op("dve", lambda e: e.scalar_tensor_tensor(out=pacc[gi][:], in0=plv[gi][lv][:, 15:15 + TB],
                                                                 scalar=psel[:, gi, lv:lv + 1], in1=pacc[gi][:],
                                                                 op0=ALU.mult, op1=ALU.add),
                         r=["plv%d_%d" % (gi, lv), "psel", "pacc" + Gk], w=["pacc" + Gk])
                if b == 0:
                    c.op("dve", lambda e: e.tensor_tensor(out=pm[gi][:], in0=pacc[gi][:], in1=invdiv[:, gi, :],
                                                          op=ALU.mult), r=["pacc" + Gk, "invdiv"], w=["pm" + Gk])
                    c.op("dve", lambda e: e.tensor_tensor(out=pd[gi][:], in0=pm[gi][:], in1=Xp[gi][:, 15:15 + TB],
                                                          op=ALU.subtract), r=["pm" + Gk, "Xp" + Gk], w=["pd" + Gk])
                else:
                    c.op("dve", lambda e: e.scalar_tensor_tensor(out=pd[gi][:], in0=pacc[gi][:],
                                                                 scalar=pv(29, gi), in1=Xp[gi][:, 15:15 + TB],
                                                                 op0=ALU.mult, op1=ALU.subtract),
                         r=["pacc" + Gk, "Xp" + Gk, "pvec"], w=["pd" + Gk])
                c.op("pe", lambda e: e.matmul(bk["D"][:], poolw[:, gi, :], pd[gi][:], start=True, stop=True),
                     r=["poolw", "pd" + Gk], w=["bkD"])
                c.op("act", lambda e: e.activation(out=po[gi][:], in_=bk["D"][:], func=AF.Identity, scale=pv(22, gi)),
                     r=["bkD", "pvec"], w=["po" + Gk])
                c.dma("sp", ypl[gi * 128:(gi + 1) * 128, t0:t0 + TB], po[gi][:], r=["po" + Gk])

            dbg(3)
            for ci in range(CPB):
                cc = chunk_counter[0]; chunk_counter[0] += 1
                pp = cc % 2
                cs = slice(ci * CH, (ci + 1) * CH)
                tm = tokmaj[pp]; sa = SA[pp]; sbb = SB_[pp]; sc = SC[pp]
                tmk, sak, sbk, sck = "tokmaj%d" % pp, "SA%d" % pp, "SB%d" % pp, "SC%d" % pp
                for hp in P2:
                    H = str(hp)
                    for j, (src, sk) in enumerate(((Bt[hp], "Bt" + H), (Kt[hp], "Kt" + H), (Vb[hp], "Vb" + H))):
                        c.op("pe", lambda e, src=src, j=j, hp=hp: e.transpose(
                            trp[0:64, j * 256 + hp * 128: j * 256 + (hp + 1) * 128], src[:, cs], identb[:]),
                            r=[sk, "identb"], w=["bktrp"])
                c.op("act", lambda e: e.copy(out=tm[:], in_=trp[0:64, 0:768]), r=["bktrp"], w=[tmk])
                dbg(4)
                def ARv(h, lo, hi):
                    hp_ = h // 2
                    t_ = ARt[hp_] if h % 2 == 0 else ARo[hp_]
                    return t_[0:64, ci * 128 + lo: ci * 128 + hi]
                def Bv(h):
                    hp_ = h // 2
                    return (Bt[hp_] if h % 2 == 0 else Bo[hp_])[0:64, cs]
                def Kv(h):
                    hp_ = h // 2
                    return (Kt[hp_] if h % 2 == 0 else Ko[hp_])[0:64, cs]
                ARk = lambda h: ("ARt%d" if h % 2 == 0 else "ARo%d") % (h // 2)
                Bk = lambda h: ("Bt%d" if h % 2 == 0 else "Bo%d") % (h // 2)
                Kk = lambda h: ("Kt%d" if h % 2 == 0 else "Ko%d") % (h // 2)
                for h in range(4):
                    c.op("pe", lambda e: e.matmul(bk["A"][0:64, h * 128:(h + 1) * 128], Bv(h), ARv(h, 0, 128),
                                                  start=True, stop=True), r=[Bk(h), ARk(h)], w=["bkA"])
                    c.op("pe", lambda e: e.matmul(bk["B"][0:64, h * 128:(h + 1) * 128], Kv(h), ARv(h, 0, 128),
                                                  start=True, stop=True), r=[Kk(h), ARk(h)], w=["bkB"])
                    c.op("pe", lambda e: e.matmul(bk["C"][0:64, h * 64:(h + 1) * 64], ARv(h, 0, 64), Bv(h),
                                                  start=True, stop=True), r=[Bk(h), ARk(h)], w=["bkC"])
                c.op("dve", lambda e: e.tensor_tensor(out=sa[:], in0=bk["A"][0:64, :], in1=maskA[:], op=ALU.mult),
                     r=["bkA", "maskA"], w=[sak])
                c.op("dve", lambda e: e.tensor_tensor(out=sbb[:], in0=bk["B"][0:64, :], in1=maskA[:], op=ALU.mult),
                     r=["bkB", "maskA"], w=[sbk])
                c.op("dve", lambda e: e.tensor_tensor(out=sc[:], in0=bk["C"][0:64, 0:256], in1=maskC[:], op=ALU.mult),
                     r=["bkC", "maskC"], w=[sck])
                Nv = lambda h: sa[:, h * 128: h * 128 + 64]
                NTv = lambda h: sc[:, h * 64:(h + 1) * 64]
                dbg(5)
                c.op("pool", lambda e: e.tensor_tensor(
                    out=TT[0][:].rearrange("p (h t) -> p h t", t=64),
                    in0=sa[:].rearrange("p (h x) -> p h x", x=128)[:, :, 0:64],
                    in1=identrep[:].rearrange("p (h t) -> p h t", t=64), op=ALU.add),
                    r=[sak, "identrep"], w=["TT0"])
                for h in range(4):
                    c.op("pe", lambda e: e.matmul(bk["D"][0:64, h * 64:(h + 1) * 64], NTv(h), Nv(h), start=True, stop=True),
                         r=[sak, sck], w=["bkD"])
                    c.op("pe", lambda e: e.matmul(bk["D"][0:64, 256 + h * 64: 256 + (h + 1) * 64], Nv(h), NTv(h),
                                                  start=True, stop=True), r=[sak, sck], w=["bkD"])
                c.op("act", lambda e: e.copy(out=PP[0][:], in_=bk["D"][0:64, :]), r=["bkD"], w=["PP0"])
                for lv in range(1, 6):
                    pi = (lv - 1) % 2; po_ = lv % 2
                    Pv = lambda h: PP[pi][:, h * 64:(h + 1) * 64]
                    PTv = lambda h: PP[pi][:, 256 + h * 64: 256 + (h + 1) * 64]
                    TTv = lambda h: TT[pi][:, h * 64:(h + 1) * 64]
                    for h in range(4):
                        c.op("pe", lambda e: e.matmul(bk["E"][0:64, h * 64:(h + 1) * 64], identb[0:64, 0:64], TTv(h),
                                                      start=True, stop=False), r=["identb", "TT%d" % pi], w=["bkE"])
                        c.op("pe", lambda e: e.matmul(bk["E"][0:64, h * 64:(h + 1) * 64], PTv(h), TTv(h),
                                                      start=False, stop=True), r=["PP%d" % pi, "TT%d" % pi], w=["bkE"])
                    if lv < 5:
                        for h in range(4):
                            c.op("pe", lambda e: e.matmul(bk["D"][0:64, h * 64:(h + 1) * 64], PTv(h), Pv(h),
                                                          start=True, stop=True), r=["PP%d" % pi], w=["bkD"])
                            c.op("pe", lambda e: e.matmul(bk["D"][0:64, 256 + h * 64: 256 + (h + 1) * 64], Pv(h), PTv(h),
                                                          start=True, stop=True), r=["PP%d" % pi], w=["bkD"])
                    c.op("dve", lambda e: e.tensor_copy(TT[po_][:], bk["E"][0:64, 0:256]), r=["bkE"], w=["TT%d" % po_])
                    if lv < 5:
                        c.op("act", lambda e: e.copy(out=PP[po_][:], in_=bk["D"][0:64, :]), r=["bkD"], w=["PP%d" % po_])
                dbg(6)
                TTf = TT[1]; TTk = "TT1"
                Mold = [M0b[h][pp] for h in range(4)]; Mnew = [M0b[h][1 - pp] for h in range(4)]
                Mok = ["M0b%d_%d" % (h, pp) for h in range(4)]; Mnk = ["M0b%d_%d" % (h, 1 - pp) for h in range(4)]
                Uc = U[pp]; Uk = "U%d" % pp
                for h in range(4):
                    c.op("pe", lambda e: e.matmul(bk["FG"][0:64, h * 64:(h + 1) * 64], ARv(h, 0, 64), Mold[h][:],
                                                  start=True, stop=False), r=[ARk(h), Mok[h]], w=["bkFG"])
                    c.op("pe", lambda e: e.matmul(bk["FG"][0:64, h * 64:(h + 1) * 64], sbb[:, h * 128: h * 128 + 64],
                                                  tm[:, 512 + h * 64: 512 + (h + 1) * 64], start=False, stop=True),
                         r=[sbk, tmk], w=["bkFG"])
                c.op("act", lambda e: e.copy(out=W1[:], in_=bk["FG"][0:64, 0:256]), r=["bkFG"], w=["W1"])
                for h in range(4):
                    c.op("pe", lambda e: e.matmul(bk["FG"][0:64, 256 + h * 64: 256 + (h + 1) * 64],
                                                  TTf[:, h * 64:(h + 1) * 64], W1[:, h * 64:(h + 1) * 64],
                                                  start=True, stop=True), r=[TTk, "W1"], w=["bkFG"])
                c.op("act", lambda e: e.copy(out=Uc[:], in_=bk["FG"][0:64, 256:512]), r=["bkFG"], w=[Uk])
                dbg(7)
                for h in range(4):
                    hp, base = h // 2, (h % 2) * 64
                    o = bk["HI"][base:base + 64, hp * 64:(hp + 1) * 64]
                    c.op("pe", lambda e: e.matmul(o, Mold[h][:], ARv(h, 64, 128), start=True, stop=False),
                         r=[ARk(h), Mok[h]], w=["bkHI"])
                    c.op("pe", lambda e: e.matmul(o, Uc[:, h * 64:(h + 1) * 64], sa[:, h * 128 + 64:(h + 1) * 128],
                                                  start=False, stop=False), r=[Uk, sak], w=["bkHI"])
                    c.op("pe", lambda e: e.matmul(o, tm[:, 512 + h * 64: 512 + (h + 1) * 64],
                                                  sbb[:, h * 128 + 64:(h + 1) * 128], start=False, stop=True),
                         r=[tmk, sbk], w=["bkHI"])
                for h in range(4):
                    o = bk["HI"][0:64, 256 + h * 64: 256 + (h + 1) * 64]
                    c.op("pe", lambda e: e.matmul(o, tm[:, 256 + h * 64: 256 + (h + 1) * 64],
                                                  tm[:, 512 + h * 64: 512 + (h + 1) * 64], start=True, stop=False),
                         r=[tmk], w=["bkHI"])
                    c.op("pe", lambda e: e.matmul(o, tm[:, h * 64:(h + 1) * 64], Uc[:, h * 64:(h + 1) * 64],
                                                  start=False, stop=False), r=[tmk, Uk], w=["bkHI"])
                    c.op("pe", lambda e: e.matmul(o, identb[0:64, 0:64], Mold[h][:],
                                                  start=False, stop=True), r=["identb", Mok[h]], w=["bkHI"])
                for hp in P2:
                    H = str(hp)
                    c.op("act", lambda e: e.copy(out=yraw[hp][:, cs], in_=bk["HI"][:, hp * 64:(hp + 1) * 64]),
                         r=["bkHI"], w=["yraw" + H])
                for h in range(4):
                    hp = h // 2
                    gsrc, gk = (GCs[hp], "GCs%d" % hp) if h % 2 == 0 else (GCo[hp], "GCo%d" % hp)
                    c.op("act", lambda e: e.activation(out=Mnew[h][:], in_=bk["HI"][0:64, 256 + h * 64: 256 + (h + 1) * 64],
                                                       func=AF.Identity, scale=gsrc[0:64, ci:ci + 1]),
                         r=["bkHI", gk], w=[Mnk[h]])

            dbg(8)
            for hp in P2:
                H = str(hp)
                rows = slice(hp * 128, (hp + 1) * 128)
                c.op("pe", lambda e: e.matmul(bk["A"][:], bo[:], yraw[hp][:], start=True, stop=True),
                     r=["bo", "yraw" + H], w=["bkA"])
                c.op("dve", lambda e: e.scalar_tensor_tensor(out=yc[hp][:], in0=bk["A"][:], scalar=-1.0 / 64,
                                                             in1=yraw[hp][:], op0=ALU.mult, op1=ALU.add),
                     r=["bkA", "yraw" + H], w=["yc" + H])
                c.op("pool", lambda e: e.tensor_tensor(out=ysq[hp][:], in0=yc[hp][:], in1=yc[hp][:], op=ALU.mult),
                     r=["yc" + H], w=["ysq"])
                c.op("pe", lambda e: e.matmul(bk["B"][:], bo[:], ysq[hp][:], start=True, stop=True),
                     r=["bo", "ysq"], w=["bkB"])
                c.op("act", lambda e: e.activation(out=yrs[hp][:], in_=bk["B"][:], func=AF.Sqrt, bias=gneps[:, 0:1],
                                                   scale=1.0 / 64), r=["bkB", "gneps"], w=["yrs"])
                c.op("dve", lambda e: e.reciprocal(out=yrs[hp][:], in_=yrs[hp][:]), r=["yrs"], w=["yrs"])
                c.op("dve", lambda e: e.tensor_tensor(out=yc[hp][:], in0=yc[hp][:], in1=yrs[hp][:], op=ALU.mult),
                     r=["yc" + H, "yrs"], w=["yc" + H])
                c.op("dve", lambda e: e.tensor_scalar(out=yc[hp][:], in0=yc[hp][:], scalar1=pv(18, hp), scalar2=pv(20, hp),
                                                      op0=ALU.mult, op1=ALU.add), r=["yc" + H, "pvec"], w=["yc" + H])
                c.op("pool", lambda e: e.tensor_tensor(out=yc[hp][:], in0=yc[hp][:], in1=bonus[hp][:], op=ALU.add),
                     r=["yc" + H, "bonus" + H], w=["yc" + H])
                c.op("pool", lambda e: e.tensor_tensor(out=yo[hp][:], in0=yc[hp][:], in1=gg[hp][:], op=ALU.mult),
                     r=["yc" + H, "gg" + H], w=["yo" + H])
                c.dma("sp", yrw[rows, t0:t0 + TB], yo[hp][:], r=["yo" + H])
    try:
        body()
    except _Stop:
        pass


def build_L2(layer1):
    nc = bass.Bass("TRN2", target_bir_lowering=False)
    dt = lambda name, shape, kind="ExternalInput": nc.dram_tensor(name, list(shape), F32, kind=kind).ap()
    A = dict(rT=dt("rT", [256, SEQ]), kT=dt("kT", [256, SEQ]), vT=dt("vT", [256, SEQ]),
             waT=dt("waT", [128, SEQ]), gdT=dt("gdT", [128, SEQ]), poolT=dt("poolT", [256, SEQ]))
    if layer1:
        A.update(vdT=dt("vdT", [32, SEQ]), vfT=dt("vfT", [256, SEQ]), vup=dt("vup", [32, 256]))
    A.update(pvec=dt("pvec", [128, 32]), wup=dt("wup", [64, 256]), aup=dt("aup", [64, 256]),
             gup=dt("gup", [128, 256]), poolw=dt("poolw", [2, 128, 128]),
             maskA=dt("maskA", [64, 512]), maskC=dt("maskC", [64, 256]), identrep=dt("identrep", [64, 256]),
             scanmask=dt("scanmask", [128, TB]), blockones=dt("blockones", [128, 128]), ident=dt("ident", [128, 128]),
             invdiv=dt("invdiv", [128, 2, TB]), psel=dt("psel", [128, 2, 4]))
    yT = dt("yT", [512, SEQ], "ExternalOutput")
    A["yrw"] = yT[0:256]; A["ypl"] = yT[256:512]
    if not layer1:
        A["vout"] = dt("vout", [256, SEQ], "ExternalOutput")
    c = Ctx(nc)
    c.begin_phase("")
    emit_L2(c, A, layer1)
    c.end_phase()
    c.finish()
    return nc


def l2_inputs(inp, l, g, projT, vfT):
    f = lambda a: np.ascontiguousarray(a, dtype=np.float32)
    mu = inp["mu_shift"][l]
    cs = slice(g * 256, (g + 1) * 256)
    pvec = np.zeros((128, 32), np.float32)
    def put(col, vec512):
        v = np.asarray(vec512)[cs].reshape(2, 128)
        pvec[:, col] = v[0]; pvec[:, col + 1] = v[1]
    put(0, mu[0:512]); put(2, mu[512:1024]); put(4, mu[1024:1536])
    put(6, inp["w0"][l]); put(8, inp["a0"][l]); put(10, inp["k_k"][l]); put(12, inp["k_a"][l])
    put(16, inp["r_k"][l].reshape(512)); put(18, inp["lnx_g"][l]); put(20, inp["lnx_b"][l])
    put(22, inp["pool_scale"][l])
    pvec[:, 24] = mu[1664:1792]; pvec[0:64, 25] = mu[1536:1600]; pvec[64:128, 25] = mu[1600:1664]
    d = dict(rT=f(projT[0:512][cs]), kT=f(projT[512:1024][cs]), vT=f(projT[1024:1536][cs]),
             waT=f(projT[1536:1664]), gdT=f(projT[1664:1792]), poolT=f(projT[1792:2304][cs]),
             wup=f(inp["w_up"][l][:, cs]), aup=f(inp["a_up"][l][:, cs]), gup=f(inp["g_up"][l][:, cs]),
             poolw=f(inp["pool_w"][l][2 * g:2 * g + 2]))
    if l > 0:
        pvec[0:32, 26] = inp["vres_mu"][l - 1]
        put(27, inp["vres_v0"][l - 1])
        d.update(vdT=f(projT[2304:2336]), vfT=f(vfT), vup=f(inp["vres_up"][l - 1][:, cs]))
    d["pvec"] = pvec
    pos = np.arange(1, TB + 1, dtype=np.float32)
    invdiv = np.stack([np.broadcast_to(1.0 / np.minimum(pos, float(POOL_WINDOWS[2 * g + gi])), (128, TB))
                       for gi in range(2)], axis=1)
    d["invdiv"] = f(invdiv)
    psel = np.zeros((128, 2, 4), np.float32)
    for gi in range(2):
        psel[:, gi, 2 * g + gi] = 1.0
        pvec[:, 29 + gi] = 1.0 / POOL_WINDOWS[2 * g + gi]
    d["psel"] = psel
    d.update(l2_consts())
    return d


EPC = 2048
def emit_L0(c, A, layers, nch):
    u_d, v_d, wo_d, pq_d, id_d = A["u"], A["v"], A["w_out"], A["peer_q"], A["ident"]
    ut_o, vb_o, wo_o, pq_o = A["UT"], A["Vb"], A["woutb"], A["pqb"]
    identf = c.sb("identf", [128, 128]); identb = c.sb("identb", [128, 128], BF16)
    uf = [c.sb("uf%d" % i, [128, D]) for i in range(2)]
    ub = [c.sb("ub%d" % i, [128, D], BF16) for i in range(2)]
    uT = [c.sb("uT%d" % i, [128, D], BF16) for i in range(2)]
    vb = [c.sb("vb%d" % i, [128, D], BF16) for i in range(2)]
    wb = [c.sb("wb%d" % i, [128, 8, 128], BF16) for i in range(2)]
    tps = [c.ps("tps%d" % i, [128, 1024], BF16) for i in range(2)]
    c.dma("sp", identf[:], id_d, w=["identf"])
    c.op("dve", lambda e: e.tensor_copy(identb[:], identf[:]), r=["identf"], w=["identb"])
    k = 0
    for l in layers:
        for ch in range(nch):
            s_ = k % 2; k += 1
            S = str(s_)
            rows = slice(ch * 128, (ch + 1) * 128)
            c.dma("sp", uf[s_][:], u_d[l, rows, :], w=["uf" + S])
            c.dma("pool", vb[s_][:], v_d[l, rows, :], w=["vb" + S])
            c.dma("sp", vb_o[l, rows, :], vb[s_][:], r=["vb" + S])
            eng = "act" if s_ == 0 else "dve"
            if eng == "act":
                c.op("act", lambda e: e.copy(out=ub[s_][:], in_=uf[s_][:]), r=["uf" + S], w=["ub" + S])
            else:
                c.op("dve", lambda e: e.tensor_copy(ub[s_][:], uf[s_][:]), r=["uf" + S], w=["ub" + S])
            for dc in range(8):
                c.op("pe", lambda e: e.transpose(tps[s_][:, dc * 128:(dc + 1) * 128], ub[s_][:, dc * 128:(dc + 1) * 128],
                                                 identb[:]), r=["ub" + S, "identb"], w=["tps" + S])
            if eng == "act":
                c.op("act", lambda e: e.copy(out=uT[s_][:], in_=tps[s_][:]), r=["tps" + S], w=["uT" + S])
            else:
                c.op("dve", lambda e: e.tensor_copy(uT[s_][:], tps[s_][:]), r=["tps" + S], w=["uT" + S])
            c.dma("sp", ut_o[l, ch], uT[s_][:].rearrange("p (dc e) -> p dc e", e=128), r=["uT" + S])
        for cc in range(8):
            s_ = k % 2; k += 1
            S = str(s_)
            c.dma("pool", vb[s_][:], wo_d[l, cc * 128:(cc + 1) * 128, :], w=["vb" + S])
            c.dma("sp", wo_o[l, cc], vb[s_][:], r=["vb" + S])
        pqv = pq_d[l].rearrange("(dc p) j -> p dc j", p=128)
        for jc in range(16):
            s_ = k % 2; k += 1
            S = str(s_)
            c.dma("pool", wb[s_][:], pqv[:, :, jc * 128:(jc + 1) * 128], w=["wb" + S])
            c.dma("sp", pq_o[l, jc], wb[s_][:], r=["wb" + S])


def build_L0():
    nc = bass.Bass("TRN2", target_bir_lowering=False)
    di = lambda name, shape: nc.dram_tensor(name, list(shape), F32, kind="ExternalInput").ap()
    do = lambda name, shape: nc.dram_tensor(name, list(shape), BF16, kind="ExternalOutput").ap()
    A = dict(u=di("u", [2, EPC, D]), v=di("v", [2, EPC, D]), w_out=di("w_out", [2, D, D]),
             peer_q=di("peer_q", [2, D, 2048]), ident=di("ident", [128, 128]),
             UT=do("UT", [2, EPC // 128, 128, 8, 128]), Vb=do("Vb", [2, EPC, D]),
             woutb=do("woutb", [2, 8, 128, D]), pqb=do("pqb", [2, 16, 128, 8, 128]))
    c = Ctx(nc)
    c.begin_phase("")
    emit_L0(c, A, range(2), EPC // 128)
    c.end_phase()
    c.finish()
    return nc


TS = 256
NST = TOK // TS
NEG = -1.0e30


def emit_L3(c, A, final):
    nc = c.nc
    x_d, c_d, adaw_d, adab_d, g_d, lnf_d = A["x"], A["c_fm"], A["ada_w"], A["ada_b_fm"], A["ln_g_fm"], A["lnf_fm"]
    wo_d, pq_d, keys_d, ut_d, vb_d, id_d, ones_d, xo_d = (A["woutb"], A["pqb"], A["keys"], A["UT"], A["Vb"],
                                                          A["ident"], A["ones"], A["xo"])
    ysel = "hsel" in A
    sb = c.sb
    c.adaw_sb = sb("adaw", [128, 8, 256])
    c_sb = sb("c_sb", [128, 8]); sig_c = sb("sig_c", [128, 8]); silu_c = sb("silu_c", [128, 8])
    adab = sb("adab", [128, 48]); lng = sb("lng", [128, 8]); lnf = sb("lnf", [128, 8])
    modT = sb("modT", [128, 32]); effs = sb("effs", [128, 8]); epsb = sb("epsb", [128, 1])
    identf = sb("identf", [128, 128]); identb = sb("identb", [128, 128], BF16); onesf = sb("onesf", [128, 128])
    diag = sb("diag", [128, 512])
    g1bc = sb("g1bc", [128, D]); g2bc = sb("g2bc", [128, D]); lnfbc = sb("lnfbc", [128, D])
    kf = sb("kf", [128, 16, 128]); keysT = sb("keysT", [128, 16, 128], BF16)
    wst = [sb("wst%d" % i, [128, D], BF16) for i in range(3)]
    x1 = [sb("x1_%d" % i, [128, D]) for i in range(2)]
    yTb = sb("yTb", [128, 8, TS], BF16)
    if ysel:
        ysa = sb("ysa", [128, 8, TS], BF16); ysb = sb("ysb", [128, 8, TS], BF16); hsel = sb("hsel_sb", [128, 2])
    junk = sb("junk", [128, D], BF16); xn = sb("xn", [128, D], BF16)
    ssq = sb("ssq", [128, 4]); rstd = sb("rstd", [128, 4])
    h2T = sb("h2T", [128, 8, TS], BF16); qT = sb("qT", [128, 16, TS], BF16)
    ST = sb("ST", [128, 16, TS]); SC = sb("SC", [128, 2048])
    wk = [sb("wk%d" % i, [128, 128]) for i in range(2)]
    stop = sb("stop", [128, 16, 16]); candh = [sb("candh%d" % i, [128, 256]) for i in range(2)]
    cwk = [sb("cwk%d" % i, [128, 256]) for i in range(2)]; ctop = sb("ctop", [128, 8, 16])
    negm = sb("negm", [128, 8]); esub = sb("esub", [128, 128]); exs = sb("exs", [128, 128])
    Zs = sb("Zs", [128, 8]); invZ = sb("invZ", [128, 8])
    PACK = sb("PACK", [128, 4, 128]); PKT = sb("PKT", [128, 4, TS])
    rep = [sb("rep%d" % i, [128, 256]) for i in range(2)]
    E0 = [sb("E0_%d" % i, [128, 128], BF16) for i in range(2)]
    D1 = [sb("D1_%d" % i, [128, 128], BF16) for i in range(2)]
    ex1 = [sb("ex1_%d" % i, [128, 128]) for i in range(2)]
    gate = sb("gate_all", [128, 128, TS], BF16)
    ut4 = [sb("ut4_%d" % i, [128, 2, D], BF16) for i in range(2)]
    vt4 = [sb("vt4_%d" % i, [128, 2, D], BF16) for i in range(2)]
    NCH = 2
    ge = [sb("ge%d" % i, [128, TS], BF16) for i in range(2)]
    AT = [sb("AT%d" % i, [128, TS], BF16) for i in range(2)]
    tmpo = sb("tmpo", [128, 512])
    B = [c.ps("b%d" % i, [128, 512]) for i in range(8)]
    Bb = c.ps

    ld = lambda dst, src, key: c.dma("sp", dst, src, w=[key])
    ld(c_sb[:], c_d, "c_sb"); ld(adab[:], adab_d, "adab"); ld(lng[:], g_d, "lng"); ld(lnf[:], lnf_d, "lnf")
    ld(identf[:], id_d, "identf"); ld(onesf[:], ones_d, "onesf")
    if ysel:
        ld(hsel[:], A["hsel"], "hsel")
    ld(kf[:], keys_d.rearrange("h p k d -> k (h p) d"), "kf")
    c.op("dve", lambda e: e.tensor_copy(identb[:], identf[:]), r=["identf"], w=["identb"])
    c.op("dve", lambda e: e.memset(epsb[:], NORM_EPS), w=["epsb"])
    c.op("act", lambda e: e.activation(out=sig_c[:], in_=c_sb[:], func=AF.Sigmoid), r=["c_sb"], w=["sig_c"])
    c.op("dve", lambda e: e.tensor_tensor(out=silu_c[:], in0=c_sb[:], in1=sig_c[:], op=ALU.mult),
         r=["c_sb", "sig_c"], w=["silu_c"])
    emit_mod_fm(c, adaw_d, 0, 32, silu_c, adab, 16, modT[:, 0:32], B[0], "modT")
    c.op("dve", lambda e: e.scalar_tensor_tensor(out=effs[:], in0=modT[:, 16:24], scalar=1.0, in1=lng[:],
                                                  op0=ALU.add, op1=ALU.mult), r=["modT", "lng"], w=["effs"])

    def bcast_rows(vec8, vkey, out_tile, okey):
        for hf in range(2):
            for q in range(4):
                jc = hf * 4 + q
                c.op("dve", lambda e: e.tensor_scalar(out=diag[:, q * 128:(q + 1) * 128], in0=identf[:],
                                                      scalar1=vec8[:, jc:jc + 1], scalar2=None, op0=ALU.mult),
                     r=["identf", vkey], w=["diag%d" % q])
                c.op("pe", lambda e: e.matmul(B[1][:, q * 128:(q + 1) * 128], onesf[:], diag[:, q * 128:(q + 1) * 128],
                                              start=True, stop=True), r=["onesf", "diag%d" % q], w=["b1"])
            c.op("act", lambda e: e.copy(out=out_tile[:, hf * 512:(hf + 1) * 512], in_=B[1][:]), r=["b1"], w=[okey])

    bcast_rows(modT[:, 0:8], "modT", g1bc, "g1bc")
    bcast_rows(modT[:, 24:32], "modT", g2bc, "g2bc")
    if final:
        bcast_rows(lnf, "lnf", lnfbc, "lnfbc")
    for q4 in range(4):
        for q in range(4):
            hp16 = q4 * 4 + q
            c.op("pe", lambda e: e.transpose(B[2][:, q * 128:(q + 1) * 128], kf[:, hp16, :], identf[:]),
                 r=["kf", "identf"], w=["b2"])
        c.op("act", lambda e: e.copy(out=keysT[:, q4 * 4:(q4 + 1) * 4, :],
                                     in_=B[2][:].rearrange("p (q k) -> p q k", k=128)), r=["b2"], w=["keysT"])
    wsi = [0]

    def wstream(src_ap):
        i = wsi[0] % 3; wsi[0] += 1
        c.dma("sp", wst[i][:], src_ap, w=["wst%d" % i])
        return wst[i], "wst%d" % i

    def body():
        dbg(1)
        for st in range(DBG_NST or NST):
            tok0 = st * TS
            if not ysel:
                c.dma("pool", yTb[:], A["yT"].rearrange("(cc p) t -> p cc t", p=128)[:, :, tok0:tok0 + TS], w=["yTb"])
            else:
                c.dma("pool", ysa[:], A["yA"].rearrange("(cc p) t -> p cc t", p=128)[:, :, tok0:tok0 + TS], w=["ysa"])
                c.dma("pool", ysb[:], A["yB"].rearrange("(cc p) t -> p cc t", p=128)[:, :, tok0:tok0 + TS], w=["ysb"])
                c.op("dve", lambda e: e.tensor_scalar(out=ysa[:], in0=ysa[:], scalar1=hsel[:, 0:1], scalar2=None,
                                                      op0=ALU.mult), r=["ysa", "hsel"], w=["ysa"])
                c.op("dve", lambda e: e.scalar_tensor_tensor(out=yTb[:], in0=ysb[:], scalar=hsel[:, 1:2], in1=ysa[:],
                                                             op0=ALU.mult, op1=ALU.add),
                     r=["ysa", "ysb", "hsel"], w=["yTb"])
            wts = []
            for tt in range(2):
                X = x1[tt]; Xk = "x1_%d" % tt
                c.dma("sp", X[:], x_d[tok0 + tt * 128: tok0 + (tt + 1) * 128, :], w=[Xk])
                for cc in range(8):
                    wt, wkk = wstream(wo_d[cc])
                    for hf in range(2):
                        c.op("pe", lambda e: e.matmul(B[hf][:], yTb[:, cc, tt * 128:(tt + 1) * 128],
                                                      wt[:, hf * 512:(hf + 1) * 512], start=(cc == 0), stop=(cc == 7)),
                             r=["yTb", wkk], w=["b%d" % hf])
                for hf in range(2):
                    c.op("dve", lambda e: e.tensor_tensor(out=tmpo[:], in0=B[hf][:], in1=g1bc[:, hf * 512:(hf + 1) * 512],
                                                          op=ALU.mult), r=["b%d" % hf, "g1bc"], w=["tmpo"])
                    c.op("dve", lambda e: e.tensor_tensor(out=X[:, hf * 512:(hf + 1) * 512], in0=tmpo[:],
                                                          in1=X[:, hf * 512:(hf + 1) * 512], op=ALU.add),
                         r=["tmpo", Xk], w=[Xk])
                c.op("dve", lambda e: e.memset(ssq[:, tt:tt + 1], 0.0), w=["ssq"])
                c.op("act", lambda e: e.activation(out=junk[:], in_=X[:], func=AF.Square, accum_out=ssq[:, tt:tt + 1]),
                     r=[Xk, "ssq"], w=["junk", "ssq"])
                c.op("act", lambda e: e.activation(out=rstd[:, tt:tt + 1], in_=ssq[:, tt:tt + 1], func=AF.Sqrt,
                                                   bias=epsb[:, 0:1], scale=1.0 / D), r=["ssq", "epsb"], w=["rstd"])
                c.op("dve", lambda e: e.reciprocal(out=rstd[:, tt:tt + 1], in_=rstd[:, tt:tt + 1]), r=["rstd"], w=["rstd"])
                c.op("dve", lambda e: e.tensor_scalar(out=xn[:], in0=X[:], scalar1=rstd[:, tt:tt + 1], scalar2=None,
                                                      op0=ALU.mult), r=[Xk, "rstd"], w=["xn"])
                tpb = B[2 + tt][:].bitcast(BF16)
                for dc in range(8):
                    c.op("pe", lambda e: e.transpose(tpb[:, dc * 128:(dc + 1) * 128], xn[:, dc * 128:(dc + 1) * 128],
                                                     identb[:]), r=["xn", "identb"], w=["b%d" % (2 + tt)])
                for dc in range(8):
                    c.op("act", lambda e: e.activation(out=h2T[:, dc, tt * 128:(tt + 1) * 128],
                                                       in_=tpb[:, dc * 128:(dc + 1) * 128], func=AF.Identity,
                                                       bias=modT[:, 8 + dc: 9 + dc], scale=effs[:, dc:dc + 1]),
                         r=["b%d" % (2 + tt), "modT", "effs"], w=["h2T"])
            dbg(2)
            for hp16 in range(16):
                wt, wkk = wstream(pq_d[hp16].rearrange("p dc j -> p (dc j)"))
                bi = 4 + (hp16 // 2) % 2
                sub = hp16 % 2
                for dc in range(8):
                    c.op("pe", lambda e: e.matmul(B[bi][:, sub * TS:(sub + 1) * TS], wt[:, dc * 128:(dc + 1) * 128],
                                                  h2T[:, dc, :], start=(dc == 0), stop=(dc == 7)),
                         r=[wkk, "h2T"], w=["b%d" % bi])
                if sub == 1:
                    c.op("act", lambda e: e.copy(out=qT[:, hp16 - 1: hp16 + 1, :],
                                                 in_=B[bi][:].rearrange("p (s t) -> p s t", t=TS)),
                         r=["b%d" % bi], w=["qT"])
            for hp16 in range(16):
                bi = 6 + (hp16 // 2) % 2
                sub = hp16 % 2
                c.op("pe", lambda e: e.matmul(B[bi][:, sub * TS:(sub + 1) * TS], keysT[:, hp16, :], qT[:, hp16, :],
                                              start=True, stop=True), r=["keysT", "qT"], w=["b%d" % bi])
                if sub == 1:
                    c.op("dve", lambda e: e.tensor_copy(ST[:, hp16 - 1: hp16 + 1, :],
                                                        B[bi][:].rearrange("p (s t) -> p s t", t=TS)),
                         r=["b%d" % bi], w=["ST"])
            dbg(3)
            stop4 = stop[:].rearrange("p (h two) a -> p h two a", two=2)
            for tt in range(2):
                for q4 in range(4):
                    for q in range(4):
                        hp16 = q4 * 4 + q
                        c.op("pe", lambda e: e.transpose(B[q4][:, q * 128:(q + 1) * 128],
                                                         ST[:, hp16, tt * 128:(tt + 1) * 128], identf[:]),
                             r=["ST", "identf"], w=["b%d" % q4])
                    c.op("act", lambda e: e.copy(out=SC[:, q4 * 512:(q4 + 1) * 512], in_=B[q4][:]),
                         r=["b%d" % q4], w=["SC%d" % q4])
                for hp16 in range(16):
                    w_ = wk[hp16 % 2]; wkk = "wk%d" % (hp16 % 2)
                    scv = SC[:, hp16 * 128:(hp16 + 1) * 128]; sck = "SC%d" % (hp16 // 4)
                    c.op("dve", lambda e: e.max(out=stop[:, hp16, 0:8], in_=scv), r=[sck], w=["stopA%d" % hp16])
                    c.op("dve", lambda e: e.match_replace(out=w_[:], in_to_replace=stop[:, hp16, 0:8], in_values=scv,
                                                          imm_value=NEG), r=[sck, "stopA%d" % hp16], w=[wkk])
                    c.op("dve", lambda e: e.max(out=stop[:, hp16, 8:16], in_=w_[:]), r=[wkk], w=["stopB%d" % hp16])
                stopkeys = ["stopA%d" % i for i in range(16)] + ["stopB%d" % i for i in range(16)]
                for h in range(8):
                    ch_ = candh[h % 2]; chk = "candh%d" % (h % 2)
                    cw_ = cwk[h % 2]; cwkk = "cwk%d" % (h % 2)
                    c.op("dve", lambda e: e.tensor_tensor(
                        out=ch_[:].rearrange("p (a b) -> p a b", b=16),
                        in0=stop4[:, h, 0, :].unsqueeze(2).broadcast_to([128, 16, 16]),
                        in1=stop4[:, h, 1, :].unsqueeze(1).broadcast_to([128, 16, 16]), op=ALU.add),
                        r=["stopA%d" % (2 * h), "stopB%d" % (2 * h), "stopA%d" % (2 * h + 1), "stopB%d" % (2 * h + 1)],
                        w=[chk])
                    c.op("dve", lambda e: e.max(out=ctop[:, h, 0:8], in_=ch_[:]), r=[chk], w=["ctopA%d" % h])
                    c.op("dve", lambda e: e.match_replace(out=cw_[:], in_to_replace=ctop[:, h, 0:8], in_values=ch_[:],
                                                          imm_value=NEG), r=[chk, "ctopA%d" % h], w=[cwkk])
                    c.op("dve", lambda e: e.max(out=ctop[:, h, 8:16], in_=cw_[:]), r=[cwkk], w=["ctopB%d" % h])
                ctk = ["ctopA%d" % h for h in range(8)] + ["ctopB%d" % h for h in range(8)]
                c.op("dve", lambda e: e.tensor_scalar(out=negm[:], in0=ctop[:, :, 0], scalar1=-1.0, scalar2=None,
                                                      op0=ALU.mult), r=ctk, w=["negm"])
                c.op("dve", lambda e: e.tensor_tensor(out=esub[:].rearrange("p (h a) -> p h a", a=16), in0=ctop[:],
                                                      in1=negm[:].unsqueeze(2).broadcast_to([128, 8, 16]), op=ALU.add),
                     r=ctk + ["negm"], w=["esub"])
                c.op("act", lambda e: e.activation(out=exs[:], in_=esub[:], func=AF.Exp), r=["esub"], w=["exs"])
                c.op("dve", lambda e: e.reduce_sum(out=Zs[:], in_=exs[:].rearrange("p (h a) -> p h a", a=16),
                                                   axis=mybir.AxisListType.X), r=["exs"], w=["Zs"])
                c.op("dve", lambda e: e.reciprocal(out=invZ[:], in_=Zs[:]), r=["Zs"], w=["invZ"])
                P3 = lambda j: PACK[:, j, :].rearrange("p (h a) -> p h a", a=16)
                c.op("dve", lambda e: e.tensor_copy(P3(0), stop4[:, :, 0, :]), r=stopkeys, w=["PACK0"])
                c.op("dve", lambda e: e.tensor_tensor(out=P3(1), in0=ctop[:, :, 15].unsqueeze(2).broadcast_to([128, 8, 16]),
                                                      in1=stop4[:, :, 0, :], op=ALU.subtract), r=stopkeys + ctk, w=["PACK1"])
                c.op("dve", lambda e: e.tensor_tensor(out=P3(2), in0=stop4[:, :, 0, :],
                                                      in1=negm[:].unsqueeze(2).broadcast_to([128, 8, 16]), op=ALU.add),
                     r=stopkeys + ["negm"], w=["PACK2"])
                c.op("dve", lambda e: e.tensor_copy(P3(3), invZ[:].unsqueeze(2).broadcast_to([128, 8, 16])),
                     r=["invZ"], w=["PACK3"])
                for j in range(4):
                    c.op("pe", lambda e: e.transpose(B[4][:, j * 128:(j + 1) * 128], PACK[:, j, :], identf[:]),
                         r=["PACK%d" % j, "identf"], w=["b4"])
                c.op("act", lambda e: e.copy(out=PKT[:, :, tt * 128:(tt + 1) * 128],
                                             in_=B[4][:].rearrange("p (j t) -> p j t", t=128)), r=["b4"], w=["PKT"])
            dbg(4)
            for t in range(TS):
                s_ = t % 2; S = str(s_)
                gs = (t // 4) % 2
                c.op("pool", lambda e: e.tensor_copy(
                    rep[s_][:].rearrange("p (two h a) -> p two h a", two=2, a=16),
                    ST[:, :, t].rearrange("p (h two) -> p two h", two=2).unsqueeze(3).broadcast_to([128, 2, 8, 16])),
                    r=["ST"], w=["rep" + S])
                c.op("pe", lambda e: e.transpose(B[0 + s_][:, 0:128], rep[s_][:, 0:128], identf[:]),
                     r=["rep" + S, "identf"], w=["b%d" % (0 + s_)])
                c.op("pe", lambda e: e.transpose(B[2 + s_][:, 0:128], rep[s_][:, 128:256], identf[:]),
                     r=["rep" + S, "identf"], w=["b%d" % (2 + s_)])
                c.op("dve", lambda e: e.tensor_scalar(out=E0[s_][:], in0=B[0 + s_][:, 0:128], scalar1=PKT[:, 0, t:t + 1],
                                                      scalar2=PKT[:, 3, t:t + 1], op0=ALU.is_equal, op1=ALU.mult),
                     r=["b%d" % (0 + s_), "PKT"], w=["E0_" + S])
                c.op("act", lambda e: e.activation(out=ex1[s_][:], in_=B[2 + s_][:, 0:128], func=AF.Exp,
                                                   bias=PKT[:, 2, t:t + 1]), r=["b%d" % (2 + s_), "PKT"], w=["ex1_" + S])
                c.op("dve", lambda e: e.scalar_tensor_tensor(out=D1[s_][:], in0=B[2 + s_][:, 0:128],
                                                             scalar=PKT[:, 1, t:t + 1], in1=ex1[s_][:],
                                                             op0=ALU.is_ge, op1=ALU.mult),
                     r=["b%d" % (2 + s_), "PKT", "ex1_" + S], w=["D1_" + S])
                c.op("pe", lambda e: e.matmul(B[4 + gs][:, (t % 4) * 128:(t % 4 + 1) * 128], D1[s_][:], E0[s_][:],
                                              start=True, stop=True), r=["D1_" + S, "E0_" + S], w=["b%d" % (4 + gs)])
                if t % 4 == 3:
                    c.op("act", lambda e: e.copy(out=gate[:, :, t - 3:t + 1].rearrange("p i t -> p t i"),
                                                 in_=B[4 + gs][:].rearrange("p (t i) -> p t i", i=128)),
                         r=["b%d" % (4 + gs)], w=["gate"])
            dbg(5)
            for g4 in range(128 // NCH):
                bf_ = g4 % 2; Bf = str(bf_)
                c.dma("sp", ut4[bf_][:], ut_d[g4 * NCH:(g4 + 1) * NCH].rearrange("c p dc e -> p c (dc e)"),
                      w=["ut4_" + Bf])
                c.dma("sp", vt4[bf_][:], vb_d[g4 * NCH * 128:(g4 + 1) * NCH * 128, :].rearrange("(c p) d -> p c d", p=128),
                      w=["vt4_" + Bf])
                for cix in range(NCH):
                    i0 = g4 * NCH + cix
                    pb = 4 + i0 % 2
                    gb = i0 % 2
                    for dc in range(8):
                        c.op("pe", lambda e: e.matmul(B[pb][:, 0:TS], ut4[bf_][:, cix, dc * 128:(dc + 1) * 128],
                                                      h2T[:, dc, :], start=(dc == 0), stop=(dc == 7)),
                             r=["ut4_" + Bf, "h2T"], w=["b%d" % pb])
                    c.op("act", lambda e: e.activation(out=ge[gb][:], in_=B[pb][:, 0:TS], func=AF.Gelu),
                         r=["b%d" % pb], w=["ge%d" % gb])
                    c.op("pool", lambda e: e.tensor_tensor(out=AT[gb][:], in0=ge[gb][:], in1=gate[:, i0, :], op=ALU.mult),
                         r=["ge%d" % gb, "gate"], w=["AT%d" % gb])
                    for tt in range(2):
                        for hf in range(2):
                            c.op("pe", lambda e: e.matmul(B[tt * 2 + hf][:], AT[gb][:, tt * 128:(tt + 1) * 128],
                                                          vt4[bf_][:, cix, hf * 512:(hf + 1) * 512],
                                                          start=(i0 == 0), stop=(i0 == 127)),
                                 r=["AT%d" % gb, "vt4_" + Bf], w=["b%d" % (tt * 2 + hf)])
            dbg(6)
            for tt in range(2):
                X = x1[tt]; Xk = "x1_%d" % tt
                for hf in range(2):
                    c.op("dve", lambda e: e.tensor_tensor(out=tmpo[:], in0=B[tt * 2 + hf][:],
                                                          in1=g2bc[:, hf * 512:(hf + 1) * 512], op=ALU.mult),
                         r=["b%d" % (tt * 2 + hf), "g2bc"], w=["tmpo"])
                    c.op("dve", lambda e: e.tensor_tensor(out=X[:, hf * 512:(hf + 1) * 512], in0=tmpo[:],
                                                          in1=X[:, hf * 512:(hf + 1) * 512], op=ALU.add),
                         r=["tmpo", Xk], w=[Xk])
                if final:
                    c.op("dve", lambda e: e.memset(ssq[:, 2 + tt:3 + tt], 0.0), w=["ssq"])
                    c.op("act", lambda e: e.activation(out=junk[:], in_=X[:], func=AF.Square,
                                                       accum_out=ssq[:, 2 + tt:3 + tt]), r=[Xk, "ssq"], w=["junk", "ssq"])
                    c.op("act", lambda e: e.activation(out=rstd[:, 2 + tt:3 + tt], in_=ssq[:, 2 + tt:3 + tt], func=AF.Sqrt,
                                                       bias=epsb[:, 0:1], scale=1.0 / D), r=["ssq", "epsb"], w=["rstd"])
                    c.op("dve", lambda e: e.reciprocal(out=rstd[:, 2 + tt:3 + tt], in_=rstd[:, 2 + tt:3 + tt]),
                         r=["rstd"], w=["rstd"])
                    c.op("dve", lambda e: e.scalar_tensor_tensor(out=X[:], in0=X[:], scalar=rstd[:, 2 + tt:3 + tt],
                                                                 in1=lnfbc[:], op0=ALU.mult, op1=ALU.mult),
                         r=[Xk, "rstd", "lnfbc"], w=[Xk])
                c.dma("sp", xo_d[tok0 + tt * 128: tok0 + (tt + 1) * 128, :], X[:], r=[Xk])

    try:
        body()
    except _Stop:
        pass


def build_L3(final):
    nc = bass.Bass("TRN2", target_bir_lowering=False)
    dt = lambda name, shape, d=F32, kind="ExternalInput": nc.dram_tensor(name, list(shape), d, kind=kind).ap()
    A = dict(x=dt("x", [TOK, D]), yT=dt("yT", [D, TOK]), c_fm=dt("c_fm", [128, 8]), ada_w=dt("ada_w", [D, 4 * D]),
             ada_b_fm=dt("ada_b_fm", [128, 48]), ln_g_fm=dt("ln_g_fm", [128, 8]), lnf_fm=dt("lnf_fm", [128, 8]),
             woutb=dt("woutb", [8, 128, D], BF16), pqb=dt("pqb", [16, 128, 8, 128], BF16),
             keys=dt("keys", [8, 2, 128, 128]), UT=dt("UT", [128, 128, 8, 128], BF16), Vb=dt("Vb", [128 * 128, D], BF16),
             ident=dt("ident", [128, 128]), ones=dt("ones", [128, 128]),
             xo=dt("xo", [TOK, D], F32, "ExternalOutput"))
    c = Ctx(nc)
    c.begin_phase("")
    emit_L3(c, A, final)
    c.end_phase()
    c.finish()
    return nc


NJC = 19


def build_fused():
    nc = bass.Bass("TRN2", target_bir_lowering=False)
    di = lambda name, shape, d=F32: nc.dram_tensor(name, list(shape), d, kind="ExternalInput").ap()
    it = lambda name, shape, d=F32: nc.dram_tensor(name, list(shape), d).ap()
    x_seq = di("x_seq", [SEQ, D]); x_mine = di("x_mine", [TOK, D]); c_fm = di("c_fm", [128, 8]); hsel = di("hsel", [128, 2])
    ada_w = di("ada_w", [2, D, 6 * D]); ada_b_fm = di("ada_b_fm", [2, 128, 48])
    ln1 = di("ln1_g_fm", [2, 128, 8]); ln2 = di("ln2_g_fm", [2, 128, 8]); lnf = di("lnf_fm", [128, 8])
    w_in = di("w_in", [2, D, 2304]); vres_down = di("vres_down", [D, 32])
    pvec = di("pvec", [2, 2, 128, 32]); psel = di("psel", [2, 128, 2, 4]); invdiv = di("invdiv", [2, 128, 2, TB])
    w_up = di("w_up", [2, 64, 512]); a_up = di("a_up", [2, 64, 512]); g_up = di("g_up", [2, 128, 512])
    vres_up = di("vres_up", [32, 512]); pool_w = di("pool_w", [2, 4, 128, 128])
    cst = {k: di(k, v.shape) for k, v in l2_consts().items()}
    ones = di("ones", [128, 128])
    nex = 128 if DBG_SKIP0 else 128 * 128
    peer_u = di("peer_u", [2, nex, D]); peer_v = di("peer_v", [2, nex, D])
    w_out = di("w_out", [2, D, D]); peer_q = di("peer_q", [2, D, 2048]); keys = di("peer_keys", [2, 8, 2, 128, 128])
    xo = nc.dram_tensor("xo", [TOK, D], F32, kind="ExternalOutput").ap()
    UT = it("UT_s", [2, 128, 128, 8, 128], BF16); Vb = it("Vb_s", [2, 128 * 128, D], BF16)
    woutb = it("woutb_s", [2, 8, 128, D], BF16); pqb = it("pqb_s", [2, 16, 128, 8, 128], BF16)
    P = it("P_s", [NJC * 128, SEQ]); Y = it("Y_s", [D, SEQ]); VF = it("VF_s", [512, SEQ])
    XO0 = it("XO0_s", [TOK, D]); X1 = it("X1_s", [SEQ, D])

    c = Ctx(nc)
    ph = [0]

    def phase(fn):
        if DBG_PHASES is not None and ph[0] >= DBG_PHASES:
            ph[0] += 1
            return
        c.begin_phase("p%d_" % ph[0]); ph[0] += 1
        fn()
        c.end_phase()

    if DBG_SKIP0:
        ph[0] += 1
    else:
        phase(lambda: emit_L0(c, dict(u=peer_u, v=peer_v, w_out=w_out, peer_q=peer_q, ident=cst["ident"],
                                       UT=UT, Vb=Vb, woutb=woutb, pqb=pqb), range(2), 128))
    for l in range(2):
        xsrc = x_seq if l == 0 else X1
        ncol = 2304 if l == 0 else 2336
        njc = (ncol + 127) // 128
        w_parts = [(w_in[l], 0, 2304)] + ([(vres_down, 2304, 32)] if l > 0 else [])
        for hf in range(2):
            xt_fn = {}
            if l > 0:
                xt_fn = dict(x_tile=lambda i, hf=hf: X1[((i // 4) * 2 + hf) * 512 + (i % 4) * 128:
                                                        ((i // 4) * 2 + hf) * 512 + (i % 4) * 128 + 128, :])
            phase(lambda: emit_L1(c, dict(xt_fn, x=xsrc[hf * TOK:(hf + 1) * TOK], c_fm=c_fm, ada_w=ada_w[l][:, 0:2 * D],
                                           ada_b_fm=ada_b_fm[l], ln_g_fm=ln1[l], w_parts=w_parts, ident=cst["ident"],
                                           out=P[0:njc * 128, hf * TOK:(hf + 1) * TOK], ncol=ncol)))
        for g in range(2):
            cs = slice(g * 256, (g + 1) * 256)
            A = dict(rT=P[g * 256:(g + 1) * 256], kT=P[512 + g * 256: 512 + (g + 1) * 256],
                     vT=P[1024 + g * 256: 1024 + (g + 1) * 256], waT=P[1536:1664], gdT=P[1664:1792],
                     poolT=P[1792 + g * 256: 1792 + (g + 1) * 256],
                     pvec=pvec[l, g], wup=w_up[l][:, cs], aup=a_up[l][:, cs], gup=g_up[l][:, cs],
                     poolw=pool_w[l][2 * g:2 * g + 2], psel=psel[g], invdiv=invdiv[g],
                     yrw=Y[g * 256:(g + 1) * 256], ypl=Y[512 + g * 256: 512 + (g + 1) * 256])
            A.update(cst)
            if l == 0:
                A["vout"] = VF[g * 256:(g + 1) * 256]
            else:
                A.update(vdT=P[2304:2336], vfT=VF[g * 256:(g + 1) * 256], vup=vres_up[:, cs])
            phase(lambda: emit_L2(c, A, l > 0))
        phase(lambda: emit_L3(c, dict(x=(x_mine if l == 0 else XO0), yA=Y[:, 0:TOK], yB=Y[:, TOK:2 * TOK], hsel=hsel,
                                       c_fm=c_fm, ada_w=ada_w[l][:, 2 * D:6 * D], ada_b_fm=ada_b_fm[l], ln_g_fm=ln2[l],
                                       lnf_fm=lnf, woutb=woutb[l], pqb=pqb[l], keys=keys[l], UT=UT[l], Vb=Vb[l],
                                       ident=cst["ident"], ones=ones, xo=(XO0 if l == 0 else xo)), l == 1))
        if l == 0 and DBG_COLL and (DBG_PHASES is None or DBG_PHASES > 6):
            for k in range(4):
                c.collective(lambda gq: gq.collective_compute(
                    "AllGather", ALU.bypass, replica_groups=[[0, 1], [2, 3], [4, 5], [6, 7]],
                    ins=[XO0[k * 512:(k + 1) * 512].opt()], outs=[X1[k * 1024:(k + 1) * 1024].opt()]))
    c.finish()
    return nc


def kernel_fused(inp):
    f = lambda a: np.ascontiguousarray(a, dtype=np.float32)
    cst = l2_consts()
    shared = dict(ada_w=f(inp["ada_w"]), ada_b_fm=f(np.stack([_fm(inp["ada_b"][l]) for l in range(2)])),
                  ln1_g_fm=f(np.stack([_fm(inp["ln1_g"][l]) for l in range(2)])),
                  ln2_g_fm=f(np.stack([_fm(inp["ln2_g"][l]) for l in range(2)])), lnf_fm=_fm(inp["lnf_g"]),
                  w_in=f(inp["w_in"]), vres_down=f(inp["vres_down"][0]), w_up=f(inp["w_up"]), a_up=f(inp["a_up"]),
                  g_up=f(inp["g_up"]), vres_up=f(inp["vres_up"][0]), pool_w=f(inp["pool_w"]),
                  ones=np.ones((128, 128), np.float32), peer_u=f(inp["peer_u"]), peer_v=f(inp["peer_v"]),
                  w_out=f(inp["w_out"]), peer_q=f(inp["peer_q"]), peer_keys=f(inp["peer_keys"]))
    shared.update(cst)
    dummyT = np.zeros((2336, 1), np.float32)
    pv = np.zeros((2, 2, 128, 32), np.float32); ps_ = np.zeros((2, 128, 2, 4), np.float32)
    idv = np.zeros((2, 128, 2, TB), np.float32)
    for l in range(2):
        for g in range(2):
            d = l2_inputs(inp, l, g, dummyT, np.zeros((1, 1), np.float32))
            pv[l, g] = d["pvec"]; ps_[g] = d["psel"]; idv[g] = d["invdiv"]
    shared.update(pvec=pv, psel=ps_, invdiv=idv)
    x = f(inp["x"])
    in_maps = []
    for core in range(NCORE):
        b, g = core // 2, core % 2
        hs = np.zeros((128, 2), np.float32); hs[:, g] = 1.0
        m = dict(shared)
        m.update(x_seq=f(x[b]), x_mine=f(x[b, g * TOK:(g + 1) * TOK]), c_fm=_fm(inp["c"][b]), hsel=hs)
        in_maps.append(m)
    if inp.get("_only_maps") is not None:
        return in_maps
    res = _run(_prog("fused", build_fused), in_maps)
    out = np.stack([np.asarray(res[core]["xo"]) for core in range(NCORE)], axis=0)
    return np.ascontiguousarray(out.reshape(NB, SEQ, D).astype(np.float32))


_PROGS = {}


def _prog(key, fn):
    if key not in _PROGS:
        _PROGS[key] = fn()
    return _PROGS[key]


def _run(nc, in_maps):
    res = run_bass_kernel_spmd(nc, in_maps, core_ids=list(range(NCORE)))
    return res.results


FUSED = True


def kernel(**inputs):
    inp = {k: np.asarray(v) for k, v in inputs.items()}
    if FUSED:
        return kernel_fused(inp)
    f = lambda a: np.ascontiguousarray(a, dtype=np.float32)
    ident = np.eye(128, dtype=np.float32); ones = np.ones((128, 128), np.float32)
    in_maps = []
    for core in range(NCORE):
        sl = slice(core * EPC, (core + 1) * EPC)
        in_maps.append(dict(u=f(inp["peer_u"][:, sl]), v=f(inp["peer_v"][:, sl]), w_out=f(inp["w_out"]),
                            peer_q=f(inp["peer_q"]), ident=ident))
    r0 = _run(_prog("L0", build_L0), in_maps)
    UT = [np.ascontiguousarray(np.concatenate([np.asarray(r0[c_]["UT"])[l] for c_ in range(NCORE)], axis=0)) for l in range(2)]
    Vb = [np.ascontiguousarray(np.concatenate([np.asarray(r0[c_]["Vb"])[l] for c_ in range(NCORE)], axis=0)) for l in range(2)]
    woutb = [np.ascontiguousarray(np.asarray(r0[0]["woutb"])[l]) for l in range(2)]
    pqb = [np.ascontiguousarray(np.asarray(r0[0]["pqb"])[l]) for l in range(2)]
    del r0
    x = f(inp["x"]).reshape(NCORE, TOK, D)
    vfirst = None
    for l in range(2):
        wfull = inp["w_in"][l] if l == 0 else np.concatenate([inp["w_in"][l], inp["vres_down"][l - 1]], axis=1)
        ncol = wfull.shape[1]
        adab_fm = _fm(inp["ada_b"][l])
        in_maps = []
        for core in range(NCORE):
            b = core // 2
            in_maps.append(dict(x=f(x[core]), c_fm=_fm(inp["c"][b]), ada_w=f(inp["ada_w"][l][:, 0:2 * D]),
                                ada_b_fm=adab_fm, ln_g_fm=_fm(inp["ln1_g"][l]), w_full=f(wfull), ident=ident))
        r1 = _run(_prog(("L1", ncol), lambda: build_L1(ncol)), in_maps)
        in_maps = []
        for core in range(NCORE):
            b, g = core // 2, core % 2
            projT = np.concatenate([r1[2 * b]["projT"], r1[2 * b + 1]["projT"]], axis=1)
            in_maps.append(l2_inputs(inp, l, g, projT, None if l == 0 else vfirst[core]))
        del r1
        r2 = _run(_prog(("L2", l > 0), lambda: build_L2(l > 0)), in_maps)
        if l == 0:
            vfirst = [np.asarray(r2[core]["vout"]) for core in range(NCORE)]
        in_maps = []
        for core in range(NCORE):
            b, hf = core // 2, core % 2
            ts = slice(hf * TOK, (hf + 1) * TOK)
            y0, y1 = r2[2 * b]["yT"], r2[2 * b + 1]["yT"]
            yT = np.concatenate([y0[0:256, ts], y1[0:256, ts], y0[256:512, ts], y1[256:512, ts]], axis=0)
            in_maps.append(dict(x=f(x[core]), yT=f(yT), c_fm=_fm(inp["c"][b]), ada_w=f(inp["ada_w"][l][:, 2 * D:6 * D]),
                                ada_b_fm=adab_fm, ln_g_fm=_fm(inp["ln2_g"][l]), lnf_fm=_fm(inp["lnf_g"]),
                                woutb=woutb[l], pqb=pqb[l], keys=f(inp["peer_keys"][l]), UT=UT[l], Vb=Vb[l],
                                ident=ident, ones=ones))
        del r2
        r3 = _run(_prog(("L3", l == 1), lambda: build_L3(l == 1)), in_maps)
        x = np.stack([np.asarray(r3[core]["xo"]) for core in range(NCORE)], axis=0)
        del r3
    return np.ascontiguousarray(x.reshape(NB, SEQ, D).astype(np.float32))
```

```python
from contextlib import ExitStack
import numpy as np
import ml_dtypes
import concourse.bass as bass
import concourse.mybir as mybir
from concourse.bass_utils import run_bass_kernel_spmd

F32 = mybir.dt.float32
BF16 = mybir.dt.bfloat16
AF = mybir.ActivationFunctionType
ALU = mybir.AluOpType

D = 1024
SEQ = 4096
NB = 4
NCORE = 8
TOK = 2048
NORM_EPS = 1e-6
GN_EPS = 64e-5
CH = 64


class Ctx:
    NDMA = 12

    def __init__(self, nc):
        self.nc = nc
        self.stack = ExitStack()
        self.E = dict(pe=nc.tensor, act=nc.scalar, dve=nc.vector, pool=nc.gpsimd, sp=nc.sync)
        self.sem = {}
        for e in ("pe", "act", "dve", "pool"):
            self.sem[e] = self.stack.enter_context(nc.semaphore("c_" + e))
        self.cnt = {e: 0 for e in self.sem}
        self.seen = {e: {} for e in self.E}
        self.dsem = {}
        self.dval = {}
        self.dnext = {}
        for q in ("sp", "pool", "act"):
            self.dsem[q] = [self.stack.enter_context(nc.semaphore("d_%s%d" % (q, i))) for i in range(self.NDMA)]
            self.dval[q] = [0] * self.NDMA
            self.dnext[q] = 0
        self.W = {}
        self.R = {}
        self.n_ins = 0
        self.nwait = {}
        self.ccsem = self.stack.enter_context(nc.semaphore("cc_sem"))
        self.ccval = 0

    scope = None
    pfx = ""

    def begin_phase(self, pfx):
        self.scope = ExitStack()
        self.pfx = pfx

    def end_phase(self):
        self.barrier()
        self.scope.close()
        self.scope = None
        self.W = {}
        self.R = {}

    def barrier(self):
        for eng in self.E:
            for e2 in self.cnt:
                if eng == "pe" and e2 == "pe":
                    continue
                self._wait(eng, e2, self.cnt[e2])
            for q in self.dsem:
                for i in range(self.NDMA):
                    self._wait(eng, (q, i), self.dval[q][i])

    def sb(self, name, shape, dt=F32):
        st = self.scope if self.scope is not None else self.stack
        return st.enter_context(self.nc.sbuf_tensor(self.pfx + name, list(shape), dt))

    def ps(self, name, shape, dt=F32):
        st = self.scope if self.scope is not None else self.stack
        return st.enter_context(self.nc.psum_tensor(self.pfx + name, list(shape), dt))

    def _semh(self, semid):
        if semid == "cc":
            return self.ccsem
        if isinstance(semid, str):
            return self.sem[semid]
        return self.dsem[semid[0]][semid[1]]

    def _wait(self, eng, semid, val):
        if val <= 0:
            return
        if self.seen[eng].get(semid, 0) >= val:
            return
        self.E[eng].wait_ge(self._semh(semid), val)
        self.seen[eng][semid] = val
        self.n_ins += 1
        self.nwait[eng] = self.nwait.get(eng, 0) + 1

    def _deps(self, r, w):
        deps = {}
        for k in r:
            for s, v in self.W.get(k, {}).items():
                deps[s] = max(deps.get(s, 0), v)
        for k in w:
            for s, v in self.W.get(k, {}).items():
                deps[s] = max(deps.get(s, 0), v)
            for s, v in self.R.get(k, {}).items():
                deps[s] = max(deps.get(s, 0), v)
        return deps

    def _record(self, semid, val, r, w):
        for k in w:
            self.W[k] = {semid: val}
            self.R[k] = {}
        for k in r:
            self.R.setdefault(k, {})[semid] = val

    def op(self, eng, fn, r=(), w=()):
        deps = self._deps(r, w)
        for s, v in deps.items():
            if eng == "pe" and s == "pe":
                continue
            self._wait(eng, s, v)
        ins = fn(self.E[eng])
        self.cnt[eng] += 1
        ins.then_inc(self.sem[eng], 1)
        self.n_ins += 1
        self._record(eng, self.cnt[eng], r, w)
        return ins

    def dma(self, q, out, in_, r=(), w=(), **kw):
        i = self.dnext[q]
        self.dnext[q] = (i + 1) % self.NDMA
        semid = (q, i)
        self._wait(q, semid, self.dval[q][i])
        deps = self._deps(r, w)
        for s, v in deps.items():
            self._wait(q, s, v)
        ins = self.E[q].dma_start(out=out, in_=in_, **kw)
        self.dval[q][i] += 16
        ins.then_inc(self.dsem[q][i], 16)
        self.n_ins += 1
        self._record(semid, self.dval[q][i], r, w)
        return ins

    def collective(self, fn):
        self.barrier()
        ins = fn(self.nc.gpsimd)
        self.ccval += 1
        ins.then_inc(self.ccsem)
        for eng in self.E:
            self._wait(eng, "cc", self.ccval)

    def finish(self):
        global LAST_CNT
        LAST_CNT = (dict(self.cnt), {q: list(v) for q, v in self.dval.items()}, self.n_ins, dict(self.nwait))
        for q in self.dsem:
            for i in range(self.NDMA):
                self._wait("sp", (q, i), self.dval[q][i])
        for e in self.cnt:
            self._wait("sp", e, self.cnt[e])
        self.stack.close()


def _fm(vec, n=None):
    v = np.ascontiguousarray(np.asarray(vec, dtype=np.float32).reshape(-1, 128).T)
    return v


def emit_mod_fm(c, adaw_dram, col0, ncolchunks, silu_c, adab_fm_sb, adab_col0, out_sb, ps_tile, tag):
    nc = c.nc
    wv = adaw_dram.rearrange("(dc p) j -> p dc j", p=128)
    cap = c.adaw_sb.shape[2] // 128
    for half in range(0, ncolchunks, cap):
        nch = min(cap, ncolchunks - half)
        wt = c.adaw_sb
        c.dma("sp", wt[:, :, 0:nch * 128], wv[:, :, col0 + half * 128: col0 + (half + nch) * 128],
              w=["adaw"])
        for jc in range(nch):
            for dc in range(8):
                c.op("pe", lambda e, jc=jc, dc=dc: e.matmul(
                    ps_tile[:, half + jc: half + jc + 1], wt[:, dc, jc * 128:(jc + 1) * 128],
                    silu_c[:, dc:dc + 1], start=(dc == 0), stop=(dc == 7)),
                    r=["adaw", "silu_c"], w=[tag + "_ps"])
    c.op("dve", lambda e: e.tensor_tensor(out=out_sb, in0=ps_tile[:, 0:ncolchunks],
                                           in1=adab_fm_sb[:, adab_col0: adab_col0 + ncolchunks], op=ALU.add),
         r=[tag + "_ps", "adab"], w=[tag])


DBG_STAGE = 99
DBG_PHASES = None
DBG_COLL = True
DBG_SKIP0 = False
LAST_CNT = None
DBG_NST = None


def emit_L1(c, A):
    ncol = A["ncol"]
    njc = (ncol + 127) // 128
    x_d, c_d, adaw_d, adab_d, g_d, id_d, out_d = (A["x"], A["c_fm"], A["ada_w"], A["ada_b_fm"], A["ln_g_fm"],
                                                   A["ident"], A["out"])
    c.adaw_sb = c.sb("adaw", [128, 8, 1024], F32)
    c_sb = c.sb("c_sb", [128, 8]); sig_c = c.sb("sig_c", [128, 8]); silu_c = c.sb("silu_c", [128, 8])
    adab = c.sb("adab", [128, 48]); lng = c.sb("lng", [128, 8])
    modT = c.sb("modT", [128, 16]); effs = c.sb("effs", [128, 8])
    identf = c.sb("identf", [128, 128]); identb = c.sb("identb", [128, 128], BF16)
    wbf = c.sb("wbf", [128, 8, njc * 128], BF16)
    hT = c.sb("hT", [128, 8, TOK], BF16)
    xt = [c.sb("xt%d" % i, [128, D]) for i in range(2)]
    junk = c.sb("junk", [128, D], BF16)
    xn = [c.sb("xn%d" % i, [128, D], BF16) for i in range(2)]
    ss = c.sb("ss", [128, 32]); rstd = c.sb("rstd", [128, 32])
    stg = [c.sb("stg%d" % i, [128, TOK]) for i in range(2)]
    mod_ps = c.ps("mod_ps", [128, 512])
    tp_ps = [c.ps("tp_ps%d" % i, [128, 1024], BF16) for i in range(2)]
    mm_ps = [c.ps("mm_ps%d" % i, [128, 512]) for i in range(4)]

    c.dma("sp", c_sb[:], c_d, w=["c_sb"])
    c.dma("sp", adab[:], adab_d, w=["adab"])
    c.dma("sp", lng[:], g_d, w=["lng"])
    c.dma("sp", identf[:], id_d, w=["identf"])
    c.op("dve", lambda e: e.tensor_copy(identb[:], identf[:]), r=["identf"], w=["identb"])
    if njc * 128 != ncol:
        c.op("pool", lambda e: e.memset(wbf[:, :, ncol:njc * 128], 0.0), w=["wbf"])
    for (w_ap, col0, ncols) in A["w_parts"]:
        wv = w_ap.rearrange("(dc p) j -> p dc j", p=128)
        for dc in range(8):
            c.dma("pool", wbf[:, dc, col0:col0 + ncols], wv[:, dc, :], w=["wbf"])
    c.op("act", lambda e: e.activation(out=sig_c[:], in_=c_sb[:], func=AF.Sigmoid), r=["c_sb"], w=["sig_c"])
    c.op("dve", lambda e: e.tensor_tensor(out=silu_c[:], in0=c_sb[:], in1=sig_c[:], op=ALU.mult),
         r=["c_sb", "sig_c"], w=["silu_c"])
    emit_mod_fm(c, adaw_d, 0, 16, silu_c, adab, 0, modT[:, 0:16], mod_ps, "modT")
    c.op("dve", lambda e: e.scalar_tensor_tensor(out=effs[:], in0=modT[:, 8:16], scalar=1.0, in1=lng[:],
                                                  op0=ALU.add, op1=ALU.mult), r=["modT", "lng"], w=["effs"])
    c.op("dve", lambda e: e.memset(ss[:], 0.0), w=["ss%d" % i for i in range(TOK // 128)])
    epsb = c.sb("epsb", [128, 1])
    c.op("dve", lambda e: e.memset(epsb[:], NORM_EPS), w=["epsb"])
    ntile = TOK // 128
    for i in range(ntile):
        s = i % 2
        c.dma("sp", xt[s][:], (A["x_tile"](i) if "x_tile" in A else x_d[i * 128:(i + 1) * 128, :]), w=["xt%d" % s])
        c.op("act", lambda e, s=s, i=i: e.activation(out=junk[:], in_=xt[s][:], func=AF.Square,
                                                     accum_out=ss[:, i:i + 1]),
             r=["xt%d" % s], w=["junk", "ss%d" % i])
        c.op("act", lambda e, i=i: e.activation(out=rstd[:, i:i + 1], in_=ss[:, i:i + 1], func=AF.Sqrt,
                                                bias=epsb[:, 0:1], scale=1.0 / D),
             r=["ss%d" % i, "epsb"], w=["rstd%d" % i])
        c.op("dve", lambda e, i=i: e.reciprocal(out=rstd[:, i:i + 1], in_=rstd[:, i:i + 1]),
             r=["rstd%d" % i], w=["rstd%d" % i])
        c.op("dve", lambda e, s=s, i=i: e.tensor_scalar(out=xn[s][:], in0=xt[s][:], scalar1=rstd[:, i:i + 1],
                                                        scalar2=None, op0=ALU.mult),
             r=["xt%d" % s, "rstd%d" % i], w=["xn%d" % s])
        for dc in range(8):
            c.op("pe", lambda e, s=s, dc=dc: e.transpose(tp_ps[s][:, dc * 128:(dc + 1) * 128],
                                                         xn[s][:, dc * 128:(dc + 1) * 128], identb[:]),
                 r=["xn%d" % s, "identb"], w=["tp%d" % s])
        for dc in range(8):
            if s == 0:
                c.op("act", lambda e, s=s, dc=dc, i=i: e.activation(
                    out=hT[:, dc, i * 128:(i + 1) * 128], in_=tp_ps[s][:, dc * 128:(dc + 1) * 128],
                    func=AF.Identity, bias=modT[:, dc:dc + 1], scale=effs[:, dc:dc + 1]),
                    r=["tp%d" % s, "modT", "effs"], w=["hT_%d_%d_%d" % (dc, i // 4, s)])
            else:
                c.op("dve", lambda e, s=s, dc=dc, i=i: e.tensor_scalar(
                    out=hT[:, dc, i * 128:(i + 1) * 128], in0=tp_ps[s][:, dc * 128:(dc + 1) * 128],
                    scalar1=effs[:, dc:dc + 1], scalar2=modT[:, dc:dc + 1], op0=ALU.mult, op1=ALU.add),
                    r=["tp%d" % s, "modT", "effs"], w=["hT_%d_%d_%d" % (dc, i // 4, s)])
    k = 0
    for jc in range(njc):
        st = stg[jc % 2]
        for tb in range(TOK // 512):
            pt = mm_ps[k % 4]; pk = "mm%d" % (k % 4); k += 1
            for dc in range(8):
                c.op("pe", lambda e, pt=pt, dc=dc, jc=jc, tb=tb: e.matmul(
                    pt[:], wbf[:, dc, jc * 128:(jc + 1) * 128], hT[:, dc, tb * 512:(tb + 1) * 512],
                    start=(dc == 0), stop=(dc == 7)),
                    r=["wbf", "hT_%d_%d_0" % (dc, tb), "hT_%d_%d_1" % (dc, tb)], w=[pk])
            eng = "act" if tb % 2 == 0 else "dve"
            if eng == "act":
                c.op("act", lambda e, pt=pt, st=st, tb=tb: e.copy(out=st[:, tb * 512:(tb + 1) * 512], in_=pt[:]),
                     r=[pk], w=["stg%d_%d" % (jc % 2, tb)])
            else:
                c.op("dve", lambda e, pt=pt, st=st, tb=tb: e.tensor_copy(st[:, tb * 512:(tb + 1) * 512], pt[:]),
                     r=[pk], w=["stg%d_%d" % (jc % 2, tb)])
        c.dma("sp", out_d[jc * 128:(jc + 1) * 128, :], st[:],
              r=["stg%d_%d" % (jc % 2, tb) for tb in range(4)])


def build_L1(ncol):
    nc = bass.Bass("TRN2", target_bir_lowering=False)
    njc = (ncol + 127) // 128
    di = lambda name, shape: nc.dram_tensor(name, list(shape), F32, kind="ExternalInput").ap()
    A = dict(x=di("x", [TOK, D]), c_fm=di("c_fm", [128, 8]), ada_w=di("ada_w", [D, 2 * D]),
             ada_b_fm=di("ada_b_fm", [128, 48]), ln_g_fm=di("ln_g_fm", [128, 8]), ident=di("ident", [128, 128]),
             ncol=ncol)
    A["w_parts"] = [(di("w_full", [D, ncol]), 0, ncol)]
    A["out"] = nc.dram_tensor("projT", [njc * 128, TOK], F32, kind="ExternalOutput").ap()
    c = Ctx(nc)
    c.begin_phase("")
    emit_L1(c, A)
    c.end_phase()
    c.finish()
    return nc


class _Stop(Exception):
    pass


def dbg(stage):
    if DBG_STAGE == stage:
        raise _Stop()


TB = 512
NBLK = SEQ // TB
CPB = TB // CH
C0 = float(np.exp(-0.5))
POOL_WINDOWS = (2, 4, 8, 16)


def l2_consts():
    s_idx = np.arange(64)[:, None]; t_idx = np.arange(64)[None, :]
    su = (t_idx > s_idx).astype(np.float32); iu = (t_idx >= s_idx).astype(np.float32)
    maskA = np.tile(np.concatenate([su, iu], axis=1), (1, 4))
    maskC = np.tile((t_idx < s_idx).astype(np.float32), (1, 4))
    identrep = np.tile(np.eye(64, dtype=np.float32), (1, 4))
    scanmask = np.ones((128, TB), np.float32); scanmask[:, ::CH] = 0.0
    blockones = np.kron(np.eye(2, dtype=np.float32), np.ones((64, 64), np.float32))
    return dict(maskA=maskA, maskC=maskC, identrep=identrep, scanmask=scanmask, blockones=blockones,
                ident=np.eye(128, dtype=np.float32))


def emit_L2(c, A, layer1):
    nc = c.nc
    rT, kT, vT, waT, gdT, poolT = A["rT"], A["kT"], A["vT"], A["waT"], A["gdT"], A["poolT"]
    if layer1:
        vdT, vfT, vup_d = A["vdT"], A["vfT"], A["vup"]
    pvec_d, wup_d, aup_d, gup_d, poolw_d = A["pvec"], A["wup"], A["aup"], A["gup"], A["poolw"]
    maskA_d, maskC_d, identrep_d = A["maskA"], A["maskC"], A["identrep"]
    scanmask_d, bo_d, id_d, invdiv_d, psel_d = A["scanmask"], A["blockones"], A["ident"], A["invdiv"], A["psel"]
    yrw, ypl = A["yrw"], A["ypl"]
    if not layer1:
        vout = A["vout"]
    sb = c.sb
    pvec = sb("pvec_sb", [128, 32]); omk = sb("omk", [128, 2])
    wup = sb("wup_sb", [128, 256], BF16); aup = sb("aup_sb", [128, 256], BF16); gup = sb("gup_sb", [128, 256], BF16)
    vup = sb("vup_sb", [32, 256], BF16); poolw = sb("poolw_sb", [128, 2, 128], BF16)
    maskA = sb("maskA_sb", [64, 512]); maskC = sb("maskC_sb", [64, 256]); identrep = sb("identrep_sb", [64, 256])
    scanmask = sb("scanmask_sb", [128, TB]); bo = sb("bo_sb", [128, 128]); identf = sb("identf", [128, 128])
    identb = sb("identb", [128, 128], BF16); invdiv = sb("invdiv_sb", [128, 2, TB])
    epsk = sb("epsk", [128, 1]); gneps = sb("gneps", [128, 1]); psel = sb("psel_sb", [128, 2, 4])
    Xwa = sb("Xwa", [128, TB + 1]); Xg = sb("Xg", [128, TB + 1]); Xvd = sb("Xvd", [32, TB + 1])
    dwa = sb("dwa", [128, TB]); swa = sb("swa", [128, TB]); th = sb("th", [64, TB], BF16); adb = sb("adb", [128, TB], BF16)
    dg = sb("dg", [128, TB]); sgd = sb("sgd", [128, TB]); sg = sb("sg", [128, TB], BF16)
    dvd = sb("dvd", [32, TB]); vdb = sb("vdb", [32, TB], BF16)
    P2 = range(2)
    Xr = [sb("Xr_sh", [128, TB + 1])] * 2; Xk = [sb("Xk_sh", [128, TB + 1])] * 2
    Xv = [sb("Xv_sh", [128, TB + 1])] * 2; Xvf = [sb("Xvf_sh", [128, TB])] * 2
    tmpd = [sb("tmpd_sh", [128, TB])] * 2
    r_s = [sb("r_s%d" % i, [128, TB]) for i in P2]; k_s = [sb("k_s%d" % i, [128, TB]) for i in P2]
    v_s = [sb("v_s%d" % i, [128, TB]) for i in P2]
    sigw = [sb("sigw%d" % i, [128, TB]) for i in P2]; cum = [sb("cum%d" % i, [128, TB]) for i in P2]
    cumx = [sb("cumx_sh", [128, TB])] * 2
    G = [sb("G%d" % i, [128, TB]) for i in P2]; Ginv = [sb("Ginv%d" % i, [128, TB]) for i in P2]
    Gex = [sb("Gex%d" % i, [128, TB]) for i in P2]
    a_ = [sb("a_%d" % i, [128, TB]) for i in P2]; gg = [sb("gg%d" % i, [128, TB]) for i in P2]
    vsig = [sb("vsig_sh", [128, TB])] * 2
    kkraw = [sb("kkraw_sh", [128, TB])] * 2; sq = [sb("sq_sh", [128, TB])] * 2
    rn = [sb("rn_sh", [128, TB])] * 2; kk = [sb("kk%d" % i, [128, TB]) for i in P2]
    fac = [sb("fac_sh", [128, TB])] * 2; kmod = [sb("kmod%d" % i, [128, TB]) for i in P2]
    rk2 = [sb("rk2_sh", [128, TB])] * 2; bonus = [sb("bonus%d" % i, [128, TB]) for i in P2]
    t1 = [sb("t1_sh", [128, TB])] * 2
    ARt = [sb("ARt%d" % i, [128, CPB * 128], BF16) for i in P2]
    Bt = [sb("Bt%d" % i, [128, TB], BF16) for i in P2]; Kt = [sb("Kt%d" % i, [128, TB], BF16) for i in P2]
    Vb = [sb("Vb%d" % i, [128, TB], BF16) for i in P2]
    yraw = [sb("yraw%d" % i, [128, TB]) for i in P2]; yc = [sb("yc%d" % i, [128, TB]) for i in P2]
    ysq = [sb("ysq_sh", [128, TB])] * 2; yrs = [sb("yrs_sh", [128, TB])] * 2
    yo = [sb("yo%d" % i, [128, TB]) for i in P2]
    M0b = [[sb("M0b%d_%d" % (i, j), [64, 64], BF16) for j in range(2)] for i in range(4)]
    ARo = [sb("ARo%d" % i, [64, CPB * 128], BF16) for i in P2]
    Bo = [sb("Bo%d" % i, [64, TB], BF16) for i in P2]; Ko = [sb("Ko%d" % i, [64, TB], BF16) for i in P2]
    GCs = [sb("GCs%d" % i, [128, CPB]) for i in P2]; GCo = [sb("GCo%d" % i, [64, CPB]) for i in P2]
    tokmaj = [sb("tokmaj%d" % j, [64, 768], BF16) for j in range(2)]
    SA = [sb("SA%d" % j, [64, 512], BF16) for j in range(2)]; SB_ = [sb("SB%d" % j, [64, 512], BF16) for j in range(2)]
    SC = [sb("SC%d" % j, [64, 256], BF16) for j in range(2)]
    TT = [sb("TT%d" % j, [64, 256], BF16) for j in range(2)]; PP = [sb("PP%d" % j, [64, 512], BF16) for j in range(2)]
    W1 = sb("W1", [64, 256], BF16); U = [sb("U%d" % j, [64, 256], BF16) for j in range(2)]
    Xp = [sb("Xp%d" % i, [128, TB + 15]) for i in P2]
    plv = [[sb("plv%d_%d" % (i, k), [128, TB + 15]) for k in range(4)] for i in P2]
    pacc = [sb("pacc%d" % i, [128, TB]) for i in P2]
    pd = [sb("pd%d" % i, [128, TB], BF16) for i in P2]; pm = [sb("pm%d" % i, [128, TB]) for i in P2]
    po = [sb("po%d" % i, [128, TB]) for i in P2]
    bk = {n: c.ps("bk_" + n, [128, 512]) for n in ("A", "B", "C", "D", "E", "FG", "HI")}
    trp = c.ps("bk_trp", [128, 1024], BF16)

    ld = lambda dst, src, key: c.dma("sp", dst, src, w=[key])
    ld(pvec[:], pvec_d, "pvec"); ld(maskA[:], maskA_d, "maskA"); ld(maskC[:], maskC_d, "maskC")
    ld(identrep[:], identrep_d, "identrep"); ld(scanmask[:], scanmask_d, "scanmask"); ld(bo[:], bo_d, "bo")
    ld(identf[:], id_d, "identf"); ld(invdiv[:], invdiv_d, "invdiv"); ld(psel[:], psel_d, "psel")
    c.dma("pool", wup[0:64, :], wup_d, w=["wup"]); c.dma("pool", aup[64:128, :], aup_d, w=["aup"])
    c.dma("pool", gup[:], gup_d, w=["gup"]); c.dma("pool", poolw[:], poolw_d.rearrange("g c d -> c g d"), w=["poolw"])
    if layer1:
        c.dma("pool", vup[0:32, :], vup_d, w=["vup"])
    c.op("dve", lambda e: e.tensor_copy(identb[:], identf[:]), r=["identf"], w=["identb"])
    c.op("dve", lambda e: e.memset(epsk[:], 1e-24), w=["epsk"])
    c.op("dve", lambda e: e.memset(gneps[:], GN_EPS), w=["gneps"])
    c.op("dve", lambda e: e.tensor_scalar(out=omk[:], in0=pvec[:, 12:14], scalar1=-1.0, scalar2=1.0,
                                          op0=ALU.mult, op1=ALU.add), r=["pvec"], w=["omk"])
    for i in range(4):
        for j in range(2):
            c.op("dve", lambda e, i=i, j=j: e.memset(M0b[i][j][:], 0.0), w=["M0b%d_%d" % (i, j)])
    for i in P2:
        for k_ in range(4):
            c.op("pool", lambda e, i=i, k_=k_: e.memset(plv[i][k_][:], 0.0), w=["plv%d_%d" % (i, k_)])
    pv = lambda col, hp=0: pvec[:, col + hp: col + hp + 1]

    def tshift(eng, X, d, out, mu, n, kX, kd, kout):
        c.op(eng, lambda e: e.tensor_tensor(out=d, in0=X[0:n, 0:TB], in1=X[0:n, 1:TB + 1], op=ALU.subtract),
             r=[kX], w=[kd])
        if eng == "dve":
            c.op(eng, lambda e: e.scalar_tensor_tensor(out=out, in0=d, scalar=mu, in1=X[0:n, 1:TB + 1],
                                                       op0=ALU.mult, op1=ALU.add), r=[kX, kd, "pvec"], w=[kout])
        else:
            c.op(eng, lambda e: e.tensor_scalar(out=d, in0=d, scalar1=mu, scalar2=None, op0=ALU.mult),
                 r=[kd, "pvec"], w=[kd])
            c.op(eng, lambda e: e.tensor_tensor(out=out, in0=d, in1=X[0:n, 1:TB + 1], op=ALU.add),
                 r=[kX, kd], w=[kout])

    def load_halo(X, src, rows, b, n, key, halo=1):
        t0 = b * TB
        if b == 0:
            c.op("pool", lambda e: e.memset(X[0:n, 0:halo], 0.0), w=[key])
            c.dma("sp", X[0:n, halo:halo + TB], src[rows, 0:TB], w=[key])
        else:
            c.dma("sp", X[0:n, :], src[rows, t0 - halo:t0 + TB], w=[key])

    chunk_counter = [0]

    def body():
        for b in range(NBLK):
            t0 = b * TB
            load_halo(Xwa, waT, slice(0, 128), b, 128, "Xwa")
            load_halo(Xg, gdT, slice(0, 128), b, 128, "Xg")
            tshift("pool", Xwa, dwa[:], swa[:], pv(25), 128, "Xwa", "dwa", "swa")
            c.op("act", lambda e: e.activation(out=th[:], in_=swa[0:64, :], func=AF.Tanh), r=["swa"], w=["th"])
            c.op("pool", lambda e: e.tensor_copy(adb[64:128, :], swa[64:128, :]), r=["swa"], w=["adb"])
            tshift("pool", Xg, dg[:], sgd[:], pv(24), 128, "Xg", "dg", "sgd")
            c.op("act", lambda e: e.activation(out=sg[:], in_=sgd[:], func=AF.Sigmoid), r=["sgd"], w=["sg"])
            if layer1:
                load_halo(Xvd, vdT, slice(0, 32), b, 32, "Xvd")
                tshift("pool", Xvd, dvd[:], vdb[:], pvec[0:32, 26:27], 32, "Xvd", "dvd", "vdb")
            dbg(1)
            for hp in P2:
                rows = slice(hp * 128, (hp + 1) * 128)
                H = str(hp)
                load_halo(Xr[hp], rT, rows, b, 128, "Xr")
                load_halo(Xk[hp], kT, rows, b, 128, "Xk")
                load_halo(Xv[hp], vT, rows, b, 128, "Xv")
                tshift("dve", Xr[hp], tmpd[hp][:], r_s[hp][:], pv(0, hp), 128, "Xr", "tmpd", "r_s" + H)
                tshift("dve", Xk[hp], tmpd[hp][:], k_s[hp][:], pv(2, hp), 128, "Xk", "tmpd", "k_s" + H)
                tshift("dve", Xv[hp], tmpd[hp][:], v_s[hp][:], pv(4, hp), 128, "Xv", "tmpd", "v_s" + H)
                cols = slice(hp * 128, (hp + 1) * 128)
                c.op("pe", lambda e: e.matmul(bk["A"][:], wup[0:64, cols], th[:], start=True, stop=True),
                     r=["wup", "th"], w=["bkA"])
                c.op("act", lambda e: e.activation(out=sigw[hp][:], in_=bk["A"][:], func=AF.Sigmoid, bias=pv(6, hp)),
                     r=["bkA", "pvec"], w=["sigw" + H])
                c.op("pe", lambda e: e.matmul(bk["B"][:], aup[64:128, cols], adb[64:128, :], start=True, stop=True),
                     r=["aup", "adb"], w=["bkB"])
                c.op("act", lambda e: e.activation(out=a_[hp][:], in_=bk["B"][:], func=AF.Sigmoid, bias=pv(8, hp)),
                     r=["bkB", "pvec"], w=["a_" + H])
                c.op("pe", lambda e: e.matmul(bk["C"][:], gup[:, cols], sg[:], start=True, stop=True),
                     r=["gup", "sg"], w=["bkC"])
                c.op("act", lambda e: e.copy(out=gg[hp][:], in_=bk["C"][:]), r=["bkC"], w=["gg" + H])
                if layer1:
                    c.dma("sp", Xvf[hp][:], vfT[rows, t0:t0 + TB], w=["Xvf"])
                    c.op("pe", lambda e: e.matmul(bk["D"][:], vup[0:32, cols], vdb[0:32, :], start=True, stop=True),
                         r=["vup", "vdb"], w=["bkD"])
                    c.op("act", lambda e: e.activation(out=vsig[hp][:], in_=bk["D"][:], func=AF.Sigmoid, bias=pv(27, hp)),
                         r=["bkD", "pvec"], w=["vsig"])
                    c.op("dve", lambda e: e.tensor_tensor(out=tmpd[hp][:], in0=Xvf[hp][:], in1=v_s[hp][:], op=ALU.subtract),
                         r=["Xvf", "v_s" + H], w=["tmpd"])
                    c.op("dve", lambda e: e.tensor_tensor(out=tmpd[hp][:], in0=tmpd[hp][:], in1=vsig[hp][:], op=ALU.mult),
                         r=["vsig", "tmpd"], w=["tmpd"])
                    c.op("dve", lambda e: e.tensor_tensor(out=v_s[hp][:], in0=v_s[hp][:], in1=tmpd[hp][:], op=ALU.add),
                         r=["v_s" + H, "tmpd"], w=["v_s" + H])
                else:
                    c.dma("sp", vout[rows, t0:t0 + TB], v_s[hp][:], r=["v_s" + H])
                c.op("dve", lambda e: e.tensor_scalar(out=kkraw[hp][:], in0=k_s[hp][:], scalar1=pv(10, hp), scalar2=None,
                                                      op0=ALU.mult), r=["k_s" + H, "pvec"], w=["kkraw"])
                c.op("pool", lambda e: e.tensor_tensor(out=sq[hp][:], in0=kkraw[hp][:], in1=kkraw[hp][:], op=ALU.mult),
                     r=["kkraw"], w=["sq"])
                c.op("pe", lambda e: e.matmul(bk["E"][:], bo[:], sq[hp][:], start=True, stop=True),
                     r=["bo", "sq"], w=["bkE"])
                c.op("act", lambda e: e.activation(out=rn[hp][:], in_=bk["E"][:], func=AF.Sqrt, bias=epsk[:, 0:1]),
                     r=["bkE", "epsk"], w=["rn"])
                c.op("dve", lambda e: e.reciprocal(out=rn[hp][:], in_=rn[hp][:]), r=["rn"], w=["rn"])
                c.op("dve", lambda e: e.tensor_tensor(out=kk[hp][:], in0=kkraw[hp][:], in1=rn[hp][:], op=ALU.mult),
                     r=["kkraw", "rn"], w=["kk" + H])
                c.op("pool", lambda e: e.tensor_scalar(out=fac[hp][:], in0=a_[hp][:], scalar1=pv(12, hp),
                                                       scalar2=omk[:, hp:hp + 1], op0=ALU.mult, op1=ALU.add),
                     r=["a_" + H, "pvec", "omk"], w=["fac"])
                c.op("pool", lambda e: e.tensor_tensor(out=kmod[hp][:], in0=k_s[hp][:], in1=fac[hp][:], op=ALU.mult),
                     r=["k_s" + H, "fac"], w=["kmod" + H])
                c.op("pool", lambda e: e.tensor_scalar(out=rk2[hp][:], in0=r_s[hp][:], scalar1=pv(16, hp), scalar2=None,
                                                       op0=ALU.mult), r=["r_s" + H, "pvec"], w=["rk2"])
                c.op("pool", lambda e: e.tensor_tensor(out=rk2[hp][:], in0=rk2[hp][:], in1=kmod[hp][:], op=ALU.mult),
                     r=["rk2", "kmod" + H], w=["rk2"])
                c.op("pe", lambda e: e.matmul(bk["FG"][:], bo[:], rk2[hp][:], start=True, stop=True),
                     r=["bo", "rk2"], w=["bkFG"])
                c.op("dve", lambda e: e.tensor_tensor(out=bonus[hp][:], in0=bk["FG"][:], in1=v_s[hp][:], op=ALU.mult),
                     r=["bkFG", "v_s" + H], w=["bonus" + H])
                c.op("dve", lambda e: e.tensor_tensor_scan(out=cum[hp][:], data0=scanmask[:], data1=sigw[hp][:],
                                                           initial=0.0, op0=ALU.mult, op1=ALU.add),
                     r=["scanmask", "sigw" + H], w=["cum" + H])
                c.op("pool", lambda e: e.tensor_tensor(out=cumx[hp][:], in0=cum[hp][:], in1=sigw[hp][:], op=ALU.subtract),
                     r=["cum" + H, "sigw" + H], w=["cumx"])
                c.op("act", lambda e: e.activation(out=G[hp][:], in_=cum[hp][:], func=AF.Exp, scale=-C0),
                     r=["cum" + H], w=["G" + H])
                c.op("act", lambda e: e.activation(out=Ginv[hp][:], in_=cum[hp][:], func=AF.Exp, scale=C0),
                     r=["cum" + H], w=["Ginv" + H])
                c.op("act", lambda e: e.activation(out=Gex[hp][:], in_=cumx[hp][:], func=AF.Exp, scale=-C0),
                     r=["cumx"], w=["Gex" + H])
                AR3 = ARt[hp][:].rearrange("p (c two t) -> p c two t", two=2, t=CH)
                v3 = lambda ap: ap.rearrange("p (c t) -> p c t", t=CH)
                c.op("dve", lambda e: e.tensor_tensor(out=AR3[:, :, 1, :], in0=v3(r_s[hp][:]), in1=v3(G[hp][:]), op=ALU.mult),
                     r=["r_s" + H, "G" + H], w=["ARt" + H])
                c.op("dve", lambda e: e.scalar_tensor_tensor(out=AR3[:, :, 0, :], in0=v3(kk[hp][:]), scalar=-1.0,
                                                             in1=v3(Gex[hp][:]), op0=ALU.mult, op1=ALU.mult),
                     r=["kk" + H, "Gex" + H], w=["ARt" + H])
                c.op("pool", lambda e: e.tensor_tensor(out=t1[hp][:], in0=kk[hp][:], in1=a_[hp][:], op=ALU.mult),
                     r=["kk" + H, "a_" + H], w=["t1"])
                c.op("dve", lambda e: e.tensor_tensor(out=Bt[hp][:], in0=t1[hp][:], in1=Ginv[hp][:], op=ALU.mult),
                     r=["t1", "Ginv" + H], w=["Bt" + H])
                c.op("pool", lambda e: e.tensor_tensor(out=Kt[hp][:], in0=kmod[hp][:], in1=Ginv[hp][:], op=ALU.mult),
                     r=["kmod" + H, "Ginv" + H], w=["Kt" + H])
                c.op("pool", lambda e: e.tensor_copy(Vb[hp][:], v_s[hp][:]), r=["v_s" + H], w=["Vb" + H])
                c.op("pool", lambda e: e.tensor_copy(GCs[hp][:], G[hp][:].rearrange("p (c t) -> p c t", t=CH)[:, :, CH - 1]),
                     r=["G" + H], w=["GCs" + H])
                c.dma("sp", ARo[hp][:], ARt[hp][64:128, :], r=["ARt" + H], w=["ARo" + H])
                c.dma("sp", Bo[hp][:], Bt[hp][64:128, :], r=["Bt" + H], w=["Bo" + H])
                c.dma("sp", Ko[hp][:], Kt[hp][64:128, :], r=["Kt" + H], w=["Ko" + H])
                c.dma("sp", GCo[hp][:], GCs[hp][64:128, :], r=["GCs" + H], w=["GCo" + H])

            dbg(2)
            for gi in P2:
                Gk = str(gi)
                load_halo(Xp[gi], poolT, slice(gi * 128, (gi + 1) * 128), b, 128, "Xp" + Gk, halo=15)
                cur = Xp[gi]; curk = "Xp" + Gk
                for lv in range(4):
                    sh = 1 << lv
                    dst = plv[gi][lv]; dk = "plv%d_%d" % (gi, lv)
                    c.op("pool", lambda e: e.tensor_tensor(out=dst[:, sh:15 + TB], in0=cur[:, sh:15 + TB],
                                                           in1=cur[:, 0:15 + TB - sh], op=ALU.add), r=[curk], w=[dk])
                    cur, curk = dst, dk
                c.op("dve", lambda e: e.tensor_scalar(out=pacc[gi][:], in0=plv[gi][0][:, 15:15 + TB],
                                                      scalar1=psel[:, gi, 0:1], scalar2=None, op0=ALU.mult),
                     r=["plv%d_0" % gi, "psel"], w=["pacc" + Gk])
                for lv in range(1, 4):
                    c.op("dve", lambda e: e.scalar_tensor_tensor(out=pacc[gi][:], in0=plv[gi][lv][:, 15:15 + TB],
                                                                 scalar=psel[:, gi, lv:lv + 1], in1=pacc[gi][:],
                                                                 op0=ALU.mult, op1=ALU.add),
                         r=["plv%d_%d" % (gi, lv), "psel", "pacc" + Gk], w=["pacc" + Gk])
                if b == 0:
                    c.op("dve", lambda e: e.tensor_tensor(out=pm[gi][:], in0=pacc[gi][:], in1=invdiv[:, gi, :],
                                                          op=ALU.mult), r=["pacc" + Gk, "invdiv"], w=["pm" + Gk])
                    c.op("dve", lambda e: e.tensor_tensor(out=pd[gi][:], in0=pm[gi][:], in1=Xp[gi][:, 15:15 + TB],
                                                          op=ALU.subtract), r=["pm" + Gk, "Xp" + Gk], w=["pd" + Gk])
                else:
                    c.op("dve", lambda e: e.scalar_tensor_tensor(out=pd[gi][:], in0=pacc[gi][:],
                                                                 scalar=pv(29, gi), in1=Xp[gi][:, 15:15 + TB],
                                                                 op0=ALU.mult, op1=ALU.subtract),
                         r=["pacc" + Gk, "Xp" + Gk, "pvec"], w=["pd" + Gk])
                c.op("pe", lambda e: e.matmul(bk["D"][:], poolw[:, gi, :], pd[gi][:], start=True, stop=True),
                     r=["poolw", "pd" + Gk], w=["bkD"])
                c.op("act", lambda e: e.activation(out=po[gi][:], in_=bk["D"][:], func=AF.Identity, scale=pv(22, gi)),
                     r=["bkD", "pvec"], w=["po" + Gk])
                c.dma("sp", ypl[gi * 128:(gi + 1) * 128, t0:t0 + TB], po[gi][:], r=["po" + Gk])

            dbg(3)
            for ci in range(CPB):
                cc = chunk_counter[0]; chunk_counter[0] += 1
                pp = cc % 2
                cs = slice(ci * CH, (ci + 1) * CH)
                tm = tokmaj[pp]; sa = SA[pp]; sbb = SB_[pp]; sc = SC[pp]
                tmk, sak, sbk, sck = "tokmaj%d" % pp, "SA%d" % pp, "SB%d" % pp, "SC%d" % pp
                for hp in P2:
                    H = str(hp)
                    for j, (src, sk) in enumerate(((Bt[hp], "Bt" + H), (Kt[hp], "Kt" + H), (Vb[hp], "Vb" + H))):
                        c.op("pe", lambda e, src=src, j=j, hp=hp: e.transpose(
                            trp[0:64, j * 256 + hp * 128: j * 256 + (hp + 1) * 128], src[:, cs], identb[:]),
                            r=[sk, "identb"], w=["bktrp"])
                c.op("act", lambda e: e.copy(out=tm[:], in_=trp[0:64, 0:768]), r=["bktrp"], w=[tmk])
                dbg(4)
                def ARv(h, lo, hi):
                    hp_ = h // 2
                    t_ = ARt[hp_] if h % 2 == 0 else ARo[hp_]
                    return t_[0:64, ci * 128 + lo: ci * 128 + hi]
                def Bv(h):
                    hp_ = h // 2
                    return (Bt[hp_] if h % 2 == 0 else Bo[hp_])[0:64, cs]
                def Kv(h):
                    hp_ = h // 2
                    return (Kt[hp_] if h % 2 == 0 else Ko[hp_])[0:64, cs]
                ARk = lambda h: ("ARt%d" if h % 2 == 0 else "ARo%d") % (h // 2)
                Bk = lambda h: ("Bt%d" if h % 2 == 0 else "Bo%d") % (h // 2)
                Kk = lambda h: ("Kt%d" if h % 2 == 0 else "Ko%d") % (h // 2)
                for h in range(4):
                    c.op("pe", lambda e: e.matmul(bk["A"][0:64, h * 128:(h + 1) * 128], Bv(h), ARv(h, 0, 128),
                                                  start=True, stop=True), r=[Bk(h), ARk(h)], w=["bkA"])
                    c.op("pe", lambda e: e.matmul(bk["B"][0:64, h * 128:(h + 1) * 128], Kv(h), ARv(h, 0, 128),
                                                  start=True, stop=True), r=[Kk(h), ARk(h)], w=["bkB"])
                    c.op("pe", lambda e: e.matmul(bk["C"][0:64, h * 64:(h + 1) * 64], ARv(h, 0, 64), Bv(h),
                                                  start=True, stop=True), r=[Bk(h), ARk(h)], w=["bkC"])
                c.op("dve", lambda e: e.tensor_tensor(out=sa[:], in0=bk["A"][0:64, :], in1=maskA[:], op=ALU.mult),
                     r=["bkA", "maskA"], w=[sak])
                c.op("dve", lambda e: e.tensor_tensor(out=sbb[:], in0=bk["B"][0:64, :], in1=maskA[:], op=ALU.mult),
                     r=["bkB", "maskA"], w=[sbk])
                c.op("dve", lambda e: e.tensor_tensor(out=sc[:], in0=bk["C"][0:64, 0:256], in1=maskC[:], op=ALU.mult),
                     r=["bkC", "maskC"], w=[sck])
                Nv = lambda h: sa[:, h * 128: h * 128 + 64]
                NTv = lambda h: sc[:, h * 64:(h + 1) * 64]
                dbg(5)
                c.op("pool", lambda e: e.tensor_tensor(
                    out=TT[0][:].rearrange("p (h t) -> p h t", t=64),
                    in0=sa[:].rearrange("p (h x) -> p h x", x=128)[:, :, 0:64],
                    in1=identrep[:].rearrange("p (h t) -> p h t", t=64), op=ALU.add),
                    r=[sak, "identrep"], w=["TT0"])
                for h in range(4):
                    c.op("pe", lambda e: e.matmul(bk["D"][0:64, h * 64:(h + 1) * 64], NTv(h), Nv(h), start=True, stop=True),
                         r=[sak, sck], w=["bkD"])
                    c.op("pe", lambda e: e.matmul(bk["D"][0:64, 256 + h * 64: 256 + (h + 1) * 64], Nv(h), NTv(h),
                                                  start=True, stop=True), r=[sak, sck], w=["bkD"])
                c.op("act", lambda e: e.copy(out=PP[0][:], in_=bk["D"][0:64, :]), r=["bkD"], w=["PP0"])
                for lv in range(1, 6):
                    pi = (lv - 1) % 2; po_ = lv % 2
                    Pv = lambda h: PP[pi][:, h * 64:(h + 1) * 64]
                    PTv = lambda h: PP[pi][:, 256 + h * 64: 256 + (h + 1) * 64]
                    TTv = lambda h: TT[pi][:, h * 64:(h + 1) * 64]
                    for h in range(4):
                        c.op("pe", lambda e: e.matmul(bk["E"][0:64, h * 64:(h + 1) * 64], identb[0:64, 0:64], TTv(h),
                                                      start=True, stop=False), r=["identb", "TT%d" % pi], w=["bkE"])
                        c.op("pe", lambda e: e.matmul(bk["E"][0:64, h * 64:(h + 1) * 64], PTv(h), TTv(h),
                                                      start=False, stop=True), r=["PP%d" % pi, "TT%d" % pi], w=["bkE"])
                    if lv < 5:
                        for h in range(4):
                            c.op("pe", lambda e: e.matmul(bk["D"][0:64, h * 64:(h + 1) * 64], PTv(h), Pv(h),
                                                          start=True, stop=True), r=["PP%d" % pi], w=["bkD"])
                            c.op("pe", lambda e: e.matmul(bk["D"][0:64, 256 + h * 64: 256 + (h + 1) * 64], Pv(h), PTv(h),
                                                          start=True, stop=True), r=["PP%d" % pi], w=["bkD"])
                    c.op("dve", lambda e: e.tensor_copy(TT[po_][:], bk["E"][0:64, 0:256]), r=["bkE"], w=["TT%d" % po_])
                    if lv < 5:
                        c.op("act", lambda e: e.copy(out=PP[po_][:], in_=bk["D"][0:64, :]), r=["bkD"], w=["PP%d" % po_])
                dbg(6)
                TTf = TT[1]; TTk = "TT1"
                Mold = [M0b[h][pp] for h in range(4)]; Mnew = [M0b[h][1 - pp] for h in range(4)]
                Mok = ["M0b%d_%d" % (h, pp) for h in range(4)]; Mnk = ["M0b%d_%d" % (h, 1 - pp) for h in range(4)]
                Uc = U[pp]; Uk = "U%d" % pp
                for h in range(4):
                    c.op("pe", lambda e: e.matmul(bk["FG"][0:64, h * 64:(h + 1) * 64], ARv(h, 0, 64), Mold[h][:],
                                                  start=True, stop=False), r=[ARk(h), Mok[h]], w=["bkFG"])
                    c.op("pe", lambda e: e.matmul(bk["FG"][0:64, h * 64:(h + 1) * 64], sbb[:, h * 128: h * 128 + 64],
                                                  tm[:, 512 + h * 64: 512 + (h + 1) * 64], start=False, stop=True),
                         r=[sbk, tmk], w=["bkFG"])
                c.op("act", lambda e: e.copy(out=W1[:], in_=bk["FG"][0:64, 0:256]), r=["bkFG"], w=["W1"])
                for h in range(4):
                    c.op("pe", lambda e: e.matmul(bk["FG"][0:64, 256 + h * 64: 256 + (h + 1) * 64],
                                                  TTf[:, h * 64:(h + 1) * 64], W1[:, h * 64:(h + 1) * 64],
                                                  start=True, stop=True), r=[TTk, "W1"], w=["bkFG"])
                c.op("act", lambda e: e.copy(out=Uc[:], in_=bk["FG"][0:64, 256:512]), r=["bkFG"], w=[Uk])
                dbg(7)
                for h in range(4):
                    hp, base = h // 2, (h % 2) * 64
                    o = bk["HI"][base:base + 64, hp * 64:(hp + 1) * 64]
                    c.op("pe", lambda e: e.matmul(o, Mold[h][:], ARv(h, 64, 128), start=True, stop=False),
                         r=[ARk(h), Mok[h]], w=["bkHI"])
                    c.op("pe", lambda e: e.matmul(o, Uc[:, h * 64:(h + 1) * 64], sa[:, h * 128 + 64:(h + 1) * 128],
                                                  start=False, stop=False), r=[Uk, sak], w=["bkHI"])
                    c.op("pe", lambda e: e.matmul(o, tm[:, 512 + h * 64: 512 + (h + 1) * 64],
                                                  sbb[:, h * 128 + 64:(h + 1) * 128], start=False, stop=True),
                         r=[tmk, sbk], w=["bkHI"])
                for h in range(4):
                    o = bk["HI"][0:64, 256 + h * 64: 256 + (h + 1) * 64]
                    c.op("pe", lambda e: e.matmul(o, tm[:, 256 + h * 64: 256 + (h + 1) * 64],
                                                  tm[:, 512 + h * 64: 512 + (h + 1) * 64], start=True, stop=False),
                         r=[tmk], w=["bkHI"])
                    c.op("pe", lambda e: e.matmul(o, tm[:, h * 64:(h + 1) * 64], Uc[:, h * 64:(h + 1) * 64],
                                                  start=False, stop=False), r=[tmk, Uk], w=["bkHI"])
                    c.op("pe", lambda e: e.matmul(o, identb[0:64, 0:64], Mold[h][:],
                                                  start=False, stop=True), r=["identb", Mok[h]], w=["bkHI"])
                for hp in P2:
                    H = str(hp)
                    c.op("act", lambda e: e.copy(out=yraw[hp][:, cs], in_=bk["HI"][:, hp * 64:(hp + 1) * 64]),
                         r=["bkHI"], w=["yraw" + H])
                for h in range(4):
                    hp = h // 2
                    gsrc, gk = (GCs[hp], "GCs%d" % hp) if h % 2 == 0 else (GCo[hp], "GCo%d" % hp)
                    c.op("act", lambda e: e.activation(out=Mnew[h][:], in_=bk["HI"][0:64, 256 + h * 64: 256 + (h + 1) * 64],
                                                       func=AF.Identity, scale=gsrc[0:64, ci:ci + 1]),
                         r=["bkHI", gk], w=[Mnk[h]])

            dbg(8)
            for hp in P2:
                H = str(hp)
                rows = slice(hp * 128, (hp + 1) * 128)
                c.op("pe", lambda e: e.matmul(bk["A"][:], bo[:], yraw[hp][:], start=True, stop=True),
                     r=["bo", "yraw" + H], w=["bkA"])
                c.op("dve", lambda e: e.scalar_tensor_tensor(out=yc[hp][:], in0=bk["A"][:], scalar=-1.0 / 64,
                                                             in1=yraw[hp][:], op0=ALU.mult, op1=ALU.add),
                     r=["bkA", "yraw" + H], w=["yc" + H])
                c.op("pool", lambda e: e.tensor_tensor(out=ysq[hp][:], in0=yc[hp][:], in1=yc[hp][:], op=ALU.mult),
                     r=["yc" + H], w=["ysq"])
                c.op("pe", lambda e: e.matmul(bk["B"][:], bo[:], ysq[hp][:], start=True, stop=True),
                     r=["bo", "ysq"], w=["bkB"])
                c.op("act", lambda e: e.activation(out=yrs[hp][:], in_=bk["B"][:], func=AF.Sqrt, bias=gneps[:, 0:1],
                                                   scale=1.0 / 64), r=["bkB", "gneps"], w=["yrs"])
                c.op("dve", lambda e: e.reciprocal(out=yrs[hp][:], in_=yrs[hp][:]), r=["yrs"], w=["yrs"])
                c.op("dve", lambda e: e.tensor_tensor(out=yc[hp][:], in0=yc[hp][:], in1=yrs[hp][:], op=ALU.mult),
                     r=["yc" + H, "yrs"], w=["yc" + H])
                c.op("dve", lambda e: e.tensor_scalar(out=yc[hp][:], in0=yc[hp][:], scalar1=pv(18, hp), scalar2=pv(20, hp),
                                                      op0=ALU.mult, op1=ALU.add), r=["yc" + H, "pvec"], w=["yc" + H])
                c.op("pool", lambda e: e.tensor_tensor(out=yc[hp][:], in0=yc[hp][:], in1=bonus[hp][:], op=ALU.add),
                     r=["yc" + H, "bonus" + H], w=["yc" + H])
                c.op("pool", lambda e: e.tensor_tensor(out=yo[hp][:], in0=yc[hp][:], in1=gg[hp][:], op=ALU.mult),
                     r=["yc" + H, "gg" + H], w=["yo" + H])
                c.dma("sp", yrw[rows, t0:t0 + TB], yo[hp][:], r=["yo" + H])
    try:
        body()
    except _Stop:
        pass


def build_L2(layer1):
    nc = bass.Bass("TRN2", target_bir_lowering=False)
    dt = lambda name, shape, kind="ExternalInput": nc.dram_tensor(name, list(shape), F32, kind=kind).ap()
    A = dict(rT=dt("rT", [256, SEQ]), kT=dt("kT", [256, SEQ]), vT=dt("vT", [256, SEQ]),
             waT=dt("waT", [128, SEQ]), gdT=dt("gdT", [128, SEQ]), poolT=dt("poolT", [256, SEQ]))
    if layer1:
        A.update(vdT=dt("vdT", [32, SEQ]), vfT=dt("vfT", [256, SEQ]), vup=dt("vup", [32, 256]))
    A.update(pvec=dt("pvec", [128, 32]), wup=dt("wup", [64, 256]), aup=dt("aup", [64, 256]),
             gup=dt("gup", [128, 256]), poolw=dt("poolw", [2, 128, 128]),
             maskA=dt("maskA", [64, 512]), maskC=dt("maskC", [64, 256]), identrep=dt("identrep", [64, 256]),
             scanmask=dt("scanmask", [128, TB]), blockones=dt("blockones", [128, 128]), ident=dt("ident", [128, 128]),
             invdiv=dt("invdiv", [128, 2, TB]), psel=dt("psel", [128, 2, 4]))
    yT = dt("yT", [512, SEQ], "ExternalOutput")
    A["yrw"] = yT[0:256]; A["ypl"] = yT[256:512]
    if not layer1:
        A["vout"] = dt("vout", [256, SEQ], "ExternalOutput")
    c = Ctx(nc)
    c.begin_phase("")
    emit_L2(c, A, layer1)
    c.end_phase()
    c.finish()
    return nc


def l2_inputs(inp, l, g, projT, vfT):
    f = lambda a: np.ascontiguousarray(a, dtype=np.float32)
    mu = inp["mu_shift"][l]
    cs = slice(g * 256, (g + 1) * 256)
    pvec = np.zeros((128, 32), np.float32)
    def put(col, vec512):
        v = np.asarray(vec512)[cs].reshape(2, 128)
        pvec[:, col] = v[0]; pvec[:, col + 1] = v[1]
    put(0, mu[0:512]); put(2, mu[512:1024]); put(4, mu[1024:1536])
    put(6, inp["w0"][l]); put(8, inp["a0"][l]); put(10, inp["k_k"][l]); put(12, inp["k_a"][l])
    put(16, inp["r_k"][l].reshape(512)); put(18, inp["lnx_g"][l]); put(20, inp["lnx_b"][l])
    put(22, inp["pool_scale"][l])
    pvec[:, 24] = mu[1664:1792]; pvec[0:64, 25] = mu[1536:1600]; pvec[64:128, 25] = mu[1600:1664]
    d = dict(rT=f(projT[0:512][cs]), kT=f(projT[512:1024][cs]), vT=f(projT[1024:1536][cs]),
             waT=f(projT[1536:1664]), gdT=f(projT[1664:1792]), poolT=f(projT[1792:2304][cs]),
             wup=f(inp["w_up"][l][:, cs]), aup=f(inp["a_up"][l][:, cs]), gup=f(inp["g_up"][l][:, cs]),
             poolw=f(inp["pool_w"][l][2 * g:2 * g + 2]))
    if l > 0:
        pvec[0:32, 26] = inp["vres_mu"][l - 1]
        put(27, inp["vres_v0"][l - 1])
        d.update(vdT=f(projT[2304:2336]), vfT=f(vfT), vup=f(inp["vres_up"][l - 1][:, cs]))
    d["pvec"] = pvec
    pos = np.arange(1, TB + 1, dtype=np.float32)
    invdiv = np.stack([np.broadcast_to(1.0 / np.minimum(pos, float(POOL_WINDOWS[2 * g + gi])), (128, TB))
                       for gi in range(2)], axis=1)
    d["invdiv"] = f(invdiv)
    psel = np.zeros((128, 2, 4), np.float32)
    for gi in range(2):
        psel[:, gi, 2 * g + gi] = 1.0
        pvec[:, 29 + gi] = 1.0 / POOL_WINDOWS[2 * g + gi]
    d["psel"] = psel
    d.update(l2_consts())
    return d


EPC = 2048
def emit_L0(c, A, layers, nch):
    u_d, v_d, wo_d, pq_d, id_d = A["u"], A["v"], A["w_out"], A["peer_q"], A["ident"]
    ut_o, vb_o, wo_o, pq_o = A["UT"], A["Vb"], A["woutb"], A["pqb"]
    identf = c.sb("identf", [128, 128]); identb = c.sb("identb", [128, 128], BF16)
    NBUF0 = 4
    uf = [c.sb("uf%d" % i, [128, D]) for i in range(NBUF0)]
    ub = [c.sb("ub%d" % i, [128, D], BF16) for i in range(NBUF0)]
    uT = [c.sb("uT%d" % i, [128, D], BF16) for i in range(NBUF0)]
    vb = [c.sb("vb%d" % i, [128, D], BF16) for i in range(NBUF0)]
    wb = [c.sb("wb%d" % i, [128, 8, 128], BF16) for i in range(NBUF0)]
    tps = [c.ps("tps%d" % i, [128, 1024], BF16) for i in range(NBUF0)]
    c.dma("sp", identf[:], id_d, w=["identf"])
    c.op("dve", lambda e: e.tensor_copy(identb[:], identf[:]), r=["identf"], w=["identb"])
    k = 0
    for l in layers:
        for ch in range(nch):
            s_ = k % NBUF0; k += 1
            S = str(s_)
            rows = slice(ch * 128, (ch + 1) * 128)
            c.dma("sp", uf[s_][:], u_d[l, rows, :], w=["uf" + S])
            c.dma("pool", vb[s_][:], v_d[l, rows, :], w=["vb" + S])
            c.dma("sp", vb_o[l, rows, :], vb[s_][:], r=["vb" + S])
            eng = "act" if s_ % 2 == 0 else "dve"
            if eng == "act":
                c.op("act", lambda e: e.copy(out=ub[s_][:], in_=uf[s_][:]), r=["uf" + S], w=["ub" + S])
            else:
                c.op("dve", lambda e: e.tensor_copy(ub[s_][:], uf[s_][:]), r=["uf" + S], w=["ub" + S])
            for dc in range(8):
                c.op("pe", lambda e: e.transpose(tps[s_][:, dc * 128:(dc + 1) * 128], ub[s_][:, dc * 128:(dc + 1) * 128],
                                                 identb[:]), r=["ub" + S, "identb"], w=["tps" + S])
            if eng == "act":
                c.op("act", lambda e: e.copy(out=uT[s_][:], in_=tps[s_][:]), r=["tps" + S], w=["uT" + S])
            else:
                c.op("dve", lambda e: e.tensor_copy(uT[s_][:], tps[s_][:]), r=["tps" + S], w=["uT" + S])
            c.dma("sp", ut_o[l, ch], uT[s_][:].rearrange("p (dc e) -> p dc e", e=128), r=["uT" + S])
        for cc in range(8):
            s_ = k % NBUF0; k += 1
            S = str(s_)
            c.dma("pool", vb[s_][:], wo_d[l, cc * 128:(cc + 1) * 128, :], w=["vb" + S])
            c.dma("sp", wo_o[l, cc], vb[s_][:], r=["vb" + S])
        pqv = pq_d[l].rearrange("(dc p) j -> p dc j", p=128)
        for jc in range(16):
            s_ = k % NBUF0; k += 1
            S = str(s_)
            c.dma("pool", wb[s_][:], pqv[:, :, jc * 128:(jc + 1) * 128], w=["wb" + S])
            c.dma("sp", pq_o[l, jc], wb[s_][:], r=["wb" + S])


def build_L0():
    nc = bass.Bass("TRN2", target_bir_lowering=False)
    di = lambda name, shape: nc.dram_tensor(name, list(shape), F32, kind="ExternalInput").ap()
    do = lambda name, shape: nc.dram_tensor(name, list(shape), BF16, kind="ExternalOutput").ap()
    A = dict(u=di("u", [2, EPC, D]), v=di("v", [2, EPC, D]), w_out=di("w_out", [2, D, D]),
             peer_q=di("peer_q", [2, D, 2048]), ident=di("ident", [128, 128]),
             UT=do("UT", [2, EPC // 128, 128, 8, 128]), Vb=do("Vb", [2, EPC, D]),
             woutb=do("woutb", [2, 8, 128, D]), pqb=do("pqb", [2, 16, 128, 8, 128]))
    c = Ctx(nc)
    c.begin_phase("")
    emit_L0(c, A, range(2), EPC // 128)
    c.end_phase()
    c.finish()
    return nc


TS = 256
NST = TOK // TS
NEG = -1.0e30


def emit_L3(c, A, final):
    nc = c.nc
    x_d, c_d, adaw_d, adab_d, g_d, lnf_d = A["x"], A["c_fm"], A["ada_w"], A["ada_b_fm"], A["ln_g_fm"], A["lnf_fm"]
    wo_d, pq_d, keys_d, ut_d, vb_d, id_d, ones_d, xo_d = (A["woutb"], A["pqb"], A["keys"], A["UT"], A["Vb"],
                                                          A["ident"], A["ones"], A["xo"])
    ysel = "hsel" in A
    sb = c.sb
    c.adaw_sb = sb("adaw", [128, 8, 256])
    c_sb = sb("c_sb", [128, 8]); sig_c = sb("sig_c", [128, 8]); silu_c = sb("silu_c", [128, 8])
    adab = sb("adab", [128, 48]); lng = sb("lng", [128, 8]); lnf = sb("lnf", [128, 8])
    modT = sb("modT", [128, 32]); effs = sb("effs", [128, 8]); epsb = sb("epsb", [128, 1])
    identf = sb("identf", [128, 128]); identb = sb("identb", [128, 128], BF16); onesf = sb("onesf", [128, 128])
    diag = sb("diag", [128, 512])
    g1bc = sb("g1bc", [128, D]); g2bc = sb("g2bc", [128, D]); lnfbc = sb("lnfbc", [128, D])
    kf = sb("kf", [128, 16, 128]); keysT = sb("keysT", [128, 16, 128], BF16)
    wst = [sb("wst%d" % i, [128, D], BF16) for i in range(3)]
    x1 = [sb("x1_%d" % i, [128, D]) for i in range(2)]
    yTb = sb("yTb", [128, 8, TS], BF16)
    if ysel:
        ysa = sb("ysa", [128, 8, TS], BF16); ysb = sb("ysb", [128, 8, TS], BF16); hsel = sb("hsel_sb", [128, 2])
    junk = sb("junk", [128, D], BF16); xn = sb("xn", [128, D], BF16)
    ssq = sb("ssq", [128, 4]); rstd = sb("rstd", [128, 4])
    h2T = sb("h2T", [128, 8, TS], BF16); qT = sb("qT", [128, 16, TS], BF16)
    ST = sb("ST", [128, 16, TS]); SC = sb("SC", [128, 2048])
    wk = [sb("wk%d" % i, [128, 128]) for i in range(2)]
    stop = sb("stop", [128, 16, 16]); candh = [sb("candh%d" % i, [128, 256]) for i in range(2)]
    cwk = [sb("cwk%d" % i, [128, 256]) for i in range(2)]; ctop = sb("ctop", [128, 8, 16])
    negm = sb("negm", [128, 8]); esub = sb("esub", [128, 128]); exs = sb("exs", [128, 128])
    Zs = sb("Zs", [128, 8]); invZ = sb("invZ", [128, 8])
    PACK = sb("PACK", [128, 4, 128]); PKT = sb("PKT", [128, 4, TS])
    rep = [sb("rep%d" % i, [128, 256]) for i in range(2)]
    E0 = [sb("E0_%d" % i, [128, 128], BF16) for i in range(2)]
    D1 = [sb("D1_%d" % i, [128, 128], BF16) for i in range(2)]
    ex1 = [sb("ex1_%d" % i, [128, 128]) for i in range(2)]
    gate = sb("gate_all", [128, 128, TS], BF16)
    ut4 = [sb("ut4_%d" % i, [128, 2, D], BF16) for i in range(2)]
    vt4 = [sb("vt4_%d" % i, [128, 2, D], BF16) for i in range(2)]
    NCH = 2
    ge = [sb("ge%d" % i, [128, TS], BF16) for i in range(2)]
    AT = [sb("AT%d" % i, [128, TS], BF16) for i in range(2)]
    tmpo = sb("tmpo", [128, 512])
    B = [c.ps("b%d" % i, [128, 512]) for i in range(8)]
    Bb = c.ps

    ld = lambda dst, src, key: c.dma("sp", dst, src, w=[key])
    ld(c_sb[:], c_d, "c_sb"); ld(adab[:], adab_d, "adab"); ld(lng[:], g_d, "lng"); ld(lnf[:], lnf_d, "lnf")
    ld(identf[:], id_d, "identf"); ld(onesf[:], ones_d, "onesf")
    if ysel:
        ld(hsel[:], A["hsel"], "hsel")
    ld(kf[:], keys_d.rearrange("h p k d -> k (h p) d"), "kf")
    c.op("dve", lambda e: e.tensor_copy(identb[:], identf[:]), r=["identf"], w=["identb"])
    c.op("dve", lambda e: e.memset(epsb[:], NORM_EPS), w=["epsb"])
    c.op("act", lambda e: e.activation(out=sig_c[:], in_=c_sb[:], func=AF.Sigmoid), r=["c_sb"], w=["sig_c"])
    c.op("dve", lambda e: e.tensor_tensor(out=silu_c[:], in0=c_sb[:], in1=sig_c[:], op=ALU.mult),
         r=["c_sb", "sig_c"], w=["silu_c"])
    emit_mod_fm(c, adaw_d, 0, 32, silu_c, adab, 16, modT[:, 0:32], B[0], "modT")
    c.op("dve", lambda e: e.scalar_tensor_tensor(out=effs[:], in0=modT[:, 16:24], scalar=1.0, in1=lng[:],
                                                  op0=ALU.add, op1=ALU.mult), r=["modT", "lng"], w=["effs"])

    def bcast_rows(vec8, vkey, out_tile, okey):
        for hf in range(2):
            for q in range(4):
                jc = hf * 4 + q
                c.op("dve", lambda e: e.tensor_scalar(out=diag[:, q * 128:(q + 1) * 128], in0=identf[:],
                                                      scalar1=vec8[:, jc:jc + 1], scalar2=None, op0=ALU.mult),
                     r=["identf", vkey], w=["diag%d" % q])
                c.op("pe", lambda e: e.matmul(B[1][:, q * 128:(q + 1) * 128], onesf[:], diag[:, q * 128:(q + 1) * 128],
                                              start=True, stop=True), r=["onesf", "diag%d" % q], w=["b1"])
            c.op("act", lambda e: e.copy(out=out_tile[:, hf * 512:(hf + 1) * 512], in_=B[1][:]), r=["b1"], w=[okey])

    bcast_rows(modT[:, 0:8], "modT", g1bc, "g1bc")
    bcast_rows(modT[:, 24:32], "modT", g2bc, "g2bc")
    if final:
        bcast_rows(lnf, "lnf", lnfbc, "lnfbc")
    for q4 in range(4):
        for q in range(4):
            hp16 = q4 * 4 + q
            c.op("pe", lambda e: e.transpose(B[2][:, q * 128:(q + 1) * 128], kf[:, hp16, :], identf[:]),
                 r=["kf", "identf"], w=["b2"])
        c.op("act", lambda e: e.copy(out=keysT[:, q4 * 4:(q4 + 1) * 4, :],
                                     in_=B[2][:].rearrange("p (q k) -> p q k", k=128)), r=["b2"], w=["keysT"])
    wsi = [0]

    def wstream(src_ap):
        i = wsi[0] % 3; wsi[0] += 1
        c.dma("sp", wst[i][:], src_ap, w=["wst%d" % i])
        return wst[i], "wst%d" % i

    def body():
        dbg(1)
        for st in range(DBG_NST or NST):
            tok0 = st * TS
            if not ysel:
                c.dma("pool", yTb[:], A["yT"].rearrange("(cc p) t -> p cc t", p=128)[:, :, tok0:tok0 + TS], w=["yTb"])
            else:
                c.dma("pool", ysa[:], A["yA"].rearrange("(cc p) t -> p cc t", p=128)[:, :, tok0:tok0 + TS], w=["ysa"])
                c.dma("pool", ysb[:], A["yB"].rearrange("(cc p) t -> p cc t", p=128)[:, :, tok0:tok0 + TS], w=["ysb"])
                c.op("dve", lambda e: e.tensor_scalar(out=ysa[:], in0=ysa[:], scalar1=hsel[:, 0:1], scalar2=None,
                                                      op0=ALU.mult), r=["ysa", "hsel"], w=["ysa"])
                c.op("dve", lambda e: e.scalar_tensor_tensor(out=yTb[:], in0=ysb[:], scalar=hsel[:, 1:2], in1=ysa[:],
                                                             op0=ALU.mult, op1=ALU.add),
                     r=["ysa", "ysb", "hsel"], w=["yTb"])
            wts = []
            for tt in range(2):
                X = x1[tt]; Xk = "x1_%d" % tt
                c.dma("sp", X[:], x_d[tok0 + tt * 128: tok0 + (tt + 1) * 128, :], w=[Xk])
                for cc in range(8):
                    wt, wkk = wstream(wo_d[cc])
                    for hf in range(2):
                        c.op("pe", lambda e: e.matmul(B[hf][:], yTb[:, cc, tt * 128:(tt + 1) * 128],
                                                      wt[:, hf * 512:(hf + 1) * 512], start=(cc == 0), stop=(cc == 7)),
                             r=["yTb", wkk], w=["b%d" % hf])
                for hf in range(2):
                    c.op("dve", lambda e: e.tensor_tensor(out=tmpo[:], in0=B[hf][:], in1=g1bc[:, hf * 512:(hf + 1) * 512],
                                                          op=ALU.mult), r=["b%d" % hf, "g1bc"], w=["tmpo"])
                    c.op("dve", lambda e: e.tensor_tensor(out=X[:, hf * 512:(hf + 1) * 512], in0=tmpo[:],
                                                          in1=X[:, hf * 512:(hf + 1) * 512], op=ALU.add),
                         r=["tmpo", Xk], w=[Xk])
                c.op("dve", lambda e: e.memset(ssq[:, tt:tt + 1], 0.0), w=["ssq"])
                c.op("act", lambda e: e.activation(out=junk[:], in_=X[:], func=AF.Square, accum_out=ssq[:, tt:tt + 1]),
                     r=[Xk, "ssq"], w=["junk", "ssq"])
                c.op("act", lambda e: e.activation(out=rstd[:, tt:tt + 1], in_=ssq[:, tt:tt + 1], func=AF.Sqrt,
                                                   bias=epsb[:, 0:1], scale=1.0 / D), r=["ssq", "epsb"], w=["rstd"])
                c.op("dve", lambda e: e.reciprocal(out=rstd[:, tt:tt + 1], in_=rstd[:, tt:tt + 1]), r=["rstd"], w=["rstd"])
                c.op("dve", lambda e: e.tensor_scalar(out=xn[:], in0=X[:], scalar1=rstd[:, tt:tt + 1], scalar2=None,
                                                      op0=ALU.mult), r=[Xk, "rstd"], w=["xn"])
                tpb = B[2 + tt][:].bitcast(BF16)
                for dc in range(8):
                    c.op("pe", lambda e: e.transpose(tpb[:, dc * 128:(dc + 1) * 128], xn[:, dc * 128:(dc + 1) * 128],
                                                     identb[:]), r=["xn", "identb"], w=["b%d" % (2 + tt)])
                for dc in range(8):
                    c.op("act", lambda e: e.activation(out=h2T[:, dc, tt * 128:(tt + 1) * 128],
                                                       in_=tpb[:, dc * 128:(dc + 1) * 128], func=AF.Identity,
                                                       bias=modT[:, 8 + dc: 9 + dc], scale=effs[:, dc:dc + 1]),
                         r=["b%d" % (2 + tt), "modT", "effs"], w=["h2T"])
            dbg(2)
            for hp16 in range(16):
                wt, wkk = wstream(pq_d[hp16].rearrange("p dc j -> p (dc j)"))
                bi = 4 + (hp16 // 2) % 2
                sub = hp16 % 2
                for dc in range(8):
                    c.op("pe", lambda e: e.matmul(B[bi][:, sub * TS:(sub + 1) * TS], wt[:, dc * 128:(dc + 1) * 128],
                                                  h2T[:, dc, :], start=(dc == 0), stop=(dc == 7)),
                         r=[wkk, "h2T"], w=["b%d" % bi])
                if sub == 1:
                    c.op("act", lambda e: e.copy(out=qT[:, hp16 - 1: hp16 + 1, :],
                                                 in_=B[bi][:].rearrange("p (s t) -> p s t", t=TS)),
                         r=["b%d" % bi], w=["qT"])
            for hp16 in range(16):
                bi = 6 + (hp16 // 2) % 2
                sub = hp16 % 2
                c.op("pe", lambda e: e.matmul(B[bi][:, sub * TS:(sub + 1) * TS], keysT[:, hp16, :], qT[:, hp16, :],
                                              start=True, stop=True), r=["keysT", "qT"], w=["b%d" % bi])
                if sub == 1:
                    c.op("dve", lambda e: e.tensor_copy(ST[:, hp16 - 1: hp16 + 1, :],
                                                        B[bi][:].rearrange("p (s t) -> p s t", t=TS)),
                         r=["b%d" % bi], w=["ST"])
            dbg(3)
            stop4 = stop[:].rearrange("p (h two) a -> p h two a", two=2)
            for tt in range(2):
                for q4 in range(4):
                    for q in range(4):
                        hp16 = q4 * 4 + q
                        c.op("pe", lambda e: e.transpose(B[q4][:, q * 128:(q + 1) * 128],
                                                         ST[:, hp16, tt * 128:(tt + 1) * 128], identf[:]),
                             r=["ST", "identf"], w=["b%d" % q4])
                    c.op("act", lambda e: e.copy(out=SC[:, q4 * 512:(q4 + 1) * 512], in_=B[q4][:]),
                         r=["b%d" % q4], w=["SC%d" % q4])
                for hp16 in range(16):
                    w_ = wk[hp16 % 2]; wkk = "wk%d" % (hp16 % 2)
                    scv = SC[:, hp16 * 128:(hp16 + 1) * 128]; sck = "SC%d" % (hp16 // 4)
                    c.op("dve", lambda e: e.max(out=stop[:, hp16, 0:8], in_=scv), r=[sck], w=["stopA%d" % hp16])
                    c.op("dve", lambda e: e.match_replace(out=w_[:], in_to_replace=stop[:, hp16, 0:8], in_values=scv,
                                                          imm_value=NEG), r=[sck, "stopA%d" % hp16], w=[wkk])
                    c.op("dve", lambda e: e.max(out=stop[:, hp16, 8:16], in_=w_[:]), r=[wkk], w=["stopB%d" % hp16])
                stopkeys = ["stopA%d" % i for i in range(16)] + ["stopB%d" % i for i in range(16)]
                for h in range(8):
                    ch_ = candh[h % 2]; chk = "candh%d" % (h % 2)
                    cw_ = cwk[h % 2]; cwkk = "cwk%d" % (h % 2)
                    c.op("dve", lambda e: e.tensor_tensor(
                        out=ch_[:].rearrange("p (a b) -> p a b", b=16),
                        in0=stop4[:, h, 0, :].unsqueeze(2).broadcast_to([128, 16, 16]),
                        in1=stop4[:, h, 1, :].unsqueeze(1).broadcast_to([128, 16, 16]), op=ALU.add),
                        r=["stopA%d" % (2 * h), "stopB%d" % (2 * h), "stopA%d" % (2 * h + 1), "stopB%d" % (2 * h + 1)],
                        w=[chk])
                    c.op("dve", lambda e: e.max(out=ctop[:, h, 0:8], in_=ch_[:]), r=[chk], w=["ctopA%d" % h])
                    c.op("dve", lambda e: e.match_replace(out=cw_[:], in_to_replace=ctop[:, h, 0:8], in_values=ch_[:],
                                                          imm_value=NEG), r=[chk, "ctopA%d" % h], w=[cwkk])
                    c.op("dve", lambda e: e.max(out=ctop[:, h, 8:16], in_=cw_[:]), r=[cwkk], w=["ctopB%d" % h])
                ctk = ["ctopA%d" % h for h in range(8)] + ["ctopB%d" % h for h in range(8)]
                c.op("dve", lambda e: e.tensor_scalar(out=negm[:], in0=ctop[:, :, 0], scalar1=-1.0, scalar2=None,
                                                      op0=ALU.mult), r=ctk, w=["negm"])
                c.op("dve", lambda e: e.tensor_tensor(out=esub[:].rearrange("p (h a) -> p h a", a=16), in0=ctop[:],
                                                      in1=negm[:].unsqueeze(2).broadcast_to([128, 8, 16]), op=ALU.add),
                     r=ctk + ["negm"], w=["esub"])
                c.op("act", lambda e: e.activation(out=exs[:], in_=esub[:], func=AF.Exp), r=["esub"], w=["exs"])
                c.op("dve", lambda e: e.reduce_sum(out=Zs[:], in_=exs[:].rearrange("p (h a) -> p h a", a=16),
                                                   axis=mybir.AxisListType.X), r=["exs"], w=["Zs"])
                c.op("dve", lambda e: e.reciprocal(out=invZ[:], in_=Zs[:]), r=["Zs"], w=["invZ"])
                P3 = lambda j: PACK[:, j, :].rearrange("p (h a) -> p h a", a=16)
                c.op("dve", lambda e: e.tensor_copy(P3(0), stop4[:, :, 0, :]), r=stopkeys, w=["PACK0"])
                c.op("dve", lambda e: e.tensor_tensor(out=P3(1), in0=ctop[:, :, 15].unsqueeze(2).broadcast_to([128, 8, 16]),
                                                      in1=stop4[:, :, 0, :], op=ALU.subtract), r=stopkeys + ctk, w=["PACK1"])
                c.op("dve", lambda e: e.tensor_tensor(out=P3(2), in0=stop4[:, :, 0, :],
                                                      in1=negm[:].unsqueeze(2).broadcast_to([128, 8, 16]), op=ALU.add),
                     r=stopkeys + ["negm"], w=["PACK2"])
                c.op("dve", lambda e: e.tensor_copy(P3(3), invZ[:].unsqueeze(2).broadcast_to([128, 8, 16])),
                     r=["invZ"], w=["PACK3"])
                for j in range(4):
                    c.op("pe", lambda e: e.transpose(B[4][:, j * 128:(j + 1) * 128], PACK[:, j, :], identf[:]),
                         r=["PACK%d" % j, "identf"], w=["b4"])
                c.op("act", lambda e: e.copy(out=PKT[:, :, tt * 128:(tt + 1) * 128],
                                             in_=B[4][:].rearrange("p (j t) -> p j t", t=128)), r=["b4"], w=["PKT"])
            dbg(4)
            for t in range(TS):
                s_ = t % 2; S = str(s_)
                gs = (t // 4) % 2
                c.op("pool", lambda e: e.tensor_copy(
                    rep[s_][:].rearrange("p (two h a) -> p two h a", two=2, a=16),
                    ST[:, :, t].rearrange("p (h two) -> p two h", two=2).unsqueeze(3).broadcast_to([128, 2, 8, 16])),
                    r=["ST"], w=["rep" + S])
                c.op("pe", lambda e: e.transpose(B[0 + s_][:, 0:128], rep[s_][:, 0:128], identf[:]),
                     r=["rep" + S, "identf"], w=["b%d" % (0 + s_)])
                c.op("pe", lambda e: e.transpose(B[2 + s_][:, 0:128], rep[s_][:, 128:256], identf[:]),
                     r=["rep" + S, "identf"], w=["b%d" % (2 + s_)])
                c.op("dve", lambda e: e.tensor_scalar(out=E0[s_][:], in0=B[0 + s_][:, 0:128], scalar1=PKT[:, 0, t:t + 1],
                                                      scalar2=PKT[:, 3, t:t + 1], op0=ALU.is_equal, op1=ALU.mult),
                     r=["b%d" % (0 + s_), "PKT"], w=["E0_" + S])
                c.op("act", lambda e: e.activation(out=ex1[s_][:], in_=B[2 + s_][:, 0:128], func=AF.Exp,
                                                   bias=PKT[:, 2, t:t + 1]), r=["b%d" % (2 + s_), "PKT"], w=["ex1_" + S])
                c.op("dve", lambda e: e.scalar_tensor_tensor(out=D1[s_][:], in0=B[2 + s_][:, 0:128],
                                                             scalar=PKT[:, 1, t:t + 1], in1=ex1[s_][:],
                                                             op0=ALU.is_ge, op1=ALU.mult),
                     r=["b%d" % (2 + s_), "PKT", "ex1_" + S], w=["D1_" + S])
                c.op("pe", lambda e: e.matmul(B[4 + gs][:, (t % 4) * 128:(t % 4 + 1) * 128], D1[s_][:], E0[s_][:],
                                              start=True, stop=True), r=["D1_" + S, "E0_" + S], w=["b%d" % (4 + gs)])
                if t % 4 == 3:
                    c.op("act", lambda e: e.copy(out=gate[:, :, t - 3:t + 1].rearrange("p i t -> p t i"),
                                                 in_=B[4 + gs][:].rearrange("p (t i) -> p t i", i=128)),
                         r=["b%d" % (4 + gs)], w=["gate"])
            dbg(5)
            for g4 in range(128 // NCH):
                bf_ = g4 % 2; Bf = str(bf_)
                c.dma("sp", ut4[bf_][:], ut_d[g4 * NCH:(g4 + 1) * NCH].rearrange("c p dc e -> p c (dc e)"),
                      w=["ut4_" + Bf])
                c.dma("sp", vt4[bf_][:], vb_d[g4 * NCH * 128:(g4 + 1) * NCH * 128, :].rearrange("(c p) d -> p c d", p=128),
                      w=["vt4_" + Bf])
                for cix in range(NCH):
                    i0 = g4 * NCH + cix
                    pb = 4 + i0 % 2
                    gb = i0 % 2
                    for dc in range(8):
                        c.op("pe", lambda e: e.matmul(B[pb][:, 0:TS], ut4[bf_][:, cix, dc * 128:(dc + 1) * 128],
                                                      h2T[:, dc, :], start=(dc == 0), stop=(dc == 7)),
                             r=["ut4_" + Bf, "h2T"], w=["b%d" % pb])
                    c.op("act", lambda e: e.activation(out=ge[gb][:], in_=B[pb][:, 0:TS], func=AF.Gelu),
                         r=["b%d" % pb], w=["ge%d" % gb])
                    c.op("pool", lambda e: e.tensor_tensor(out=AT[gb][:], in0=ge[gb][:], in1=gate[:, i0, :], op=ALU.mult),
                         r=["ge%d" % gb, "gate"], w=["AT%d" % gb])
                    for tt in range(2):
                        for hf in range(2):
                            c.op("pe", lambda e: e.matmul(B[tt * 2 + hf][:], AT[gb][:, tt * 128:(tt + 1) * 128],
                                                          vt4[bf_][:, cix, hf * 512:(hf + 1) * 512],
                                                          start=(i0 == 0), stop=(i0 == 127)),
                                 r=["AT%d" % gb, "vt4_" + Bf], w=["b%d" % (tt * 2 + hf)])
            dbg(6)
            for tt in range(2):
                X = x1[tt]; Xk = "x1_%d" % tt
                for hf in range(2):
                    c.op("dve", lambda e: e.tensor_tensor(out=tmpo[:], in0=B[tt * 2 + hf][:],
                                                          in1=g2bc[:, hf * 512:(hf + 1) * 512], op=ALU.mult),
                         r=["b%d" % (tt * 2 + hf), "g2bc"], w=["tmpo"])
                    c.op("dve", lambda e: e.tensor_tensor(out=X[:, hf * 512:(hf + 1) * 512], in0=tmpo[:],
                                                          in1=X[:, hf * 512:(hf + 1) * 512], op=ALU.add),
                         r=["tmpo", Xk], w=[Xk])
                if final:
                    c.op("dve", lambda e: e.memset(ssq[:, 2 + tt:3 + tt], 0.0), w=["ssq"])
                    c.op("act", lambda e: e.activation(out=junk[:], in_=X[:], func=AF.Square,
                                                       accum_out=ssq[:, 2 + tt:3 + tt]), r=[Xk, "ssq"], w=["junk", "ssq"])
                    c.op("act", lambda e: e.activation(out=rstd[:, 2 + tt:3 + tt], in_=ssq[:, 2 + tt:3 + tt], func=AF.Sqrt,
                                                       bias=epsb[:, 0:1], scale=1.0 / D), r=["ssq", "epsb"], w=["rstd"])
                    c.op("dve", lambda e: e.reciprocal(out=rstd[:, 2 + tt:3 + tt], in_=rstd[:, 2 + tt:3 + tt]),
                         r=["rstd"], w=["rstd"])
                    c.op("dve", lambda e: e.scalar_tensor_tensor(out=X[:], in0=X[:], scalar=rstd[:, 2 + tt:3 + tt],
                                                                 in1=lnfbc[:], op0=ALU.mult, op1=ALU.mult),
                         r=[Xk, "rstd", "lnfbc"], w=[Xk])
                c.dma("sp", xo_d[tok0 + tt * 128: tok0 + (tt + 1) * 128, :], X[:], r=[Xk])

    try:
        body()
    except _Stop:
        pass


def build_L3(final):
    nc = bass.Bass("TRN2", target_bir_lowering=False)
    dt = lambda name, shape, d=F32, kind="ExternalInput": nc.dram_tensor(name, list(shape), d, kind=kind).ap()
    A = dict(x=dt("x", [TOK, D]), yT=dt("yT", [D, TOK]), c_fm=dt("c_fm", [128, 8]), ada_w=dt("ada_w", [D, 4 * D]),
             ada_b_fm=dt("ada_b_fm", [128, 48]), ln_g_fm=dt("ln_g_fm", [128, 8]), lnf_fm=dt("lnf_fm", [128, 8]),
             woutb=dt("woutb", [8, 128, D], BF16), pqb=dt("pqb", [16, 128, 8, 128], BF16),
             keys=dt("keys", [8, 2, 128, 128]), UT=dt("UT", [128, 128, 8, 128], BF16), Vb=dt("Vb", [128 * 128, D], BF16),
             ident=dt("ident", [128, 128]), ones=dt("ones", [128, 128]),
             xo=dt("xo", [TOK, D], F32, "ExternalOutput"))
    c = Ctx(nc)
    c.begin_phase("")
    emit_L3(c, A, final)
    c.end_phase()
    c.finish()
    return nc


NJC = 19


def build_fused():
    nc = bass.Bass("TRN2", target_bir_lowering=False)
    di = lambda name, shape, d=F32: nc.dram_tensor(name, list(shape), d, kind="ExternalInput").ap()
    it = lambda name, shape, d=F32: nc.dram_tensor(name, list(shape), d).ap()
    x_seq = di("x_seq", [SEQ, D]); x_mine = di("x_mine", [TOK, D]); c_fm = di("c_fm", [128, 8]); hsel = di("hsel", [128, 2])
    ada_w = di("ada_w", [2, D, 6 * D]); ada_b_fm = di("ada_b_fm", [2, 128, 48])
    ln1 = di("ln1_g_fm", [2, 128, 8]); ln2 = di("ln2_g_fm", [2, 128, 8]); lnf = di("lnf_fm", [128, 8])
    w_in = di("w_in", [2, D, 2304]); vres_down = di("vres_down", [D, 32])
    pvec = di("pvec", [2, 2, 128, 32]); psel = di("psel", [2, 128, 2, 4]); invdiv = di("invdiv", [2, 128, 2, TB])
    w_up = di("w_up", [2, 64, 512]); a_up = di("a_up", [2, 64, 512]); g_up = di("g_up", [2, 128, 512])
    vres_up = di("vres_up", [32, 512]); pool_w = di("pool_w", [2, 4, 128, 128])
    cst = {k: di(k, v.shape) for k, v in l2_consts().items()}
    ones = di("ones", [128, 128])
    nex = 128 if DBG_SKIP0 else 128 * 128
    peer_u = di("peer_u", [2, nex, D]); peer_v = di("peer_v", [2, nex, D])
    w_out = di("w_out", [2, D, D]); peer_q = di("peer_q", [2, D, 2048]); keys = di("peer_keys", [2, 8, 2, 128, 128])
    xo = nc.dram_tensor("xo", [TOK, D], F32, kind="ExternalOutput").ap()
    UT = it("UT_s", [2, 128, 128, 8, 128], BF16); Vb = it("Vb_s", [2, 128 * 128, D], BF16)
    woutb = it("woutb_s", [2, 8, 128, D], BF16); pqb = it("pqb_s", [2, 16, 128, 8, 128], BF16)
    P = it("P_s", [NJC * 128, SEQ]); Y = it("Y_s", [D, SEQ]); VF = it("VF_s", [512, SEQ])
    XO0 = it("XO0_s", [TOK, D]); X1 = it("X1_s", [SEQ, D])

    c = Ctx(nc)
    ph = [0]

    def phase(fn):
        if DBG_PHASES is not None and ph[0] >= DBG_PHASES:
            ph[0] += 1
            return
        c.begin_phase("p%d_" % ph[0]); ph[0] += 1
        fn()
        c.end_phase()

    if DBG_SKIP0:
        ph[0] += 1
    else:
        phase(lambda: emit_L0(c, dict(u=peer_u, v=peer_v, w_out=w_out, peer_q=peer_q, ident=cst["ident"],
                                       UT=UT, Vb=Vb, woutb=woutb, pqb=pqb), range(2), 128))
    for l in range(2):
        xsrc = x_seq if l == 0 else X1
        ncol = 2304 if l == 0 else 2336
        njc = (ncol + 127) // 128
        w_parts = [(w_in[l], 0, 2304)] + ([(vres_down, 2304, 32)] if l > 0 else [])
        for hf in range(2):
            xt_fn = {}
            if l > 0:
                xt_fn = dict(x_tile=lambda i, hf=hf: X1[((i // 4) * 2 + hf) * 512 + (i % 4) * 128:
                                                        ((i // 4) * 2 + hf) * 512 + (i % 4) * 128 + 128, :])
            phase(lambda: emit_L1(c, dict(xt_fn, x=xsrc[hf * TOK:(hf + 1) * TOK], c_fm=c_fm, ada_w=ada_w[l][:, 0:2 * D],
                                           ada_b_fm=ada_b_fm[l], ln_g_fm=ln1[l], w_parts=w_parts, ident=cst["ident"],
                                           out=P[0:njc * 128, hf * TOK:(hf + 1) * TOK], ncol=ncol)))
        for g in range(2):
            cs = slice(g * 256, (g + 1) * 256)
            A = dict(rT=P[g * 256:(g + 1) * 256], kT=P[512 + g * 256: 512 + (g + 1) * 256],
                     vT=P[1024 + g * 256: 1024 + (g + 1) * 256], waT=P[1536:1664], gdT=P[1664:1792],
                     poolT=P[1792 + g * 256: 1792 + (g + 1) * 256],
                     pvec=pvec[l, g], wup=w_up[l][:, cs], aup=a_up[l][:, cs], gup=g_up[l][:, cs],
                     poolw=pool_w[l][2 * g:2 * g + 2], psel=psel[g], invdiv=invdiv[g],
                     yrw=Y[g * 256:(g + 1) * 256], ypl=Y[512 + g * 256: 512 + (g + 1) * 256])
            A.update(cst)
            if l == 0:
                A["vout"] = VF[g * 256:(g + 1) * 256]
            else:
                A.update(vdT=P[2304:2336], vfT=VF[g * 256:(g + 1) * 256], vup=vres_up[:, cs])
            phase(lambda: emit_L2(c, A, l > 0))
        phase(lambda: emit_L3(c, dict(x=(x_mine if l == 0 else XO0), yA=Y[:, 0:TOK], yB=Y[:, TOK:2 * TOK], hsel=hsel,
                                       c_fm=c_fm, ada_w=ada_w[l][:, 2 * D:6 * D], ada_b_fm=ada_b_fm[l], ln_g_fm=ln2[l],
                                       lnf_fm=lnf, woutb=woutb[l], pqb=pqb[l], keys=keys[l], UT=UT[l], Vb=Vb[l],
                                       ident=cst["ident"], ones=ones, xo=(XO0 if l == 0 else xo)), l == 1))
        if l == 0 and DBG_COLL and (DBG_PHASES is None or DBG_PHASES > 6):
            for k in range(4):
                c.collective(lambda gq: gq.collective_compute(
                    "AllGather", ALU.bypass, replica_groups=[[0, 1], [2, 3], [4, 5], [6, 7]],
                    ins=[XO0[k * 512:(k + 1) * 512].opt()], outs=[X1[k * 1024:(k + 1) * 1024].opt()]))
    c.finish()
    return nc


def kernel_fused(inp):
    f = lambda a: np.ascontiguousarray(a, dtype=np.float32)
    cst = l2_consts()
    shared = dict(ada_w=f(inp["ada_w"]), ada_b_fm=f(np.stack([_fm(inp["ada_b"][l]) for l in range(2)])),
                  ln1_g_fm=f(np.stack([_fm(inp["ln1_g"][l]) for l in range(2)])),
                  ln2_g_fm=f(np.stack([_fm(inp["ln2_g"][l]) for l in range(2)])), lnf_fm=_fm(inp["lnf_g"]),
                  w_in=f(inp["w_in"]), vres_down=f(inp["vres_down"][0]), w_up=f(inp["w_up"]), a_up=f(inp["a_up"]),
                  g_up=f(inp["g_up"]), vres_up=f(inp["vres_up"][0]), pool_w=f(inp["pool_w"]),
                  ones=np.ones((128, 128), np.float32), peer_u=f(inp["peer_u"]), peer_v=f(inp["peer_v"]),
                  w_out=f(inp["w_out"]), peer_q=f(inp["peer_q"]), peer_keys=f(inp["peer_keys"]))
    shared.update(cst)
    dummyT = np.zeros((2336, 1), np.float32)
    pv = np.zeros((2, 2, 128, 32), np.float32); ps_ = np.zeros((2, 128, 2, 4), np.float32)
    idv = np.zeros((2, 128, 2, TB), np.float32)
    for l in range(2):
        for g in range(2):
            d = l2_inputs(inp, l, g, dummyT, np.zeros((1, 1), np.float32))
            pv[l, g] = d["pvec"]; ps_[g] = d["psel"]; idv[g] = d["invdiv"]
    shared.update(pvec=pv, psel=ps_, invdiv=idv)
    x = f(inp["x"])
    in_maps = []
    for core in range(NCORE):
        b, g = core // 2, core % 2
        hs = np.zeros((128, 2), np.float32); hs[:, g] = 1.0
        m = dict(shared)
        m.update(x_seq=f(x[b]), x_mine=f(x[b, g * TOK:(g + 1) * TOK]), c_fm=_fm(inp["c"][b]), hsel=hs)
        in_maps.append(m)
    if inp.get("_only_maps") is not None:
        return in_maps
    res = _run(_prog("fused", build_fused), in_maps)
    out = np.stack([np.asarray(res[core]["xo"]) for core in range(NCORE)], axis=0)
    return np.ascontiguousarray(out.reshape(NB, SEQ, D).astype(np.float32))


_PROGS = {}


def _prog(key, fn):
    if key not in _PROGS:
        _PROGS[key] = fn()
    return _PROGS[key]


def _run(nc, in_maps):
    res = run_bass_kernel_spmd(nc, in_maps, core_ids=list(range(NCORE)))
    return res.results


FUSED = True


def kernel(**inputs):
    inp = {k: np.asarray(v) for k, v in inputs.items()}
    if FUSED:
        return kernel_fused(inp)
    f = lambda a: np.ascontiguousarray(a, dtype=np.float32)
    ident = np.eye(128, dtype=np.float32); ones = np.ones((128, 128), np.float32)
    in_maps = []
    for core in range(NCORE):
        sl = slice(core * EPC, (core + 1) * EPC)
        in_maps.append(dict(u=f(inp["peer_u"][:, sl]), v=f(inp["peer_v"][:, sl]), w_out=f(inp["w_out"]),
                            peer_q=f(inp["peer_q"]), ident=ident))
    r0 = _run(_prog("L0", build_L0), in_maps)
    UT = [np.ascontiguousarray(np.concatenate([np.asarray(r0[c_]["UT"])[l] for c_ in range(NCORE)], axis=0)) for l in range(2)]
    Vb = [np.ascontiguousarray(np.concatenate([np.asarray(r0[c_]["Vb"])[l] for c_ in range(NCORE)], axis=0)) for l in range(2)]
    woutb = [np.ascontiguousarray(np.asarray(r0[0]["woutb"])[l]) for l in range(2)]
    pqb = [np.ascontiguousarray(np.asarray(r0[0]["pqb"])[l]) for l in range(2)]
    del r0
    x = f(inp["x"]).reshape(NCORE, TOK, D)
    vfirst = None
    for l in range(2):
        wfull = inp["w_in"][l] if l == 0 else np.concatenate([inp["w_in"][l], inp["vres_down"][l - 1]], axis=1)
        ncol = wfull.shape[1]
        adab_fm = _fm(inp["ada_b"][l])
        in_maps = []
        for core in range(NCORE):
            b = core // 2
            in_maps.append(dict(x=f(x[core]), c_fm=_fm(inp["c"][b]), ada_w=f(inp["ada_w"][l][:, 0:2 * D]),
                                ada_b_fm=adab_fm, ln_g_fm=_fm(inp["ln1_g"][l]), w_full=f(wfull), ident=ident))
        r1 = _run(_prog(("L1", ncol), lambda: build_L1(ncol)), in_maps)
        in_maps = []
        for core in range(NCORE):
            b, g = core // 2, core % 2
            projT = np.concatenate([r1[2 * b]["projT"], r1[2 * b + 1]["projT"]], axis=1)
            in_maps.append(l2_inputs(inp, l, g, projT, None if l == 0 else vfirst[core]))
        del r1
        r2 = _run(_prog(("L2", l > 0), lambda: build_L2(l > 0)), in_maps)
        if l == 0:
            vfirst = [np.asarray(r2[core]["vout"]) for core in range(NCORE)]
        in_maps = []
        for core in range(NCORE):
            b, hf = core // 2, core % 2
            ts = slice(hf * TOK, (hf + 1) * TOK)
            y0, y1 = r2[2 * b]["yT"], r2[2 * b + 1]["yT"]
            yT = np.concatenate([y0[0:256, ts], y1[0:256, ts], y0[256:512, ts], y1[256:512, ts]], axis=0)
            in_maps.append(dict(x=f(x[core]), yT=f(yT), c_fm=_fm(inp["c"][b]), ada_w=f(inp["ada_w"][l][:, 2 * D:6 * D]),
                                ada_b_fm=adab_fm, ln_g_fm=_fm(inp["ln2_g"][l]), lnf_fm=_fm(inp["lnf_g"]),
                                woutb=woutb[l], pqb=pqb[l], keys=f(inp["peer_keys"][l]), UT=UT[l], Vb=Vb[l],
                                ident=ident, ones=ones))
        del r2
        r3 = _run(_prog(("L3", l == 1), lambda: build_L3(l == 1)), in_maps)
        x = np.stack([np.asarray(r3[core]["xo"]) for core in range(NCORE)], axis=0)
        del r3
    return np.ascontiguousarray(x.reshape(NB, SEQ, D).astype(np.float32))
```

```python
from contextlib import ExitStack
import numpy as np
import ml_dtypes
import concourse.bass as bass
import concourse.mybir as mybir
from concourse.bass_utils import run_bass_kernel_spmd

F32 = mybir.dt.float32
BF16 = mybir.dt.bfloat16
AF = mybir.ActivationFunctionType
ALU = mybir.AluOpType

D = 1024
SEQ = 4096
NB = 4
NCORE = 8
TOK = 2048
NORM_EPS = 1e-6
GN_EPS = 64e-5
CH = 64


class Ctx:
    NDMA = 12

    def __init__(self, nc):
        self.nc = nc
        self.stack = ExitStack()
        self.E = dict(pe=nc.tensor, act=nc.scalar, dve=nc.vector, pool=nc.gpsimd, sp=nc.sync)
        self.sem = {}
        for e in ("pe", "act", "dve", "pool"):
            self.sem[e] = self.stack.enter_context(nc.semaphore("c_" + e))
        self.cnt = {e: 0 for e in self.sem}
        self.seen = {e: {} for e in self.E}
        self.dsem = {}
        self.dval = {}
        self.dnext = {}
        for q in ("sp", "pool", "act"):
            self.dsem[q] = [self.stack.enter_context(nc.semaphore("d_%s%d" % (q, i))) for i in range(self.NDMA)]
            self.dval[q] = [0] * self.NDMA
            self.dnext[q] = 0
        self.W = {}
        self.R = {}
        self.n_ins = 0
        self.nwait = {}
        self.ccsem = self.stack.enter_context(nc.semaphore("cc_sem"))
        self.ccval = 0

    scope = None
    pfx = ""

    def begin_phase(self, pfx):
        self.scope = ExitStack()
        self.pfx = pfx

    def end_phase(self):
        self.barrier()
        self.scope.close()
        self.scope = None
        self.W = {}
        self.R = {}

    def barrier(self):
        for eng in self.E:
            for e2 in self.cnt:
                if eng == "pe" and e2 == "pe":
                    continue
                self._wait(eng, e2, self.cnt[e2])
            for q in self.dsem:
                for i in range(self.NDMA):
                    self._wait(eng, (q, i), self.dval[q][i])

    def sb(self, name, shape, dt=F32):
        st = self.scope if self.scope is not None else self.stack
        return st.enter_context(self.nc.sbuf_tensor(self.pfx + name, list(shape), dt))

    def ps(self, name, shape, dt=F32):
        st = self.scope if self.scope is not None else self.stack
        return st.enter_context(self.nc.psum_tensor(self.pfx + name, list(shape), dt))

    def _semh(self, semid):
        if semid == "cc":
            return self.ccsem
        if isinstance(semid, str):
            return self.sem[semid]
        return self.dsem[semid[0]][semid[1]]

    def _wait(self, eng, semid, val):
        if val <= 0:
            return
        if self.seen[eng].get(semid, 0) >= val:
            return
        self.E[eng].wait_ge(self._semh(semid), val)
        self.seen[eng][semid] = val
        self.n_ins += 1
        self.nwait[eng] = self.nwait.get(eng, 0) + 1

    def _deps(self, r, w):
        deps = {}
        for k in r:
            for s, v in self.W.get(k, {}).items():
                deps[s] = max(deps.get(s, 0), v)
        for k in w:
            for s, v in self.W.get(k, {}).items():
                deps[s] = max(deps.get(s, 0), v)
            for s, v in self.R.get(k, {}).items():
                deps[s] = max(deps.get(s, 0), v)
        return deps

    def _record(self, semid, val, r, w):
        for k in w:
            self.W[k] = {semid: val}
            self.R[k] = {}
        for k in r:
            self.R.setdefault(k, {})[semid] = val

    def op(self, eng, fn, r=(), w=()):
        deps = self._deps(r, w)
        for s, v in deps.items():
            if eng == "pe" and s == "pe":
                continue
            self._wait(eng, s, v)
        ins = fn(self.E[eng])
        self.cnt[eng] += 1
        ins.then_inc(self.sem[eng], 1)
        self.n_ins += 1
        self._record(eng, self.cnt[eng], r, w)
        return ins

    def dma(self, q, out, in_, r=(), w=(), **kw):
        i = self.dnext[q]
        self.dnext[q] = (i + 1) % self.NDMA
        semid = (q, i)
        self._wait(q, semid, self.dval[q][i])
        deps = self._deps(r, w)
        for s, v in deps.items():
            self._wait(q, s, v)
        ins = self.E[q].dma_start(out=out, in_=in_, **kw)
        self.dval[q][i] += 16
        ins.then_inc(self.dsem[q][i], 16)
        self.n_ins += 1
        self._record(semid, self.dval[q][i], r, w)
        return ins

    def collective(self, fn):
        self.barrier()
        ins = fn(self.nc.gpsimd)
        self.ccval += 1
        ins.then_inc(self.ccsem)
        for eng in self.E:
            self._wait(eng, "cc", self.ccval)

    def finish(self):
        global LAST_CNT
        LAST_CNT = (dict(self.cnt), {q: list(v) for q, v in self.dval.items()}, self.n_ins, dict(self.nwait))
        for q in self.dsem:
            for i in range(self.NDMA):
                self._wait("sp", (q, i), self.dval[q][i])
        for e in self.cnt:
            self._wait("sp", e, self.cnt[e])
        self.stack.close()


def _fm(vec, n=None):
    v = np.ascontiguousarray(np.asarray(vec, dtype=np.float32).reshape(-1, 128).T)
    return v


def emit_mod_fm(c, adaw_dram, col0, ncolchunks, silu_c, adab_fm_sb, adab_col0, out_sb, ps_tile, tag):
    nc = c.nc
    wv = adaw_dram.rearrange("(dc p) j -> p dc j", p=128)
    cap = c.adaw_sb.shape[2] // 128
    for half in range(0, ncolchunks, cap):
        nch = min(cap, ncolchunks - half)
        wt = c.adaw_sb
        c.dma("sp", wt[:, :, 0:nch * 128], wv[:, :, col0 + half * 128: col0 + (half + nch) * 128],
              w=["adaw"])
        for jc in range(nch):
            for dc in range(8):
                c.op("pe", lambda e, jc=jc, dc=dc: e.matmul(
                    ps_tile[:, half + jc: half + jc + 1], wt[:, dc, jc * 128:(jc + 1) * 128],
                    silu_c[:, dc:dc + 1], start=(dc == 0), stop=(dc == 7)),
                    r=["adaw", "silu_c"], w=[tag + "_ps"])
    c.op("dve", lambda e: e.tensor_tensor(out=out_sb, in0=ps_tile[:, 0:ncolchunks],
                                           in1=adab_fm_sb[:, adab_col0: adab_col0 + ncolchunks], op=ALU.add),
         r=[tag + "_ps", "adab"], w=[tag])


DBG_STAGE = 99
DBG_PHASES = None
DBG_COLL = True
DBG_SKIP0 = False
LAST_CNT = None
DBG_NST = None


def emit_L1(c, A):
    ncol = A["ncol"]
    njc = (ncol + 127) // 128
    x_d, c_d, adaw_d, adab_d, g_d, id_d, out_d = (A["x"], A["c_fm"], A["ada_w"], A["ada_b_fm"], A["ln_g_fm"],
                                                   A["ident"], A["out"])
    c.adaw_sb = c.sb("adaw", [128, 8, 1024], F32)
    c_sb = c.sb("c_sb", [128, 8]); sig_c = c.sb("sig_c", [128, 8]); silu_c = c.sb("silu_c", [128, 8])
    adab = c.sb("adab", [128, 48]); lng = c.sb("lng", [128, 8])
    modT = c.sb("modT", [128, 16]); effs = c.sb("effs", [128, 8])
    identf = c.sb("identf", [128, 128]); identb = c.sb("identb", [128, 128], BF16)
    wbf = c.sb("wbf", [128, 8, njc * 128], BF16)
    hT = c.sb("hT", [128, 8, TOK], BF16)
    xt = [c.sb("xt%d" % i, [128, D]) for i in range(2)]
    junk = c.sb("junk", [128, D], BF16)
    xn = [c.sb("xn%d" % i, [128, D], BF16) for i in range(2)]
    ss = c.sb("ss", [128, 32]); rstd = c.sb("rstd", [128, 32])
    stg = [c.sb("stg%d" % i, [128, TOK]) for i in range(2)]
    mod_ps = c.ps("mod_ps", [128, 512])
    tp_ps = [c.ps("tp_ps%d" % i, [128, 1024], BF16) for i in range(2)]
    mm_ps = [c.ps("mm_ps%d" % i, [128, 512]) for i in range(4)]

    c.dma("sp", c_sb[:], c_d, w=["c_sb"])
    c.dma("sp", adab[:], adab_d, w=["adab"])
    c.dma("sp", lng[:], g_d, w=["lng"])
    c.dma("sp", identf[:], id_d, w=["identf"])
    c.op("dve", lambda e: e.tensor_copy(identb[:], identf[:]), r=["identf"], w=["identb"])
    if njc * 128 != ncol:
        c.op("pool", lambda e: e.memset(wbf[:, :, ncol:njc * 128], 0.0), w=["wbf"])
    for (w_ap, col0, ncols) in A["w_parts"]:
        wv = w_ap.rearrange("(dc p) j -> p dc j", p=128)
        for dc in range(8):
            c.dma("pool", wbf[:, dc, col0:col0 + ncols], wv[:, dc, :], w=["wbf"])
    c.op("act", lambda e: e.activation(out=sig_c[:], in_=c_sb[:], func=AF.Sigmoid), r=["c_sb"], w=["sig_c"])
    c.op("dve", lambda e: e.tensor_tensor(out=silu_c[:], in0=c_sb[:], in1=sig_c[:], op=ALU.mult),
         r=["c_sb", "sig_c"], w=["silu_c"])
    emit_mod_fm(c, adaw_d, 0, 16, silu_c, adab, 0, modT[:, 0:16], mod_ps, "modT")
    c.op("dve", lambda e: e.scalar_tensor_tensor(out=effs[:], in0=modT[:, 8:16], scalar=1.0, in1=lng[:],
                                                  op0=ALU.add, op1=ALU.mult), r=["modT", "lng"], w=["effs"])
    c.op("dve", lambda e: e.memset(ss[:], 0.0), w=["ss%d" % i for i in range(TOK // 128)])
    epsb = c.sb("epsb", [128, 1])
    c.op("dve", lambda e: e.memset(epsb[:], NORM_EPS), w=["epsb"])
    ntile = TOK // 128
    for i in range(ntile):
        s = i % 2
        c.dma("sp", xt[s][:], (A["x_tile"](i) if "x_tile" in A else x_d[i * 128:(i + 1) * 128, :]), w=["xt%d" % s])
        c.op("act", lambda e, s=s, i=i: e.activation(out=junk[:], in_=xt[s][:], func=AF.Square,
                                                     accum_out=ss[:, i:i + 1]),
             r=["xt%d" % s], w=["junk", "ss%d" % i])
        c.op("act", lambda e, i=i: e.activation(out=rstd[:, i:i + 1], in_=ss[:, i:i + 1], func=AF.Sqrt,
                                                bias=epsb[:, 0:1], scale=1.0 / D),
             r=["ss%d" % i, "epsb"], w=["rstd%d" % i])
        c.op("dve", lambda e, i=i: e.reciprocal(out=rstd[:, i:i + 1], in_=rstd[:, i:i + 1]),
             r=["rstd%d" % i], w=["rstd%d" % i])
        c.op("dve", lambda e, s=s, i=i: e.tensor_scalar(out=xn[s][:], in0=xt[s][:], scalar1=rstd[:, i:i + 1],
                                                        scalar2=None, op0=ALU.mult),
             r=["xt%d" % s, "rstd%d" % i], w=["xn%d" % s])
        for dc in range(8):
            c.op("pe", lambda e, s=s, dc=dc: e.transpose(tp_ps[s][:, dc * 128:(dc + 1) * 128],
                                                         xn[s][:, dc * 128:(dc + 1) * 128], identb[:]),
                 r=["xn%d" % s, "identb"], w=["tp%d" % s])
        for dc in range(8):
            if s == 0:
                c.op("act", lambda e, s=s, dc=dc, i=i: e.activation(
                    out=hT[:, dc, i * 128:(i + 1) * 128], in_=tp_ps[s][:, dc * 128:(dc + 1) * 128],
                    func=AF.Identity, bias=modT[:, dc:dc + 1], scale=effs[:, dc:dc + 1]),
                    r=["tp%d" % s, "modT", "effs"], w=["hT_%d_%d_%d" % (dc, i // 4, s)])
            else:
                c.op("dve", lambda e, s=s, dc=dc, i=i: e.tensor_scalar(
                    out=hT[:, dc, i * 128:(i + 1) * 128], in0=tp_ps[s][:, dc * 128:(dc + 1) * 128],
                    scalar1=effs[:, dc:dc + 1], scalar2=modT[:, dc:dc + 1], op0=ALU.mult, op1=ALU.add),
                    r=["tp%d" % s, "modT", "effs"], w=["hT_%d_%d_%d" % (dc, i // 4, s)])
    k = 0
    for jc in range(njc):
        st = stg[jc % 2]
        for tb in range(TOK // 512):
            pt = mm_ps[k % 4]; pk = "mm%d" % (k % 4); k += 1
            for dc in range(8):
                c.op("pe", lambda e, pt=pt, dc=dc, jc=jc, tb=tb: e.matmul(
                    pt[:], wbf[:, dc, jc * 128:(jc + 1) * 128], hT[:, dc, tb * 512:(tb + 1) * 512],
                    start=(dc == 0), stop=(dc == 7)),
                    r=["wbf", "hT_%d_%d_0" % (dc, tb), "hT_%d_%d_1" % (dc, tb)], w=[pk])
            eng = "act" if tb % 2 == 0 else "dve"
            if eng == "act":
                c.op("act", lambda e, pt=pt, st=st, tb=tb: e.copy(out=st[:, tb * 512:(tb + 1) * 512], in_=pt[:]),
                     r=[pk], w=["stg%d_%d" % (jc % 2, tb)])
            else:
                c.op("dve", lambda e, pt=pt, st=st, tb=tb: e.tensor_copy(st[:, tb * 512:(tb + 1) * 512], pt[:]),
                     r=[pk], w=["stg%d_%d" % (jc % 2, tb)])
        c.dma("sp", out_d[jc * 128:(jc + 1) * 128, :], st[:],
              r=["stg%d_%d" % (jc % 2, tb) for tb in range(4)])


def build_L1(ncol):
    nc = bass.Bass("TRN2", target_bir_lowering=False)
    njc = (ncol + 127) // 128
    di = lambda name, shape: nc.dram_tensor(name, list(shape), F32, kind="ExternalInput").ap()
    A = dict(x=di("x", [TOK, D]), c_fm=di("c_fm", [128, 8]), ada_w=di("ada_w", [D, 2 * D]),
             ada_b_fm=di("ada_b_fm", [128, 48]), ln_g_fm=di("ln_g_fm", [128, 8]), ident=di("ident", [128, 128]),
             ncol=ncol)
    A["w_parts"] = [(di("w_full", [D, ncol]), 0, ncol)]
    A["out"] = nc.dram_tensor("projT", [njc * 128, TOK], F32, kind="ExternalOutput").ap()
    c = Ctx(nc)
    c.begin_phase("")
    emit_L1(c, A)
    c.end_phase()
    c.finish()
    return nc


class _Stop(Exception):
    pass


def dbg(stage):
    if DBG_STAGE == stage:
        raise _Stop()


TB = 512
NBLK = SEQ // TB
CPB = TB // CH
C0 = float(np.exp(-0.5))
POOL_WINDOWS = (2, 4, 8, 16)


def l2_consts():
    s_idx = np.arange(64)[:, None]; t_idx = np.arange(64)[None, :]
    su = (t_idx > s_idx).astype(np.float32); iu = (t_idx >= s_idx).astype(np.float32)
    maskA = np.tile(np.concatenate([su, iu], axis=1), (1, 4))
    maskC = np.tile((t_idx < s_idx).astype(np.float32), (1, 4))
    identrep = np.tile(np.eye(64, dtype=np.float32), (1, 4))
    scanmask = np.ones((128, TB), np.float32); scanmask[:, ::CH] = 0.0
    blockones = np.kron(np.eye(2, dtype=np.float32), np.ones((64, 64), np.float32))
    return dict(maskA=maskA, maskC=maskC, identrep=identrep, scanmask=scanmask, blockones=blockones,
                ident=np.eye(128, dtype=np.float32))


def emit_L2(c, A, layer1):
    nc = c.nc
    rT, kT, vT, waT, gdT, poolT = A["rT"], A["kT"], A["vT"], A["waT"], A["gdT"], A["poolT"]
    if layer1:
        vdT, vfT, vup_d = A["vdT"], A["vfT"], A["vup"]
    pvec_d, wup_d, aup_d, gup_d, poolw_d = A["pvec"], A["wup"], A["aup"], A["gup"], A["poolw"]
    maskA_d, maskC_d, identrep_d = A["maskA"], A["maskC"], A["identrep"]
    scanmask_d, bo_d, id_d, invdiv_d, psel_d = A["scanmask"], A["blockones"], A["ident"], A["invdiv"], A["psel"]
    yrw, ypl = A["yrw"], A["ypl"]
    if not layer1:
        vout = A["vout"]
    sb = c.sb
    pvec = sb("pvec_sb", [128, 32]); omk = sb("omk", [128, 2])
    wup = sb("wup_sb", [128, 256], BF16); aup = sb("aup_sb", [128, 256], BF16); gup = sb("gup_sb", [128, 256], BF16)
    vup = sb("vup_sb", [32, 256], BF16); poolw = sb("poolw_sb", [128, 2, 128], BF16)
    maskA = sb("maskA_sb", [64, 512]); maskC = sb("maskC_sb", [64, 256]); identrep = sb("identrep_sb", [64, 256])
    scanmask = sb("scanmask_sb", [128, TB]); bo = sb("bo_sb", [128, 128]); identf = sb("identf", [128, 128])
    identb = sb("identb", [128, 128], BF16); invdiv = sb("invdiv_sb", [128, 2, TB])
    epsk = sb("epsk", [128, 1]); gneps = sb("gneps", [128, 1]); psel = sb("psel_sb", [128, 2, 4])
    Xwa = sb("Xwa", [128, TB + 1]); Xg = sb("Xg", [128, TB + 1]); Xvd = sb("Xvd", [32, TB + 1])
    dwa = sb("dwa", [128, TB]); swa = sb("swa", [128, TB]); th = sb("th", [64, TB], BF16); adb = sb("adb", [128, TB], BF16)
    dg = sb("dg", [128, TB]); sgd = sb("sgd", [128, TB]); sg = sb("sg", [128, TB], BF16)
    dvd = sb("dvd", [32, TB]); vdb = sb("vdb", [32, TB], BF16)
    P2 = range(2)
    Xr = [sb("Xr_sh", [128, TB + 1])] * 2; Xk = [sb("Xk_sh", [128, TB + 1])] * 2
    Xv = [sb("Xv_sh", [128, TB + 1])] * 2; Xvf = [sb("Xvf_sh", [128, TB])] * 2
    tmpd = [sb("tmpd_sh", [128, TB])] * 2
    r_s = [sb("r_s%d" % i, [128, TB]) for i in P2]; k_s = [sb("k_s%d" % i, [128, TB]) for i in P2]
    v_s = [sb("v_s%d" % i, [128, TB]) for i in P2]
    sigw = [sb("sigw%d" % i, [128, TB]) for i in P2]; cum = [sb("cum%d" % i, [128, TB]) for i in P2]
    cumx = [sb("cumx_sh", [128, TB])] * 2
    G = [sb("G%d" % i, [128, TB]) for i in P2]; Ginv = [sb("Ginv%d" % i, [128, TB]) for i in P2]
    Gex = [sb("Gex%d" % i, [128, TB]) for i in P2]
    a_ = [sb("a_%d" % i, [128, TB]) for i in P2]; gg = [sb("gg%d" % i, [128, TB]) for i in P2]
    vsig = [sb("vsig_sh", [128, TB])] * 2
    kkraw = [sb("kkraw_sh", [128, TB])] * 2; sq = [sb("sq_sh", [128, TB])] * 2
    rn = [sb("rn_sh", [128, TB])] * 2; kk = [sb("kk%d" % i, [128, TB]) for i in P2]
    fac = [sb("fac_sh", [128, TB])] * 2; kmod = [sb("kmod%d" % i, [128, TB]) for i in P2]
    rk2 = [sb("rk2_sh", [128, TB])] * 2; bonus = [sb("bonus%d" % i, [128, TB]) for i in P2]
    t1 = [sb("t1_sh", [128, TB])] * 2
    ARt = [sb("ARt%d" % i, [128, CPB * 128], BF16) for i in P2]
    Bt = [sb("Bt%d" % i, [128, TB], BF16) for i in P2]; Kt = [sb("Kt%d" % i, [128, TB], BF16) for i in P2]
    Vb = [sb("Vb%d" % i, [128, TB], BF16) for i in P2]
    yraw = [sb("yraw%d" % i, [128, TB]) for i in P2]; yc = [sb("yc%d" % i, [128, TB]) for i in P2]
    ysq = [sb("ysq_sh", [128, TB])] * 2; yrs = [sb("yrs_sh", [128, TB])] * 2
    yo = [sb("yo%d" % i, [128, TB]) for i in P2]
    M0b = [[sb("M0b%d_%d" % (i, j), [64, 64], BF16) for j in range(2)] for i in range(4)]
    ARo = [sb("ARo%d" % i, [64, CPB * 128], BF16) for i in P2]
    Bo = [sb("Bo%d" % i, [64, TB], BF16) for i in P2]; Ko = [sb("Ko%d" % i, [64, TB], BF16) for i in P2]
    GCs = [sb("GCs%d" % i, [128, CPB]) for i in P2]; GCo = [sb("GCo%d" % i, [64, CPB]) for i in P2]
    tokmaj = [sb("tokmaj%d" % j, [64, 768], BF16) for j in range(2)]
    SA = [sb("SA%d" % j, [64, 512], BF16) for j in range(2)]; SB_ = [sb("SB%d" % j, [64, 512], BF16) for j in range(2)]
    SC = [sb("SC%d" % j, [64, 256], BF16) for j in range(2)]
    TT = [sb("TT%d" % j, [64, 256], BF16) for j in range(2)]; PP = [sb("PP%d" % j, [64, 512], BF16) for j in range(2)]
    W1 = sb("W1", [64, 256], BF16); U = [sb("U%d" % j, [64, 256], BF16) for j in range(2)]
    TTFs = [sb("TTF%d" % j, [64, 256], BF16) for j in range(2)]
    Xp = [sb("Xp%d" % i, [128, TB + 15]) for i in P2]
    plv = [[sb("plv%d_%d" % (i, k), [128, TB + 15]) for k in range(4)] for i in P2]
    pacc = [sb("pacc%d" % i, [128, TB]) for i in P2]
    pd = [sb("pd%d" % i, [128, TB], BF16) for i in P2]; pm = [sb("pm%d" % i, [128, TB]) for i in P2]
    po = [sb("po%d" % i, [128, TB]) for i in P2]
    bk = {n: c.ps("bk_" + n, [128, 512]) for n in ("A", "B", "C", "D", "E", "FG", "HI")}
    trp = c.ps("bk_trp", [128, 1024], BF16)

    ld = lambda dst, src, key: c.dma("sp", dst, src, w=[key])
    ld(pvec[:], pvec_d, "pvec"); ld(maskA[:], maskA_d, "maskA"); ld(maskC[:], maskC_d, "maskC")
    ld(identrep[:], identrep_d, "identrep"); ld(scanmask[:], scanmask_d, "scanmask"); ld(bo[:], bo_d, "bo")
    ld(identf[:], id_d, "identf"); ld(invdiv[:], invdiv_d, "invdiv"); ld(psel[:], psel_d, "psel")
    c.dma("pool", wup[0:64, :], wup_d, w=["wup"]); c.dma("pool", aup[64:128, :], aup_d, w=["aup"])
    c.dma("pool", gup[:], gup_d, w=["gup"]); c.dma("pool", poolw[:], poolw_d.rearrange("g c d -> c g d"), w=["poolw"])
    if layer1:
        c.dma("pool", vup[0:32, :], vup_d, w=["vup"])
    c.op("dve", lambda e: e.tensor_copy(identb[:], identf[:]), r=["identf"], w=["identb"])
    c.op("dve", lambda e: e.memset(epsk[:], 1e-24), w=["epsk"])
    c.op("dve", lambda e: e.memset(gneps[:], GN_EPS), w=["gneps"])
    c.op("dve", lambda e: e.tensor_scalar(out=omk[:], in0=pvec[:, 12:14], scalar1=-1.0, scalar2=1.0,
                                          op0=ALU.mult, op1=ALU.add), r=["pvec"], w=["omk"])
    for i in range(4):
        for j in range(2):
            c.op("dve", lambda e, i=i, j=j: e.memset(M0b[i][j][:], 0.0), w=["M0b%d_%d" % (i, j)])
    for i in P2:
        for k_ in range(4):
            c.op("pool", lambda e, i=i, k_=k_: e.memset(plv[i][k_][:], 0.0), w=["plv%d_%d" % (i, k_)])
    pv = lambda col, hp=0: pvec[:, col + hp: col + hp + 1]

    def tshift(eng, X, d, out, mu, n, kX, kd, kout):
        c.op(eng, lambda e: e.tensor_tensor(out=d, in0=X[0:n, 0:TB], in1=X[0:n, 1:TB + 1], op=ALU.subtract),
             r=[kX], w=[kd])
        if eng == "dve":
            c.op(eng, lambda e: e.scalar_tensor_tensor(out=out, in0=d, scalar=mu, in1=X[0:n, 1:TB + 1],
                                                       op0=ALU.mult, op1=ALU.add), r=[kX, kd, "pvec"], w=[kout])
        else:
            c.op(eng, lambda e: e.tensor_scalar(out=d, in0=d, scalar1=mu, scalar2=None, op0=ALU.mult),
                 r=[kd, "pvec"], w=[kd])
            c.op(eng, lambda e: e.tensor_tensor(out=out, in0=d, in1=X[0:n, 1:TB + 1], op=ALU.add),
                 r=[kX, kd], w=[kout])

    def load_halo(X, src, rows, b, n, key, halo=1):
        t0 = b * TB
        if b == 0:
            c.op("pool", lambda e: e.memset(X[0:n, 0:halo], 0.0), w=[key])
            c.dma("sp", X[0:n, halo:halo + TB], src[rows, 0:TB], w=[key])
        else:
            c.dma("sp", X[0:n, :], src[rows, t0 - halo:t0 + TB], w=[key])

    chunk_counter = [0]

    def body():
        for b in range(NBLK):
            t0 = b * TB
            load_halo(Xwa, waT, slice(0, 128), b, 128, "Xwa")
            load_halo(Xg, gdT, slice(0, 128), b, 128, "Xg")
            tshift("pool", Xwa, dwa[:], swa[:], pv(25), 128, "Xwa", "dwa", "swa")
            c.op("act", lambda e: e.activation(out=th[:], in_=swa[0:64, :], func=AF.Tanh), r=["swa"], w=["th"])
            c.op("pool", lambda e: e.tensor_copy(adb[64:128, :], swa[64:128, :]), r=["swa"], w=["adb"])
            tshift("pool", Xg, dg[:], sgd[:], pv(24), 128, "Xg", "dg", "sgd")
            c.op("act", lambda e: e.activation(out=sg[:], in_=sgd[:], func=AF.Sigmoid), r=["sgd"], w=["sg"])
            if layer1:
                load_halo(Xvd, vdT, slice(0, 32), b, 32, "Xvd")
                tshift("pool", Xvd, dvd[:], vdb[:], pvec[0:32, 26:27], 32, "Xvd", "dvd", "vdb")
            dbg(1)
            for hp in P2:
                rows = slice(hp * 128, (hp + 1) * 128)
                H = str(hp)
                load_halo(Xr[hp], rT, rows, b, 128, "Xr")
                load_halo(Xk[hp], kT, rows, b, 128, "Xk")
                load_halo(Xv[hp], vT, rows, b, 128, "Xv")
                tshift("dve", Xr[hp], tmpd[hp][:], r_s[hp][:], pv(0, hp), 128, "Xr", "tmpd", "r_s" + H)
                tshift("dve", Xk[hp], tmpd[hp][:], k_s[hp][:], pv(2, hp), 128, "Xk", "tmpd", "k_s" + H)
                tshift("dve", Xv[hp], tmpd[hp][:], v_s[hp][:], pv(4, hp), 128, "Xv", "tmpd", "v_s" + H)
                cols = slice(hp * 128, (hp + 1) * 128)
                c.op("pe", lambda e: e.matmul(bk["A"][:], wup[0:64, cols], th[:], start=True, stop=True),
                     r=["wup", "th"], w=["bkA"])
                c.op("act", lambda e: e.activation(out=sigw[hp][:], in_=bk["A"][:], func=AF.Sigmoid, bias=pv(6, hp)),
                     r=["bkA", "pvec"], w=["sigw" + H])
                c.op("pe", lambda e: e.matmul(bk["B"][:], aup[64:128, cols], adb[64:128, :], start=True, stop=True),
                     r=["aup", "adb"], w=["bkB"])
                c.op("act", lambda e: e.activation(out=a_[hp][:], in_=bk["B"][:], func=AF.Sigmoid, bias=pv(8, hp)),
                     r=["bkB", "pvec"], w=["a_" + H])
                c.op("pe", lambda e: e.matmul(bk["C"][:], gup[:, cols], sg[:], start=True, stop=True),
                     r=["gup", "sg"], w=["bkC"])
                c.op("act", lambda e: e.copy(out=gg[hp][:], in_=bk["C"][:]), r=["bkC"], w=["gg" + H])
                if layer1:
                    c.dma("sp", Xvf[hp][:], vfT[rows, t0:t0 + TB], w=["Xvf"])
                    c.op("pe", lambda e: e.matmul(bk["D"][:], vup[0:32, cols], vdb[0:32, :], start=True, stop=True),
                         r=["vup", "vdb"], w=["bkD"])
                    c.op("act", lambda e: e.activation(out=vsig[hp][:], in_=bk["D"][:], func=AF.Sigmoid, bias=pv(27, hp)),
                         r=["bkD", "pvec"], w=["vsig"])
                    c.op("dve", lambda e: e.tensor_tensor(out=tmpd[hp][:], in0=Xvf[hp][:], in1=v_s[hp][:], op=ALU.subtract),
                         r=["Xvf", "v_s" + H], w=["tmpd"])
                    c.op("dve", lambda e: e.tensor_tensor(out=tmpd[hp][:], in0=tmpd[hp][:], in1=vsig[hp][:], op=ALU.mult),
                         r=["vsig", "tmpd"], w=["tmpd"])
                    c.op("dve", lambda e: e.tensor_tensor(out=v_s[hp][:], in0=v_s[hp][:], in1=tmpd[hp][:], op=ALU.add),
                         r=["v_s" + H, "tmpd"], w=["v_s" + H])
                else:
                    c.dma("sp", vout[rows, t0:t0 + TB], v_s[hp][:], r=["v_s" + H])
                c.op("dve", lambda e: e.tensor_scalar(out=kkraw[hp][:], in0=k_s[hp][:], scalar1=pv(10, hp), scalar2=None,
                                                      op0=ALU.mult), r=["k_s" + H, "pvec"], w=["kkraw"])
                c.op("pool", lambda e: e.tensor_tensor(out=sq[hp][:], in0=kkraw[hp][:], in1=kkraw[hp][:], op=ALU.mult),
                     r=["kkraw"], w=["sq"])
                c.op("pe", lambda e: e.matmul(bk["E"][:], bo[:], sq[hp][:], start=True, stop=True),
                     r=["bo", "sq"], w=["bkE"])
                c.op("act", lambda e: e.activation(out=rn[hp][:], in_=bk["E"][:], func=AF.Sqrt, bias=epsk[:, 0:1]),
                     r=["bkE", "epsk"], w=["rn"])
                c.op("dve", lambda e: e.reciprocal(out=rn[hp][:], in_=rn[hp][:]), r=["rn"], w=["rn"])
                c.op("dve", lambda e: e.tensor_tensor(out=kk[hp][:], in0=kkraw[hp][:], in1=rn[hp][:], op=ALU.mult),
                     r=["kkraw", "rn"], w=["kk" + H])
                c.op("pool", lambda e: e.tensor_scalar(out=fac[hp][:], in0=a_[hp][:], scalar1=pv(12, hp),
                                                       scalar2=omk[:, hp:hp + 1], op0=ALU.mult, op1=ALU.add),
                     r=["a_" + H, "pvec", "omk"], w=["fac"])
                c.op("pool", lambda e: e.tensor_tensor(out=kmod[hp][:], in0=k_s[hp][:], in1=fac[hp][:], op=ALU.mult),
                     r=["k_s" + H, "fac"], w=["kmod" + H])
                c.op("pool", lambda e: e.tensor_scalar(out=rk2[hp][:], in0=r_s[hp][:], scalar1=pv(16, hp), scalar2=None,
                                                       op0=ALU.mult), r=["r_s" + H, "pvec"], w=["rk2"])
                c.op("pool", lambda e: e.tensor_tensor(out=rk2[hp][:], in0=rk2[hp][:], in1=kmod[hp][:], op=ALU.mult),
                     r=["rk2", "kmod" + H], w=["rk2"])
                c.op("pe", lambda e: e.matmul(bk["FG"][:], bo[:], rk2[hp][:], start=True, stop=True),
                     r=["bo", "rk2"], w=["bkFG"])
                c.op("dve", lambda e: e.tensor_tensor(out=bonus[hp][:], in0=bk["FG"][:], in1=v_s[hp][:], op=ALU.mult),
                     r=["bkFG", "v_s" + H], w=["bonus" + H])
                c.op("dve", lambda e: e.tensor_tensor_scan(out=cum[hp][:], data0=scanmask[:], data1=sigw[hp][:],
                                                           initial=0.0, op0=ALU.mult, op1=ALU.add),
                     r=["scanmask", "sigw" + H], w=["cum" + H])
                c.op("pool", lambda e: e.tensor_tensor(out=cumx[hp][:], in0=cum[hp][:], in1=sigw[hp][:], op=ALU.subtract),
                     r=["cum" + H, "sigw" + H], w=["cumx"])
                c.op("act", lambda e: e.activation(out=G[hp][:], in_=cum[hp][:], func=AF.Exp, scale=-C0),
                     r=["cum" + H], w=["G" + H])
                c.op("act", lambda e: e.activation(out=Ginv[hp][:], in_=cum[hp][:], func=AF.Exp, scale=C0),
                     r=["cum" + H], w=["Ginv" + H])
                c.op("act", lambda e: e.activation(out=Gex[hp][:], in_=cumx[hp][:], func=AF.Exp, scale=-C0),
                     r=["cumx"], w=["Gex" + H])
                AR3 = ARt[hp][:].rearrange("p (c two t) -> p c two t", two=2, t=CH)
                v3 = lambda ap: ap.rearrange("p (c t) -> p c t", t=CH)
                c.op("dve", lambda e: e.tensor_tensor(out=AR3[:, :, 1, :], in0=v3(r_s[hp][:]), in1=v3(G[hp][:]), op=ALU.mult),
                     r=["r_s" + H, "G" + H], w=["ARt" + H])
                c.op("dve", lambda e: e.scalar_tensor_tensor(out=AR3[:, :, 0, :], in0=v3(kk[hp][:]), scalar=-1.0,
                                                             in1=v3(Gex[hp][:]), op0=ALU.mult, op1=ALU.mult),
                     r=["kk" + H, "Gex" + H], w=["ARt" + H])
                c.op("pool", lambda e: e.tensor_tensor(out=t1[hp][:], in0=kk[hp][:], in1=a_[hp][:], op=ALU.mult),
                     r=["kk" + H, "a_" + H], w=["t1"])
                c.op("dve", lambda e: e.tensor_tensor(out=Bt[hp][:], in0=t1[hp][:], in1=Ginv[hp][:], op=ALU.mult),
                     r=["t1", "Ginv" + H], w=["Bt" + H])
                c.op("pool", lambda e: e.tensor_tensor(out=Kt[hp][:], in0=kmod[hp][:], in1=Ginv[hp][:], op=ALU.mult),
                     r=["kmod" + H, "Ginv" + H], w=["Kt" + H])
                c.op("pool", lambda e: e.tensor_copy(Vb[hp][:], v_s[hp][:]), r=["v_s" + H], w=["Vb" + H])
                c.op("pool", lambda e: e.tensor_copy(GCs[hp][:], G[hp][:].rearrange("p (c t) -> p c t", t=CH)[:, :, CH - 1]),
                     r=["G" + H], w=["GCs" + H])
                c.dma("sp", ARo[hp][:], ARt[hp][64:128, :], r=["ARt" + H], w=["ARo" + H])
                c.dma("sp", Bo[hp][:], Bt[hp][64:128, :], r=["Bt" + H], w=["Bo" + H])
                c.dma("sp", Ko[hp][:], Kt[hp][64:128, :], r=["Kt" + H], w=["Ko" + H])
                c.dma("sp", GCo[hp][:], GCs[hp][64:128, :], r=["GCs" + H], w=["GCo" + H])

            dbg(2)
            for gi in P2:
                Gk = str(gi)
                load_halo(Xp[gi], poolT, slice(gi * 128, (gi + 1) * 128), b, 128, "Xp" + Gk, halo=15)
                cur = Xp[gi]; curk = "Xp" + Gk
                for lv in range(4):
                    sh = 1 << lv
                    dst = plv[gi][lv]; dk = "plv%d_%d" % (gi, lv)
                    c.op("pool", lambda e: e.tensor_tensor(out=dst[:, sh:15 + TB], in0=cur[:, sh:15 + TB],
                                                           in1=cur[:, 0:15 + TB - sh], op=ALU.add), r=[curk], w=[dk])
                    cur, curk = dst, dk
                c.op("dve", lambda e: e.tensor_scalar(out=pacc[gi][:], in0=plv[gi][0][:, 15:15 + TB],
                                                      scalar1=psel[:, gi, 0:1], scalar2=None, op0=ALU.mult),
                     r=["plv%d_0" % gi, "psel"], w=["pacc" + Gk])
                for lv in range(1, 4):
                    c.op("dve", lambda e: e.scalar_tensor_tensor(out=pacc[gi][:], in0=plv[gi][lv][:, 15:15 + TB],
                                                                 scalar=psel[:, gi, lv:lv + 1], in1=pacc[gi][:],
                                                                 op0=ALU.mult, op1=ALU.add),
                         r=["plv%d_%d" % (gi, lv), "psel", "pacc" + Gk], w=["pacc" + Gk])
                if b == 0:
                    c.op("dve", lambda e: e.tensor_tensor(out=pm[gi][:], in0=pacc[gi][:], in1=invdiv[:, gi, :],
                                                          op=ALU.mult), r=["pacc" + Gk, "invdiv"], w=["pm" + Gk])
                    c.op("dve", lambda e: e.tensor_tensor(out=pd[gi][:], in0=pm[gi][:], in1=Xp[gi][:, 15:15 + TB],
                                                          op=ALU.subtract), r=["pm" + Gk, "Xp" + Gk], w=["pd" + Gk])
                else:
                    c.op("dve", lambda e: e.scalar_tensor_tensor(out=pd[gi][:], in0=pacc[gi][:],
                                                                 scalar=pv(29, gi), in1=Xp[gi][:, 15:15 + TB],
                                                                 op0=ALU.mult, op1=ALU.subtract),
                         r=["pacc" + Gk, "Xp" + Gk, "pvec"], w=["pd" + Gk])
                c.op("pe", lambda e: e.matmul(bk["D"][:], poolw[:, gi, :], pd[gi][:], start=True, stop=True),
                     r=["poolw", "pd" + Gk], w=["bkD"])
                c.op("act", lambda e: e.activation(out=po[gi][:], in_=bk["D"][:], func=AF.Identity, scale=pv(22, gi)),
                     r=["bkD", "pvec"], w=["po" + Gk])
                c.dma("sp", ypl[gi * 128:(gi + 1) * 128, t0:t0 + TB], po[gi][:], r=["po" + Gk])

            dbg(3)
            def emit_chunk(ci, cc, part):
                pp = cc % 2
                cs = slice(ci * CH, (ci + 1) * CH)
                tm = tokmaj[pp]; sa = SA[pp]; sbb = SB_[pp]; sc = SC[pp]
                tmk, sak, sbk, sck = "tokmaj%d" % pp, "SA%d" % pp, "SB%d" % pp, "SC%d" % pp
                TTF = TTFs[pp]; TTFk = "TTF%d" % pp
                def ARv(h, lo, hi):
                    hp_ = h // 2
                    t_ = ARt[hp_] if h % 2 == 0 else ARo[hp_]
                    return t_[0:64, ci * 128 + lo: ci * 128 + hi]
                def Bv(h):
                    hp_ = h // 2
                    return (Bt[hp_] if h % 2 == 0 else Bo[hp_])[0:64, cs]
                def Kv(h):
                    hp_ = h // 2
                    return (Kt[hp_] if h % 2 == 0 else Ko[hp_])[0:64, cs]
                ARk = lambda h: ("ARt%d" if h % 2 == 0 else "ARo%d") % (h // 2)
                Bk = lambda h: ("Bt%d" if h % 2 == 0 else "Bo%d") % (h // 2)
                Kk = lambda h: ("Kt%d" if h % 2 == 0 else "Ko%d") % (h // 2)
                if part == "I":
                    for hp in P2:
                        H = str(hp)
                        for j, (src, sk) in enumerate(((Bt[hp], "Bt" + H), (Kt[hp], "Kt" + H), (Vb[hp], "Vb" + H))):
                            c.op("pe", lambda e, src=src, j=j, hp=hp: e.transpose(
                                trp[0:64, j * 256 + hp * 128: j * 256 + (hp + 1) * 128], src[:, cs], identb[:]),
                                r=[sk, "identb"], w=["bktrp"])
                    c.op("act", lambda e: e.copy(out=tm[:], in_=trp[0:64, 0:768]), r=["bktrp"], w=[tmk])
                    yield
                    dbg(4)
                    for h in range(4):
                        c.op("pe", lambda e: e.matmul(bk["A"][0:64, h * 128:(h + 1) * 128], Bv(h), ARv(h, 0, 128),
                                                      start=True, stop=True), r=[Bk(h), ARk(h)], w=["bkA"])
                        c.op("pe", lambda e: e.matmul(bk["B"][0:64, h * 128:(h + 1) * 128], Kv(h), ARv(h, 0, 128),
                                                      start=True, stop=True), r=[Kk(h), ARk(h)], w=["bkB"])
                        c.op("pe", lambda e: e.matmul(bk["C"][0:64, h * 64:(h + 1) * 64], ARv(h, 0, 64), Bv(h),
                                                      start=True, stop=True), r=[Bk(h), ARk(h)], w=["bkC"])
                    c.op("dve", lambda e: e.tensor_tensor(out=sa[:], in0=bk["A"][0:64, :], in1=maskA[:], op=ALU.mult),
                         r=["bkA", "maskA"], w=[sak])
                    c.op("dve", lambda e: e.tensor_tensor(out=sbb[:], in0=bk["B"][0:64, :], in1=maskA[:], op=ALU.mult),
                         r=["bkB", "maskA"], w=[sbk])
                    c.op("dve", lambda e: e.tensor_tensor(out=sc[:], in0=bk["C"][0:64, 0:256], in1=maskC[:], op=ALU.mult),
                         r=["bkC", "maskC"], w=[sck])
                    yield
                    Nv = lambda h: sa[:, h * 128: h * 128 + 64]
                    NTv = lambda h: sc[:, h * 64:(h + 1) * 64]
                    dbg(5)
                    c.op("pool", lambda e: e.tensor_tensor(
                        out=TT[0][:].rearrange("p (h t) -> p h t", t=64),
                        in0=sa[:].rearrange("p (h x) -> p h x", x=128)[:, :, 0:64],
                        in1=identrep[:].rearrange("p (h t) -> p h t", t=64), op=ALU.add),
                        r=[sak, "identrep"], w=["TT0"])
                    for h in range(4):
                        c.op("pe", lambda e: e.matmul(bk["D"][0:64, h * 64:(h + 1) * 64], NTv(h), Nv(h), start=True, stop=True),
                             r=[sak, sck], w=["bkD"])
                        c.op("pe", lambda e: e.matmul(bk["D"][0:64, 256 + h * 64: 256 + (h + 1) * 64], Nv(h), NTv(h),
                                                      start=True, stop=True), r=[sak, sck], w=["bkD"])
                    c.op("act", lambda e: e.copy(out=PP[0][:], in_=bk["D"][0:64, :]), r=["bkD"], w=["PP0"])
                    yield
                    for lv in range(1, 6):
                        pi = (lv - 1) % 2; po_ = lv % 2
                        Pv = lambda h: PP[pi][:, h * 64:(h + 1) * 64]
                        PTv = lambda h: PP[pi][:, 256 + h * 64: 256 + (h + 1) * 64]
                        TTv = lambda h: TT[pi][:, h * 64:(h + 1) * 64]
                        for h in range(4):
                            c.op("pe", lambda e: e.matmul(bk["E"][0:64, h * 64:(h + 1) * 64], identb[0:64, 0:64], TTv(h),
                                                          start=True, stop=False), r=["identb", "TT%d" % pi], w=["bkE"])
                            c.op("pe", lambda e: e.matmul(bk["E"][0:64, h * 64:(h + 1) * 64], PTv(h), TTv(h),
                                                          start=False, stop=True), r=["PP%d" % pi, "TT%d" % pi], w=["bkE"])
                        if lv < 5:
                            for h in range(4):
                                c.op("pe", lambda e: e.matmul(bk["D"][0:64, h * 64:(h + 1) * 64], PTv(h), Pv(h),
                                                              start=True, stop=True), r=["PP%d" % pi], w=["bkD"])
                                c.op("pe", lambda e: e.matmul(bk["D"][0:64, 256 + h * 64: 256 + (h + 1) * 64], Pv(h), PTv(h),
                                                              start=True, stop=True), r=["PP%d" % pi], w=["bkD"])
                        if lv == 5:
                            c.op("dve", lambda e: e.tensor_copy(TTF[:], bk["E"][0:64, 0:256]), r=["bkE"], w=[TTFk])
                        else:
                            c.op("dve", lambda e: e.tensor_copy(TT[po_][:], bk["E"][0:64, 0:256]), r=["bkE"], w=["TT%d" % po_])
                        if lv < 5:
                            c.op("act", lambda e: e.copy(out=PP[po_][:], in_=bk["D"][0:64, :]), r=["bkD"], w=["PP%d" % po_])
                        yield
                if part == "D":
                    TTf = TTF; TTk = TTFk
                    Mold = [M0b[h][pp] for h in range(4)]; Mnew = [M0b[h][1 - pp] for h in range(4)]
                    Mok = ["M0b%d_%d" % (h, pp) for h in range(4)]; Mnk = ["M0b%d_%d" % (h, 1 - pp) for h in range(4)]
                    Uc = U[pp]; Uk = "U%d" % pp
                    for h in range(4):
                        c.op("pe", lambda e: e.matmul(bk["FG"][0:64, h * 64:(h + 1) * 64], ARv(h, 0, 64), Mold[h][:],
                                                      start=True, stop=False), r=[ARk(h), Mok[h]], w=["bkFG"])
                        c.op("pe", lambda e: e.matmul(bk["FG"][0:64, h * 64:(h + 1) * 64], sbb[:, h * 128: h * 128 + 64],
                                                      tm[:, 512 + h * 64: 512 + (h + 1) * 64], start=False, stop=True),
                             r=[sbk, tmk], w=["bkFG"])
                    c.op("act", lambda e: e.copy(out=W1[:], in_=bk["FG"][0:64, 0:256]), r=["bkFG"], w=["W1"])
                    yield
                    for h in range(4):
                        c.op("pe", lambda e: e.matmul(bk["FG"][0:64, 256 + h * 64: 256 + (h + 1) * 64],
                                                      TTf[:, h * 64:(h + 1) * 64], W1[:, h * 64:(h + 1) * 64],
                                                      start=True, stop=True), r=[TTk, "W1"], w=["bkFG"])
                    c.op("act", lambda e: e.copy(out=Uc[:], in_=bk["FG"][0:64, 256:512]), r=["bkFG"], w=[Uk])
                    yield
                    dbg(7)
                    for h in range(4):
                        hp, base = h // 2, (h % 2) * 64
                        o = bk["HI"][base:base + 64, hp * 64:(hp + 1) * 64]
                        c.op("pe", lambda e: e.matmul(o, Mold[h][:], ARv(h, 64, 128), start=True, stop=False),
                             r=[ARk(h), Mok[h]], w=["bkHI"])
                        c.op("pe", lambda e: e.matmul(o, Uc[:, h * 64:(h + 1) * 64], sa[:, h * 128 + 64:(h + 1) * 128],
                                                      start=False, stop=False), r=[Uk, sak], w=["bkHI"])
                        c.op("pe", lambda e: e.matmul(o, tm[:, 512 + h * 64: 512 + (h + 1) * 64],
                                                      sbb[:, h * 128 + 64:(h + 1) * 128], start=False, stop=True),
                             r=[tmk, sbk], w=["bkHI"])
                    for h in range(4):
                        o = bk["HI"][0:64, 256 + h * 64: 256 + (h + 1) * 64]
                        c.op("pe", lambda e: e.matmul(o, tm[:, 256 + h * 64: 256 + (h + 1) * 64],
                                                      tm[:, 512 + h * 64: 512 + (h + 1) * 64], start=True, stop=False),
                             r=[tmk], w=["bkHI"])
                        c.op("pe", lambda e: e.matmul(o, tm[:, h * 64:(h + 1) * 64], Uc[:, h * 64:(h + 1) * 64],
                                                      start=False, stop=False), r=[tmk, Uk], w=["bkHI"])
                        c.op("pe", lambda e: e.matmul(o, identb[0:64, 0:64], Mold[h][:],
                                                      start=False, stop=True), r=["identb", Mok[h]], w=["bkHI"])
                    yield
                    for hp in P2:
                        H = str(hp)
                        c.op("act", lambda e: e.copy(out=yraw[hp][:, cs], in_=bk["HI"][:, hp * 64:(hp + 1) * 64]),
                             r=["bkHI"], w=["yraw" + H])
                    for h in range(4):
                        hp = h // 2
                        gsrc, gk = (GCs[hp], "GCs%d" % hp) if h % 2 == 0 else (GCo[hp], "GCo%d" % hp)
                        c.op("act", lambda e: e.activation(out=Mnew[h][:], in_=bk["HI"][0:64, 256 + h * 64: 256 + (h + 1) * 64],
                                                           func=AF.Identity, scale=gsrc[0:64, ci:ci + 1]),
                             r=["bkHI", gk], w=[Mnk[h]])

            cc0 = chunk_counter[0]; chunk_counter[0] += CPB

            def drive(gens):
                gens = list(gens)
                while gens:
                    for g_ in list(gens):
                        try:
                            next(g_)
                        except StopIteration:
                            gens.remove(g_)

            drive([emit_chunk(0, cc0, "I")])
            for ci in range(CPB):
                gens = [emit_chunk(ci, cc0 + ci, "D")]
                if ci + 1 < CPB:
                    gens.insert(0, emit_chunk(ci + 1, cc0 + ci + 1, "I"))
                drive(gens)
            dbg(8)
            for hp in P2:
                H = str(hp)
                rows = slice(hp * 128, (hp + 1) * 128)
                c.op("pe", lambda e: e.matmul(bk["A"][:], bo[:], yraw[hp][:], start=True, stop=True),
                     r=["bo", "yraw" + H], w=["bkA"])
                c.op("dve", lambda e: e.scalar_tensor_tensor(out=yc[hp][:], in0=bk["A"][:], scalar=-1.0 / 64,
                                                             in1=yraw[hp][:], op0=ALU.mult, op1=ALU.add),
                     r=["bkA", "yraw" + H], w=["yc" + H])
                c.op("pool", lambda e: e.tensor_tensor(out=ysq[hp][:], in0=yc[hp][:], in1=yc[hp][:], op=ALU.mult),
                     r=["yc" + H], w=["ysq"])
                c.op("pe", lambda e: e.matmul(bk["B"][:], bo[:], ysq[hp][:], start=True, stop=True),
                     r=["bo", "ysq"], w=["bkB"])
                c.op("act", lambda e: e.activation(out=yrs[hp][:], in_=bk["B"][:], func=AF.Sqrt, bias=gneps[:, 0:1],
                                                   scale=1.0 / 64), r=["bkB", "gneps"], w=["yrs"])
                c.op("dve", lambda e: e.reciprocal(out=yrs[hp][:], in_=yrs[hp][:]), r=["yrs"], w=["yrs"])
                c.op("dve", lambda e: e.tensor_tensor(out=yc[hp][:], in0=yc[hp][:], in1=yrs[hp][:], op=ALU.mult),
                     r=["yc" + H, "yrs"], w=["yc" + H])
                c.op("dve", lambda e: e.tensor_scalar(out=yc[hp][:], in0=yc[hp][:], scalar1=pv(18, hp), scalar2=pv(20, hp),
                                                      op0=ALU.mult, op1=ALU.add), r=["yc" + H, "pvec"], w=["yc" + H])
                c.op("pool", lambda e: e.tensor_tensor(out=yc[hp][:], in0=yc[hp][:], in1=bonus[hp][:], op=ALU.add),
                     r=["yc" + H, "bonus" + H], w=["yc" + H])
                c.op("pool", lambda e: e.tensor_tensor(out=yo[hp][:], in0=yc[hp][:], in1=gg[hp][:], op=ALU.mult),
                     r=["yc" + H, "gg" + H], w=["yo" + H])
                c.dma("sp", yrw[rows, t0:t0 + TB], yo[hp][:], r=["yo" + H])
    try:
        body()
    except _Stop:
        pass


def build_L2(layer1):
    nc = bass.Bass("TRN2", target_bir_lowering=False)
    dt = lambda name, shape, kind="ExternalInput": nc.dram_tensor(name, list(shape), F32, kind=kind).ap()
    A = dict(rT=dt("rT", [256, SEQ]), kT=dt("kT", [256, SEQ]), vT=dt("vT", [256, SEQ]),
             waT=dt("waT", [128, SEQ]), gdT=dt("gdT", [128, SEQ]), poolT=dt("poolT", [256, SEQ]))
    if layer1:
        A.update(vdT=dt("vdT", [32, SEQ]), vfT=dt("vfT", [256, SEQ]), vup=dt("vup", [32, 256]))
    A.update(pvec=dt("pvec", [128, 32]), wup=dt("wup", [64, 256]), aup=dt("aup", [64, 256]),
             gup=dt("gup", [128, 256]), poolw=dt("poolw", [2, 128, 128]),
             maskA=dt("maskA", [64, 512]), maskC=dt("maskC", [64, 256]), identrep=dt("identrep", [64, 256]),
             scanmask=dt("scanmask", [128, TB]), blockones=dt("blockones", [128, 128]), ident=dt("ident", [128, 128]),
             invdiv=dt("invdiv", [128, 2, TB]), psel=dt("psel", [128, 2, 4]))
    yT = dt("yT", [512, SEQ], "ExternalOutput")
    A["yrw"] = yT[0:256]; A["ypl"] = yT[256:512]
    if not layer1:
        A["vout"] = dt("vout", [256, SEQ], "ExternalOutput")
    c = Ctx(nc)
    c.begin_phase("")
    emit_L2(c, A, layer1)
    c.end_phase()
    c.finish()
    return nc


def l2_inputs(inp, l, g, projT, vfT):
    f = lambda a: np.ascontiguousarray(a, dtype=np.float32)
    mu = inp["mu_shift"][l]
    cs = slice(g * 256, (g + 1) * 256)
    pvec = np.zeros((128, 32), np.float32)
    def put(col, vec512):
        v = np.asarray(vec512)[cs].reshape(2, 128)
        pvec[:, col] = v[0]; pvec[:, col + 1] = v[1]
    put(0, mu[0:512]); put(2, mu[512:1024]); put(4, mu[1024:1536])
    put(6, inp["w0"][l]); put(8, inp["a0"][l]); put(10, inp["k_k"][l]); put(12, inp["k_a"][l])
    put(16, inp["r_k"][l].reshape(512)); put(18, inp["lnx_g"][l]); put(20, inp["lnx_b"][l])
    put(22, inp["pool_scale"][l])
    pvec[:, 24] = mu[1664:1792]; pvec[0:64, 25] = mu[1536:1600]; pvec[64:128, 25] = mu[1600:1664]
    d = dict(rT=f(projT[0:512][cs]), kT=f(projT[512:1024][cs]), vT=f(projT[1024:1536][cs]),
             waT=f(projT[1536:1664]), gdT=f(projT[1664:1792]), poolT=f(projT[1792:2304][cs]),
             wup=f(inp["w_up"][l][:, cs]), aup=f(inp["a_up"][l][:, cs]), gup=f(inp["g_up"][l][:, cs]),
             poolw=f(inp["pool_w"][l][2 * g:2 * g + 2]))
    if l > 0:
        pvec[0:32, 26] = inp["vres_mu"][l - 1]
        put(27, inp["vres_v0"][l - 1])
        d.update(vdT=f(projT[2304:2336]), vfT=f(vfT), vup=f(inp["vres_up"][l - 1][:, cs]))
    d["pvec"] = pvec
    pos = np.arange(1, TB + 1, dtype=np.float32)
    invdiv = np.stack([np.broadcast_to(1.0 / np.minimum(pos, float(POOL_WINDOWS[2 * g + gi])), (128, TB))
                       for gi in range(2)], axis=1)
    d["invdiv"] = f(invdiv)
    psel = np.zeros((128, 2, 4), np.float32)
    for gi in range(2):
        psel[:, gi, 2 * g + gi] = 1.0
        pvec[:, 29 + gi] = 1.0 / POOL_WINDOWS[2 * g + gi]
    d["psel"] = psel
    d.update(l2_consts())
    return d


EPC = 2048
def emit_L0(c, A, layers, nch):
    u_d, v_d, wo_d, pq_d, id_d = A["u"], A["v"], A["w_out"], A["peer_q"], A["ident"]
    ut_o, vb_o, wo_o, pq_o = A["UT"], A["Vb"], A["woutb"], A["pqb"]
    identf = c.sb("identf", [128, 128]); identb = c.sb("identb", [128, 128], BF16)
    NBUF0 = 4
    uf = [c.sb("uf%d" % i, [128, D]) for i in range(NBUF0)]
    ub = [c.sb("ub%d" % i, [128, D], BF16) for i in range(NBUF0)]
    uT = [c.sb("uT%d" % i, [128, D], BF16) for i in range(NBUF0)]
    vb = [c.sb("vb%d" % i, [128, D], BF16) for i in range(NBUF0)]
    wb = [c.sb("wb%d" % i, [128, 8, 128], BF16) for i in range(NBUF0)]
    tps = [c.ps("tps%d" % i, [128, 1024], BF16) for i in range(NBUF0)]
    c.dma("sp", identf[:], id_d, w=["identf"])
    c.op("dve", lambda e: e.tensor_copy(identb[:], identf[:]), r=["identf"], w=["identb"])
    k = 0
    for l in layers:
        for ch in range(nch):
            s_ = k % NBUF0; k += 1
            S = str(s_)
            rows = slice(ch * 128, (ch + 1) * 128)
            c.dma("sp", uf[s_][:], u_d[l, rows, :], w=["uf" + S])
            c.dma("pool", vb[s_][:], v_d[l, rows, :], w=["vb" + S])
            c.dma("sp", vb_o[l, rows, :], vb[s_][:], r=["vb" + S])
            eng = "act" if s_ % 2 == 0 else "dve"
            if eng == "act":
                c.op("act", lambda e: e.copy(out=ub[s_][:], in_=uf[s_][:]), r=["uf" + S], w=["ub" + S])
            else:
                c.op("dve", lambda e: e.tensor_copy(ub[s_][:], uf[s_][:]), r=["uf" + S], w=["ub" + S])
            for dc in range(8):
                c.op("pe", lambda e: e.transpose(tps[s_][:, dc * 128:(dc + 1) * 128], ub[s_][:, dc * 128:(dc + 1) * 128],
                                                 identb[:]), r=["ub" + S, "identb"], w=["tps" + S])
            if eng == "act":
                c.op("act", lambda e: e.copy(out=uT[s_][:], in_=tps[s_][:]), r=["tps" + S], w=["uT" + S])
            else:
                c.op("dve", lambda e: e.tensor_copy(uT[s_][:], tps[s_][:]), r=["tps" + S], w=["uT" + S])
            c.dma("sp", ut_o[l, ch], uT[s_][:].rearrange("p (dc e) -> p dc e", e=128), r=["uT" + S])
        for cc in range(8):
            s_ = k % NBUF0; k += 1
            S = str(s_)
            c.dma("pool", vb[s_][:], wo_d[l, cc * 128:(cc + 1) * 128, :], w=["vb" + S])
            c.dma("sp", wo_o[l, cc], vb[s_][:], r=["vb" + S])
        pqv = pq_d[l].rearrange("(dc p) j -> p dc j", p=128)
        for jc in range(16):
            s_ = k % NBUF0; k += 1
            S = str(s_)
            c.dma("pool", wb[s_][:], pqv[:, :, jc * 128:(jc + 1) * 128], w=["wb" + S])
            c.dma("sp", pq_o[l, jc], wb[s_][:], r=["wb" + S])


def build_L0():
    nc = bass.Bass("TRN2", target_bir_lowering=False)
    di = lambda name, shape: nc.dram_tensor(name, list(shape), F32, kind="ExternalInput").ap()
    do = lambda name, shape: nc.dram_tensor(name, list(shape), BF16, kind="ExternalOutput").ap()
    A = dict(u=di("u", [2, EPC, D]), v=di("v", [2, EPC, D]), w_out=di("w_out", [2, D, D]),
             peer_q=di("peer_q", [2, D, 2048]), ident=di("ident", [128, 128]),
             UT=do("UT", [2, EPC // 128, 128, 8, 128]), Vb=do("Vb", [2, EPC, D]),
             woutb=do("woutb", [2, 8, 128, D]), pqb=do("pqb", [2, 16, 128, 8, 128]))
    c = Ctx(nc)
    c.begin_phase("")
    emit_L0(c, A, range(2), EPC // 128)
    c.end_phase()
    c.finish()
    return nc


TS = 256
NST = TOK // TS
NEG = -1.0e30


def emit_L3(c, A, final):
    nc = c.nc
    x_d, c_d, adaw_d, adab_d, g_d, lnf_d = A["x"], A["c_fm"], A["ada_w"], A["ada_b_fm"], A["ln_g_fm"], A["lnf_fm"]
    wo_d, pq_d, keys_d, ut_d, vb_d, id_d, ones_d, xo_d = (A["woutb"], A["pqb"], A["keys"], A["UT"], A["Vb"],
                                                          A["ident"], A["ones"], A["xo"])
    ysel = "hsel" in A
    sb = c.sb
    c.adaw_sb = sb("adaw", [128, 8, 256])
    c_sb = sb("c_sb", [128, 8]); sig_c = sb("sig_c", [128, 8]); silu_c = sb("silu_c", [128, 8])
    adab = sb("adab", [128, 48]); lng = sb("lng", [128, 8]); lnf = sb("lnf", [128, 8])
    modT = sb("modT", [128, 32]); effs = sb("effs", [128, 8]); epsb = sb("epsb", [128, 1])
    identf = sb("identf", [128, 128]); identb = sb("identb", [128, 128], BF16); onesf = sb("onesf", [128, 128])
    diag = sb("diag", [128, 512])
    g1bc = sb("g1bc", [128, D]); g2bc = sb("g2bc", [128, D]); lnfbc = sb("lnfbc", [128, D])
    keysT = sb("keysT", [128, 16, 128], BF16)
    wst = [sb("wst%d" % i, [128, D], BF16) for i in range(3)]
    x1 = [sb("x1_%d" % i, [128, D]) for i in range(2)]
    yTb = sb("yTb", [128, 8, TS], BF16)
    if ysel:
        ysa = sb("ysa", [128, 8, TS], BF16); ysb = sb("ysb", [128, 8, TS], BF16); hsel = sb("hsel_sb", [128, 2])
    junk = sb("junk", [128, D], BF16); xn = sb("xn", [128, D], BF16)
    ssq = sb("ssq", [128, 4]); rstd = sb("rstd", [128, 4])
    h2T = sb("h2T", [128, 8, TS], BF16); qT = sb("qT", [128, 16, TS], BF16)
    ST = sb("ST", [128, 16, TS]); SC = sb("SC", [128, 2048])
    kf = SC[:].rearrange("p (a d) -> p a d", d=128)
    wk = [sb("wk%d" % i, [128, 128]) for i in range(2)]
    stop = sb("stop", [128, 16, 16]); candh = [sb("candh%d" % i, [128, 256]) for i in range(2)]
    cwk = [sb("cwk%d" % i, [128, 256]) for i in range(2)]; ctop = sb("ctop", [128, 8, 16])
    negm = sb("negm", [128, 8]); esub = sb("esub", [128, 128]); exs = sb("exs", [128, 128])
    Zs = sb("Zs", [128, 8]); invZ = sb("invZ", [128, 8])
    PACK = sb("PACK", [128, 4, 128]); PKT = sb("PKT", [128, 4, TS])
    rep = [sb("rep%d" % i, [128, 256]) for i in range(2)]
    E0 = [sb("E0_%d" % i, [128, 128], BF16) for i in range(2)]
    D1 = [sb("D1_%d" % i, [128, 128], BF16) for i in range(2)]
    ex1 = [sb("ex1_%d" % i, [128, 128]) for i in range(2)]
    gate = sb("gate_all", [128, 128, TS], BF16)
    ut4 = [sb("ut4_%d" % i, [128, 2, D], BF16) for i in range(3)]
    vt4 = [sb("vt4_%d" % i, [128, 2, D], BF16) for i in range(3)]
    NCH = 2
    ge = [sb("ge%d" % i, [128, TS], BF16) for i in range(2)]
    AT = [sb("AT%d" % i, [128, TS], BF16) for i in range(2)]
    tmpo = sb("tmpo", [128, 512])
    B = [c.ps("b%d" % i, [128, 512]) for i in range(8)]
    Bb = c.ps

    ld = lambda dst, src, key: c.dma("sp", dst, src, w=[key])
    ld(c_sb[:], c_d, "c_sb"); ld(adab[:], adab_d, "adab"); ld(lng[:], g_d, "lng"); ld(lnf[:], lnf_d, "lnf")
    ld(identf[:], id_d, "identf"); ld(onesf[:], ones_d, "onesf")
    if ysel:
        ld(hsel[:], A["hsel"], "hsel")
    ld(kf, keys_d.rearrange("h p k d -> k (h p) d"), "kf")
    c.op("dve", lambda e: e.tensor_copy(identb[:], identf[:]), r=["identf"], w=["identb"])
    c.op("dve", lambda e: e.memset(epsb[:], NORM_EPS), w=["epsb"])
    c.op("act", lambda e: e.activation(out=sig_c[:], in_=c_sb[:], func=AF.Sigmoid), r=["c_sb"], w=["sig_c"])
    c.op("dve", lambda e: e.tensor_tensor(out=silu_c[:], in0=c_sb[:], in1=sig_c[:], op=ALU.mult),
         r=["c_sb", "sig_c"], w=["silu_c"])
    emit_mod_fm(c, adaw_d, 0, 32, silu_c, adab, 16, modT[:, 0:32], B[0], "modT")
    c.op("dve", lambda e: e.scalar_tensor_tensor(out=effs[:], in0=modT[:, 16:24], scalar=1.0, in1=lng[:],
                                                  op0=ALU.add, op1=ALU.mult), r=["modT", "lng"], w=["effs"])

    def bcast_rows(vec8, vkey, out_tile, okey):
        for hf in range(2):
            for q in range(4):
                jc = hf * 4 + q
                c.op("dve", lambda e: e.tensor_scalar(out=diag[:, q * 128:(q + 1) * 128], in0=identf[:],
                                                      scalar1=vec8[:, jc:jc + 1], scalar2=None, op0=ALU.mult),
                     r=["identf", vkey], w=["diag%d" % q])
                c.op("pe", lambda e: e.matmul(B[1][:, q * 128:(q + 1) * 128], onesf[:], diag[:, q * 128:(q + 1) * 128],
                                              start=True, stop=True), r=["onesf", "diag%d" % q], w=["b1"])
            c.op("act", lambda e: e.copy(out=out_tile[:, hf * 512:(hf + 1) * 512], in_=B[1][:]), r=["b1"], w=[okey])

    bcast_rows(modT[:, 0:8], "modT", g1bc, "g1bc")
    bcast_rows(modT[:, 24:32], "modT", g2bc, "g2bc")
    if final:
        bcast_rows(lnf, "lnf", lnfbc, "lnfbc")
    for q4 in range(4):
        for q in range(4):
            hp16 = q4 * 4 + q
            c.op("pe", lambda e: e.transpose(B[2][:, q * 128:(q + 1) * 128], kf[:, hp16, :], identf[:]),
                 r=["kf", "identf"], w=["b2"])
        c.op("act", lambda e: e.copy(out=keysT[:, q4 * 4:(q4 + 1) * 4, :],
                                     in_=B[2][:].rearrange("p (q k) -> p q k", k=128)), r=["b2"], w=["keysT"])
    wsi = [0]

    def wstream(src_ap):
        i = wsi[0] % 3; wsi[0] += 1
        c.dma("sp", wst[i][:], src_ap, w=["wst%d" % i])
        return wst[i], "wst%d" % i

    def body():
        dbg(1)
        for st in range(DBG_NST or NST):
            tok0 = st * TS
            if not ysel:
                c.dma("pool", yTb[:], A["yT"].rearrange("(cc p) t -> p cc t", p=128)[:, :, tok0:tok0 + TS], w=["yTb"])
            else:
                c.dma("pool", ysa[:], A["yA"].rearrange("(cc p) t -> p cc t", p=128)[:, :, tok0:tok0 + TS], w=["ysa"])
                c.dma("pool", ysb[:], A["yB"].rearrange("(cc p) t -> p cc t", p=128)[:, :, tok0:tok0 + TS], w=["ysb"])
                c.op("dve", lambda e: e.tensor_scalar(out=ysa[:], in0=ysa[:], scalar1=hsel[:, 0:1], scalar2=None,
                                                      op0=ALU.mult), r=["ysa", "hsel"], w=["ysa"])
                c.op("dve", lambda e: e.scalar_tensor_tensor(out=yTb[:], in0=ysb[:], scalar=hsel[:, 1:2], in1=ysa[:],
                                                             op0=ALU.mult, op1=ALU.add),
                     r=["ysa", "ysb", "hsel"], w=["yTb"])
            wts = []
            for tt in range(2):
                X = x1[tt]; Xk = "x1_%d" % tt
                c.dma("sp", X[:], x_d[tok0 + tt * 128: tok0 + (tt + 1) * 128, :], w=[Xk])
                for cc in range(8):
                    wt, wkk = wstream(wo_d[cc])
                    for hf in range(2):
                        c.op("pe", lambda e: e.matmul(B[hf][:], yTb[:, cc, tt * 128:(tt + 1) * 128],
                                                      wt[:, hf * 512:(hf + 1) * 512], start=(cc == 0), stop=(cc == 7)),
                             r=["yTb", wkk], w=["b%d" % hf])
                for hf in range(2):
                    c.op("dve", lambda e: e.tensor_tensor(out=tmpo[:], in0=B[hf][:], in1=g1bc[:, hf * 512:(hf + 1) * 512],
                                                          op=ALU.mult), r=["b%d" % hf, "g1bc"], w=["tmpo"])
                    c.op("dve", lambda e: e.tensor_tensor(out=X[:, hf * 512:(hf + 1) * 512], in0=tmpo[:],
                                                          in1=X[:, hf * 512:(hf + 1) * 512], op=ALU.add),
                         r=["tmpo", Xk], w=[Xk])
                c.op("dve", lambda e: e.memset(ssq[:, tt:tt + 1], 0.0), w=["ssq"])
                c.op("act", lambda e: e.activation(out=junk[:], in_=X[:], func=AF.Square, accum_out=ssq[:, tt:tt + 1]),
                     r=[Xk, "ssq"], w=["junk", "ssq"])
                c.op("act", lambda e: e.activation(out=rstd[:, tt:tt + 1], in_=ssq[:, tt:tt + 1], func=AF.Sqrt,
                                                   bias=epsb[:, 0:1], scale=1.0 / D), r=["ssq", "epsb"], w=["rstd"])
                c.op("dve", lambda e: e.reciprocal(out=rstd[:, tt:tt + 1], in_=rstd[:, tt:tt + 1]), r=["rstd"], w=["rstd"])
                c.op("dve", lambda e: e.tensor_scalar(out=xn[:], in0=X[:], scalar1=rstd[:, tt:tt + 1], scalar2=None,
                                                      op0=ALU.mult), r=[Xk, "rstd"], w=["xn"])
                tpb = B[2 + tt][:].bitcast(BF16)
                for dc in range(8):
                    c.op("pe", lambda e: e.transpose(tpb[:, dc * 128:(dc + 1) * 128], xn[:, dc * 128:(dc + 1) * 128],
                                                     identb[:]), r=["xn", "identb"], w=["b%d" % (2 + tt)])
                for dc in range(8):
                    c.op("act", lambda e: e.activation(out=h2T[:, dc, tt * 128:(tt + 1) * 128],
                                                       in_=tpb[:, dc * 128:(dc + 1) * 128], func=AF.Identity,
                                                       bias=modT[:, 8 + dc: 9 + dc], scale=effs[:, dc:dc + 1]),
                         r=["b%d" % (2 + tt), "modT", "effs"], w=["h2T"])
            dbg(2)
            for hp16 in range(16):
                wt, wkk = wstream(pq_d[hp16].rearrange("p dc j -> p (dc j)"))
                bi = 4 + (hp16 // 2) % 2
                sub = hp16 % 2
                for dc in range(8):
                    c.op("pe", lambda e: e.matmul(B[bi][:, sub * TS:(sub + 1) * TS], wt[:, dc * 128:(dc + 1) * 128],
                                                  h2T[:, dc, :], start=(dc == 0), stop=(dc == 7)),
                         r=[wkk, "h2T"], w=["b%d" % bi])
                if sub == 1:
                    c.op("act", lambda e: e.copy(out=qT[:, hp16 - 1: hp16 + 1, :],
                                                 in_=B[bi][:].rearrange("p (s t) -> p s t", t=TS)),
                         r=["b%d" % bi], w=["qT"])
            for hp16 in range(16):
                bi = 6 + (hp16 // 2) % 2
                sub = hp16 % 2
                c.op("pe", lambda e: e.matmul(B[bi][:, sub * TS:(sub + 1) * TS], keysT[:, hp16, :], qT[:, hp16, :],
                                              start=True, stop=True), r=["keysT", "qT"], w=["b%d" % bi])
                if sub == 1:
                    c.op("dve", lambda e: e.tensor_copy(ST[:, hp16 - 1: hp16 + 1, :],
                                                        B[bi][:].rearrange("p (s t) -> p s t", t=TS)),
                         r=["b%d" % bi], w=["ST"])
            dbg(3)
            stop4 = stop[:].rearrange("p (h two) a -> p h two a", two=2)
            for tt in range(2):
                for q4 in range(4):
                    for q in range(4):
                        hp16 = q4 * 4 + q
                        c.op("pe", lambda e: e.transpose(B[q4][:, q * 128:(q + 1) * 128],
                                                         ST[:, hp16, tt * 128:(tt + 1) * 128], identf[:]),
                             r=["ST", "identf"], w=["b%d" % q4])
                    c.op("act", lambda e: e.copy(out=SC[:, q4 * 512:(q4 + 1) * 512], in_=B[q4][:]),
                         r=["b%d" % q4], w=["SC%d" % q4, "kf"])
                for hp16 in range(16):
                    w_ = wk[hp16 % 2]; wkk = "wk%d" % (hp16 % 2)
                    scv = SC[:, hp16 * 128:(hp16 + 1) * 128]; sck = "SC%d" % (hp16 // 4)
                    c.op("dve", lambda e: e.max(out=stop[:, hp16, 0:8], in_=scv), r=[sck], w=["stopA%d" % hp16])
                    c.op("dve", lambda e: e.match_replace(out=w_[:], in_to_replace=stop[:, hp16, 0:8], in_values=scv,
                                                          imm_value=NEG), r=[sck, "stopA%d" % hp16], w=[wkk])
                    c.op("dve", lambda e: e.max(out=stop[:, hp16, 8:16], in_=w_[:]), r=[wkk], w=["stopB%d" % hp16])
                stopkeys = ["stopA%d" % i for i in range(16)] + ["stopB%d" % i for i in range(16)]
                for h in range(8):
                    ch_ = candh[h % 2]; chk = "candh%d" % (h % 2)
                    cw_ = cwk[h % 2]; cwkk = "cwk%d" % (h % 2)
                    c.op("dve", lambda e: e.tensor_tensor(
                        out=ch_[:].rearrange("p (a b) -> p a b", b=16),
                        in0=stop4[:, h, 0, :].unsqueeze(2).broadcast_to([128, 16, 16]),
                        in1=stop4[:, h, 1, :].unsqueeze(1).broadcast_to([128, 16, 16]), op=ALU.add),
                        r=["stopA%d" % (2 * h), "stopB%d" % (2 * h), "stopA%d" % (2 * h + 1), "stopB%d" % (2 * h + 1)],
                        w=[chk])
                    c.op("dve", lambda e: e.max(out=ctop[:, h, 0:8], in_=ch_[:]), r=[chk], w=["ctopA%d" % h])
                    c.op("dve", lambda e: e.match_replace(out=cw_[:], in_to_replace=ctop[:, h, 0:8], in_values=ch_[:],
                                                          imm_value=NEG), r=[chk, "ctopA%d" % h], w=[cwkk])
                    c.op("dve", lambda e: e.max(out=ctop[:, h, 8:16], in_=cw_[:]), r=[cwkk], w=["ctopB%d" % h])
                ctk = ["ctopA%d" % h for h in range(8)] + ["ctopB%d" % h for h in range(8)]
                c.op("dve", lambda e: e.tensor_scalar(out=negm[:], in0=ctop[:, :, 0], scalar1=-1.0, scalar2=None,
                                                      op0=ALU.mult), r=ctk, w=["negm"])
                c.op("dve", lambda e: e.tensor_tensor(out=esub[:].rearrange("p (h a) -> p h a", a=16), in0=ctop[:],
                                                      in1=negm[:].unsqueeze(2).broadcast_to([128, 8, 16]), op=ALU.add),
                     r=ctk + ["negm"], w=["esub"])
                c.op("act", lambda e: e.activation(out=exs[:], in_=esub[:], func=AF.Exp), r=["esub"], w=["exs"])
                c.op("dve", lambda e: e.reduce_sum(out=Zs[:], in_=exs[:].rearrange("p (h a) -> p h a", a=16),
                                                   axis=mybir.AxisListType.X), r=["exs"], w=["Zs"])
                c.op("dve", lambda e: e.reciprocal(out=invZ[:], in_=Zs[:]), r=["Zs"], w=["invZ"])
                P3 = lambda j: PACK[:, j, :].rearrange("p (h a) -> p h a", a=16)
                c.op("dve", lambda e: e.tensor_copy(P3(0), stop4[:, :, 0, :]), r=stopkeys, w=["PACK0"])
                c.op("dve", lambda e: e.tensor_tensor(out=P3(1), in0=ctop[:, :, 15].unsqueeze(2).broadcast_to([128, 8, 16]),
                                                      in1=stop4[:, :, 0, :], op=ALU.subtract), r=stopkeys + ctk, w=["PACK1"])
                c.op("dve", lambda e: e.tensor_tensor(out=P3(2), in0=stop4[:, :, 0, :],
                                                      in1=negm[:].unsqueeze(2).broadcast_to([128, 8, 16]), op=ALU.add),
                     r=stopkeys + ["negm"], w=["PACK2"])
                c.op("dve", lambda e: e.tensor_copy(P3(3), invZ[:].unsqueeze(2).broadcast_to([128, 8, 16])),
                     r=["invZ"], w=["PACK3"])
                for j in range(4):
                    c.op("pe", lambda e: e.transpose(B[4][:, j * 128:(j + 1) * 128], PACK[:, j, :], identf[:]),
                         r=["PACK%d" % j, "identf"], w=["b4"])
                c.op("act", lambda e: e.copy(out=PKT[:, :, tt * 128:(tt + 1) * 128],
                                             in_=B[4][:].rearrange("p (j t) -> p j t", t=128)), r=["b4"], w=["PKT"])
            dbg(4)
            def s6A(t):
                s_ = t % 2; S = str(s_)
                c.op("pool", lambda e: e.tensor_copy(
                    rep[s_][:].rearrange("p (two h a) -> p two h a", two=2, a=16),
                    ST[:, :, t].rearrange("p (h two) -> p two h", two=2).unsqueeze(3).broadcast_to([128, 2, 8, 16])),
                    r=["ST"], w=["rep" + S])
                c.op("pe", lambda e: e.transpose(B[0 + s_][:, 0:128], rep[s_][:, 0:128], identf[:]),
                     r=["rep" + S, "identf"], w=["b%d" % (0 + s_)])
                c.op("pe", lambda e: e.transpose(B[2 + s_][:, 0:128], rep[s_][:, 128:256], identf[:]),
                     r=["rep" + S, "identf"], w=["b%d" % (2 + s_)])

            def s6B(t):
                s_ = t % 2; S = str(s_)
                c.op("dve", lambda e: e.tensor_scalar(out=E0[s_][:], in0=B[0 + s_][:, 0:128], scalar1=PKT[:, 0, t:t + 1],
                                                      scalar2=PKT[:, 3, t:t + 1], op0=ALU.is_equal, op1=ALU.mult),
                     r=["b%d" % (0 + s_), "PKT"], w=["E0_" + S])
                c.op("act", lambda e: e.activation(out=ex1[s_][:], in_=B[2 + s_][:, 0:128], func=AF.Exp,
                                                   bias=PKT[:, 2, t:t + 1]), r=["b%d" % (2 + s_), "PKT"], w=["ex1_" + S])
                c.op("dve", lambda e: e.scalar_tensor_tensor(out=D1[s_][:], in0=B[2 + s_][:, 0:128],
                                                             scalar=PKT[:, 1, t:t + 1], in1=ex1[s_][:],
                                                             op0=ALU.is_ge, op1=ALU.mult),
                     r=["b%d" % (2 + s_), "PKT", "ex1_" + S], w=["D1_" + S])

            def s6C(t):
                s_ = t % 2; S = str(s_)
                gs = (t // 4) % 2
                c.op("pe", lambda e: e.matmul(B[4 + gs][:, (t % 4) * 128:(t % 4 + 1) * 128], D1[s_][:], E0[s_][:],
                                              start=True, stop=True), r=["D1_" + S, "E0_" + S], w=["b%d" % (4 + gs)])
                if t % 4 == 3:
                    c.op("act", lambda e: e.copy(out=gate[:, :, t - 3:t + 1].rearrange("p i t -> p t i"),
                                                 in_=B[4 + gs][:].rearrange("p (t i) -> p t i", i=128)),
                         r=["b%d" % (4 + gs)], w=["gate"])

            s6A(0)
            for t in range(TS):
                if t + 1 < TS:
                    s6A(t + 1)
                s6B(t)
                s6C(t)
            dbg(5)
            def s7U(i0):
                g4, cix = i0 // NCH, i0 % NCH
                bf_ = g4 % 3; Bf = str(bf_)
                if cix == 0:
                    c.dma("sp", ut4[bf_][:], ut_d[g4 * NCH:(g4 + 1) * NCH].rearrange("c p dc e -> p c (dc e)"),
                          w=["ut4_" + Bf])
                    c.dma("sp", vt4[bf_][:],
                          vb_d[g4 * NCH * 128:(g4 + 1) * NCH * 128, :].rearrange("(c p) d -> p c d", p=128),
                          w=["vt4_" + Bf])
                pb = 4 + i0 % 2
                for dc in range(8):
                    c.op("pe", lambda e: e.matmul(B[pb][:, 0:TS], ut4[bf_][:, cix, dc * 128:(dc + 1) * 128],
                                                  h2T[:, dc, :], start=(dc == 0), stop=(dc == 7)),
                         r=["ut4_" + Bf, "h2T"], w=["b%d" % pb])

            def s7V(i0):
                g4, cix = i0 // NCH, i0 % NCH
                bf_ = g4 % 3; Bf = str(bf_)
                pb = 4 + i0 % 2
                gb = i0 % 2
                c.op("act", lambda e: e.activation(out=ge[gb][:], in_=B[pb][:, 0:TS], func=AF.Gelu),
                     r=["b%d" % pb], w=["ge%d" % gb])
                c.op("pool", lambda e: e.tensor_tensor(out=AT[gb][:], in0=ge[gb][:], in1=gate[:, i0, :], op=ALU.mult),
                     r=["ge%d" % gb, "gate"], w=["AT%d" % gb])
                for tt in range(2):
                    for hf in range(2):
                        c.op("pe", lambda e: e.matmul(B[tt * 2 + hf][:], AT[gb][:, tt * 128:(tt + 1) * 128],
                                                      vt4[bf_][:, cix, hf * 512:(hf + 1) * 512],
                                                      start=(i0 == 0), stop=(i0 == 127)),
                             r=["AT%d" % gb, "vt4_" + Bf], w=["b%d" % (tt * 2 + hf)])

            s7U(0)
            for i0 in range(128):
                if i0 + 1 < 128:
                    s7U(i0 + 1)
                s7V(i0)
            dbg(6)
            for tt in range(2):
                X = x1[tt]; Xk = "x1_%d" % tt
                for hf in range(2):
                    c.op("dve", lambda e: e.tensor_tensor(out=tmpo[:], in0=B[tt * 2 + hf][:],
                                                          in1=g2bc[:, hf * 512:(hf + 1) * 512], op=ALU.mult),
                         r=["b%d" % (tt * 2 + hf), "g2bc"], w=["tmpo"])
                    c.op("dve", lambda e: e.tensor_tensor(out=X[:, hf * 512:(hf + 1) * 512], in0=tmpo[:],
                                                          in1=X[:, hf * 512:(hf + 1) * 512], op=ALU.add),
                         r=["tmpo", Xk], w=[Xk])
                if final:
                    c.op("dve", lambda e: e.memset(ssq[:, 2 + tt:3 + tt], 0.0), w=["ssq"])
                    c.op("act", lambda e: e.activation(out=junk[:], in_=X[:], func=AF.Square,
                                                       accum_out=ssq[:, 2 + tt:3 + tt]), r=[Xk, "ssq"], w=["junk", "ssq"])
                    c.op("act", lambda e: e.activation(out=rstd[:, 2 + tt:3 + tt], in_=ssq[:, 2 + tt:3 + tt], func=AF.Sqrt,
                                                       bias=epsb[:, 0:1], scale=1.0 / D), r=["ssq", "epsb"], w=["rstd"])
                    c.op("dve", lambda e: e.reciprocal(out=rstd[:, 2 + tt:3 + tt], in_=rstd[:, 2 + tt:3 + tt]),
                         r=["rstd"], w=["rstd"])
                    c.op("dve", lambda e: e.scalar_tensor_tensor(out=X[:], in0=X[:], scalar=rstd[:, 2 + tt:3 + tt],
                                                                 in1=lnfbc[:], op0=ALU.mult, op1=ALU.mult),
                         r=[Xk, "rstd", "lnfbc"], w=[Xk])
                c.dma("sp", xo_d[tok0 + tt * 128: tok0 + (tt + 1) * 128, :], X[:], r=[Xk])

    try:
        body()
    except _Stop:
        pass


def build_L3(final):
    nc = bass.Bass("TRN2", target_bir_lowering=False)
    dt = lambda name, shape, d=F32, kind="ExternalInput": nc.dram_tensor(name, list(shape), d, kind=kind).ap()
    A = dict(x=dt("x", [TOK, D]), yT=dt("yT", [D, TOK]), c_fm=dt("c_fm", [128, 8]), ada_w=dt("ada_w", [D, 4 * D]),
             ada_b_fm=dt("ada_b_fm", [128, 48]), ln_g_fm=dt("ln_g_fm", [128, 8]), lnf_fm=dt("lnf_fm", [128, 8]),
             woutb=dt("woutb", [8, 128, D], BF16), pqb=dt("pqb", [16, 128, 8, 128], BF16),
             keys=dt("keys", [8, 2, 128, 128]), UT=dt("UT", [128, 128, 8, 128], BF16), Vb=dt("Vb", [128 * 128, D], BF16),
             ident=dt("ident", [128, 128]), ones=dt("ones", [128, 128]),
             xo=dt("xo", [TOK, D], F32, "ExternalOutput"))
    c = Ctx(nc)
    c.begin_phase("")
    emit_L3(c, A, final)
    c.end_phase()
    c.finish()
    return nc


NJC = 19


def build_fused():
    nc = bass.Bass("TRN2", target_bir_lowering=False)
    di = lambda name, shape, d=F32: nc.dram_tensor(name, list(shape), d, kind="ExternalInput").ap()
    it = lambda name, shape, d=F32: nc.dram_tensor(name, list(shape), d).ap()
    x_seq = di("x_seq", [SEQ, D]); x_mine = di("x_mine", [TOK, D]); c_fm = di("c_fm", [128, 8]); hsel = di("hsel", [128, 2])
    ada_w = di("ada_w", [2, D, 6 * D]); ada_b_fm = di("ada_b_fm", [2, 128, 48])
    ln1 = di("ln1_g_fm", [2, 128, 8]); ln2 = di("ln2_g_fm", [2, 128, 8]); lnf = di("lnf_fm", [128, 8])
    w_in = di("w_in", [2, D, 2304]); vres_down = di("vres_down", [D, 32])
    pvec = di("pvec", [2, 2, 128, 32]); psel = di("psel", [2, 128, 2, 4]); invdiv = di("invdiv", [2, 128, 2, TB])
    w_up = di("w_up", [2, 64, 512]); a_up = di("a_up", [2, 64, 512]); g_up = di("g_up", [2, 128, 512])
    vres_up = di("vres_up", [32, 512]); pool_w = di("pool_w", [2, 4, 128, 128])
    cst = {k: di(k, v.shape) for k, v in l2_consts().items()}
    ones = di("ones", [128, 128])
    nex = 128 if DBG_SKIP0 else 128 * 128
    peer_u = di("peer_u", [2, nex, D]); peer_v = di("peer_v", [2, nex, D])
    w_out = di("w_out", [2, D, D]); peer_q = di("peer_q", [2, D, 2048]); keys = di("peer_keys", [2, 8, 2, 128, 128])
    xo = nc.dram_tensor("xo", [TOK, D], F32, kind="ExternalOutput").ap()
    UT = it("UT_s", [2, 128, 128, 8, 128], BF16); Vb = it("Vb_s", [2, 128 * 128, D], BF16)
    woutb = it("woutb_s", [2, 8, 128, D], BF16); pqb = it("pqb_s", [2, 16, 128, 8, 128], BF16)
    P = it("P_s", [NJC * 128, SEQ]); Y = it("Y_s", [D, SEQ]); VF = it("VF_s", [512, SEQ])
    XO0 = it("XO0_s", [TOK, D]); X1 = it("X1_s", [SEQ, D])

    c = Ctx(nc)
    ph = [0]

    def phase(fn):
        if DBG_PHASES is not None and ph[0] >= DBG_PHASES:
            ph[0] += 1
            return
        c.begin_phase("p%d_" % ph[0]); ph[0] += 1
        fn()
        c.end_phase()

    if DBG_SKIP0:
        ph[0] += 1
    else:
        phase(lambda: emit_L0(c, dict(u=peer_u, v=peer_v, w_out=w_out, peer_q=peer_q, ident=cst["ident"],
                                       UT=UT, Vb=Vb, woutb=woutb, pqb=pqb), range(2), 128))
    for l in range(2):
        xsrc = x_seq if l == 0 else X1
        ncol = 2304 if l == 0 else 2336
        njc = (ncol + 127) // 128
        w_parts = [(w_in[l], 0, 2304)] + ([(vres_down, 2304, 32)] if l > 0 else [])
        for hf in range(2):
            xt_fn = {}
            if l > 0:
                xt_fn = dict(x_tile=lambda i, hf=hf: X1[((i // 4) * 2 + hf) * 512 + (i % 4) * 128:
                                                        ((i // 4) * 2 + hf) * 512 + (i % 4) * 128 + 128, :])
            phase(lambda: emit_L1(c, dict(xt_fn, x=xsrc[hf * TOK:(hf + 1) * TOK], c_fm=c_fm, ada_w=ada_w[l][:, 0:2 * D],
                                           ada_b_fm=ada_b_fm[l], ln_g_fm=ln1[l], w_parts=w_parts, ident=cst["ident"],
                                           out=P[0:njc * 128, hf * TOK:(hf + 1) * TOK], ncol=ncol)))
        for g in range(2):
            cs = slice(g * 256, (g + 1) * 256)
            A = dict(rT=P[g * 256:(g + 1) * 256], kT=P[512 + g * 256: 512 + (g + 1) * 256],
                     vT=P[1024 + g * 256: 1024 + (g + 1) * 256], waT=P[1536:1664], gdT=P[1664:1792],
                     poolT=P[1792 + g * 256: 1792 + (g + 1) * 256],
                     pvec=pvec[l, g], wup=w_up[l][:, cs], aup=a_up[l][:, cs], gup=g_up[l][:, cs],
                     poolw=pool_w[l][2 * g:2 * g + 2], psel=psel[g], invdiv=invdiv[g],
                     yrw=Y[g * 256:(g + 1) * 256], ypl=Y[512 + g * 256: 512 + (g + 1) * 256])
            A.update(cst)
            if l == 0:
                A["vout"] = VF[g * 256:(g + 1) * 256]
            else:
                A.update(vdT=P[2304:2336], vfT=VF[g * 256:(g + 1) * 256], vup=vres_up[:, cs])
            phase(lambda: emit_L2(c, A, l > 0))
        phase(lambda: emit_L3(c, dict(x=(x_mine if l == 0 else XO0), yA=Y[:, 0:TOK], yB=Y[:, TOK:2 * TOK], hsel=hsel,
                                       c_fm=c_fm, ada_w=ada_w[l][:, 2 * D:6 * D], ada_b_fm=ada_b_fm[l], ln_g_fm=ln2[l],
                                       lnf_fm=lnf, woutb=woutb[l], pqb=pqb[l], keys=keys[l], UT=UT[l], Vb=Vb[l],
                                       ident=cst["ident"], ones=ones, xo=(XO0 if l == 0 else xo)), l == 1))
        if l == 0 and DBG_COLL and (DBG_PHASES is None or DBG_PHASES > 6):
            for k in range(4):
                c.collective(lambda gq: gq.collective_compute(
                    "AllGather", ALU.bypass, replica_groups=[[0, 1], [2, 3], [4, 5], [6, 7]],
                    ins=[XO0[k * 512:(k + 1) * 512].opt()], outs=[X1[k * 1024:(k + 1) * 1024].opt()]))
    c.finish()
    return nc


def kernel_fused(inp):
    f = lambda a: np.ascontiguousarray(a, dtype=np.float32)
    cst = l2_consts()
    shared = dict(ada_w=f(inp["ada_w"]), ada_b_fm=f(np.stack([_fm(inp["ada_b"][l]) for l in range(2)])),
                  ln1_g_fm=f(np.stack([_fm(inp["ln1_g"][l]) for l in range(2)])),
                  ln2_g_fm=f(np.stack([_fm(inp["ln2_g"][l]) for l in range(2)])), lnf_fm=_fm(inp["lnf_g"]),
                  w_in=f(inp["w_in"]), vres_down=f(inp["vres_down"][0]), w_up=f(inp["w_up"]), a_up=f(inp["a_up"]),
                  g_up=f(inp["g_up"]), vres_up=f(inp["vres_up"][0]), pool_w=f(inp["pool_w"]),
                  ones=np.ones((128, 128), np.float32), peer_u=f(inp["peer_u"]), peer_v=f(inp["peer_v"]),
                  w_out=f(inp["w_out"]), peer_q=f(inp["peer_q"]), peer_keys=f(inp["peer_keys"]))
    shared.update(cst)
    dummyT = np.zeros((2336, 1), np.float32)
    pv = np.zeros((2, 2, 128, 32), np.float32); ps_ = np.zeros((2, 128, 2, 4), np.float32)
    idv = np.zeros((2, 128, 2, TB), np.float32)
    for l in range(2):
        for g in range(2):
            d = l2_inputs(inp, l, g, dummyT, np.zeros((1, 1), np.float32))
            pv[l, g] = d["pvec"]; ps_[g] = d["psel"]; idv[g] = d["invdiv"]
    shared.update(pvec=pv, psel=ps_, invdiv=idv)
    x = f(inp["x"])
    in_maps = []
    for core in range(NCORE):
        b, g = core // 2, core % 2
        hs = np.zeros((128, 2), np.float32); hs[:, g] = 1.0
        m = dict(shared)
        m.update(x_seq=f(x[b]), x_mine=f(x[b, g * TOK:(g + 1) * TOK]), c_fm=_fm(inp["c"][b]), hsel=hs)
        in_maps.append(m)
    if inp.get("_only_maps") is not None:
        return in_maps
    res = _run(_prog("fused", build_fused), in_maps)
    out = np.stack([np.asarray(res[core]["xo"]) for core in range(NCORE)], axis=0)
    return np.ascontiguousarray(out.reshape(NB, SEQ, D).astype(np.float32))


_PROGS = {}


def _prog(key, fn):
    if key not in _PROGS:
        _PROGS[key] = fn()
    return _PROGS[key]


def _run(nc, in_maps):
    res = run_bass_kernel_spmd(nc, in_maps, core_ids=list(range(NCORE)))
    return res.results


FUSED = True


def kernel(**inputs):
    inp = {k: np.asarray(v) for k, v in inputs.items()}
    if FUSED:
        return kernel_fused(inp)
    f = lambda a: np.ascontiguousarray(a, dtype=np.float32)
    ident = np.eye(128, dtype=np.float32); ones = np.ones((128, 128), np.float32)
    in_maps = []
    for core in range(NCORE):
        sl = slice(core * EPC, (core + 1) * EPC)
        in_maps.append(dict(u=f(inp["peer_u"][:, sl]), v=f(inp["peer_v"][:, sl]), w_out=f(inp["w_out"]),
                            peer_q=f(inp["peer_q"]), ident=ident))
    r0 = _run(_prog("L0", build_L0), in_maps)
    UT = [np.ascontiguousarray(np.concatenate([np.asarray(r0[c_]["UT"])[l] for c_ in range(NCORE)], axis=0)) for l in range(2)]
    Vb = [np.ascontiguousarray(np.concatenate([np.asarray(r0[c_]["Vb"])[l] for c_ in range(NCORE)], axis=0)) for l in range(2)]
    woutb = [np.ascontiguousarray(np.asarray(r0[0]["woutb"])[l]) for l in range(2)]
    pqb = [np.ascontiguousarray(np.asarray(r0[0]["pqb"])[l]) for l in range(2)]
    del r0
    x = f(inp["x"]).reshape(NCORE, TOK, D)
    vfirst = None
    for l in range(2):
        wfull = inp["w_in"][l] if l == 0 else np.concatenate([inp["w_in"][l], inp["vres_down"][l - 1]], axis=1)
        ncol = wfull.shape[1]
        adab_fm = _fm(inp["ada_b"][l])
        in_maps = []
        for core in range(NCORE):
            b = core // 2
            in_maps.append(dict(x=f(x[core]), c_fm=_fm(inp["c"][b]), ada_w=f(inp["ada_w"][l][:, 0:2 * D]),
                                ada_b_fm=adab_fm, ln_g_fm=_fm(inp["ln1_g"][l]), w_full=f(wfull), ident=ident))
        r1 = _run(_prog(("L1", ncol), lambda: build_L1(ncol)), in_maps)
        in_maps = []
        for core in range(NCORE):
            b, g = core // 2, core % 2
            projT = np.concatenate([r1[2 * b]["projT"], r1[2 * b + 1]["projT"]], axis=1)
            in_maps.append(l2_inputs(inp, l, g, projT, None if l == 0 else vfirst[core]))
        del r1
        r2 = _run(_prog(("L2", l > 0), lambda: build_L2(l > 0)), in_maps)
        if l == 0:
            vfirst = [np.asarray(r2[core]["vout"]) for core in range(NCORE)]
        in_maps = []
        for core in range(NCORE):
            b, hf = core // 2, core % 2
            ts = slice(hf * TOK, (hf + 1) * TOK)
            y0, y1 = r2[2 * b]["yT"], r2[2 * b + 1]["yT"]
            yT = np.concatenate([y0[0:256, ts], y1[0:256, ts], y0[256:512, ts], y1[256:512, ts]], axis=0)
            in_maps.append(dict(x=f(x[core]), yT=f(yT), c_fm=_fm(inp["c"][b]), ada_w=f(inp["ada_w"][l][:, 2 * D:6 * D]),
                                ada_b_fm=adab_fm, ln_g_fm=_fm(inp["ln2_g"][l]), lnf_fm=_fm(inp["lnf_g"]),
                                woutb=woutb[l], pqb=pqb[l], keys=f(inp["peer_keys"][l]), UT=UT[l], Vb=Vb[l],
                                ident=ident, ones=ones))
        del r2
        r3 = _run(_prog(("L3", l == 1), lambda: build_L3(l == 1)), in_maps)
        x = np.stack([np.asarray(r3[core]["xo"]) for core in range(NCORE)], axis=0)
        del r3
    return np.ascontiguousarray(x.reshape(NB, SEQ, D).astype(np.float32))
```

```python
from contextlib import ExitStack
import numpy as np
import ml_dtypes
import concourse.bass as bass
import concourse.mybir as mybir
from concourse.bass_utils import run_bass_kernel_spmd

F32 = mybir.dt.float32
BF16 = mybir.dt.bfloat16
AF = mybir.ActivationFunctionType
ALU = mybir.AluOpType

D = 1024
SEQ = 4096
NB = 4
NCORE = 8
TOK = 2048
NORM_EPS = 1e-6
GN_EPS = 64e-5
CH = 64


class Ctx:
    NDMA = 12

    def __init__(self, nc):
        self.nc = nc
        self.stack = ExitStack()
        self.E = dict(pe=nc.tensor, act=nc.scalar, dve=nc.vector, pool=nc.gpsimd, sp=nc.sync)
        self.sem = {}
        for e in ("pe", "act", "dve", "pool"):
            self.sem[e] = self.stack.enter_context(nc.semaphore("c_" + e))
        self.cnt = {e: 0 for e in self.sem}
        self.seen = {e: {} for e in self.E}
        self.dsem = {}
        self.dval = {}
        self.dnext = {}
        for q in ("sp", "pool", "act"):
            self.dsem[q] = [self.stack.enter_context(nc.semaphore("d_%s%d" % (q, i))) for i in range(self.NDMA)]
            self.dval[q] = [0] * self.NDMA
            self.dnext[q] = 0
        self.W = {}
        self.R = {}
        self.n_ins = 0
        self.nwait = {}
        self.ccsem = self.stack.enter_context(nc.semaphore("cc_sem"))
        self.ccval = 0

    scope = None
    pfx = ""

    def begin_phase(self, pfx):
        self.scope = ExitStack()
        self.pfx = pfx

    def end_phase(self):
        self.barrier()
        self.scope.close()
        self.scope = None
        self.W = {}
        self.R = {}

    def barrier(self):
        for eng in self.E:
            for e2 in self.cnt:
                if eng == "pe" and e2 == "pe":
                    continue
                self._wait(eng, e2, self.cnt[e2])
            for q in self.dsem:
                for i in range(self.NDMA):
                    self._wait(eng, (q, i), self.dval[q][i])

    def sb(self, name, shape, dt=F32):
        st = self.scope if self.scope is not None else self.stack
        return st.enter_context(self.nc.sbuf_tensor(self.pfx + name, list(shape), dt))

    def ps(self, name, shape, dt=F32):
        st = self.scope if self.scope is not None else self.stack
        return st.enter_context(self.nc.psum_tensor(self.pfx + name, list(shape), dt))

    def _semh(self, semid):
        if semid == "cc":
            return self.ccsem
        if isinstance(semid, str):
            return self.sem[semid]
        return self.dsem[semid[0]][semid[1]]

    def _wait(self, eng, semid, val):
        if val <= 0:
            return
        if self.seen[eng].get(semid, 0) >= val:
            return
        self.E[eng].wait_ge(self._semh(semid), val)
        self.seen[eng][semid] = val
        self.n_ins += 1
        self.nwait[eng] = self.nwait.get(eng, 0) + 1

    def _deps(self, r, w):
        deps = {}
        for k in r:
            for s, v in self.W.get(k, {}).items():
                deps[s] = max(deps.get(s, 0), v)
        for k in w:
            for s, v in self.W.get(k, {}).items():
                deps[s] = max(deps.get(s, 0), v)
            for s, v in self.R.get(k, {}).items():
                deps[s] = max(deps.get(s, 0), v)
        return deps

    def _record(self, semid, val, r, w):
        for k in w:
            self.W[k] = {semid: val}
            self.R[k] = {}
        for k in r:
            self.R.setdefault(k, {})[semid] = val

    def op(self, eng, fn, r=(), w=()):
        deps = self._deps(r, w)
        for s, v in deps.items():
            if eng == "pe" and s == "pe":
                continue
            self._wait(eng, s, v)
        ins = fn(self.E[eng])
        self.cnt[eng] += 1
        ins.then_inc(self.sem[eng], 1)
        self.n_ins += 1
        self._record(eng, self.cnt[eng], r, w)
        return ins

    def dma(self, q, out, in_, r=(), w=(), **kw):
        i = self.dnext[q]
        self.dnext[q] = (i + 1) % self.NDMA
        semid = (q, i)
        self._wait(q, semid, self.dval[q][i])
        deps = self._deps(r, w)
        for s, v in deps.items():
            self._wait(q, s, v)
        ins = self.E[q].dma_start(out=out, in_=in_, **kw)
        self.dval[q][i] += 16
        ins.then_inc(self.dsem[q][i], 16)
        self.n_ins += 1
        self._record(semid, self.dval[q][i], r, w)
        return ins

    def collective(self, fn):
        self.barrier()
        ins = fn(self.nc.gpsimd)
        self.ccval += 1
        ins.then_inc(self.ccsem)
        for eng in self.E:
            self._wait(eng, "cc", self.ccval)

    def finish(self):
        global LAST_CNT
        LAST_CNT = (dict(self.cnt), {q: list(v) for q, v in self.dval.items()}, self.n_ins, dict(self.nwait))
        for q in self.dsem:
            for i in range(self.NDMA):
                self._wait("sp", (q, i), self.dval[q][i])
        for e in self.cnt:
            self._wait("sp", e, self.cnt[e])
        self.stack.close()


def _fm(vec, n=None):
    v = np.ascontiguousarray(np.asarray(vec, dtype=np.float32).reshape(-1, 128).T)
    return v


def emit_mod_fm(c, adaw_dram, col0, ncolchunks, silu_c, adab_fm_sb, adab_col0, out_sb, ps_tile, tag):
    nc = c.nc
    wv = adaw_dram.rearrange("(dc p) j -> p dc j", p=128)
    cap = c.adaw_sb.shape[2] // 128
    for half in range(0, ncolchunks, cap):
        nch = min(cap, ncolchunks - half)
        wt = c.adaw_sb
        c.dma("sp", wt[:, :, 0:nch * 128], wv[:, :, col0 + half * 128: col0 + (half + nch) * 128],
              w=["adaw"])
        for jc in range(nch):
            for dc in range(8):
                c.op("pe", lambda e, jc=jc, dc=dc: e.matmul(
                    ps_tile[:, half + jc: half + jc + 1], wt[:, dc, jc * 128:(jc + 1) * 128],
                    silu_c[:, dc:dc + 1], start=(dc == 0), stop=(dc == 7)),
                    r=["adaw", "silu_c"], w=[tag + "_ps"])
    c.op("dve", lambda e: e.tensor_tensor(out=out_sb, in0=ps_tile[:, 0:ncolchunks],
                                           in1=adab_fm_sb[:, adab_col0: adab_col0 + ncolchunks], op=ALU.add),
         r=[tag + "_ps", "adab"], w=[tag])


DBG_STAGE = 99
DBG_PHASES = None
DBG_COLL = True
DBG_SKIP0 = False
LAST_CNT = None
DBG_NST = None


def emit_L1(c, A):
    ncol = A["ncol"]
    njc = (ncol + 127) // 128
    x_d, c_d, adaw_d, adab_d, g_d, id_d, out_d = (A["x"], A["c_fm"], A["ada_w"], A["ada_b_fm"], A["ln_g_fm"],
                                                   A["ident"], A["out"])
    c.adaw_sb = c.sb("adaw", [128, 8, 1024], F32)
    c_sb = c.sb("c_sb", [128, 8]); sig_c = c.sb("sig_c", [128, 8]); silu_c = c.sb("silu_c", [128, 8])
    adab = c.sb("adab", [128, 48]); lng = c.sb("lng", [128, 8])
    modT = c.sb("modT", [128, 16]); effs = c.sb("effs", [128, 8])
    identf = c.sb("identf", [128, 128]); identb = c.sb("identb", [128, 128], BF16)
    wbf = c.sb("wbf", [128, 8, njc * 128], BF16)
    hT = c.sb("hT", [128, 8, TOK], BF16)
    xt = [c.sb("xt%d" % i, [128, D]) for i in range(2)]
    junk = c.sb("junk", [128, D], BF16)
    xn = [c.sb("xn%d" % i, [128, D], BF16) for i in range(2)]
    ss = c.sb("ss", [128, 32]); rstd = c.sb("rstd", [128, 32])
    stg = [c.sb("stg%d" % i, [128, TOK]) for i in range(2)]
    mod_ps = c.ps("mod_ps", [128, 512])
    tp_ps = [c.ps("tp_ps%d" % i, [128, 1024], BF16) for i in range(2)]
    mm_ps = [c.ps("mm_ps%d" % i, [128, 512]) for i in range(4)]

    c.dma("sp", c_sb[:], c_d, w=["c_sb"])
    c.dma("sp", adab[:], adab_d, w=["adab"])
    c.dma("sp", lng[:], g_d, w=["lng"])
    c.dma("sp", identf[:], id_d, w=["identf"])
    c.op("dve", lambda e: e.tensor_copy(identb[:], identf[:]), r=["identf"], w=["identb"])
    if njc * 128 != ncol:
        c.op("pool", lambda e: e.memset(wbf[:, :, ncol:njc * 128], 0.0), w=["wbf"])
    for (w_ap, col0, ncols) in A["w_parts"]:
        wv = w_ap.rearrange("(dc p) j -> p dc j", p=128)
        for dc in range(8):
            c.dma("pool", wbf[:, dc, col0:col0 + ncols], wv[:, dc, :], w=["wbf"])
    c.op("act", lambda e: e.activation(out=sig_c[:], in_=c_sb[:], func=AF.Sigmoid), r=["c_sb"], w=["sig_c"])
    c.op("dve", lambda e: e.tensor_tensor(out=silu_c[:], in0=c_sb[:], in1=sig_c[:], op=ALU.mult),
         r=["c_sb", "sig_c"], w=["silu_c"])
    emit_mod_fm(c, adaw_d, 0, 16, silu_c, adab, 0, modT[:, 0:16], mod_ps, "modT")
    c.op("dve", lambda e: e.scalar_tensor_tensor(out=effs[:], in0=modT[:, 8:16], scalar=1.0, in1=lng[:],
                                                  op0=ALU.add, op1=ALU.mult), r=["modT", "lng"], w=["effs"])
    c.op("dve", lambda e: e.memset(ss[:], 0.0), w=["ss%d" % i for i in range(TOK // 128)])
    epsb = c.sb("epsb", [128, 1])
    c.op("dve", lambda e: e.memset(epsb[:], NORM_EPS), w=["epsb"])
    ntile = TOK // 128
    for i in range(ntile):
        s = i % 2
        c.dma("sp", xt[s][:], (A["x_tile"](i) if "x_tile" in A else x_d[i * 128:(i + 1) * 128, :]), w=["xt%d" % s])
        c.op("act", lambda e, s=s, i=i: e.activation(out=junk[:], in_=xt[s][:], func=AF.Square,
                                                     accum_out=ss[:, i:i + 1]),
             r=["xt%d" % s], w=["junk", "ss%d" % i])
        c.op("act", lambda e, i=i: e.activation(out=rstd[:, i:i + 1], in_=ss[:, i:i + 1], func=AF.Sqrt,
                                                bias=epsb[:, 0:1], scale=1.0 / D),
             r=["ss%d" % i, "epsb"], w=["rstd%d" % i])
        c.op("dve", lambda e, i=i: e.reciprocal(out=rstd[:, i:i + 1], in_=rstd[:, i:i + 1]),
             r=["rstd%d" % i], w=["rstd%d" % i])
        c.op("dve", lambda e, s=s, i=i: e.tensor_scalar(out=xn[s][:], in0=xt[s][:], scalar1=rstd[:, i:i + 1],
                                                        scalar2=None, op0=ALU.mult),
             r=["xt%d" % s, "rstd%d" % i], w=["xn%d" % s])
        for dc in range(8):
            c.op("pe", lambda e, s=s, dc=dc: e.transpose(tp_ps[s][:, dc * 128:(dc + 1) * 128],
                                                         xn[s][:, dc * 128:(dc + 1) * 128], identb[:]),
                 r=["xn%d" % s, "identb"], w=["tp%d" % s])
        for dc in range(8):
            if s == 0:
                c.op("act", lambda e, s=s, dc=dc, i=i: e.activation(
                    out=hT[:, dc, i * 128:(i + 1) * 128], in_=tp_ps[s][:, dc * 128:(dc + 1) * 128],
                    func=AF.Identity, bias=modT[:, dc:dc + 1], scale=effs[:, dc:dc + 1]),
                    r=["tp%d" % s, "modT", "effs"], w=["hT_%d_%d_%d" % (dc, i // 4, s)])
            else:
                c.op("dve", lambda e, s=s, dc=dc, i=i: e.tensor_scalar(
                    out=hT[:, dc, i * 128:(i + 1) * 128], in0=tp_ps[s][:, dc * 128:(dc + 1) * 128],
                    scalar1=effs[:, dc:dc + 1], scalar2=modT[:, dc:dc + 1], op0=ALU.mult, op1=ALU.add),
                    r=["tp%d" % s, "modT", "effs"], w=["hT_%d_%d_%d" % (dc, i // 4, s)])
    k = 0
    for jc in range(njc):
        st = stg[jc % 2]
        for tb in range(TOK // 512):
            pt = mm_ps[k % 4]; pk = "mm%d" % (k % 4); k += 1
            for dc in range(8):
                c.op("pe", lambda e, pt=pt, dc=dc, jc=jc, tb=tb: e.matmul(
                    pt[:], wbf[:, dc, jc * 128:(jc + 1) * 128], hT[:, dc, tb * 512:(tb + 1) * 512],
                    start=(dc == 0), stop=(dc == 7)),
                    r=["wbf", "hT_%d_%d_0" % (dc, tb), "hT_%d_%d_1" % (dc, tb)], w=[pk])
            eng = "act" if tb % 2 == 0 else "dve"
            if eng == "act":
                c.op("act", lambda e, pt=pt, st=st, tb=tb: e.copy(out=st[:, tb * 512:(tb + 1) * 512], in_=pt[:]),
                     r=[pk], w=["stg%d_%d" % (jc % 2, tb)])
            else:
                c.op("dve", lambda e, pt=pt, st=st, tb=tb: e.tensor_copy(st[:, tb * 512:(tb + 1) * 512], pt[:]),
                     r=[pk], w=["stg%d_%d" % (jc % 2, tb)])
        c.dma("sp", out_d[jc * 128:(jc + 1) * 128, :], st[:],
              r=["stg%d_%d" % (jc % 2, tb) for tb in range(4)])


def build_L1(ncol):
    nc = bass.Bass("TRN2", target_bir_lowering=False)
    njc = (ncol + 127) // 128
    di = lambda name, shape: nc.dram_tensor(name, list(shape), F32, kind="ExternalInput").ap()
    A = dict(x=di("x", [TOK, D]), c_fm=di("c_fm", [128, 8]), ada_w=di("ada_w", [D, 2 * D]),
             ada_b_fm=di("ada_b_fm", [128, 48]), ln_g_fm=di("ln_g_fm", [128, 8]), ident=di("ident", [128, 128]),
             ncol=ncol)
    A["w_parts"] = [(di("w_full", [D, ncol]), 0, ncol)]
    A["out"] = nc.dram_tensor("projT", [njc * 128, TOK], F32, kind="ExternalOutput").ap()
    c = Ctx(nc)
    c.begin_phase("")
    emit_L1(c, A)
    c.end_phase()
    c.finish()
    return nc


class _Stop(Exception):
    pass


def dbg(stage):
    if DBG_STAGE == stage:
        raise _Stop()


TB = 512
NBLK = SEQ // TB
CPB = TB // CH
C0 = float(np.exp(-0.5))
POOL_WINDOWS = (2, 4, 8, 16)


def l2_consts():
    s_idx = np.arange(64)[:, None]; t_idx = np.arange(64)[None, :]
    su = (t_idx > s_idx).astype(np.float32); iu = (t_idx >= s_idx).astype(np.float32)
    maskA = np.tile(np.concatenate([su, iu], axis=1), (1, 4))
    maskC = np.tile((t_idx < s_idx).astype(np.float32), (1, 4))
    identrep = np.tile(np.eye(64, dtype=np.float32), (1, 4))
    scanmask = np.ones((128, TB), np.float32); scanmask[:, ::CH] = 0.0
    blockones = np.kron(np.eye(2, dtype=np.float32), np.ones((64, 64), np.float32))
    return dict(maskA=maskA, maskC=maskC, identrep=identrep, scanmask=scanmask, blockones=blockones,
                ident=np.eye(128, dtype=np.float32))


def emit_L2(c, A, layer1):
    nc = c.nc
    rT, kT, vT, waT, gdT, poolT = A["rT"], A["kT"], A["vT"], A["waT"], A["gdT"], A["poolT"]
    if layer1:
        vdT, vfT, vup_d = A["vdT"], A["vfT"], A["vup"]
    pvec_d, wup_d, aup_d, gup_d, poolw_d = A["pvec"], A["wup"], A["aup"], A["gup"], A["poolw"]
    maskA_d, maskC_d, identrep_d = A["maskA"], A["maskC"], A["identrep"]
    scanmask_d, bo_d, id_d, invdiv_d, psel_d = A["scanmask"], A["blockones"], A["ident"], A["invdiv"], A["psel"]
    yrw, ypl = A["yrw"], A["ypl"]
    if not layer1:
        vout = A["vout"]
    sb = c.sb
    pvec = sb("pvec_sb", [128, 32]); omk = sb("omk", [128, 2])
    wup = sb("wup_sb", [128, 256], BF16); aup = sb("aup_sb", [128, 256], BF16); gup = sb("gup_sb", [128, 256], BF16)
    vup = sb("vup_sb", [32, 256], BF16); poolw = sb("poolw_sb", [128, 2, 128], BF16)
    maskA = sb("maskA_sb", [64, 512]); maskC = sb("maskC_sb", [64, 256]); identrep = sb("identrep_sb", [64, 256])
    scanmask = sb("scanmask_sb", [128, TB]); bo = sb("bo_sb", [128, 128]); identf = sb("identf", [128, 128])
    identb = sb("identb", [128, 128], BF16); invdiv = sb("invdiv_sb", [128, 2, TB])
    epsk = sb("epsk", [128, 1]); gneps = sb("gneps", [128, 1]); psel = sb("psel_sb", [128, 2, 4])
    Xwa = sb("Xwa", [128, TB + 1]); Xg = sb("Xg", [128, TB + 1]); Xvd = sb("Xvd", [32, TB + 1])
    dwa = sb("dwa", [128, TB]); swa = sb("swa", [128, TB]); th = sb("th", [64, TB], BF16); adb = sb("adb", [128, TB], BF16)
    dg = sb("dg", [128, TB]); sgd = sb("sgd", [128, TB]); sg = sb("sg", [128, TB], BF16)
    dvd = sb("dvd", [32, TB]); vdb = sb("vdb", [32, TB], BF16)
    P2 = range(2)
    Xr = [sb("Xr_sh", [128, TB + 1])] * 2; Xk = [sb("Xk_sh", [128, TB + 1])] * 2
    Xv = [sb("Xv_sh", [128, TB + 1])] * 2; Xvf = [sb("Xvf_sh", [128, TB])] * 2
    tmpd = [sb("tmpd_sh", [128, TB])] * 2
    r_s = [sb("r_s%d" % i, [128, TB]) for i in P2]; k_s = [sb("k_s%d" % i, [128, TB]) for i in P2]
    v_s = [sb("v_s%d" % i, [128, TB]) for i in P2]
    sigw = [sb("sigw%d" % i, [128, TB]) for i in P2]; cum = [sb("cum%d" % i, [128, TB]) for i in P2]
    cumx = [sb("cumx_sh", [128, TB])] * 2
    G = [sb("G%d" % i, [128, TB]) for i in P2]; Ginv = [sb("Ginv%d" % i, [128, TB]) for i in P2]
    Gex = [sb("Gex%d" % i, [128, TB]) for i in P2]
    a_ = [sb("a_%d" % i, [128, TB]) for i in P2]; gg = [sb("gg%d" % i, [128, TB]) for i in P2]
    vsig = [sb("vsig_sh", [128, TB])] * 2
    kkraw = [sb("kkraw_sh", [128, TB])] * 2; sq = [sb("sq_sh", [128, TB])] * 2
    rn = [sb("rn_sh", [128, TB])] * 2; kk = [sb("kk%d" % i, [128, TB]) for i in P2]
    fac = [sb("fac_sh", [128, TB])] * 2; kmod = [sb("kmod%d" % i, [128, TB]) for i in P2]
    rk2 = [sb("rk2_sh", [128, TB])] * 2; bonus = [sb("bonus%d" % i, [128, TB]) for i in P2]
    t1 = [sb("t1_sh", [128, TB])] * 2
    ARt = [sb("ARt%d" % i, [128, CPB * 128], BF16) for i in P2]
    Bt = [sb("Bt%d" % i, [128, TB], BF16) for i in P2]; Kt = [sb("Kt%d" % i, [128, TB], BF16) for i in P2]
    Vb = [sb("Vb%d" % i, [128, TB], BF16) for i in P2]
    yraw = [sb("yraw%d" % i, [128, TB]) for i in P2]; yc = [sb("yc%d" % i, [128, TB]) for i in P2]
    ysq = [sb("ysq_sh", [128, TB])] * 2; yrs = [sb("yrs_sh", [128, TB])] * 2
    yo = [sb("yo%d" % i, [128, TB]) for i in P2]
    M0b = [[sb("M0b%d_%d" % (i, j), [64, 64], BF16) for j in range(2)] for i in range(4)]
    ARo = [sb("ARo%d" % i, [64, CPB * 128], BF16) for i in P2]
    Bo = [sb("Bo%d" % i, [64, TB], BF16) for i in P2]; Ko = [sb("Ko%d" % i, [64, TB], BF16) for i in P2]
    GCs = [sb("GCs%d" % i, [128, CPB]) for i in P2]; GCo = [sb("GCo%d" % i, [64, CPB]) for i in P2]
    tokmaj = [sb("tokmaj%d" % j, [64, 768], BF16) for j in range(2)]
    SA = [sb("SA%d" % j, [64, 512], BF16) for j in range(2)]; SB_ = [sb("SB%d" % j, [64, 512], BF16) for j in range(2)]
    SC = [sb("SC%d" % j, [64, 256], BF16) for j in range(2)]
    TT = [sb("TT%d" % j, [64, 256], BF16) for j in range(2)]; PP = [sb("PP%d" % j, [64, 512], BF16) for j in range(2)]
    W1 = sb("W1", [64, 256], BF16); U = [sb("U%d" % j, [64, 256], BF16) for j in range(2)]
    TTFs = [sb("TTF%d" % j, [64, 256], BF16) for j in range(2)]
    Xp = [sb("Xp%d" % i, [128, TB + 15]) for i in P2]
    plv = [[sb("plv%d_%d" % (i, k), [128, TB + 15]) for k in range(4)] for i in P2]
    pacc = [sb("pacc%d" % i, [128, TB]) for i in P2]
    pd = [sb("pd%d" % i, [128, TB], BF16) for i in P2]; pm = [sb("pm%d" % i, [128, TB]) for i in P2]
    po = [sb("po%d" % i, [128, TB]) for i in P2]
    bk = {n: c.ps("bk_" + n, [128, 512]) for n in ("A", "B", "C", "D", "E", "FG", "HI")}
    trp = c.ps("bk_trp", [128, 1024], BF16)

    ld = lambda dst, src, key: c.dma("sp", dst, src, w=[key])
    ld(pvec[:], pvec_d, "pvec"); ld(maskA[:], maskA_d, "maskA"); ld(maskC[:], maskC_d, "maskC")
    ld(identrep[:], identrep_d, "identrep"); ld(scanmask[:], scanmask_d, "scanmask"); ld(bo[:], bo_d, "bo")
    ld(identf[:], id_d, "identf"); ld(invdiv[:], invdiv_d, "invdiv"); ld(psel[:], psel_d, "psel")
    c.dma("pool", wup[0:64, :], wup_d, w=["wup"]); c.dma("pool", aup[64:128, :], aup_d, w=["aup"])
    c.dma("pool", gup[:], gup_d, w=["gup"]); c.dma("pool", poolw[:], poolw_d.rearrange("g c d -> c g d"), w=["poolw"])
    if layer1:
        c.dma("pool", vup[0:32, :], vup_d, w=["vup"])
    c.op("dve", lambda e: e.tensor_copy(identb[:], identf[:]), r=["identf"], w=["identb"])
    c.op("dve", lambda e: e.memset(epsk[:], 1e-24), w=["epsk"])
    c.op("dve", lambda e: e.memset(gneps[:], GN_EPS), w=["gneps"])
    c.op("dve", lambda e: e.tensor_scalar(out=omk[:], in0=pvec[:, 12:14], scalar1=-1.0, scalar2=1.0,
                                          op0=ALU.mult, op1=ALU.add), r=["pvec"], w=["omk"])
    for i in range(4):
        for j in range(2):
            c.op("dve", lambda e, i=i, j=j: e.memset(M0b[i][j][:], 0.0), w=["M0b%d_%d" % (i, j)])
    for i in P2:
        for k_ in range(4):
            c.op("pool", lambda e, i=i, k_=k_: e.memset(plv[i][k_][:], 0.0), w=["plv%d_%d" % (i, k_)])
    pv = lambda col, hp=0: pvec[:, col + hp: col + hp + 1]

    def tshift(eng, X, d, out, mu, n, kX, kd, kout):
        c.op(eng, lambda e: e.tensor_tensor(out=d, in0=X[0:n, 0:TB], in1=X[0:n, 1:TB + 1], op=ALU.subtract),
             r=[kX], w=[kd])
        if eng == "dve":
            c.op(eng, lambda e: e.scalar_tensor_tensor(out=out, in0=d, scalar=mu, in1=X[0:n, 1:TB + 1],
                                                       op0=ALU.mult, op1=ALU.add), r=[kX, kd, "pvec"], w=[kout])
        else:
            c.op(eng, lambda e: e.tensor_scalar(out=d, in0=d, scalar1=mu, scalar2=None, op0=ALU.mult),
                 r=[kd, "pvec"], w=[kd])
            c.op(eng, lambda e: e.tensor_tensor(out=out, in0=d, in1=X[0:n, 1:TB + 1], op=ALU.add),
                 r=[kX, kd], w=[kout])

    def load_halo(X, src, rows, b, n, key, halo=1):
        t0 = b * TB
        if b == 0:
            c.op("pool", lambda e: e.memset(X[0:n, 0:halo], 0.0), w=[key])
            c.dma("sp", X[0:n, halo:halo + TB], src[rows, 0:TB], w=[key])
        else:
            c.dma("sp", X[0:n, :], src[rows, t0 - halo:t0 + TB], w=[key])

    chunk_counter = [0]

    def body():
        for b in range(NBLK):
            t0 = b * TB
            load_halo(Xwa, waT, slice(0, 128), b, 128, "Xwa")
            load_halo(Xg, gdT, slice(0, 128), b, 128, "Xg")
            tshift("pool", Xwa, dwa[:], swa[:], pv(25), 128, "Xwa", "dwa", "swa")
            c.op("act", lambda e: e.activation(out=th[:], in_=swa[0:64, :], func=AF.Tanh), r=["swa"], w=["th"])
            c.op("pool", lambda e: e.tensor_copy(adb[64:128, :], swa[64:128, :]), r=["swa"], w=["adb"])
            tshift("pool", Xg, dg[:], sgd[:], pv(24), 128, "Xg", "dg", "sgd")
            c.op("act", lambda e: e.activation(out=sg[:], in_=sgd[:], func=AF.Sigmoid), r=["sgd"], w=["sg"])
            if layer1:
                load_halo(Xvd, vdT, slice(0, 32), b, 32, "Xvd")
                tshift("pool", Xvd, dvd[:], vdb[:], pvec[0:32, 26:27], 32, "Xvd", "dvd", "vdb")
            dbg(1)
            for hp in P2:
                rows = slice(hp * 128, (hp + 1) * 128)
                H = str(hp)
                load_halo(Xr[hp], rT, rows, b, 128, "Xr")
                load_halo(Xk[hp], kT, rows, b, 128, "Xk")
                load_halo(Xv[hp], vT, rows, b, 128, "Xv")
                tshift("dve", Xr[hp], tmpd[hp][:], r_s[hp][:], pv(0, hp), 128, "Xr", "tmpd", "r_s" + H)
                tshift("dve", Xk[hp], tmpd[hp][:], k_s[hp][:], pv(2, hp), 128, "Xk", "tmpd", "k_s" + H)
                tshift("dve", Xv[hp], tmpd[hp][:], v_s[hp][:], pv(4, hp), 128, "Xv", "tmpd", "v_s" + H)
                cols = slice(hp * 128, (hp + 1) * 128)
                c.op("pe", lambda e: e.matmul(bk["A"][:], wup[0:64, cols], th[:], start=True, stop=True),
                     r=["wup", "th"], w=["bkA"])
                c.op("act", lambda e: e.activation(out=sigw[hp][:], in_=bk["A"][:], func=AF.Sigmoid, bias=pv(6, hp)),
                     r=["bkA", "pvec"], w=["sigw" + H])
                c.op("pe", lambda e: e.matmul(bk["B"][:], aup[64:128, cols], adb[64:128, :], start=True, stop=True),
                     r=["aup", "adb"], w=["bkB"])
                c.op("act", lambda e: e.activation(out=a_[hp][:], in_=bk["B"][:], func=AF.Sigmoid, bias=pv(8, hp)),
                     r=["bkB", "pvec"], w=["a_" + H])
                c.op("pe", lambda e: e.matmul(bk["C"][:], gup[:, cols], sg[:], start=True, stop=True),
                     r=["gup", "sg"], w=["bkC"])
                c.op("act", lambda e: e.copy(out=gg[hp][:], in_=bk["C"][:]), r=["bkC"], w=["gg" + H])
                if layer1:
                    c.dma("sp", Xvf[hp][:], vfT[rows, t0:t0 + TB], w=["Xvf"])
                    c.op("pe", lambda e: e.matmul(bk["D"][:], vup[0:32, cols], vdb[0:32, :], start=True, stop=True),
                         r=["vup", "vdb"], w=["bkD"])
                    c.op("act", lambda e: e.activation(out=vsig[hp][:], in_=bk["D"][:], func=AF.Sigmoid, bias=pv(27, hp)),
                         r=["bkD", "pvec"], w=["vsig"])
                    c.op("dve", lambda e: e.tensor_tensor(out=tmpd[hp][:], in0=Xvf[hp][:], in1=v_s[hp][:], op=ALU.subtract),
                         r=["Xvf", "v_s" + H], w=["tmpd"])
                    c.op("dve", lambda e: e.tensor_tensor(out=tmpd[hp][:], in0=tmpd[hp][:], in1=vsig[hp][:], op=ALU.mult),
                         r=["vsig", "tmpd"], w=["tmpd"])
                    c.op("dve", lambda e: e.tensor_tensor(out=v_s[hp][:], in0=v_s[hp][:], in1=tmpd[hp][:], op=ALU.add),
                         r=["v_s" + H, "tmpd"], w=["v_s" + H])
                else:
                    c.dma("sp", vout[rows, t0:t0 + TB], v_s[hp][:], r=["v_s" + H])
                c.op("dve", lambda e: e.tensor_scalar(out=kkraw[hp][:], in0=k_s[hp][:], scalar1=pv(10, hp), scalar2=None,
                                                      op0=ALU.mult), r=["k_s" + H, "pvec"], w=["kkraw"])
                c.op("pool", lambda e: e.tensor_tensor(out=sq[hp][:], in0=kkraw[hp][:], in1=kkraw[hp][:], op=ALU.mult),
                     r=["kkraw"], w=["sq"])
                c.op("pe", lambda e: e.matmul(bk["E"][:], bo[:], sq[hp][:], start=True, stop=True),
                     r=["bo", "sq"], w=["bkE"])
                c.op("act", lambda e: e.activation(out=rn[hp][:], in_=bk["E"][:], func=AF.Sqrt, bias=epsk[:, 0:1]),
                     r=["bkE", "epsk"], w=["rn"])
                c.op("dve", lambda e: e.reciprocal(out=rn[hp][:], in_=rn[hp][:]), r=["rn"], w=["rn"])
                c.op("dve", lambda e: e.tensor_tensor(out=kk[hp][:], in0=kkraw[hp][:], in1=rn[hp][:], op=ALU.mult),
                     r=["kkraw", "rn"], w=["kk" + H])
                c.op("pool", lambda e: e.tensor_scalar(out=fac[hp][:], in0=a_[hp][:], scalar1=pv(12, hp),
                                                       scalar2=omk[:, hp:hp + 1], op0=ALU.mult, op1=ALU.add),
                     r=["a_" + H, "pvec", "omk"], w=["fac"])
                c.op("pool", lambda e: e.tensor_tensor(out=kmod[hp][:], in0=k_s[hp][:], in1=fac[hp][:], op=ALU.mult),
                     r=["k_s" + H, "fac"], w=["kmod" + H])
                c.op("pool", lambda e: e.tensor_scalar(out=rk2[hp][:], in0=r_s[hp][:], scalar1=pv(16, hp), scalar2=None,
                                                       op0=ALU.mult), r=["r_s" + H, "pvec"], w=["rk2"])
                c.op("pool", lambda e: e.tensor_tensor(out=rk2[hp][:], in0=rk2[hp][:], in1=kmod[hp][:], op=ALU.mult),
                     r=["rk2", "kmod" + H], w=["rk2"])
                c.op("pe", lambda e: e.matmul(bk["FG"][:], bo[:], rk2[hp][:], start=True, stop=True),
                     r=["bo", "rk2"], w=["bkFG"])
                c.op("dve", lambda e: e.tensor_tensor(out=bonus[hp][:], in0=bk["FG"][:], in1=v_s[hp][:], op=ALU.mult),
                     r=["bkFG", "v_s" + H], w=["bonus" + H])
                c.op("dve", lambda e: e.tensor_tensor_scan(out=cum[hp][:], data0=scanmask[:], data1=sigw[hp][:],
                                                           initial=0.0, op0=ALU.mult, op1=ALU.add),
                     r=["scanmask", "sigw" + H], w=["cum" + H])
                c.op("pool", lambda e: e.tensor_tensor(out=cumx[hp][:], in0=cum[hp][:], in1=sigw[hp][:], op=ALU.subtract),
                     r=["cum" + H, "sigw" + H], w=["cumx"])
                c.op("act", lambda e: e.activation(out=G[hp][:], in_=cum[hp][:], func=AF.Exp, scale=-C0),
                     r=["cum" + H], w=["G" + H])
                c.op("act", lambda e: e.activation(out=Ginv[hp][:], in_=cum[hp][:], func=AF.Exp, scale=C0),
                     r=["cum" + H], w=["Ginv" + H])
                c.op("act", lambda e: e.activation(out=Gex[hp][:], in_=cumx[hp][:], func=AF.Exp, scale=-C0),
                     r=["cumx"], w=["Gex" + H])
                AR3 = ARt[hp][:].rearrange("p (c two t) -> p c two t", two=2, t=CH)
                v3 = lambda ap: ap.rearrange("p (c t) -> p c t", t=CH)
                c.op("dve", lambda e: e.tensor_tensor(out=AR3[:, :, 1, :], in0=v3(r_s[hp][:]), in1=v3(G[hp][:]), op=ALU.mult),
                     r=["r_s" + H, "G" + H], w=["ARt" + H])
                c.op("dve", lambda e: e.scalar_tensor_tensor(out=AR3[:, :, 0, :], in0=v3(kk[hp][:]), scalar=-1.0,
                                                             in1=v3(Gex[hp][:]), op0=ALU.mult, op1=ALU.mult),
                     r=["kk" + H, "Gex" + H], w=["ARt" + H])
                c.op("pool", lambda e: e.tensor_tensor(out=t1[hp][:], in0=kk[hp][:], in1=a_[hp][:], op=ALU.mult),
                     r=["kk" + H, "a_" + H], w=["t1"])
                c.op("dve", lambda e: e.tensor_tensor(out=Bt[hp][:], in0=t1[hp][:], in1=Ginv[hp][:], op=ALU.mult),
                     r=["t1", "Ginv" + H], w=["Bt" + H])
                c.op("pool", lambda e: e.tensor_tensor(out=Kt[hp][:], in0=kmod[hp][:], in1=Ginv[hp][:], op=ALU.mult),
                     r=["kmod" + H, "Ginv" + H], w=["Kt" + H])
                c.op("pool", lambda e: e.tensor_copy(Vb[hp][:], v_s[hp][:]), r=["v_s" + H], w=["Vb" + H])
                c.op("pool", lambda e: e.tensor_copy(GCs[hp][:], G[hp][:].rearrange("p (c t) -> p c t", t=CH)[:, :, CH - 1]),
                     r=["G" + H], w=["GCs" + H])
                c.dma("sp", ARo[hp][:], ARt[hp][64:128, :], r=["ARt" + H], w=["ARo" + H])
                c.dma("sp", Bo[hp][:], Bt[hp][64:128, :], r=["Bt" + H], w=["Bo" + H])
                c.dma("sp", Ko[hp][:], Kt[hp][64:128, :], r=["Kt" + H], w=["Ko" + H])
                c.dma("sp", GCo[hp][:], GCs[hp][64:128, :], r=["GCs" + H], w=["GCo" + H])

            dbg(2)
            for gi in P2:
                Gk = str(gi)
                load_halo(Xp[gi], poolT, slice(gi * 128, (gi + 1) * 128), b, 128, "Xp" + Gk, halo=15)
                cur = Xp[gi]; curk = "Xp" + Gk
                for lv in range(4):
                    sh = 1 << lv
                    dst = plv[gi][lv]; dk = "plv%d_%d" % (gi, lv)
                    c.op("pool", lambda e: e.tensor_tensor(out=dst[:, sh:15 + TB], in0=cur[:, sh:15 + TB],
                                                           in1=cur[:, 0:15 + TB - sh], op=ALU.add), r=[curk], w=[dk])
                    cur, curk = dst, dk
                c.op("dve", lambda e: e.tensor_scalar(out=pacc[gi][:], in0=plv[gi][0][:, 15:15 + TB],
                                                      scalar1=psel[:, gi, 0:1], scalar2=None, op0=ALU.mult),
                     r=["plv%d_0" % gi, "psel"], w=["pacc" + Gk])
                for lv in range(1, 4):
                    c.op("dve", lambda e: e.scalar_tensor_tensor(out=pacc[gi][:], in0=plv[gi][lv][:, 15:15 + TB],
                                                                 scalar=psel[:, gi, lv:lv + 1], in1=pacc[gi][:],
                                                                 op0=ALU.mult, op1=ALU.add),
                         r=["plv%d_%d" % (gi, lv), "psel", "pacc" + Gk], w=["pacc" + Gk])
                if b == 0:
                    c.op("dve", lambda e: e.tensor_tensor(out=pm[gi][:], in0=pacc[gi][:], in1=invdiv[:, gi, :],
                                                          op=ALU.mult), r=["pacc" + Gk, "invdiv"], w=["pm" + Gk])
                    c.op("dve", lambda e: e.tensor_tensor(out=pd[gi][:], in0=pm[gi][:], in1=Xp[gi][:, 15:15 + TB],
                                                          op=ALU.subtract), r=["pm" + Gk, "Xp" + Gk], w=["pd" + Gk])
                else:
                    c.op("dve", lambda e: e.scalar_tensor_tensor(out=pd[gi][:], in0=pacc[gi][:],
                                                                 scalar=pv(29, gi), in1=Xp[gi][:, 15:15 + TB],
                                                                 op0=ALU.mult, op1=ALU.subtract),
                         r=["pacc" + Gk, "Xp" + Gk, "pvec"], w=["pd" + Gk])
                c.op("pe", lambda e: e.matmul(bk["D"][:], poolw[:, gi, :], pd[gi][:], start=True, stop=True),
                     r=["poolw", "pd" + Gk], w=["bkD"])
                c.op("act", lambda e: e.activation(out=po[gi][:], in_=bk["D"][:], func=AF.Identity, scale=pv(22, gi)),
                     r=["bkD", "pvec"], w=["po" + Gk])
                c.dma("sp", ypl[gi * 128:(gi + 1) * 128, t0:t0 + TB], po[gi][:], r=["po" + Gk])

            dbg(3)
            def emit_chunk(ci, cc, part):
                pp = cc % 2
                cs = slice(ci * CH, (ci + 1) * CH)
                tm = tokmaj[pp]; sa = SA[pp]; sbb = SB_[pp]; sc = SC[pp]
                tmk, sak, sbk, sck = "tokmaj%d" % pp, "SA%d" % pp, "SB%d" % pp, "SC%d" % pp
                TTF = TTFs[pp]; TTFk = "TTF%d" % pp
                def ARv(h, lo, hi):
                    hp_ = h // 2
                    t_ = ARt[hp_] if h % 2 == 0 else ARo[hp_]
                    return t_[0:64, ci * 128 + lo: ci * 128 + hi]
                def Bv(h):
                    hp_ = h // 2
                    return (Bt[hp_] if h % 2 == 0 else Bo[hp_])[0:64, cs]
                def Kv(h):
                    hp_ = h // 2
                    return (Kt[hp_] if h % 2 == 0 else Ko[hp_])[0:64, cs]
                ARk = lambda h: ("ARt%d" if h % 2 == 0 else "ARo%d") % (h // 2)
                Bk = lambda h: ("Bt%d" if h % 2 == 0 else "Bo%d") % (h // 2)
                Kk = lambda h: ("Kt%d" if h % 2 == 0 else "Ko%d") % (h // 2)
                if part == "I":
                    for hp in P2:
                        H = str(hp)
                        for j, (src, sk) in enumerate(((Bt[hp], "Bt" + H), (Kt[hp], "Kt" + H), (Vb[hp], "Vb" + H))):
                            c.op("pe", lambda e, src=src, j=j, hp=hp: e.transpose(
                                trp[0:64, j * 256 + hp * 128: j * 256 + (hp + 1) * 128], src[:, cs], identb[:]),
                                r=[sk, "identb"], w=["bktrp"])
                    c.op("act", lambda e: e.copy(out=tm[:], in_=trp[0:64, 0:768]), r=["bktrp"], w=[tmk])
                    yield
                    dbg(4)
                    for h in range(4):
                        c.op("pe", lambda e: e.matmul(bk["A"][0:64, h * 128:(h + 1) * 128], Bv(h), ARv(h, 0, 128),
                                                      start=True, stop=True), r=[Bk(h), ARk(h)], w=["bkA"])
                        c.op("pe", lambda e: e.matmul(bk["B"][0:64, h * 128:(h + 1) * 128], Kv(h), ARv(h, 0, 128),
                                                      start=True, stop=True), r=[Kk(h), ARk(h)], w=["bkB"])
                        c.op("pe", lambda e: e.matmul(bk["C"][0:64, h * 64:(h + 1) * 64], ARv(h, 0, 64), Bv(h),
                                                      start=True, stop=True), r=[Bk(h), ARk(h)], w=["bkC"])
                    c.op("dve", lambda e: e.tensor_tensor(out=sa[:], in0=bk["A"][0:64, :], in1=maskA[:], op=ALU.mult),
                         r=["bkA", "maskA"], w=[sak])
                    c.op("dve", lambda e: e.tensor_tensor(out=sbb[:], in0=bk["B"][0:64, :], in1=maskA[:], op=ALU.mult),
                         r=["bkB", "maskA"], w=[sbk])
                    c.op("dve", lambda e: e.tensor_tensor(out=sc[:], in0=bk["C"][0:64, 0:256], in1=maskC[:], op=ALU.mult),
                         r=["bkC", "maskC"], w=[sck])
                    yield
                    Nv = lambda h: sa[:, h * 128: h * 128 + 64]
                    NTv = lambda h: sc[:, h * 64:(h + 1) * 64]
                    dbg(5)
                    c.op("pool", lambda e: e.tensor_tensor(
                        out=TT[0][:].rearrange("p (h t) -> p h t", t=64),
                        in0=sa[:].rearrange("p (h x) -> p h x", x=128)[:, :, 0:64],
                        in1=identrep[:].rearrange("p (h t) -> p h t", t=64), op=ALU.add),
                        r=[sak, "identrep"], w=["TT0"])
                    for h in range(4):
                        c.op("pe", lambda e: e.matmul(bk["D"][0:64, h * 64:(h + 1) * 64], NTv(h), Nv(h), start=True, stop=True),
                             r=[sak, sck], w=["bkD"])
                        c.op("pe", lambda e: e.matmul(bk["D"][0:64, 256 + h * 64: 256 + (h + 1) * 64], Nv(h), NTv(h),
                                                      start=True, stop=True), r=[sak, sck], w=["bkD"])
                    c.op("act", lambda e: e.copy(out=PP[0][:], in_=bk["D"][0:64, :]), r=["bkD"], w=["PP0"])
                    yield
                    for lv in range(1, 6):
                        pi = (lv - 1) % 2; po_ = lv % 2
                        Pv = lambda h: PP[pi][:, h * 64:(h + 1) * 64]
                        PTv = lambda h: PP[pi][:, 256 + h * 64: 256 + (h + 1) * 64]
                        TTv = lambda h: TT[pi][:, h * 64:(h + 1) * 64]
                        for h in range(4):
                            c.op("pe", lambda e: e.matmul(bk["E"][0:64, h * 64:(h + 1) * 64], identb[0:64, 0:64], TTv(h),
                                                          start=True, stop=False), r=["identb", "TT%d" % pi], w=["bkE"])
                            c.op("pe", lambda e: e.matmul(bk["E"][0:64, h * 64:(h + 1) * 64], PTv(h), TTv(h),
                                                          start=False, stop=True), r=["PP%d" % pi, "TT%d" % pi], w=["bkE"])
                        if lv < 5:
                            for h in range(4):
                                c.op("pe", lambda e: e.matmul(bk["D"][0:64, h * 64:(h + 1) * 64], PTv(h), Pv(h),
                                                              start=True, stop=True), r=["PP%d" % pi], w=["bkD"])
                                c.op("pe", lambda e: e.matmul(bk["D"][0:64, 256 + h * 64: 256 + (h + 1) * 64], Pv(h), PTv(h),
                                                              start=True, stop=True), r=["PP%d" % pi], w=["bkD"])
                        if lv == 5:
                            c.op("dve", lambda e: e.tensor_copy(TTF[:], bk["E"][0:64, 0:256]), r=["bkE"], w=[TTFk])
                        else:
                            c.op("dve", lambda e: e.tensor_copy(TT[po_][:], bk["E"][0:64, 0:256]), r=["bkE"], w=["TT%d" % po_])
                        if lv < 5:
                            c.op("act", lambda e: e.copy(out=PP[po_][:], in_=bk["D"][0:64, :]), r=["bkD"], w=["PP%d" % po_])
                        yield
                if part == "D":
                    TTf = TTF; TTk = TTFk
                    Mold = [M0b[h][pp] for h in range(4)]; Mnew = [M0b[h][1 - pp] for h in range(4)]
                    Mok = ["M0b%d_%d" % (h, pp) for h in range(4)]; Mnk = ["M0b%d_%d" % (h, 1 - pp) for h in range(4)]
                    Uc = U[pp]; Uk = "U%d" % pp
                    for h in range(4):
                        c.op("pe", lambda e: e.matmul(bk["FG"][0:64, h * 64:(h + 1) * 64], ARv(h, 0, 64), Mold[h][:],
                                                      start=True, stop=False), r=[ARk(h), Mok[h]], w=["bkFG"])
                        c.op("pe", lambda e: e.matmul(bk["FG"][0:64, h * 64:(h + 1) * 64], sbb[:, h * 128: h * 128 + 64],
                                                      tm[:, 512 + h * 64: 512 + (h + 1) * 64], start=False, stop=True),
                             r=[sbk, tmk], w=["bkFG"])
                    c.op("act", lambda e: e.copy(out=W1[:], in_=bk["FG"][0:64, 0:256]), r=["bkFG"], w=["W1"])
                    yield
                    for h in range(4):
                        c.op("pe", lambda e: e.matmul(bk["FG"][0:64, 256 + h * 64: 256 + (h + 1) * 64],
                                                      TTf[:, h * 64:(h + 1) * 64], W1[:, h * 64:(h + 1) * 64],
                                                      start=True, stop=True), r=[TTk, "W1"], w=["bkFG"])
                    c.op("act", lambda e: e.copy(out=Uc[:], in_=bk["FG"][0:64, 256:512]), r=["bkFG"], w=[Uk])
                    yield
                    dbg(7)
                    for h in range(4):
                        hp, base = h // 2, (h % 2) * 64
                        o = bk["HI"][base:base + 64, hp * 64:(hp + 1) * 64]
                        c.op("pe", lambda e: e.matmul(o, Mold[h][:], ARv(h, 64, 128), start=True, stop=False),
                             r=[ARk(h), Mok[h]], w=["bkHI"])
                        c.op("pe", lambda e: e.matmul(o, Uc[:, h * 64:(h + 1) * 64], sa[:, h * 128 + 64:(h + 1) * 128],
                                                      start=False, stop=False), r=[Uk, sak], w=["bkHI"])
                        c.op("pe", lambda e: e.matmul(o, tm[:, 512 + h * 64: 512 + (h + 1) * 64],
                                                      sbb[:, h * 128 + 64:(h + 1) * 128], start=False, stop=True),
                             r=[tmk, sbk], w=["bkHI"])
                    for h in range(4):
                        o = bk["HI"][0:64, 256 + h * 64: 256 + (h + 1) * 64]
                        c.op("pe", lambda e: e.matmul(o, tm[:, 256 + h * 64: 256 + (h + 1) * 64],
                                                      tm[:, 512 + h * 64: 512 + (h + 1) * 64], start=True, stop=False),
                             r=[tmk], w=["bkHI"])
                        c.op("pe", lambda e: e.matmul(o, tm[:, h * 64:(h + 1) * 64], Uc[:, h * 64:(h + 1) * 64],
                                                      start=False, stop=False), r=[tmk, Uk], w=["bkHI"])
                        c.op("pe", lambda e: e.matmul(o, identb[0:64, 0:64], Mold[h][:],
                                                      start=False, stop=True), r=["identb", Mok[h]], w=["bkHI"])
                    yield
                    for hp in P2:
                        H = str(hp)
                        c.op("act", lambda e: e.copy(out=yraw[hp][:, cs], in_=bk["HI"][:, hp * 64:(hp + 1) * 64]),
                             r=["bkHI"], w=["yraw" + H])
                    for h in range(4):
                        hp = h // 2
                        gsrc, gk = (GCs[hp], "GCs%d" % hp) if h % 2 == 0 else (GCo[hp], "GCo%d" % hp)
                        c.op("act", lambda e: e.activation(out=Mnew[h][:], in_=bk["HI"][0:64, 256 + h * 64: 256 + (h + 1) * 64],
                                                           func=AF.Identity, scale=gsrc[0:64, ci:ci + 1]),
                             r=["bkHI", gk], w=[Mnk[h]])

            cc0 = chunk_counter[0]; chunk_counter[0] += CPB

            def drive(gens):
                gens = list(gens)
                while gens:
                    for g_ in list(gens):
                        try:
                            next(g_)
                        except StopIteration:
                            gens.remove(g_)

            drive([emit_chunk(0, cc0, "I")])
            for ci in range(CPB):
                gens = [emit_chunk(ci, cc0 + ci, "D")]
                if ci + 1 < CPB:
                    gens.insert(0, emit_chunk(ci + 1, cc0 + ci + 1, "I"))
                drive(gens)
            dbg(8)
            for hp in P2:
                H = str(hp)
                rows = slice(hp * 128, (hp + 1) * 128)
                c.op("pe", lambda e: e.matmul(bk["A"][:], bo[:], yraw[hp][:], start=True, stop=True),
                     r=["bo", "yraw" + H], w=["bkA"])
                c.op("dve", lambda e: e.scalar_tensor_tensor(out=yc[hp][:], in0=bk["A"][:], scalar=-1.0 / 64,
                                                             in1=yraw[hp][:], op0=ALU.mult, op1=ALU.add),
                     r=["bkA", "yraw" + H], w=["yc" + H])
                c.op("pool", lambda e: e.tensor_tensor(out=ysq[hp][:], in0=yc[hp][:], in1=yc[hp][:], op=ALU.mult),
                     r=["yc" + H], w=["ysq"])
                c.op("pe", lambda e: e.matmul(bk["B"][:], bo[:], ysq[hp][:], start=True, stop=True),
                     r=["bo", "ysq"], w=["bkB"])
                c.op("act", lambda e: e.activation(out=yrs[hp][:], in_=bk["B"][:], func=AF.Sqrt, bias=gneps[:, 0:1],
                                                   scale=1.0 / 64), r=["bkB", "gneps"], w=["yrs"])
                c.op("dve", lambda e: e.reciprocal(out=yrs[hp][:], in_=yrs[hp][:]), r=["yrs"], w=["yrs"])
                c.op("dve", lambda e: e.tensor_tensor(out=yc[hp][:], in0=yc[hp][:], in1=yrs[hp][:], op=ALU.mult),
                     r=["yc" + H, "yrs"], w=["yc" + H])
                c.op("dve", lambda e: e.tensor_scalar(out=yc[hp][:], in0=yc[hp][:], scalar1=pv(18, hp), scalar2=pv(20, hp),
                                                      op0=ALU.mult, op1=ALU.add), r=["yc" + H, "pvec"], w=["yc" + H])
                c.op("pool", lambda e: e.tensor_tensor(out=yc[hp][:], in0=yc[hp][:], in1=bonus[hp][:], op=ALU.add),
                     r=["yc" + H, "bonus" + H], w=["yc" + H])
                c.op("pool", lambda e: e.tensor_tensor(out=yo[hp][:], in0=yc[hp][:], in1=gg[hp][:], op=ALU.mult),
                     r=["yc" + H, "gg" + H], w=["yo" + H])
                c.dma("sp", yrw[rows, t0:t0 + TB], yo[hp][:], r=["yo" + H])
    try:
        body()
    except _Stop:
        pass


def build_L2(layer1):
    nc = bass.Bass("TRN2", target_bir_lowering=False)
    dt = lambda name, shape, kind="ExternalInput": nc.dram_tensor(name, list(shape), F32, kind=kind).ap()
    A = dict(rT=dt("rT", [256, SEQ]), kT=dt("kT", [256, SEQ]), vT=dt("vT", [256, SEQ]),
             waT=dt("waT", [128, SEQ]), gdT=dt("gdT", [128, SEQ]), poolT=dt("poolT", [256, SEQ]))
    if layer1:
        A.update(vdT=dt("vdT", [32, SEQ]), vfT=dt("vfT", [256, SEQ]), vup=dt("vup", [32, 256]))
    A.update(pvec=dt("pvec", [128, 32]), wup=dt("wup", [64, 256]), aup=dt("aup", [64, 256]),
             gup=dt("gup", [128, 256]), poolw=dt("poolw", [2, 128, 128]),
             maskA=dt("maskA", [64, 512]), maskC=dt("maskC", [64, 256]), identrep=dt("identrep", [64, 256]),
             scanmask=dt("scanmask", [128, TB]), blockones=dt("blockones", [128, 128]), ident=dt("ident", [128, 128]),
             invdiv=dt("invdiv", [128, 2, TB]), psel=dt("psel", [128, 2, 4]))
    yT = dt("yT", [512, SEQ], "ExternalOutput")
    A["yrw"] = yT[0:256]; A["ypl"] = yT[256:512]
    if not layer1:
        A["vout"] = dt("vout", [256, SEQ], "ExternalOutput")
    c = Ctx(nc)
    c.begin_phase("")
    emit_L2(c, A, layer1)
    c.end_phase()
    c.finish()
    return nc


def l2_inputs(inp, l, g, projT, vfT):
    f = lambda a: np.ascontiguousarray(a, dtype=np.float32)
    mu = inp["mu_shift"][l]
    cs = slice(g * 256, (g + 1) * 256)
    pvec = np.zeros((128, 32), np.float32)
    def put(col, vec512):
        v = np.asarray(vec512)[cs].reshape(2, 128)
        pvec[:, col] = v[0]; pvec[:, col + 1] = v[1]
    put(0, mu[0:512]); put(2, mu[512:1024]); put(4, mu[1024:1536])
    put(6, inp["w0"][l]); put(8, inp["a0"][l]); put(10, inp["k_k"][l]); put(12, inp["k_a"][l])
    put(16, inp["r_k"][l].reshape(512)); put(18, inp["lnx_g"][l]); put(20, inp["lnx_b"][l])
    put(22, inp["pool_scale"][l])
    pvec[:, 24] = mu[1664:1792]; pvec[0:64, 25] = mu[1536:1600]; pvec[64:128, 25] = mu[1600:1664]
    d = dict(rT=f(projT[0:512][cs]), kT=f(projT[512:1024][cs]), vT=f(projT[1024:1536][cs]),
             waT=f(projT[1536:1664]), gdT=f(projT[1664:1792]), poolT=f(projT[1792:2304][cs]),
             wup=f(inp["w_up"][l][:, cs]), aup=f(inp["a_up"][l][:, cs]), gup=f(inp["g_up"][l][:, cs]),
             poolw=f(inp["pool_w"][l][2 * g:2 * g + 2]))
    if l > 0:
        pvec[0:32, 26] = inp["vres_mu"][l - 1]
        put(27, inp["vres_v0"][l - 1])
        d.update(vdT=f(projT[2304:2336]), vfT=f(vfT), vup=f(inp["vres_up"][l - 1][:, cs]))
    d["pvec"] = pvec
    pos = np.arange(1, TB + 1, dtype=np.float32)
    invdiv = np.stack([np.broadcast_to(1.0 / np.minimum(pos, float(POOL_WINDOWS[2 * g + gi])), (128, TB))
                       for gi in range(2)], axis=1)
    d["invdiv"] = f(invdiv)
    psel = np.zeros((128, 2, 4), np.float32)
    for gi in range(2):
        psel[:, gi, 2 * g + gi] = 1.0
        pvec[:, 29 + gi] = 1.0 / POOL_WINDOWS[2 * g + gi]
    d["psel"] = psel
    d.update(l2_consts())
    return d


EPC = 2048
def emit_L0(c, A, layers, nch):
    u_d, v_d, wo_d, pq_d, id_d = A["u"], A["v"], A["w_out"], A["peer_q"], A["ident"]
    ut_o, vb_o, wo_o, pq_o = A["UT"], A["Vb"], A["woutb"], A["pqb"]
    identf = c.sb("identf", [128, 128]); identb = c.sb("identb", [128, 128], BF16)
    NBUF0 = 4
    uf = [c.sb("uf%d" % i, [128, D]) for i in range(NBUF0)]
    ub = [c.sb("ub%d" % i, [128, D], BF16) for i in range(NBUF0)]
    uT = [c.sb("uT%d" % i, [128, D], BF16) for i in range(NBUF0)]
    vb = [c.sb("vb%d" % i, [128, D], BF16) for i in range(NBUF0)]
    wb = [c.sb("wb%d" % i, [128, 8, 128], BF16) for i in range(NBUF0)]
    tps = [c.ps("tps%d" % i, [128, 1024], BF16) for i in range(NBUF0)]
    c.dma("sp", identf[:], id_d, w=["identf"])
    c.op("dve", lambda e: e.tensor_copy(identb[:], identf[:]), r=["identf"], w=["identb"])
    k = 0
    for l in layers:
        for ch in range(nch):
            s_ = k % NBUF0; k += 1
            S = str(s_)
            rows = slice(ch * 128, (ch + 1) * 128)
            c.dma("sp", uf[s_][:], u_d[l, rows, :], w=["uf" + S])
            c.dma("pool", vb[s_][:], v_d[l, rows, :], w=["vb" + S])
            c.dma("sp", vb_o[l, rows, :], vb[s_][:], r=["vb" + S])
            eng = "act" if s_ % 2 == 0 else "dve"
            if eng == "act":
                c.op("act", lambda e: e.copy(out=ub[s_][:], in_=uf[s_][:]), r=["uf" + S], w=["ub" + S])
            else:
                c.op("dve", lambda e: e.tensor_copy(ub[s_][:], uf[s_][:]), r=["uf" + S], w=["ub" + S])
            for dc in range(8):
                c.op("pe", lambda e: e.transpose(tps[s_][:, dc * 128:(dc + 1) * 128], ub[s_][:, dc * 128:(dc + 1) * 128],
                                                 identb[:]), r=["ub" + S, "identb"], w=["tps" + S])
            if eng == "act":
                c.op("act", lambda e: e.copy(out=uT[s_][:], in_=tps[s_][:]), r=["tps" + S], w=["uT" + S])
            else:
                c.op("dve", lambda e: e.tensor_copy(uT[s_][:], tps[s_][:]), r=["tps" + S], w=["uT" + S])
            c.dma("sp", ut_o[l, ch], uT[s_][:].rearrange("p (dc e) -> p dc e", e=128), r=["uT" + S])
        for cc in range(8):
            s_ = k % NBUF0; k += 1
            S = str(s_)
            c.dma("pool", vb[s_][:], wo_d[l, cc * 128:(cc + 1) * 128, :], w=["vb" + S])
            c.dma("sp", wo_o[l, cc], vb[s_][:], r=["vb" + S])
        pqv = pq_d[l].rearrange("(dc p) j -> p dc j", p=128)
        for jc in range(16):
            s_ = k % NBUF0; k += 1
            S = str(s_)
            c.dma("pool", wb[s_][:], pqv[:, :, jc * 128:(jc + 1) * 128], w=["wb" + S])
            c.dma("sp", pq_o[l, jc], wb[s_][:], r=["wb" + S])


def build_L0():
    nc = bass.Bass("TRN2", target_bir_lowering=False)
    di = lambda name, shape: nc.dram_tensor(name, list(shape), F32, kind="ExternalInput").ap()
    do = lambda name, shape: nc.dram_tensor(name, list(shape), BF16, kind="ExternalOutput").ap()
    A = dict(u=di("u", [2, EPC, D]), v=di("v", [2, EPC, D]), w_out=di("w_out", [2, D, D]),
             peer_q=di("peer_q", [2, D, 2048]), ident=di("ident", [128, 128]),
             UT=do("UT", [2, EPC // 128, 128, 8, 128]), Vb=do("Vb", [2, EPC, D]),
             woutb=do("woutb", [2, 8, 128, D]), pqb=do("pqb", [2, 16, 128, 8, 128]))
    c = Ctx(nc)
    c.begin_phase("")
    emit_L0(c, A, range(2), EPC // 128)
    c.end_phase()
    c.finish()
    return nc


TS = 256
NST = TOK // TS
NEG = -1.0e30


def emit_L3(c, A, final):
    nc = c.nc
    x_d, c_d, adaw_d, adab_d, g_d, lnf_d = A["x"], A["c_fm"], A["ada_w"], A["ada_b_fm"], A["ln_g_fm"], A["lnf_fm"]
    wo_d, pq_d, keys_d, ut_d, vb_d, id_d, ones_d, xo_d = (A["woutb"], A["pqb"], A["keys"], A["UT"], A["Vb"],
                                                          A["ident"], A["ones"], A["xo"])
    ysel = "hsel" in A
    sb = c.sb
    c.adaw_sb = sb("adaw", [128, 8, 256])
    c_sb = sb("c_sb", [128, 8]); sig_c = sb("sig_c", [128, 8]); silu_c = sb("silu_c", [128, 8])
    adab = sb("adab", [128, 48]); lng = sb("lng", [128, 8]); lnf = sb("lnf", [128, 8])
    modT = sb("modT", [128, 32]); effs = sb("effs", [128, 8]); epsb = sb("epsb", [128, 1])
    identf = sb("identf", [128, 128]); identb = sb("identb", [128, 128], BF16); onesf = sb("onesf", [128, 128])
    diag = sb("diag", [128, 512])
    g1bc = sb("g1bc", [128, D]); g2bc = sb("g2bc", [128, D]); lnfbc = sb("lnfbc", [128, D])
    keysT = sb("keysT", [128, 16, 128], BF16)
    wst = [sb("wst%d" % i, [128, D], BF16) for i in range(3)]
    x1 = [sb("x1_%d" % i, [128, D]) for i in range(2)]
    yTb = sb("yTb", [128, 8, TS], BF16)
    if ysel:
        ysa = sb("ysa", [128, 8, TS], BF16); ysb = sb("ysb", [128, 8, TS], BF16); hsel = sb("hsel_sb", [128, 2])
    junk = sb("junk", [128, D], BF16); xn = sb("xn", [128, D], BF16)
    ssq = sb("ssq", [128, 4]); rstd = sb("rstd", [128, 4])
    h2T = sb("h2T", [128, 8, TS], BF16); qT = sb("qT", [128, 16, TS], BF16)
    ST = sb("ST", [128, 16, TS]); SC = sb("SC", [128, 2048])
    kf = SC[:].rearrange("p (a d) -> p a d", d=128)
    wk = [sb("wk%d" % i, [128, 128]) for i in range(2)]
    stop = sb("stop", [128, 16, 16]); candh = [sb("candh%d" % i, [128, 256]) for i in range(2)]
    cwk = [sb("cwk%d" % i, [128, 256]) for i in range(2)]; ctop = sb("ctop", [128, 8, 16])
    negm = sb("negm", [128, 8]); esub = sb("esub", [128, 128]); exs = sb("exs", [128, 128])
    Zs = sb("Zs", [128, 8]); invZ = sb("invZ", [128, 8])
    PACK = sb("PACK", [128, 4, 128]); PKT = sb("PKT", [128, 4, TS])
    rep = [sb("rep%d" % i, [128, 256]) for i in range(3)]
    E0 = [sb("E0_%d" % i, [128, 128], BF16) for i in range(3)]
    D1 = [sb("D1_%d" % i, [128, 128], BF16) for i in range(3)]
    ex1 = [sb("ex1_%d" % i, [128, 128]) for i in range(3)]
    gate = sb("gate_all", [128, 128, TS], BF16)
    ut4 = [sb("ut4_%d" % i, [128, 2, D], BF16) for i in range(3)]
    vt4 = [sb("vt4_%d" % i, [128, 2, D], BF16) for i in range(3)]
    NCH = 2
    ge = [sb("ge%d" % i, [128, TS], BF16) for i in range(2)]
    AT = [sb("AT%d" % i, [128, TS], BF16) for i in range(2)]
    tmpo = sb("tmpo", [128, 512])
    B = [c.ps("b%d" % i, [128, 512]) for i in range(8)]
    Bb = c.ps

    ld = lambda dst, src, key: c.dma("sp", dst, src, w=[key])
    ld(c_sb[:], c_d, "c_sb"); ld(adab[:], adab_d, "adab"); ld(lng[:], g_d, "lng"); ld(lnf[:], lnf_d, "lnf")
    ld(identf[:], id_d, "identf"); ld(onesf[:], ones_d, "onesf")
    if ysel:
        ld(hsel[:], A["hsel"], "hsel")
    ld(kf, keys_d.rearrange("h p k d -> k (h p) d"), "kf")
    c.op("dve", lambda e: e.tensor_copy(identb[:], identf[:]), r=["identf"], w=["identb"])
    c.op("dve", lambda e: e.memset(epsb[:], NORM_EPS), w=["epsb"])
    c.op("act", lambda e: e.activation(out=sig_c[:], in_=c_sb[:], func=AF.Sigmoid), r=["c_sb"], w=["sig_c"])
    c.op("dve", lambda e: e.tensor_tensor(out=silu_c[:], in0=c_sb[:], in1=sig_c[:], op=ALU.mult),
         r=["c_sb", "sig_c"], w=["silu_c"])
    emit_mod_fm(c, adaw_d, 0, 32, silu_c, adab, 16, modT[:, 0:32], B[0], "modT")
    c.op("dve", lambda e: e.scalar_tensor_tensor(out=effs[:], in0=modT[:, 16:24], scalar=1.0, in1=lng[:],
                                                  op0=ALU.add, op1=ALU.mult), r=["modT", "lng"], w=["effs"])

    def bcast_rows(vec8, vkey, out_tile, okey):
        for hf in range(2):
            for q in range(4):
                jc = hf * 4 + q
                c.op("dve", lambda e: e.tensor_scalar(out=diag[:, q * 128:(q + 1) * 128], in0=identf[:],
                                                      scalar1=vec8[:, jc:jc + 1], scalar2=None, op0=ALU.mult),
                     r=["identf", vkey], w=["diag%d" % q])
                c.op("pe", lambda e: e.matmul(B[1][:, q * 128:(q + 1) * 128], onesf[:], diag[:, q * 128:(q + 1) * 128],
                                              start=True, stop=True), r=["onesf", "diag%d" % q], w=["b1"])
            c.op("act", lambda e: e.copy(out=out_tile[:, hf * 512:(hf + 1) * 512], in_=B[1][:]), r=["b1"], w=[okey])

    bcast_rows(modT[:, 0:8], "modT", g1bc, "g1bc")
    bcast_rows(modT[:, 24:32], "modT", g2bc, "g2bc")
    if final:
        bcast_rows(lnf, "lnf", lnfbc, "lnfbc")
    for q4 in range(4):
        for q in range(4):
            hp16 = q4 * 4 + q
            c.op("pe", lambda e: e.transpose(B[2][:, q * 128:(q + 1) * 128], kf[:, hp16, :], identf[:]),
                 r=["kf", "identf"], w=["b2"])
        c.op("act", lambda e: e.copy(out=keysT[:, q4 * 4:(q4 + 1) * 4, :],
                                     in_=B[2][:].rearrange("p (q k) -> p q k", k=128)), r=["b2"], w=["keysT"])
    wsi = [0]

    def wstream(src_ap):
        i = wsi[0] % 3; wsi[0] += 1
        c.dma("sp", wst[i][:], src_ap, w=["wst%d" % i])
        return wst[i], "wst%d" % i

    def body():
        dbg(1)
        for st in range(DBG_NST or NST):
            tok0 = st * TS
            if not ysel:
                c.dma("pool", yTb[:], A["yT"].rearrange("(cc p) t -> p cc t", p=128)[:, :, tok0:tok0 + TS], w=["yTb"])
            else:
                c.dma("pool", ysa[:], A["yA"].rearrange("(cc p) t -> p cc t", p=128)[:, :, tok0:tok0 + TS], w=["ysa"])
                c.dma("pool", ysb[:], A["yB"].rearrange("(cc p) t -> p cc t", p=128)[:, :, tok0:tok0 + TS], w=["ysb"])
                c.op("dve", lambda e: e.tensor_scalar(out=ysa[:], in0=ysa[:], scalar1=hsel[:, 0:1], scalar2=None,
                                                      op0=ALU.mult), r=["ysa", "hsel"], w=["ysa"])
                c.op("dve", lambda e: e.scalar_tensor_tensor(out=yTb[:], in0=ysb[:], scalar=hsel[:, 1:2], in1=ysa[:],
                                                             op0=ALU.mult, op1=ALU.add),
                     r=["ysa", "ysb", "hsel"], w=["yTb"])
            wts = []
            for tt in range(2):
                X = x1[tt]; Xk = "x1_%d" % tt
                c.dma("sp", X[:], x_d[tok0 + tt * 128: tok0 + (tt + 1) * 128, :], w=[Xk])
                for cc in range(8):
                    wt, wkk = wstream(wo_d[cc])
                    for hf in range(2):
                        c.op("pe", lambda e: e.matmul(B[hf][:], yTb[:, cc, tt * 128:(tt + 1) * 128],
                                                      wt[:, hf * 512:(hf + 1) * 512], start=(cc == 0), stop=(cc == 7)),
                             r=["yTb", wkk], w=["b%d" % hf])
                for hf in range(2):
                    c.op("dve", lambda e: e.tensor_tensor(out=tmpo[:], in0=B[hf][:], in1=g1bc[:, hf * 512:(hf + 1) * 512],
                                                          op=ALU.mult), r=["b%d" % hf, "g1bc"], w=["tmpo"])
                    c.op("dve", lambda e: e.tensor_tensor(out=X[:, hf * 512:(hf + 1) * 512], in0=tmpo[:],
                                                          in1=X[:, hf * 512:(hf + 1) * 512], op=ALU.add),
                         r=["tmpo", Xk], w=[Xk])
                c.op("dve", lambda e: e.memset(ssq[:, tt:tt + 1], 0.0), w=["ssq"])
                c.op("act", lambda e: e.activation(out=junk[:], in_=X[:], func=AF.Square, accum_out=ssq[:, tt:tt + 1]),
                     r=[Xk, "ssq"], w=["junk", "ssq"])
                c.op("act", lambda e: e.activation(out=rstd[:, tt:tt + 1], in_=ssq[:, tt:tt + 1], func=AF.Sqrt,
                                                   bias=epsb[:, 0:1], scale=1.0 / D), r=["ssq", "epsb"], w=["rstd"])
                c.op("dve", lambda e: e.reciprocal(out=rstd[:, tt:tt + 1], in_=rstd[:, tt:tt + 1]), r=["rstd"], w=["rstd"])
                c.op("dve", lambda e: e.tensor_scalar(out=xn[:], in0=X[:], scalar1=rstd[:, tt:tt + 1], scalar2=None,
                                                      op0=ALU.mult), r=[Xk, "rstd"], w=["xn"])
                tpb = B[2 + tt][:].bitcast(BF16)
                for dc in range(8):
                    c.op("pe", lambda e: e.transpose(tpb[:, dc * 128:(dc + 1) * 128], xn[:, dc * 128:(dc + 1) * 128],
                                                     identb[:]), r=["xn", "identb"], w=["b%d" % (2 + tt)])
                for dc in range(8):
                    c.op("act", lambda e: e.activation(out=h2T[:, dc, tt * 128:(tt + 1) * 128],
                                                       in_=tpb[:, dc * 128:(dc + 1) * 128], func=AF.Identity,
                                                       bias=modT[:, 8 + dc: 9 + dc], scale=effs[:, dc:dc + 1]),
                         r=["b%d" % (2 + tt), "modT", "effs"], w=["h2T"])
            dbg(2)
            for hp16 in range(16):
                wt, wkk = wstream(pq_d[hp16].rearrange("p dc j -> p (dc j)"))
                bi = 4 + (hp16 // 2) % 2
                sub = hp16 % 2
                for dc in range(8):
                    c.op("pe", lambda e: e.matmul(B[bi][:, sub * TS:(sub + 1) * TS], wt[:, dc * 128:(dc + 1) * 128],
                                                  h2T[:, dc, :], start=(dc == 0), stop=(dc == 7)),
                         r=[wkk, "h2T"], w=["b%d" % bi])
                if sub == 1:
                    c.op("act", lambda e: e.copy(out=qT[:, hp16 - 1: hp16 + 1, :],
                                                 in_=B[bi][:].rearrange("p (s t) -> p s t", t=TS)),
                         r=["b%d" % bi], w=["qT"])
            for hp16 in range(16):
                bi = 6 + (hp16 // 2) % 2
                sub = hp16 % 2
                c.op("pe", lambda e: e.matmul(B[bi][:, sub * TS:(sub + 1) * TS], keysT[:, hp16, :], qT[:, hp16, :],
                                              start=True, stop=True), r=["keysT", "qT"], w=["b%d" % bi])
                if sub == 1:
                    c.op("dve", lambda e: e.tensor_copy(ST[:, hp16 - 1: hp16 + 1, :],
                                                        B[bi][:].rearrange("p (s t) -> p s t", t=TS)),
                         r=["b%d" % bi], w=["ST"])
            dbg(3)
            stop4 = stop[:].rearrange("p (h two) a -> p h two a", two=2)
            for tt in range(2):
                for q4 in range(4):
                    for q in range(4):
                        hp16 = q4 * 4 + q
                        c.op("pe", lambda e: e.transpose(B[q4][:, q * 128:(q + 1) * 128],
                                                         ST[:, hp16, tt * 128:(tt + 1) * 128], identf[:]),
                             r=["ST", "identf"], w=["b%d" % q4])
                    c.op("act", lambda e: e.copy(out=SC[:, q4 * 512:(q4 + 1) * 512], in_=B[q4][:]),
                         r=["b%d" % q4], w=["SC%d" % q4, "kf"])
                for hp16 in range(16):
                    w_ = wk[hp16 % 2]; wkk = "wk%d" % (hp16 % 2)
                    scv = SC[:, hp16 * 128:(hp16 + 1) * 128]; sck = "SC%d" % (hp16 // 4)
                    c.op("dve", lambda e: e.max(out=stop[:, hp16, 0:8], in_=scv), r=[sck], w=["stopA%d" % hp16])
                    c.op("dve", lambda e: e.match_replace(out=w_[:], in_to_replace=stop[:, hp16, 0:8], in_values=scv,
                                                          imm_value=NEG), r=[sck, "stopA%d" % hp16], w=[wkk])
                    c.op("dve", lambda e: e.max(out=stop[:, hp16, 8:16], in_=w_[:]), r=[wkk], w=["stopB%d" % hp16])
                stopkeys = ["stopA%d" % i for i in range(16)] + ["stopB%d" % i for i in range(16)]
                for h in range(8):
                    ch_ = candh[h % 2]; chk = "candh%d" % (h % 2)
                    cw_ = cwk[h % 2]; cwkk = "cwk%d" % (h % 2)
                    c.op("dve", lambda e: e.tensor_tensor(
                        out=ch_[:].rearrange("p (a b) -> p a b", b=16),
                        in0=stop4[:, h, 0, :].unsqueeze(2).broadcast_to([128, 16, 16]),
                        in1=stop4[:, h, 1, :].unsqueeze(1).broadcast_to([128, 16, 16]), op=ALU.add),
                        r=["stopA%d" % (2 * h), "stopB%d" % (2 * h), "stopA%d" % (2 * h + 1), "stopB%d" % (2 * h + 1)],
                        w=[chk])
                    c.op("dve", lambda e: e.max(out=ctop[:, h, 0:8], in_=ch_[:]), r=[chk], w=["ctopA%d" % h])
                    c.op("dve", lambda e: e.match_replace(out=cw_[:], in_to_replace=ctop[:, h, 0:8], in_values=ch_[:],
                                                          imm_value=NEG), r=[chk, "ctopA%d" % h], w=[cwkk])
                    c.op("dve", lambda e: e.max(out=ctop[:, h, 8:16], in_=cw_[:]), r=[cwkk], w=["ctopB%d" % h])
                ctk = ["ctopA%d" % h for h in range(8)] + ["ctopB%d" % h for h in range(8)]
                c.op("dve", lambda e: e.tensor_scalar(out=negm[:], in0=ctop[:, :, 0], scalar1=-1.0, scalar2=None,
                                                      op0=ALU.mult), r=ctk, w=["negm"])
                c.op("dve", lambda e: e.tensor_tensor(out=esub[:].rearrange("p (h a) -> p h a", a=16), in0=ctop[:],
                                                      in1=negm[:].unsqueeze(2).broadcast_to([128, 8, 16]), op=ALU.add),
                     r=ctk + ["negm"], w=["esub"])
                c.op("act", lambda e: e.activation(out=exs[:], in_=esub[:], func=AF.Exp), r=["esub"], w=["exs"])
                c.op("dve", lambda e: e.reduce_sum(out=Zs[:], in_=exs[:].rearrange("p (h a) -> p h a", a=16),
                                                   axis=mybir.AxisListType.X), r=["exs"], w=["Zs"])
                c.op("dve", lambda e: e.reciprocal(out=invZ[:], in_=Zs[:]), r=["Zs"], w=["invZ"])
                P3 = lambda j: PACK[:, j, :].rearrange("p (h a) -> p h a", a=16)
                c.op("dve", lambda e: e.tensor_copy(P3(0), stop4[:, :, 0, :]), r=stopkeys, w=["PACK0"])
                c.op("dve", lambda e: e.tensor_tensor(out=P3(1), in0=ctop[:, :, 15].unsqueeze(2).broadcast_to([128, 8, 16]),
                                                      in1=stop4[:, :, 0, :], op=ALU.subtract), r=stopkeys + ctk, w=["PACK1"])
                c.op("dve", lambda e: e.tensor_tensor(out=P3(2), in0=stop4[:, :, 0, :],
                                                      in1=negm[:].unsqueeze(2).broadcast_to([128, 8, 16]), op=ALU.add),
                     r=stopkeys + ["negm"], w=["PACK2"])
                c.op("dve", lambda e: e.tensor_copy(P3(3), invZ[:].unsqueeze(2).broadcast_to([128, 8, 16])),
                     r=["invZ"], w=["PACK3"])
                for j in range(4):
                    c.op("pe", lambda e: e.transpose(B[4][:, j * 128:(j + 1) * 128], PACK[:, j, :], identf[:]),
                         r=["PACK%d" % j, "identf"], w=["b4"])
                c.op("act", lambda e: e.copy(out=PKT[:, :, tt * 128:(tt + 1) * 128],
                                             in_=B[4][:].rearrange("p (j t) -> p j t", t=128)), r=["b4"], w=["PKT"])
            dbg(4)
            def s6A(t):
                s_ = t % 3; S = str(s_)
                c.op("pool", lambda e: e.tensor_copy(
                    rep[s_][:].rearrange("p (two h a) -> p two h a", two=2, a=16),
                    ST[:, :, t].rearrange("p (h two) -> p two h", two=2).unsqueeze(3).broadcast_to([128, 2, 8, 16])),
                    r=["ST"], w=["rep" + S])
                c.op("pe", lambda e: e.transpose(B[0 + s_][:, 0:128], rep[s_][:, 0:128], identf[:]),
                     r=["rep" + S, "identf"], w=["b%d" % (0 + s_)])
                c.op("pe", lambda e: e.transpose(B[3 + s_][:, 0:128], rep[s_][:, 128:256], identf[:]),
                     r=["rep" + S, "identf"], w=["b%d" % (3 + s_)])

            def s6B(t):
                s_ = t % 3; S = str(s_)
                c.op("dve", lambda e: e.tensor_scalar(out=E0[s_][:], in0=B[0 + s_][:, 0:128], scalar1=PKT[:, 0, t:t + 1],
                                                      scalar2=PKT[:, 3, t:t + 1], op0=ALU.is_equal, op1=ALU.mult),
                     r=["b%d" % (0 + s_), "PKT"], w=["E0_" + S])
                c.op("act", lambda e: e.activation(out=ex1[s_][:], in_=B[3 + s_][:, 0:128], func=AF.Exp,
                                                   bias=PKT[:, 2, t:t + 1]), r=["b%d" % (3 + s_), "PKT"], w=["ex1_" + S])
                c.op("dve", lambda e: e.scalar_tensor_tensor(out=D1[s_][:], in0=B[3 + s_][:, 0:128],
                                                             scalar=PKT[:, 1, t:t + 1], in1=ex1[s_][:],
                                                             op0=ALU.is_ge, op1=ALU.mult),
                     r=["b%d" % (3 + s_), "PKT", "ex1_" + S], w=["D1_" + S])

            def s6C(t):
                s_ = t % 3; S = str(s_)
                gs = (t // 4) % 2
                c.op("pe", lambda e: e.matmul(B[6 + gs][:, (t % 4) * 128:(t % 4 + 1) * 128], D1[s_][:], E0[s_][:],
                                              start=True, stop=True), r=["D1_" + S, "E0_" + S], w=["b%d" % (6 + gs)])
                if t % 4 == 3:
                    c.op("act", lambda e: e.copy(out=gate[:, :, t - 3:t + 1].rearrange("p i t -> p t i"),
                                                 in_=B[6 + gs][:].rearrange("p (t i) -> p t i", i=128)),
                         r=["b%d" % (6 + gs)], w=["gate"])

            s6A(0)
            s6A(1)
            for t in range(TS):
                if t + 2 < TS:
                    s6A(t + 2)
                s6B(t)
                s6C(t)
            dbg(5)
            def s7U(i0):
                g4, cix = i0 // NCH, i0 % NCH
                bf_ = g4 % 3; Bf = str(bf_)
                if cix == 0:
                    c.dma("sp", ut4[bf_][:], ut_d[g4 * NCH:(g4 + 1) * NCH].rearrange("c p dc e -> p c (dc e)"),
                          w=["ut4_" + Bf])
                    c.dma("sp", vt4[bf_][:],
                          vb_d[g4 * NCH * 128:(g4 + 1) * NCH * 128, :].rearrange("(c p) d -> p c d", p=128),
                          w=["vt4_" + Bf])
                pb = 4 + i0 % 4
                for dc in range(8):
                    c.op("pe", lambda e: e.matmul(B[pb][:, 0:TS], ut4[bf_][:, cix, dc * 128:(dc + 1) * 128],
                                                  h2T[:, dc, :], start=(dc == 0), stop=(dc == 7)),
                         r=["ut4_" + Bf, "h2T"], w=["b%d" % pb])

            def s7V(i0):
                g4, cix = i0 // NCH, i0 % NCH
                bf_ = g4 % 3; Bf = str(bf_)
                pb = 4 + i0 % 4
                gb = i0 % 2
                c.op("act", lambda e: e.activation(out=ge[gb][:], in_=B[pb][:, 0:TS], func=AF.Gelu),
                     r=["b%d" % pb], w=["ge%d" % gb])
                c.op("pool", lambda e: e.tensor_tensor(out=AT[gb][:], in0=ge[gb][:], in1=gate[:, i0, :], op=ALU.mult),
                     r=["ge%d" % gb, "gate"], w=["AT%d" % gb])
                for tt in range(2):
                    for hf in range(2):
                        c.op("pe", lambda e: e.matmul(B[tt * 2 + hf][:], AT[gb][:, tt * 128:(tt + 1) * 128],
                                                      vt4[bf_][:, cix, hf * 512:(hf + 1) * 512],
                                                      start=(i0 == 0), stop=(i0 == 127)),
                             r=["AT%d" % gb, "vt4_" + Bf], w=["b%d" % (tt * 2 + hf)])

            s7U(0)
            s7U(1)
            for i0 in range(128):
                if i0 + 2 < 128:
                    s7U(i0 + 2)
                s7V(i0)
            dbg(6)
            for tt in range(2):
                X = x1[tt]; Xk = "x1_%d" % tt
                for hf in range(2):
                    c.op("dve", lambda e: e.tensor_tensor(out=tmpo[:], in0=B[tt * 2 + hf][:],
                                                          in1=g2bc[:, hf * 512:(hf + 1) * 512], op=ALU.mult),
                         r=["b%d" % (tt * 2 + hf), "g2bc"], w=["tmpo"])
                    c.op("dve", lambda e: e.tensor_tensor(out=X[:, hf * 512:(hf + 1) * 512], in0=tmpo[:],
                                                          in1=X[:, hf * 512:(hf + 1) * 512], op=ALU.add),
                         r=["tmpo", Xk], w=[Xk])
                if final:
                    c.op("dve", lambda e: e.memset(ssq[:, 2 + tt:3 + tt], 0.0), w=["ssq"])
                    c.op("act", lambda e: e.activation(out=junk[:], in_=X[:], func=AF.Square,
                                                       accum_out=ssq[:, 2 + tt:3 + tt]), r=[Xk, "ssq"], w=["junk", "ssq"])
                    c.op("act", lambda e: e.activation(out=rstd[:, 2 + tt:3 + tt], in_=ssq[:, 2 + tt:3 + tt], func=AF.Sqrt,
                                                       bias=epsb[:, 0:1], scale=1.0 / D), r=["ssq", "epsb"], w=["rstd"])
                    c.op("dve", lambda e: e.reciprocal(out=rstd[:, 2 + tt:3 + tt], in_=rstd[:, 2 + tt:3 + tt]),
                         r=["rstd"], w=["rstd"])
                    c.op("dve", lambda e: e.scalar_tensor_tensor(out=X[:], in0=X[:], scalar=rstd[:, 2 + tt:3 + tt],
                                                                 in1=lnfbc[:], op0=ALU.mult, op1=ALU.mult),
                         r=[Xk, "rstd", "lnfbc"], w=[Xk])
                c.dma("sp", xo_d[tok0 + tt * 128: tok0 + (tt + 1) * 128, :], X[:], r=[Xk])

    try:
        body()
    except _Stop:
        pass


def build_L3(final):
    nc = bass.Bass("TRN2", target_bir_lowering=False)
    dt = lambda name, shape, d=F32, kind="ExternalInput": nc.dram_tensor(name, list(shape), d, kind=kind).ap()
    A = dict(x=dt("x", [TOK, D]), yT=dt("yT", [D, TOK]), c_fm=dt("c_fm", [128, 8]), ada_w=dt("ada_w", [D, 4 * D]),
             ada_b_fm=dt("ada_b_fm", [128, 48]), ln_g_fm=dt("ln_g_fm", [128, 8]), lnf_fm=dt("lnf_fm", [128, 8]),
             woutb=dt("woutb", [8, 128, D], BF16), pqb=dt("pqb", [16, 128, 8, 128], BF16),
             keys=dt("keys", [8, 2, 128, 128]), UT=dt("UT", [128, 128, 8, 128], BF16), Vb=dt("Vb", [128 * 128, D], BF16),
             ident=dt("ident", [128, 128]), ones=dt("ones", [128, 128]),
             xo=dt("xo", [TOK, D], F32, "ExternalOutput"))
    c = Ctx(nc)
    c.begin_phase("")
    emit_L3(c, A, final)
    c.end_phase()
    c.finish()
    return nc


NJC = 19


def build_fused():
    nc = bass.Bass("TRN2", target_bir_lowering=False)
    di = lambda name, shape, d=F32: nc.dram_tensor(name, list(shape), d, kind="ExternalInput").ap()
    it = lambda name, shape, d=F32: nc.dram_tensor(name, list(shape), d).ap()
    x_seq = di("x_seq", [SEQ, D]); x_mine = di("x_mine", [TOK, D]); c_fm = di("c_fm", [128, 8]); hsel = di("hsel", [128, 2])
    ada_w = di("ada_w", [2, D, 6 * D]); ada_b_fm = di("ada_b_fm", [2, 128, 48])
    ln1 = di("ln1_g_fm", [2, 128, 8]); ln2 = di("ln2_g_fm", [2, 128, 8]); lnf = di("lnf_fm", [128, 8])
    w_in = di("w_in", [2, D, 2304]); vres_down = di("vres_down", [D, 32])
    pvec = di("pvec", [2, 2, 128, 32]); psel = di("psel", [2, 128, 2, 4]); invdiv = di("invdiv", [2, 128, 2, TB])
    w_up = di("w_up", [2, 64, 512]); a_up = di("a_up", [2, 64, 512]); g_up = di("g_up", [2, 128, 512])
    vres_up = di("vres_up", [32, 512]); pool_w = di("pool_w", [2, 4, 128, 128])
    cst = {k: di(k, v.shape) for k, v in l2_consts().items()}
    ones = di("ones", [128, 128])
    nex = 128 if DBG_SKIP0 else 128 * 128
    peer_u = di("peer_u", [2, nex, D]); peer_v = di("peer_v", [2, nex, D])
    w_out = di("w_out", [2, D, D]); peer_q = di("peer_q", [2, D, 2048]); keys = di("peer_keys", [2, 8, 2, 128, 128])
    xo = nc.dram_tensor("xo", [TOK, D], F32, kind="ExternalOutput").ap()
    UT = it("UT_s", [2, 128, 128, 8, 128], BF16); Vb = it("Vb_s", [2, 128 * 128, D], BF16)
    woutb = it("woutb_s", [2, 8, 128, D], BF16); pqb = it("pqb_s", [2, 16, 128, 8, 128], BF16)
    P = it("P_s", [NJC * 128, SEQ]); Y = it("Y_s", [D, SEQ]); VF = it("VF_s", [512, SEQ])
    XO0 = it("XO0_s", [TOK, D]); X1 = it("X1_s", [SEQ, D])

    c = Ctx(nc)
    ph = [0]

    def phase(fn):
        if DBG_PHASES is not None and ph[0] >= DBG_PHASES:
            ph[0] += 1
            return
        c.begin_phase("p%d_" % ph[0]); ph[0] += 1
        fn()
        c.end_phase()

    if DBG_SKIP0:
        ph[0] += 1
    else:
        phase(lambda: emit_L0(c, dict(u=peer_u, v=peer_v, w_out=w_out, peer_q=peer_q, ident=cst["ident"],
                                       UT=UT, Vb=Vb, woutb=woutb, pqb=pqb), range(2), 128))
    for l in range(2):
        xsrc = x_seq if l == 0 else X1
        ncol = 2304 if l == 0 else 2336
        njc = (ncol + 127) // 128
        w_parts = [(w_in[l], 0, 2304)] + ([(vres_down, 2304, 32)] if l > 0 else [])
        for hf in range(2):
            xt_fn = {}
            if l > 0:
                xt_fn = dict(x_tile=lambda i, hf=hf: X1[((i // 4) * 2 + hf) * 512 + (i % 4) * 128:
                                                        ((i // 4) * 2 + hf) * 512 + (i % 4) * 128 + 128, :])
            phase(lambda: emit_L1(c, dict(xt_fn, x=xsrc[hf * TOK:(hf + 1) * TOK], c_fm=c_fm, ada_w=ada_w[l][:, 0:2 * D],
                                           ada_b_fm=ada_b_fm[l], ln_g_fm=ln1[l], w_parts=w_parts, ident=cst["ident"],
                                           out=P[0:njc * 128, hf * TOK:(hf + 1) * TOK], ncol=ncol)))
        for g in range(2):
            cs = slice(g * 256, (g + 1) * 256)
            A = dict(rT=P[g * 256:(g + 1) * 256], kT=P[512 + g * 256: 512 + (g + 1) * 256],
                     vT=P[1024 + g * 256: 1024 + (g + 1) * 256], waT=P[1536:1664], gdT=P[1664:1792],
                     poolT=P[1792 + g * 256: 1792 + (g + 1) * 256],
                     pvec=pvec[l, g], wup=w_up[l][:, cs], aup=a_up[l][:, cs], gup=g_up[l][:, cs],
                     poolw=pool_w[l][2 * g:2 * g + 2], psel=psel[g], invdiv=invdiv[g],
                     yrw=Y[g * 256:(g + 1) * 256], ypl=Y[512 + g * 256: 512 + (g + 1) * 256])
            A.update(cst)
            if l == 0:
                A["vout"] = VF[g * 256:(g + 1) * 256]
            else:
                A.update(vdT=P[2304:2336], vfT=VF[g * 256:(g + 1) * 256], vup=vres_up[:, cs])
            phase(lambda: emit_L2(c, A, l > 0))
        phase(lambda: emit_L3(c, dict(x=(x_mine if l == 0 else XO0), yA=Y[:, 0:TOK], yB=Y[:, TOK:2 * TOK], hsel=hsel,
                                       c_fm=c_fm, ada_w=ada_w[l][:, 2 * D:6 * D], ada_b_fm=ada_b_fm[l], ln_g_fm=ln2[l],
                                       lnf_fm=lnf, woutb=woutb[l], pqb=pqb[l], keys=keys[l], UT=UT[l], Vb=Vb[l],
                                       ident=cst["ident"], ones=ones, xo=(XO0 if l == 0 else xo)), l == 1))
        if l == 0 and DBG_COLL and (DBG_PHASES is None or DBG_PHASES > 6):
            for k in range(4):
                c.collective(lambda gq: gq.collective_compute(
                    "AllGather", ALU.bypass, replica_groups=[[0, 1], [2, 3], [4, 5], [6, 7]],
                    ins=[XO0[k * 512:(k + 1) * 512].opt()], outs=[X1[k * 1024:(k + 1) * 1024].opt()]))
    c.finish()
    return nc


def kernel_fused(inp):
    f = lambda a: np.ascontiguousarray(a, dtype=np.float32)
    cst = l2_consts()
    shared = dict(ada_w=f(inp["ada_w"]), ada_b_fm=f(np.stack([_fm(inp["ada_b"][l]) for l in range(2)])),
                  ln1_g_fm=f(np.stack([_fm(inp["ln1_g"][l]) for l in range(2)])),
                  ln2_g_fm=f(np.stack([_fm(inp["ln2_g"][l]) for l in range(2)])), lnf_fm=_fm(inp["lnf_g"]),
                  w_in=f(inp["w_in"]), vres_down=f(inp["vres_down"][0]), w_up=f(inp["w_up"]), a_up=f(inp["a_up"]),
                  g_up=f(inp["g_up"]), vres_up=f(inp["vres_up"][0]), pool_w=f(inp["pool_w"]),
                  ones=np.ones((128, 128), np.float32), peer_u=f(inp["peer_u"]), peer_v=f(inp["peer_v"]),
                  w_out=f(inp["w_out"]), peer_q=f(inp["peer_q"]), peer_keys=f(inp["peer_keys"]))
    shared.update(cst)
    dummyT = np.zeros((2336, 1), np.float32)
    pv = np.zeros((2, 2, 128, 32), np.float32); ps_ = np.zeros((2, 128, 2, 4), np.float32)
    idv = np.zeros((2, 128, 2, TB), np.float32)
    for l in range(2):
        for g in range(2):
            d = l2_inputs(inp, l, g, dummyT, np.zeros((1, 1), np.float32))
            pv[l, g] = d["pvec"]; ps_[g] = d["psel"]; idv[g] = d["invdiv"]
    shared.update(pvec=pv, psel=ps_, invdiv=idv)
    x = f(inp["x"])
    in_maps = []
    for core in range(NCORE):
        b, g = core // 2, core % 2
        hs = np.zeros((128, 2), np.float32); hs[:, g] = 1.0
        m = dict(shared)
        m.update(x_seq=f(x[b]), x_mine=f(x[b, g * TOK:(g + 1) * TOK]), c_fm=_fm(inp["c"][b]), hsel=hs)
        in_maps.append(m)
    if inp.get("_only_maps") is not None:
        return in_maps
    res = _run(_prog("fused", build_fused), in_maps)
    out = np.stack([np.asarray(res[core]["xo"]) for core in range(NCORE)], axis=0)
    return np.ascontiguousarray(out.reshape(NB, SEQ, D).astype(np.float32))


_PROGS = {}


def _prog(key, fn):
    if key not in _PROGS:
        _PROGS[key] = fn()
    return _PROGS[key]


def _run(nc, in_maps):
    res = run_bass_kernel_spmd(nc, in_maps, core_ids=list(range(NCORE)))
    return res.results


FUSED = True


def kernel(**inputs):
    inp = {k: np.asarray(v) for k, v in inputs.items()}
    if FUSED:
        return kernel_fused(inp)
    f = lambda a: np.ascontiguousarray(a, dtype=np.float32)
    ident = np.eye(128, dtype=np.float32); ones = np.ones((128, 128), np.float32)
    in_maps = []
    for core in range(NCORE):
        sl = slice(core * EPC, (core + 1) * EPC)
        in_maps.append(dict(u=f(inp["peer_u"][:, sl]), v=f(inp["peer_v"][:, sl]), w_out=f(inp["w_out"]),
                            peer_q=f(inp["peer_q"]), ident=ident))
    r0 = _run(_prog("L0", build_L0), in_maps)
    UT = [np.ascontiguousarray(np.concatenate([np.asarray(r0[c_]["UT"])[l] for c_ in range(NCORE)], axis=0)) for l in range(2)]
    Vb = [np.ascontiguousarray(np.concatenate([np.asarray(r0[c_]["Vb"])[l] for c_ in range(NCORE)], axis=0)) for l in range(2)]
    woutb = [np.ascontiguousarray(np.asarray(r0[0]["woutb"])[l]) for l in range(2)]
    pqb = [np.ascontiguousarray(np.asarray(r0[0]["pqb"])[l]) for l in range(2)]
    del r0
    x = f(inp["x"]).reshape(NCORE, TOK, D)
    vfirst = None
    for l in range(2):
        wfull = inp["w_in"][l] if l == 0 else np.concatenate([inp["w_in"][l], inp["vres_down"][l - 1]], axis=1)
        ncol = wfull.shape[1]
        adab_fm = _fm(inp["ada_b"][l])
        in_maps = []
        for core in range(NCORE):
            b = core // 2
            in_maps.append(dict(x=f(x[core]), c_fm=_fm(inp["c"][b]), ada_w=f(inp["ada_w"][l][:, 0:2 * D]),
                                ada_b_fm=adab_fm, ln_g_fm=_fm(inp["ln1_g"][l]), w_full=f(wfull), ident=ident))
        r1 = _run(_prog(("L1", ncol), lambda: build_L1(ncol)), in_maps)
        in_maps = []
        for core in range(NCORE):
            b, g = core // 2, core % 2
            projT = np.concatenate([r1[2 * b]["projT"], r1[2 * b + 1]["projT"]], axis=1)
            in_maps.append(l2_inputs(inp, l, g, projT, None if l == 0 else vfirst[core]))
        del r1
        r2 = _run(_prog(("L2", l > 0), lambda: build_L2(l > 0)), in_maps)
        if l == 0:
            vfirst = [np.asarray(r2[core]["vout"]) for core in range(NCORE)]
        in_maps = []
        for core in range(NCORE):
            b, hf = core // 2, core % 2
            ts = slice(hf * TOK, (hf + 1) * TOK)
            y0, y1 = r2[2 * b]["yT"], r2[2 * b + 1]["yT"]
            yT = np.concatenate([y0[0:256, ts], y1[0:256, ts], y0[256:512, ts], y1[256:512, ts]], axis=0)
            in_maps.append(dict(x=f(x[core]), yT=f(yT), c_fm=_fm(inp["c"][b]), ada_w=f(inp["ada_w"][l][:, 2 * D:6 * D]),
                                ada_b_fm=adab_fm, ln_g_fm=_fm(inp["ln2_g"][l]), lnf_fm=_fm(inp["lnf_g"]),
                                woutb=woutb[l], pqb=pqb[l], keys=f(inp["peer_keys"][l]), UT=UT[l], Vb=Vb[l],
                                ident=ident, ones=ones))
        del r2
        r3 = _run(_prog(("L3", l == 1), lambda: build_L3(l == 1)), in_maps)
        x = np.stack([np.asarray(r3[core]["xo"]) for core in range(NCORE)], axis=0)
        del r3
    return np.ascontiguousarray(x.reshape(NB, SEQ, D).astype(np.float32))
```

```python
from contextlib import ExitStack
import numpy as np
import ml_dtypes
import concourse.bass as bass
import concourse.mybir as mybir
from concourse.bass_utils import run_bass_kernel_spmd

F32 = mybir.dt.float32
BF16 = mybir.dt.bfloat16
AF = mybir.ActivationFunctionType
ALU = mybir.AluOpType

D = 1024
SEQ = 4096
NB = 4
NCORE = 8
TOK = 2048
NORM_EPS = 1e-6
GN_EPS = 64e-5
CH = 64


class Ctx:
    NDMA = 12

    def __init__(self, nc):
        self.nc = nc
        self.stack = ExitStack()
        self.E = dict(pe=nc.tensor, act=nc.scalar, dve=nc.vector, pool=nc.gpsimd, sp=nc.sync)
        self.sem = {}
        for e in ("pe", "act", "dve", "pool"):
            self.sem[e] = self.stack.enter_context(nc.semaphore("c_" + e))
        self.cnt = {e: 0 for e in self.sem}
        self.seen = {e: {} for e in self.E}
        self.dsem = {}
        self.dval = {}
        self.dnext = {}
        for q in ("sp", "pool", "act"):
            self.dsem[q] = [self.stack.enter_context(nc.semaphore("d_%s%d" % (q, i))) for i in range(self.NDMA)]
            self.dval[q] = [0] * self.NDMA
            self.dnext[q] = 0
        self.W = {}
        self.R = {}
        self.n_ins = 0
        self.nwait = {}
        self.ccsem = self.stack.enter_context(nc.semaphore("cc_sem"))
        self.ccval = 0

    scope = None
    pfx = ""

    def begin_phase(self, pfx):
        self.scope = ExitStack()
        self.pfx = pfx

    def end_phase(self):
        self.barrier()
        self.scope.close()
        self.scope = None
        self.W = {}
        self.R = {}

    def barrier(self):
        for eng in self.E:
            for e2 in self.cnt:
                if eng == "pe" and e2 == "pe":
                    continue
                self._wait(eng, e2, self.cnt[e2])
            for q in self.dsem:
                for i in range(self.NDMA):
                    self._wait(eng, (q, i), self.dval[q][i])

    def sb(self, name, shape, dt=F32):
        st = self.scope if self.scope is not None else self.stack
        return st.enter_context(self.nc.sbuf_tensor(self.pfx + name, list(shape), dt))

    def ps(self, name, shape, dt=F32):
        st = self.scope if self.scope is not None else self.stack
        return st.enter_context(self.nc.psum_tensor(self.pfx + name, list(shape), dt))

    def _semh(self, semid):
        if semid == "cc":
            return self.ccsem
        if isinstance(semid, str):
            return self.sem[semid]
        return self.dsem[semid[0]][semid[1]]

    def _wait(self, eng, semid, val):
        if val <= 0:
            return
        if self.seen[eng].get(semid, 0) >= val:
            return
        self.E[eng].wait_ge(self._semh(semid), val)
        self.seen[eng][semid] = val
        self.n_ins += 1
        self.nwait[eng] = self.nwait.get(eng, 0) + 1

    def _deps(self, r, w):
        deps = {}
        for k in r:
            for s, v in self.W.get(k, {}).items():
                deps[s] = max(deps.get(s, 0), v)
        for k in w:
            for s, v in self.W.get(k, {}).items():
                deps[s] = max(deps.get(s, 0), v)
            for s, v in self.R.get(k, {}).items():
                deps[s] = max(deps.get(s, 0), v)
        return deps

    def _record(self, semid, val, r, w):
        for k in w:
            self.W[k] = {semid: val}
            self.R[k] = {}
        for k in r:
            self.R.setdefault(k, {})[semid] = val

    def op(self, eng, fn, r=(), w=()):
        deps = self._deps(r, w)
        for s, v in deps.items():
            if eng == "pe" and s == "pe":
                continue
            self._wait(eng, s, v)
        ins = fn(self.E[eng])
        self.cnt[eng] += 1
        ins.then_inc(self.sem[eng], 1)
        self.n_ins += 1
        self._record(eng, self.cnt[eng], r, w)
        return ins

    def dma(self, q, out, in_, r=(), w=(), **kw):
        i = self.dnext[q]
        self.dnext[q] = (i + 1) % self.NDMA
        semid = (q, i)
        self._wait(q, semid, self.dval[q][i])
        deps = self._deps(r, w)
        for s, v in deps.items():
            self._wait(q, s, v)
        ins = self.E[q].dma_start(out=out, in_=in_, **kw)
        self.dval[q][i] += 16
        ins.then_inc(self.dsem[q][i], 16)
        self.n_ins += 1
        self._record(semid, self.dval[q][i], r, w)
        return ins

    def collective(self, fn):
        self.barrier()
        ins = fn(self.nc.gpsimd)
        self.ccval += 1
        ins.then_inc(self.ccsem)
        for eng in self.E:
            self._wait(eng, "cc", self.ccval)

    def finish(self):
        global LAST_CNT
        LAST_CNT = (dict(self.cnt), {q: list(v) for q, v in self.dval.items()}, self.n_ins, dict(self.nwait))
        for q in self.dsem:
            for i in range(self.NDMA):
                self._wait("sp", (q, i), self.dval[q][i])
        for e in self.cnt:
            self._wait("sp", e, self.cnt[e])
        self.stack.close()


def _fm(vec, n=None):
    v = np.ascontiguousarray(np.asarray(vec, dtype=np.float32).reshape(-1, 128).T)
    return v


def emit_mod_fm(c, adaw_dram, col0, ncolchunks, silu_c, adab_fm_sb, adab_col0, out_sb, ps_tile, tag):
    nc = c.nc
    wv = adaw_dram.rearrange("(dc p) j -> p dc j", p=128)
    cap = c.adaw_sb.shape[2] // 128
    for half in range(0, ncolchunks, cap):
        nch = min(cap, ncolchunks - half)
        wt = c.adaw_sb
        c.dma("sp", wt[:, :, 0:nch * 128], wv[:, :, col0 + half * 128: col0 + (half + nch) * 128],
              w=["adaw"])
        for jc in range(nch):
            for dc in range(8):
                c.op("pe", lambda e, jc=jc, dc=dc: e.matmul(
                    ps_tile[:, half + jc: half + jc + 1], wt[:, dc, jc * 128:(jc + 1) * 128],
                    silu_c[:, dc:dc + 1], start=(dc == 0), stop=(dc == 7)),
                    r=["adaw", "silu_c"], w=[tag + "_ps"])
    c.op("dve", lambda e: e.tensor_tensor(out=out_sb, in0=ps_tile[:, 0:ncolchunks],
                                           in1=adab_fm_sb[:, adab_col0: adab_col0 + ncolchunks], op=ALU.add),
         r=[tag + "_ps", "adab"], w=[tag])


DBG_STAGE = 99
DBG_PHASES = None
DBG_COLL = True
DBG_SKIP0 = False
LAST_CNT = None
DBG_NST = None


def emit_L1(c, A):
    ncol = A["ncol"]
    njc = (ncol + 127) // 128
    x_d, c_d, adaw_d, adab_d, g_d, id_d, out_d = (A["x"], A["c_fm"], A["ada_w"], A["ada_b_fm"], A["ln_g_fm"],
                                                   A["ident"], A["out"])
    c.adaw_sb = c.sb("adaw", [128, 8, 1024], F32)
    c_sb = c.sb("c_sb", [128, 8]); sig_c = c.sb("sig_c", [128, 8]); silu_c = c.sb("silu_c", [128, 8])
    adab = c.sb("adab", [128, 48]); lng = c.sb("lng", [128, 8])
    modT = c.sb("modT", [128, 16]); effs = c.sb("effs", [128, 8])
    identf = c.sb("identf", [128, 128]); identb = c.sb("identb", [128, 128], BF16)
    wbf = c.sb("wbf", [128, 8, njc * 128], BF16)
    hT = c.sb("hT", [128, 8, TOK], BF16)
    xt = [c.sb("xt%d" % i, [128, D]) for i in range(2)]
    junk = c.sb("junk", [128, D], BF16)
    xn = [c.sb("xn%d" % i, [128, D], BF16) for i in range(2)]
    ss = c.sb("ss", [128, 32]); rstd = c.sb("rstd", [128, 32])
    stg = [c.sb("stg%d" % i, [128, TOK]) for i in range(2)]
    mod_ps = c.ps("mod_ps", [128, 512])
    tp_ps = [c.ps("tp_ps%d" % i, [128, 1024], BF16) for i in range(2)]
    mm_ps = [c.ps("mm_ps%d" % i, [128, 512]) for i in range(4)]

    c.dma("sp", c_sb[:], c_d, w=["c_sb"])
    c.dma("sp", adab[:], adab_d, w=["adab"])
    c.dma("sp", lng[:], g_d, w=["lng"])
    c.dma("sp", identf[:], id_d, w=["identf"])
    c.op("dve", lambda e: e.tensor_copy(identb[:], identf[:]), r=["identf"], w=["identb"])
    if njc * 128 != ncol:
        c.op("pool", lambda e: e.memset(wbf[:, :, ncol:njc * 128], 0.0), w=["wbf"])
    for (w_ap, col0, ncols) in A["w_parts"]:
        wv = w_ap.rearrange("(dc p) j -> p dc j", p=128)
        for dc in range(8):
            c.dma("pool", wbf[:, dc, col0:col0 + ncols], wv[:, dc, :], w=["wbf"])
    c.op("act", lambda e: e.activation(out=sig_c[:], in_=c_sb[:], func=AF.Sigmoid), r=["c_sb"], w=["sig_c"])
    c.op("dve", lambda e: e.tensor_tensor(out=silu_c[:], in0=c_sb[:], in1=sig_c[:], op=ALU.mult),
         r=["c_sb", "sig_c"], w=["silu_c"])
    emit_mod_fm(c, adaw_d, 0, 16, silu_c, adab, 0, modT[:, 0:16], mod_ps, "modT")
    c.op("dve", lambda e: e.scalar_tensor_tensor(out=effs[:], in0=modT[:, 8:16], scalar=1.0, in1=lng[:],
                                                  op0=ALU.add, op1=ALU.mult), r=["modT", "lng"], w=["effs"])
    c.op("dve", lambda e: e.memset(ss[:], 0.0), w=["ss%d" % i for i in range(TOK // 128)])
    epsb = c.sb("epsb", [128, 1])
    c.op("dve", lambda e: e.memset(epsb[:], NORM_EPS), w=["epsb"])
    ntile = TOK // 128
    for i in range(ntile):
        s = i % 2
        c.dma("sp", xt[s][:], (A["x_tile"](i) if "x_tile" in A else x_d[i * 128:(i + 1) * 128, :]), w=["xt%d" % s])
        c.op("act", lambda e, s=s, i=i: e.activation(out=junk[:], in_=xt[s][:], func=AF.Square,
                                                     accum_out=ss[:, i:i + 1]),
             r=["xt%d" % s], w=["junk", "ss%d" % i])
        c.op("act", lambda e, i=i: e.activation(out=rstd[:, i:i + 1], in_=ss[:, i:i + 1], func=AF.Sqrt,
                                                bias=epsb[:, 0:1], scale=1.0 / D),
             r=["ss%d" % i, "epsb"], w=["rstd%d" % i])
        c.op("dve", lambda e, i=i: e.reciprocal(out=rstd[:, i:i + 1], in_=rstd[:, i:i + 1]),
             r=["rstd%d" % i], w=["rstd%d" % i])
        c.op("dve", lambda e, s=s, i=i: e.tensor_scalar(out=xn[s][:], in0=xt[s][:], scalar1=rstd[:, i:i + 1],
                                                        scalar2=None, op0=ALU.mult),
             r=["xt%d" % s, "rstd%d" % i], w=["xn%d" % s])
        for dc in range(8):
            c.op("pe", lambda e, s=s, dc=dc: e.transpose(tp_ps[s][:, dc * 128:(dc + 1) * 128],
                                                         xn[s][:, dc * 128:(dc + 1) * 128], identb[:]),
                 r=["xn%d" % s, "identb"], w=["tp%d" % s])
        for dc in range(8):
            if s == 0:
                c.op("act", lambda e, s=s, dc=dc, i=i: e.activation(
                    out=hT[:, dc, i * 128:(i + 1) * 128], in_=tp_ps[s][:, dc * 128:(dc + 1) * 128],
                    func=AF.Identity, bias=modT[:, dc:dc + 1], scale=effs[:, dc:dc + 1]),
                    r=["tp%d" % s, "modT", "effs"], w=["hT_%d_%d_%d" % (dc, i // 4, s)])
            else:
                c.op("dve", lambda e, s=s, dc=dc, i=i: e.tensor_scalar(
                    out=hT[:, dc, i * 128:(i + 1) * 128], in0=tp_ps[s][:, dc * 128:(dc + 1) * 128],
                    scalar1=effs[:, dc:dc + 1], scalar2=modT[:, dc:dc + 1], op0=ALU.mult, op1=ALU.add),
                    r=["tp%d" % s, "modT", "effs"], w=["hT_%d_%d_%d" % (dc, i // 4, s)])
    k = 0
    for jc in range(njc):
        st = stg[jc % 2]
        for tb in range(TOK // 512):
            pt = mm_ps[k % 4]; pk = "mm%d" % (k % 4); k += 1
            for dc in range(8):
                c.op("pe", lambda e, pt=pt, dc=dc, jc=jc, tb=tb: e.matmul(
                    pt[:], wbf[:, dc, jc * 128:(jc + 1) * 128], hT[:, dc, tb * 512:(tb + 1) * 512],
                    start=(dc == 0), stop=(dc == 7)),
                    r=["wbf", "hT_%d_%d_0" % (dc, tb), "hT_%d_%d_1" % (dc, tb)], w=[pk])
            eng = "act" if tb % 2 == 0 else "dve"
            if eng == "act":
                c.op("act", lambda e, pt=pt, st=st, tb=tb: e.copy(out=st[:, tb * 512:(tb + 1) * 512], in_=pt[:]),
                     r=[pk], w=["stg%d_%d" % (jc % 2, tb)])
            else:
                c.op("dve", lambda e, pt=pt, st=st, tb=tb: e.tensor_copy(st[:, tb * 512:(tb + 1) * 512], pt[:]),
                     r=[pk], w=["stg%d_%d" % (jc % 2, tb)])
        c.dma("sp", out_d[jc * 128:(jc + 1) * 128, :], st[:],
              r=["stg%d_%d" % (jc % 2, tb) for tb in range(4)])


def build_L1(ncol):
    nc = bass.Bass("TRN2", target_bir_lowering=False)
    njc = (ncol + 127) // 128
    di = lambda name, shape: nc.dram_tensor(name, list(shape), F32, kind="ExternalInput").ap()
    A = dict(x=di("x", [TOK, D]), c_fm=di("c_fm", [128, 8]), ada_w=di("ada_w", [D, 2 * D]),
             ada_b_fm=di("ada_b_fm", [128, 48]), ln_g_fm=di("ln_g_fm", [128, 8]), ident=di("ident", [128, 128]),
             ncol=ncol)
    A["w_parts"] = [(di("w_full", [D, ncol]), 0, ncol)]
    A["out"] = nc.dram_tensor("projT", [njc * 128, TOK], F32, kind="ExternalOutput").ap()
    c = Ctx(nc)
    c.begin_phase("")
    emit_L1(c, A)
    c.end_phase()
    c.finish()
    return nc


class _Stop(Exception):
    pass


def dbg(stage):
    if DBG_STAGE == stage:
        raise _Stop()


TB = 512
NBLK = SEQ // TB
CPB = TB // CH
C0 = float(np.exp(-0.5))
POOL_WINDOWS = (2, 4, 8, 16)


def l2_consts():
    s_idx = np.arange(64)[:, None]; t_idx = np.arange(64)[None, :]
    su = (t_idx > s_idx).astype(np.float32); iu = (t_idx >= s_idx).astype(np.float32)
    maskA = np.tile(np.concatenate([su, iu], axis=1), (1, 4))
    maskC = np.tile((t_idx < s_idx).astype(np.float32), (1, 4))
    identrep = np.tile(np.eye(64, dtype=np.float32), (1, 4))
    scanmask = np.ones((128, TB), np.float32); scanmask[:, ::CH] = 0.0
    blockones = np.kron(np.eye(2, dtype=np.float32), np.ones((64, 64), np.float32))
    return dict(maskA=maskA, maskC=maskC, identrep=identrep, scanmask=scanmask, blockones=blockones,
                ident=np.eye(128, dtype=np.float32))


def emit_L2(c, A, layer1):
    nc = c.nc
    rT, kT, vT, waT, gdT, poolT = A["rT"], A["kT"], A["vT"], A["waT"], A["gdT"], A["poolT"]
    if layer1:
        vdT, vfT, vup_d = A["vdT"], A["vfT"], A["vup"]
    pvec_d, wup_d, aup_d, gup_d, poolw_d = A["pvec"], A["wup"], A["aup"], A["gup"], A["poolw"]
    maskA_d, maskC_d, identrep_d = A["maskA"], A["maskC"], A["identrep"]
    scanmask_d, bo_d, id_d, invdiv_d, psel_d = A["scanmask"], A["blockones"], A["ident"], A["invdiv"], A["psel"]
    yrw, ypl = A["yrw"], A["ypl"]
    if not layer1:
        vout = A["vout"]
    sb = c.sb
    pvec = sb("pvec_sb", [128, 32]); omk = sb("omk", [128, 2])
    wup = sb("wup_sb", [128, 256], BF16); aup = sb("aup_sb", [128, 256], BF16); gup = sb("gup_sb", [128, 256], BF16)
    vup = sb("vup_sb", [32, 256], BF16); poolw = sb("poolw_sb", [128, 2, 128], BF16)
    maskA = sb("maskA_sb", [64, 512]); maskC = sb("maskC_sb", [64, 256]); identrep = sb("identrep_sb", [64, 256])
    scanmask = sb("scanmask_sb", [128, TB]); bo = sb("bo_sb", [128, 128]); identf = sb("identf", [128, 128])
    identb = sb("identb", [128, 128], BF16); invdiv = sb("invdiv_sb", [128, 2, TB])
    epsk = sb("epsk", [128, 1]); gneps = sb("gneps", [128, 1]); psel = sb("psel_sb", [128, 2, 4])
    Xwa = sb("Xwa", [128, TB + 1]); Xg = sb("Xg", [128, TB + 1]); Xvd = sb("Xvd", [32, TB + 1])
    dwa = sb("dwa", [128, TB]); swa = sb("swa", [128, TB]); th = sb("th", [64, TB], BF16); adb = sb("adb", [128, TB], BF16)
    dg = sb("dg", [128, TB]); sgd = sb("sgd", [128, TB]); sg = sb("sg", [128, TB], BF16)
    dvd = sb("dvd", [32, TB]); vdb = sb("vdb", [32, TB], BF16)
    P2 = range(2)
    Xr = [sb("Xr_sh", [128, TB + 1])] * 2; Xk = [sb("Xk_sh", [128, TB + 1])] * 2
    Xv = [sb("Xv_sh", [128, TB + 1])] * 2; Xvf = [sb("Xvf_sh", [128, TB])] * 2
    tmpd = [sb("tmpd_sh", [128, TB])] * 2
    r_s = [sb("r_s%d" % i, [128, TB]) for i in P2]; k_s = [sb("k_s%d" % i, [128, TB]) for i in P2]
    v_s = [sb("v_s%d" % i, [128, TB]) for i in P2]
    sigw = [sb("sigw%d" % i, [128, TB]) for i in P2]; cum = [sb("cum%d" % i, [128, TB]) for i in P2]
    cumx = [sb("cumx_sh", [128, TB])] * 2
    G = [sb("G%d" % i, [128, TB]) for i in P2]; Ginv = [sb("Ginv%d" % i, [128, TB]) for i in P2]
    Gex = [sb("Gex%d" % i, [128, TB]) for i in P2]
    a_ = [sb("a_%d" % i, [128, TB]) for i in P2]; gg = [sb("gg%d" % i, [128, TB]) for i in P2]
    vsig = [sb("vsig_sh", [128, TB])] * 2
    kkraw = [sb("kkraw_sh", [128, TB])] * 2; sq = [sb("sq_sh", [128, TB])] * 2
    rn = [sb("rn_sh", [128, TB])] * 2; kk = [sb("kk%d" % i, [128, TB]) for i in P2]
    fac = [sb("fac_sh", [128, TB])] * 2; kmod = [sb("kmod%d" % i, [128, TB]) for i in P2]
    rk2 = [sb("rk2_sh", [128, TB])] * 2; bonus = [sb("bonus%d" % i, [128, TB]) for i in P2]
    t1 = [sb("t1_sh", [128, TB])] * 2
    ARt = [sb("ARt%d" % i, [128, CPB * 128], BF16) for i in P2]
    Bt = [sb("Bt%d" % i, [128, TB], BF16) for i in P2]; Kt = [sb("Kt%d" % i, [128, TB], BF16) for i in P2]
    Vb = [sb("Vb%d" % i, [128, TB], BF16) for i in P2]
    yraw = [sb("yraw%d" % i, [128, TB]) for i in P2]; yc = [sb("yc%d" % i, [128, TB]) for i in P2]
    ysq = [sb("ysq_sh", [128, TB])] * 2; yrs = [sb("yrs_sh", [128, TB])] * 2
    yo = [sb("yo%d" % i, [128, TB]) for i in P2]
    M0b = [[sb("M0b%d_%d" % (i, j), [64, 64], BF16) for j in range(2)] for i in range(4)]
    ARo = [sb("ARo%d" % i, [64, CPB * 128], BF16) for i in P2]
    Bo = [sb("Bo%d" % i, [64, TB], BF16) for i in P2]; Ko = [sb("Ko%d" % i, [64, TB], BF16) for i in P2]
    GCs = [sb("GCs%d" % i, [128, CPB]) for i in P2]; GCo = [sb("GCo%d" % i, [64, CPB]) for i in P2]
    tokmaj = [sb("tokmaj%d" % j, [64, 768], BF16) for j in range(2)]
    SA = [sb("SA%d" % j, [64, 512], BF16) for j in range(2)]; SB_ = [sb("SB%d" % j, [64, 512], BF16) for j in range(2)]
    SC = [sb("SC%d" % j, [64, 256], BF16) for j in range(2)]
    TT = [sb("TT%d" % j, [64, 256], BF16) for j in range(2)]; PP = [sb("PP%d" % j, [64, 512], BF16) for j in range(2)]
    W1 = sb("W1", [64, 256], BF16); U = [sb("U%d" % j, [64, 256], BF16) for j in range(2)]
    TTFs = [sb("TTF%d" % j, [64, 256], BF16) for j in range(2)]
    Xp = [sb("Xp%d" % i, [128, TB + 15]) for i in P2]
    plv = [[sb("plv%d_%d" % (i, k), [128, TB + 15]) for k in range(4)] for i in P2]
    pacc = [sb("pacc%d" % i, [128, TB]) for i in P2]
    pd = [sb("pd%d" % i, [128, TB], BF16) for i in P2]; pm = [sb("pm%d" % i, [128, TB]) for i in P2]
    po = [sb("po%d" % i, [128, TB]) for i in P2]
    bk = {n: c.ps("bk_" + n, [128, 512]) for n in ("A", "B", "C", "D", "E", "FG", "HI")}
    trp = c.ps("bk_trp", [128, 1024], BF16)

    ld = lambda dst, src, key: c.dma("sp", dst, src, w=[key])
    ld(pvec[:], pvec_d, "pvec"); ld(maskA[:], maskA_d, "maskA"); ld(maskC[:], maskC_d, "maskC")
    ld(identrep[:], identrep_d, "identrep"); ld(scanmask[:], scanmask_d, "scanmask"); ld(bo[:], bo_d, "bo")
    ld(identf[:], id_d, "identf"); ld(invdiv[:], invdiv_d, "invdiv"); ld(psel[:], psel_d, "psel")
    c.dma("pool", wup[0:64, :], wup_d, w=["wup"]); c.dma("pool", aup[64:128, :], aup_d, w=["aup"])
    c.dma("pool", gup[:], gup_d, w=["gup"]); c.dma("pool", poolw[:], poolw_d.rearrange("g c d -> c g d"), w=["poolw"])
    if layer1:
        c.dma("pool", vup[0:32, :], vup_d, w=["vup"])
    c.op("dve", lambda e: e.tensor_copy(identb[:], identf[:]), r=["identf"], w=["identb"])
    c.op("dve", lambda e: e.memset(epsk[:], 1e-24), w=["epsk"])
    c.op("dve", lambda e: e.memset(gneps[:], GN_EPS), w=["gneps"])
    c.op("dve", lambda e: e.tensor_scalar(out=omk[:], in0=pvec[:, 12:14], scalar1=-1.0, scalar2=1.0,
                                          op0=ALU.mult, op1=ALU.add), r=["pvec"], w=["omk"])
    for i in range(4):
        for j in range(2):
            c.op("dve", lambda e, i=i, j=j: e.memset(M0b[i][j][:], 0.0), w=["M0b%d_%d" % (i, j)])
    for i in P2:
        for k_ in range(4):
            c.op("pool", lambda e, i=i, k_=k_: e.memset(plv[i][k_][:], 0.0), w=["plv%d_%d" % (i, k_)])
    pv = lambda col, hp=0: pvec[:, col + hp: col + hp + 1]

    def tshift(eng, X, d, out, mu, n, kX, kd, kout):
        c.op(eng, lambda e: e.tensor_tensor(out=d, in0=X[0:n, 0:TB], in1=X[0:n, 1:TB + 1], op=ALU.subtract),
             r=[kX], w=[kd])
        if eng == "dve":
            c.op(eng, lambda e: e.scalar_tensor_tensor(out=out, in0=d, scalar=mu, in1=X[0:n, 1:TB + 1],
                                                       op0=ALU.mult, op1=ALU.add), r=[kX, kd, "pvec"], w=[kout])
        else:
            c.op(eng, lambda e: e.tensor_scalar(out=d, in0=d, scalar1=mu, scalar2=None, op0=ALU.mult),
                 r=[kd, "pvec"], w=[kd])
            c.op(eng, lambda e: e.tensor_tensor(out=out, in0=d, in1=X[0:n, 1:TB + 1], op=ALU.add),
                 r=[kX, kd], w=[kout])

    def load_halo(X, src, rows, b, n, key, halo=1):
        t0 = b * TB
        if b == 0:
            c.op("pool", lambda e: e.memset(X[0:n, 0:halo], 0.0), w=[key])
            c.dma("sp", X[0:n, halo:halo + TB], src[rows, 0:TB], w=[key])
        else:
            c.dma("sp", X[0:n, :], src[rows, t0 - halo:t0 + TB], w=[key])

    chunk_counter = [0]

    def body():
        for b in range(NBLK):
            t0 = b * TB
            load_halo(Xwa, waT, slice(0, 128), b, 128, "Xwa")
            load_halo(Xg, gdT, slice(0, 128), b, 128, "Xg")
            tshift("pool", Xwa, dwa[:], swa[:], pv(25), 128, "Xwa", "dwa", "swa")
            c.op("act", lambda e: e.activation(out=th[:], in_=swa[0:64, :], func=AF.Tanh), r=["swa"], w=["th"])
            c.op("pool", lambda e: e.tensor_copy(adb[64:128, :], swa[64:128, :]), r=["swa"], w=["adb"])
            tshift("pool", Xg, dg[:], sgd[:], pv(24), 128, "Xg", "dg", "sgd")
            c.op("act", lambda e: e.activation(out=sg[:], in_=sgd[:], func=AF.Sigmoid), r=["sgd"], w=["sg"])
            if layer1:
                load_halo(Xvd, vdT, slice(0, 32), b, 32, "Xvd")
                tshift("pool", Xvd, dvd[:], vdb[:], pvec[0:32, 26:27], 32, "Xvd", "dvd", "vdb")
            dbg(1)
            for hp in P2:
                rows = slice(hp * 128, (hp + 1) * 128)
                H = str(hp)
                load_halo(Xr[hp], rT, rows, b, 128, "Xr")
                load_halo(Xk[hp], kT, rows, b, 128, "Xk")
                load_halo(Xv[hp], vT, rows, b, 128, "Xv")
                tshift("dve", Xr[hp], tmpd[hp][:], r_s[hp][:], pv(0, hp), 128, "Xr", "tmpd", "r_s" + H)
                tshift("dve", Xk[hp], tmpd[hp][:], k_s[hp][:], pv(2, hp), 128, "Xk", "tmpd", "k_s" + H)
                tshift("dve", Xv[hp], tmpd[hp][:], v_s[hp][:], pv(4, hp), 128, "Xv", "tmpd", "v_s" + H)
                cols = slice(hp * 128, (hp + 1) * 128)
                c.op("pe", lambda e: e.matmul(bk["A"][:], wup[0:64, cols], th[:], start=True, stop=True),
                     r=["wup", "th"], w=["bkA"])
                c.op("act", lambda e: e.activation(out=sigw[hp][:], in_=bk["A"][:], func=AF.Sigmoid, bias=pv(6, hp)),
                     r=["bkA", "pvec"], w=["sigw" + H])
                c.op("pe", lambda e: e.matmul(bk["B"][:], aup[64:128, cols], adb[64:128, :], start=True, stop=True),
                     r=["aup", "adb"], w=["bkB"])
                c.op("act", lambda e: e.activation(out=a_[hp][:], in_=bk["B"][:], func=AF.Sigmoid, bias=pv(8, hp)),
                     r=["bkB", "pvec"], w=["a_" + H])
                c.op("pe", lambda e: e.matmul(bk["C"][:], gup[:, cols], sg[:], start=True, stop=True),
                     r=["gup", "sg"], w=["bkC"])
                c.op("act", lambda e: e.copy(out=gg[hp][:], in_=bk["C"][:]), r=["bkC"], w=["gg" + H])
                if layer1:
                    c.dma("sp", Xvf[hp][:], vfT[rows, t0:t0 + TB], w=["Xvf"])
                    c.op("pe", lambda e: e.matmul(bk["D"][:], vup[0:32, cols], vdb[0:32, :], start=True, stop=True),
                         r=["vup", "vdb"], w=["bkD"])
                    c.op("act", lambda e: e.activation(out=vsig[hp][:], in_=bk["D"][:], func=AF.Sigmoid, bias=pv(27, hp)),
                         r=["bkD", "pvec"], w=["vsig"])
                    c.op("dve", lambda e: e.tensor_tensor(out=tmpd[hp][:], in0=Xvf[hp][:], in1=v_s[hp][:], op=ALU.subtract),
                         r=["Xvf", "v_s" + H], w=["tmpd"])
                    c.op("dve", lambda e: e.tensor_tensor(out=tmpd[hp][:], in0=tmpd[hp][:], in1=vsig[hp][:], op=ALU.mult),
                         r=["vsig", "tmpd"], w=["tmpd"])
                    c.op("dve", lambda e: e.tensor_tensor(out=v_s[hp][:], in0=v_s[hp][:], in1=tmpd[hp][:], op=ALU.add),
                         r=["v_s" + H, "tmpd"], w=["v_s" + H])
                else:
                    c.dma("sp", vout[rows, t0:t0 + TB], v_s[hp][:], r=["v_s" + H])
                c.op("dve", lambda e: e.tensor_scalar(out=kkraw[hp][:], in0=k_s[hp][:], scalar1=pv(10, hp), scalar2=None,
                                                      op0=ALU.mult), r=["k_s" + H, "pvec"], w=["kkraw"])
                c.op("pool", lambda e: e.tensor_tensor(out=sq[hp][:], in0=kkraw[hp][:], in1=kkraw[hp][:], op=ALU.mult),
                     r=["kkraw"], w=["sq"])
                c.op("pe", lambda e: e.matmul(bk["E"][:], bo[:], sq[hp][:], start=True, stop=True),
                     r=["bo", "sq"], w=["bkE"])
                c.op("act", lambda e: e.activation(out=rn[hp][:], in_=bk["E"][:], func=AF.Sqrt, bias=epsk[:, 0:1]),
                     r=["bkE", "epsk"], w=["rn"])
                c.op("dve", lambda e: e.reciprocal(out=rn[hp][:], in_=rn[hp][:]), r=["rn"], w=["rn"])
                c.op("dve", lambda e: e.tensor_tensor(out=kk[hp][:], in0=kkraw[hp][:], in1=rn[hp][:], op=ALU.mult),
                     r=["kkraw", "rn"], w=["kk" + H])
                c.op("pool", lambda e: e.tensor_scalar(out=fac[hp][:], in0=a_[hp][:], scalar1=pv(12, hp),
                                                       scalar2=omk[:, hp:hp + 1], op0=ALU.mult, op1=ALU.add),
                     r=["a_" + H, "pvec", "omk"], w=["fac"])
                c.op("pool", lambda e: e.tensor_tensor(out=kmod[hp][:], in0=k_s[hp][:], in1=fac[hp][:], op=ALU.mult),
                     r=["k_s" + H, "fac"], w=["kmod" + H])
                c.op("pool", lambda e: e.tensor_scalar(out=rk2[hp][:], in0=r_s[hp][:], scalar1=pv(16, hp), scalar2=None,
                                                       op0=ALU.mult), r=["r_s" + H, "pvec"], w=["rk2"])
                c.op("pool", lambda e: e.tensor_tensor(out=rk2[hp][:], in0=rk2[hp][:], in1=kmod[hp][:], op=ALU.mult),
                     r=["rk2", "kmod" + H], w=["rk2"])
                c.op("pe", lambda e: e.matmul(bk["FG"][:], bo[:], rk2[hp][:], start=True, stop=True),
                     r=["bo", "rk2"], w=["bkFG"])
                c.op("dve", lambda e: e.tensor_tensor(out=bonus[hp][:], in0=bk["FG"][:], in1=v_s[hp][:], op=ALU.mult),
                     r=["bkFG", "v_s" + H], w=["bonus" + H])
                c.op("dve", lambda e: e.tensor_tensor_scan(out=cum[hp][:], data0=scanmask[:], data1=sigw[hp][:],
                                                           initial=0.0, op0=ALU.mult, op1=ALU.add),
                     r=["scanmask", "sigw" + H], w=["cum" + H])
                c.op("pool", lambda e: e.tensor_tensor(out=cumx[hp][:], in0=cum[hp][:], in1=sigw[hp][:], op=ALU.subtract),
                     r=["cum" + H, "sigw" + H], w=["cumx"])
                c.op("act", lambda e: e.activation(out=G[hp][:], in_=cum[hp][:], func=AF.Exp, scale=-C0),
                     r=["cum" + H], w=["G" + H])
                c.op("act", lambda e: e.activation(out=Ginv[hp][:], in_=cum[hp][:], func=AF.Exp, scale=C0),
                     r=["cum" + H], w=["Ginv" + H])
                c.op("act", lambda e: e.activation(out=Gex[hp][:], in_=cumx[hp][:], func=AF.Exp, scale=-C0),
                     r=["cumx"], w=["Gex" + H])
                AR3 = ARt[hp][:].rearrange("p (c two t) -> p c two t", two=2, t=CH)
                v3 = lambda ap: ap.rearrange("p (c t) -> p c t", t=CH)
                c.op("dve", lambda e: e.tensor_tensor(out=AR3[:, :, 1, :], in0=v3(r_s[hp][:]), in1=v3(G[hp][:]), op=ALU.mult),
                     r=["r_s" + H, "G" + H], w=["ARt" + H])
                c.op("dve", lambda e: e.scalar_tensor_tensor(out=AR3[:, :, 0, :], in0=v3(kk[hp][:]), scalar=-1.0,
                                                             in1=v3(Gex[hp][:]), op0=ALU.mult, op1=ALU.mult),
                     r=["kk" + H, "Gex" + H], w=["ARt" + H])
                c.op("pool", lambda e: e.tensor_tensor(out=t1[hp][:], in0=kk[hp][:], in1=a_[hp][:], op=ALU.mult),
                     r=["kk" + H, "a_" + H], w=["t1"])
                c.op("dve", lambda e: e.tensor_tensor(out=Bt[hp][:], in0=t1[hp][:], in1=Ginv[hp][:], op=ALU.mult),
                     r=["t1", "Ginv" + H], w=["Bt" + H])
                c.op("pool", lambda e: e.tensor_tensor(out=Kt[hp][:], in0=kmod[hp][:], in1=Ginv[hp][:], op=ALU.mult),
                     r=["kmod" + H, "Ginv" + H], w=["Kt" + H])
                c.op("pool", lambda e: e.tensor_copy(Vb[hp][:], v_s[hp][:]), r=["v_s" + H], w=["Vb" + H])
                c.op("pool", lambda e: e.tensor_copy(GCs[hp][:], G[hp][:].rearrange("p (c t) -> p c t", t=CH)[:, :, CH - 1]),
                     r=["G" + H], w=["GCs" + H])
                c.dma("sp", ARo[hp][:], ARt[hp][64:128, :], r=["ARt" + H], w=["ARo" + H])
                c.dma("sp", Bo[hp][:], Bt[hp][64:128, :], r=["Bt" + H], w=["Bo" + H])
                c.dma("sp", Ko[hp][:], Kt[hp][64:128, :], r=["Kt" + H], w=["Ko" + H])
                c.dma("sp", GCo[hp][:], GCs[hp][64:128, :], r=["GCs" + H], w=["GCo" + H])

            dbg(2)
            for gi in P2:
                Gk = str(gi)
                load_halo(Xp[gi], poolT, slice(gi * 128, (gi + 1) * 128), b, 128, "Xp" + Gk, halo=15)
                cur = Xp[gi]; curk = "Xp" + Gk
                for lv in range(4):
                    sh = 1 << lv
                    dst = plv[gi][lv]; dk = "plv%d_%d" % (gi, lv)
                    c.op("pool", lambda e: e.tensor_tensor(out=dst[:, sh:15 + TB], in0=cur[:, sh:15 + TB],
                                                           in1=cur[:, 0:15 + TB - sh], op=ALU.add), r=[curk], w=[dk])
                    cur, curk = dst, dk
                c.op("dve", lambda e: e.tensor_scalar(out=pacc[gi][:], in0=plv[gi][0][:, 15:15 + TB],
                                                      scalar1=psel[:, gi, 0:1], scalar2=None, op0=ALU.mult),
                     r=["plv%d_0" % gi, "psel"], w=["pacc" + Gk])
                for lv in range(1, 4):
                    c.op("dve", lambda e: e.scalar_tensor_tensor(out=pacc[gi][:], in0=plv[gi][lv][:, 15:15 + TB],
                                                                 scalar=psel[:, gi, lv:lv + 1], in1=pacc[gi][:],
                                                                 op0=ALU.mult, op1=ALU.add),
                         r=["plv%d_%d" % (gi, lv), "psel", "pacc" + Gk], w=["pacc" + Gk])
                if b == 0:
                    c.op("dve", lambda e: e.tensor_tensor(out=pm[gi][:], in0=pacc[gi][:], in1=invdiv[:, gi, :],
                                                          op=ALU.mult), r=["pacc" + Gk, "invdiv"], w=["pm" + Gk])
                    c.op("dve", lambda e: e.tensor_tensor(out=pd[gi][:], in0=pm[gi][:], in1=Xp[gi][:, 15:15 + TB],
                                                          op=ALU.subtract), r=["pm" + Gk, "Xp" + Gk], w=["pd" + Gk])
                else:
                    c.op("dve", lambda e: e.scalar_tensor_tensor(out=pd[gi][:], in0=pacc[gi][:],
                                                                 scalar=pv(29, gi), in1=Xp[gi][:, 15:15 + TB],
                                                                 op0=ALU.mult, op1=ALU.subtract),
                         r=["pacc" + Gk, "Xp" + Gk, "pvec"], w=["pd" + Gk])
                c.op("pe", lambda e: e.matmul(bk["D"][:], poolw[:, gi, :], pd[gi][:], start=True, stop=True),
                     r=["poolw", "pd" + Gk], w=["bkD"])
                c.op("act", lambda e: e.activation(out=po[gi][:], in_=bk["D"][:], func=AF.Identity, scale=pv(22, gi)),
                     r=["bkD", "pvec"], w=["po" + Gk])
                c.dma("sp", ypl[gi * 128:(gi + 1) * 128, t0:t0 + TB], po[gi][:], r=["po" + Gk])

            dbg(3)
            def emit_chunk(ci, cc, part):
                pp = cc % 2
                cs = slice(ci * CH, (ci + 1) * CH)
                tm = tokmaj[pp]; sa = SA[pp]; sbb = SB_[pp]; sc = SC[pp]
                tmk, sak, sbk, sck = "tokmaj%d" % pp, "SA%d" % pp, "SB%d" % pp, "SC%d" % pp
                TTF = TTFs[pp]; TTFk = "TTF%d" % pp
                def ARv(h, lo, hi):
                    hp_ = h // 2
                    t_ = ARt[hp_] if h % 2 == 0 else ARo[hp_]
                    return t_[0:64, ci * 128 + lo: ci * 128 + hi]
                def Bv(h):
                    hp_ = h // 2
                    return (Bt[hp_] if h % 2 == 0 else Bo[hp_])[0:64, cs]
                def Kv(h):
                    hp_ = h // 2
                    return (Kt[hp_] if h % 2 == 0 else Ko[hp_])[0:64, cs]
                ARk = lambda h: ("ARt%d" if h % 2 == 0 else "ARo%d") % (h // 2)
                Bk = lambda h: ("Bt%d" if h % 2 == 0 else "Bo%d") % (h // 2)
                Kk = lambda h: ("Kt%d" if h % 2 == 0 else "Ko%d") % (h // 2)
                if part == "I":
                    for hp in P2:
                        H = str(hp)
                        for j, (src, sk) in enumerate(((Bt[hp], "Bt" + H), (Kt[hp], "Kt" + H), (Vb[hp], "Vb" + H))):
                            c.op("pe", lambda e, src=src, j=j, hp=hp: e.transpose(
                                trp[0:64, j * 256 + hp * 128: j * 256 + (hp + 1) * 128], src[:, cs], identb[:]),
                                r=[sk, "identb"], w=["bktrp"])
                    c.op("act", lambda e: e.copy(out=tm[:], in_=trp[0:64, 0:768]), r=["bktrp"], w=[tmk])
                    yield
                    dbg(4)
                    for h in range(4):
                        c.op("pe", lambda e: e.matmul(bk["A"][0:64, h * 128:(h + 1) * 128], Bv(h), ARv(h, 0, 128),
                                                      start=True, stop=True), r=[Bk(h), ARk(h)], w=["bkA"])
                        c.op("pe", lambda e: e.matmul(bk["B"][0:64, h * 128:(h + 1) * 128], Kv(h), ARv(h, 0, 128),
                                                      start=True, stop=True), r=[Kk(h), ARk(h)], w=["bkB"])
                        c.op("pe", lambda e: e.matmul(bk["C"][0:64, h * 64:(h + 1) * 64], ARv(h, 0, 64), Bv(h),
                                                      start=True, stop=True), r=[Bk(h), ARk(h)], w=["bkC"])
                    c.op("dve", lambda e: e.tensor_tensor(out=sa[:], in0=bk["A"][0:64, :], in1=maskA[:], op=ALU.mult),
                         r=["bkA", "maskA"], w=[sak])
                    c.op("dve", lambda e: e.tensor_tensor(out=sbb[:], in0=bk["B"][0:64, :], in1=maskA[:], op=ALU.mult),
                         r=["bkB", "maskA"], w=[sbk])
                    c.op("dve", lambda e: e.tensor_tensor(out=sc[:], in0=bk["C"][0:64, 0:256], in1=maskC[:], op=ALU.mult),
                         r=["bkC", "maskC"], w=[sck])
                    yield
                    Nv = lambda h: sa[:, h * 128: h * 128 + 64]
                    NTv = lambda h: sc[:, h * 64:(h + 1) * 64]
                    dbg(5)
                    c.op("pool", lambda e: e.tensor_tensor(
                        out=TT[0][:].rearrange("p (h t) -> p h t", t=64),
                        in0=sa[:].rearrange("p (h x) -> p h x", x=128)[:, :, 0:64],
                        in1=identrep[:].rearrange("p (h t) -> p h t", t=64), op=ALU.add),
                        r=[sak, "identrep"], w=["TT0"])
                    for h in range(4):
                        c.op("pe", lambda e: e.matmul(bk["D"][0:64, h * 64:(h + 1) * 64], NTv(h), Nv(h), start=True, stop=True),
                             r=[sak, sck], w=["bkD"])
                        c.op("pe", lambda e: e.matmul(bk["D"][0:64, 256 + h * 64: 256 + (h + 1) * 64], Nv(h), NTv(h),
                                                      start=True, stop=True), r=[sak, sck], w=["bkD"])
                    c.op("act", lambda e: e.copy(out=PP[0][:], in_=bk["D"][0:64, :]), r=["bkD"], w=["PP0"])
                    yield
                    for lv in range(1, 6):
                        pi = (lv - 1) % 2; po_ = lv % 2
                        Pv = lambda h: PP[pi][:, h * 64:(h + 1) * 64]
                        PTv = lambda h: PP[pi][:, 256 + h * 64: 256 + (h + 1) * 64]
                        TTv = lambda h: TT[pi][:, h * 64:(h + 1) * 64]
                        for h in range(4):
                            c.op("pe", lambda e: e.matmul(bk["E"][0:64, h * 64:(h + 1) * 64], identb[0:64, 0:64], TTv(h),
                                                          start=True, stop=False), r=["identb", "TT%d" % pi], w=["bkE"])
                            c.op("pe", lambda e: e.matmul(bk["E"][0:64, h * 64:(h + 1) * 64], PTv(h), TTv(h),
                                                          start=False, stop=True), r=["PP%d" % pi, "TT%d" % pi], w=["bkE"])
                        if lv < 5:
                            for h in range(4):
                                c.op("pe", lambda e: e.matmul(bk["D"][0:64, h * 64:(h + 1) * 64], PTv(h), Pv(h),
                                                              start=True, stop=True), r=["PP%d" % pi], w=["bkD"])
                                c.op("pe", lambda e: e.matmul(bk["D"][0:64, 256 + h * 64: 256 + (h + 1) * 64], Pv(h), PTv(h),
                                                              start=True, stop=True), r=["PP%d" % pi], w=["bkD"])
                        if lv == 5:
                            c.op("dve", lambda e: e.tensor_copy(TTF[:], bk["E"][0:64, 0:256]), r=["bkE"], w=[TTFk])
                        else:
                            c.op("dve", lambda e: e.tensor_copy(TT[po_][:], bk["E"][0:64, 0:256]), r=["bkE"], w=["TT%d" % po_])
                        if lv < 5:
                            c.op("act", lambda e: e.copy(out=PP[po_][:], in_=bk["D"][0:64, :]), r=["bkD"], w=["PP%d" % po_])
                        yield
                if part == "D":
                    TTf = TTF; TTk = TTFk
                    Mold = [M0b[h][pp] for h in range(4)]; Mnew = [M0b[h][1 - pp] for h in range(4)]
                    Mok = ["M0b%d_%d" % (h, pp) for h in range(4)]; Mnk = ["M0b%d_%d" % (h, 1 - pp) for h in range(4)]
                    Uc = U[pp]; Uk = "U%d" % pp
                    for h in range(4):
                        c.op("pe", lambda e: e.matmul(bk["FG"][0:64, h * 64:(h + 1) * 64], ARv(h, 0, 64), Mold[h][:],
                                                      start=True, stop=False), r=[ARk(h), Mok[h]], w=["bkFG"])
                        c.op("pe", lambda e: e.matmul(bk["FG"][0:64, h * 64:(h + 1) * 64], sbb[:, h * 128: h * 128 + 64],
                                                      tm[:, 512 + h * 64: 512 + (h + 1) * 64], start=False, stop=True),
                             r=[sbk, tmk], w=["bkFG"])
                    c.op("act", lambda e: e.copy(out=W1[:], in_=bk["FG"][0:64, 0:256]), r=["bkFG"], w=["W1"])
                    yield
                    for h in range(4):
                        c.op("pe", lambda e: e.matmul(bk["FG"][0:64, 256 + h * 64: 256 + (h + 1) * 64],
                                                      TTf[:, h * 64:(h + 1) * 64], W1[:, h * 64:(h + 1) * 64],
                                                      start=True, stop=True), r=[TTk, "W1"], w=["bkFG"])
                    c.op("act", lambda e: e.copy(out=Uc[:], in_=bk["FG"][0:64, 256:512]), r=["bkFG"], w=[Uk])
                    yield
                    dbg(7)
                    for h in range(4):
                        hp, base = h // 2, (h % 2) * 64
                        o = bk["HI"][base:base + 64, hp * 64:(hp + 1) * 64]
                        c.op("pe", lambda e: e.matmul(o, Mold[h][:], ARv(h, 64, 128), start=True, stop=False),
                             r=[ARk(h), Mok[h]], w=["bkHI"])
                        c.op("pe", lambda e: e.matmul(o, Uc[:, h * 64:(h + 1) * 64], sa[:, h * 128 + 64:(h + 1) * 128],
                                                      start=False, stop=False), r=[Uk, sak], w=["bkHI"])
                        c.op("pe", lambda e: e.matmul(o, tm[:, 512 + h * 64: 512 + (h + 1) * 64],
                                                      sbb[:, h * 128 + 64:(h + 1) * 128], start=False, stop=True),
                             r=[tmk, sbk], w=["bkHI"])
                    for h in range(4):
                        o = bk["HI"][0:64, 256 + h * 64: 256 + (h + 1) * 64]
                        c.op("pe", lambda e: e.matmul(o, tm[:, 256 + h * 64: 256 + (h + 1) * 64],
                                                      tm[:, 512 + h * 64: 512 + (h + 1) * 64], start=True, stop=False),
                             r=[tmk], w=["bkHI"])
                        c.op("pe", lambda e: e.matmul(o, tm[:, h * 64:(h + 1) * 64], Uc[:, h * 64:(h + 1) * 64],
                                                      start=False, stop=False), r=[tmk, Uk], w=["bkHI"])
                        c.op("pe", lambda e: e.matmul(o, identb[0:64, 0:64], Mold[h][:],
                                                      start=False, stop=True), r=["identb", Mok[h]], w=["bkHI"])
                    yield
                    for hp in P2:
                        H = str(hp)
                        c.op("act", lambda e: e.copy(out=yraw[hp][:, cs], in_=bk["HI"][:, hp * 64:(hp + 1) * 64]),
                             r=["bkHI"], w=["yraw" + H])
                    for h in range(4):
                        hp = h // 2
                        gsrc, gk = (GCs[hp], "GCs%d" % hp) if h % 2 == 0 else (GCo[hp], "GCo%d" % hp)
                        c.op("act", lambda e: e.activation(out=Mnew[h][:], in_=bk["HI"][0:64, 256 + h * 64: 256 + (h + 1) * 64],
                                                           func=AF.Identity, scale=gsrc[0:64, ci:ci + 1]),
                             r=["bkHI", gk], w=[Mnk[h]])

            cc0 = chunk_counter[0]; chunk_counter[0] += CPB

            def drive(gens):
                gens = list(gens)
                while gens:
                    for g_ in list(gens):
                        try:
                            next(g_)
                        except StopIteration:
                            gens.remove(g_)

            drive([emit_chunk(0, cc0, "I")])
            for ci in range(CPB):
                gens = [emit_chunk(ci, cc0 + ci, "D")]
                if ci + 1 < CPB:
                    gens.insert(0, emit_chunk(ci + 1, cc0 + ci + 1, "I"))
                drive(gens)
            dbg(8)
            for hp in P2:
                H = str(hp)
                rows = slice(hp * 128, (hp + 1) * 128)
                c.op("pe", lambda e: e.matmul(bk["A"][:], bo[:], yraw[hp][:], start=True, stop=True),
                     r=["bo", "yraw" + H], w=["bkA"])
                c.op("dve", lambda e: e.scalar_tensor_tensor(out=yc[hp][:], in0=bk["A"][:], scalar=-1.0 / 64,
                                                             in1=yraw[hp][:], op0=ALU.mult, op1=ALU.add),
                     r=["bkA", "yraw" + H], w=["yc" + H])
                c.op("pool", lambda e: e.tensor_tensor(out=ysq[hp][:], in0=yc[hp][:], in1=yc[hp][:], op=ALU.mult),
                     r=["yc" + H], w=["ysq"])
                c.op("pe", lambda e: e.matmul(bk["B"][:], bo[:], ysq[hp][:], start=True, stop=True),
                     r=["bo", "ysq"], w=["bkB"])
                c.op("act", lambda e: e.activation(out=yrs[hp][:], in_=bk["B"][:], func=AF.Sqrt, bias=gneps[:, 0:1],
                                                   scale=1.0 / 64), r=["bkB", "gneps"], w=["yrs"])
                c.op("dve", lambda e: e.reciprocal(out=yrs[hp][:], in_=yrs[hp][:]), r=["yrs"], w=["yrs"])
                c.op("dve", lambda e: e.tensor_tensor(out=yc[hp][:], in0=yc[hp][:], in1=yrs[hp][:], op=ALU.mult),
                     r=["yc" + H, "yrs"], w=["yc" + H])
                c.op("dve", lambda e: e.tensor_scalar(out=yc[hp][:], in0=yc[hp][:], scalar1=pv(18, hp), scalar2=pv(20, hp),
                                                      op0=ALU.mult, op1=ALU.add), r=["yc" + H, "pvec"], w=["yc" + H])
                c.op("pool", lambda e: e.tensor_tensor(out=yc[hp][:], in0=yc[hp][:], in1=bonus[hp][:], op=ALU.add),
                     r=["yc" + H, "bonus" + H], w=["yc" + H])
                c.op("pool", lambda e: e.tensor_tensor(out=yo[hp][:], in0=yc[hp][:], in1=gg[hp][:], op=ALU.mult),
                     r=["yc" + H, "gg" + H], w=["yo" + H])
                c.dma("sp", yrw[rows, t0:t0 + TB], yo[hp][:], r=["yo" + H])
    try:
        body()
    except _Stop:
        pass


def build_L2(layer1):
    nc = bass.Bass("TRN2", target_bir_lowering=False)
    dt = lambda name, shape, kind="ExternalInput": nc.dram_tensor(name, list(shape), F32, kind=kind).ap()
    A = dict(rT=dt("rT", [256, SEQ]), kT=dt("kT", [256, SEQ]), vT=dt("vT", [256, SEQ]),
             waT=dt("waT", [128, SEQ]), gdT=dt("gdT", [128, SEQ]), poolT=dt("poolT", [256, SEQ]))
    if layer1:
        A.update(vdT=dt("vdT", [32, SEQ]), vfT=dt("vfT", [256, SEQ]), vup=dt("vup", [32, 256]))
    A.update(pvec=dt("pvec", [128, 32]), wup=dt("wup", [64, 256]), aup=dt("aup", [64, 256]),
             gup=dt("gup", [128, 256]), poolw=dt("poolw", [2, 128, 128]),
             maskA=dt("maskA", [64, 512]), maskC=dt("maskC", [64, 256]), identrep=dt("identrep", [64, 256]),
             scanmask=dt("scanmask", [128, TB]), blockones=dt("blockones", [128, 128]), ident=dt("ident", [128, 128]),
             invdiv=dt("invdiv", [128, 2, TB]), psel=dt("psel", [128, 2, 4]))
    yT = dt("yT", [512, SEQ], "ExternalOutput")
    A["yrw"] = yT[0:256]; A["ypl"] = yT[256:512]
    if not layer1:
        A["vout"] = dt("vout", [256, SEQ], "ExternalOutput")
    c = Ctx(nc)
    c.begin_phase("")
    emit_L2(c, A, layer1)
    c.end_phase()
    c.finish()
    return nc


def l2_inputs(inp, l, g, projT, vfT):
    f = lambda a: np.ascontiguousarray(a, dtype=np.float32)
    mu = inp["mu_shift"][l]
    cs = slice(g * 256, (g + 1) * 256)
    pvec = np.zeros((128, 32), np.float32)
    def put(col, vec512):
        v = np.asarray(vec512)[cs].reshape(2, 128)
        pvec[:, col] = v[0]; pvec[:, col + 1] = v[1]
    put(0, mu[0:512]); put(2, mu[512:1024]); put(4, mu[1024:1536])
    put(6, inp["w0"][l]); put(8, inp["a0"][l]); put(10, inp["k_k"][l]); put(12, inp["k_a"][l])
    put(16, inp["r_k"][l].reshape(512)); put(18, inp["lnx_g"][l]); put(20, inp["lnx_b"][l])
    put(22, inp["pool_scale"][l])
    pvec[:, 24] = mu[1664:1792]; pvec[0:64, 25] = mu[1536:1600]; pvec[64:128, 25] = mu[1600:1664]
    d = dict(rT=f(projT[0:512][cs]), kT=f(projT[512:1024][cs]), vT=f(projT[1024:1536][cs]),
             waT=f(projT[1536:1664]), gdT=f(projT[1664:1792]), poolT=f(projT[1792:2304][cs]),
             wup=f(inp["w_up"][l][:, cs]), aup=f(inp["a_up"][l][:, cs]), gup=f(inp["g_up"][l][:, cs]),
             poolw=f(inp["pool_w"][l][2 * g:2 * g + 2]))
    if l > 0:
        pvec[0:32, 26] = inp["vres_mu"][l - 1]
        put(27, inp["vres_v0"][l - 1])
        d.update(vdT=f(projT[2304:2336]), vfT=f(vfT), vup=f(inp["vres_up"][l - 1][:, cs]))
    d["pvec"] = pvec
    pos = np.arange(1, TB + 1, dtype=np.float32)
    invdiv = np.stack([np.broadcast_to(1.0 / np.minimum(pos, float(POOL_WINDOWS[2 * g + gi])), (128, TB))
                       for gi in range(2)], axis=1)
    d["invdiv"] = f(invdiv)
    psel = np.zeros((128, 2, 4), np.float32)
    for gi in range(2):
        psel[:, gi, 2 * g + gi] = 1.0
        pvec[:, 29 + gi] = 1.0 / POOL_WINDOWS[2 * g + gi]
    d["psel"] = psel
    d.update(l2_consts())
    return d


EPC = 2048
def emit_L0(c, A, layers, nch):
    u_d, v_d, wo_d, pq_d, id_d = A["u"], A["v"], A["w_out"], A["peer_q"], A["ident"]
    ut_o, vb_o, wo_o, pq_o = A["UT"], A["Vb"], A["woutb"], A["pqb"]
    identf = c.sb("identf", [128, 128]); identb = c.sb("identb", [128, 128], BF16)
    NBUF0 = 4
    uf = [c.sb("uf%d" % i, [128, D]) for i in range(NBUF0)]
    ub = [c.sb("ub%d" % i, [128, D], BF16) for i in range(NBUF0)]
    uT = [c.sb("uT%d" % i, [128, D], BF16) for i in range(NBUF0)]
    vb = [c.sb("vb%d" % i, [128, D], BF16) for i in range(NBUF0)]
    wb = [c.sb("wb%d" % i, [128, 8, 128], BF16) for i in range(NBUF0)]
    tps = [c.ps("tps%d" % i, [128, 1024], BF16) for i in range(NBUF0)]
    c.dma("sp", identf[:], id_d, w=["identf"])
    c.op("dve", lambda e: e.tensor_copy(identb[:], identf[:]), r=["identf"], w=["identb"])
    k = 0
    for l in layers:
        for ch in range(nch):
            s_ = k % NBUF0; k += 1
            S = str(s_)
            rows = slice(ch * 128, (ch + 1) * 128)
            c.dma("sp", uf[s_][:], u_d[l, rows, :], w=["uf" + S])
            c.dma("pool", vb[s_][:], v_d[l, rows, :], w=["vb" + S])
            c.dma("sp", vb_o[l, rows, :], vb[s_][:], r=["vb" + S])
            eng = "act" if s_ % 2 == 0 else "dve"
            if eng == "act":
                c.op("act", lambda e: e.copy(out=ub[s_][:], in_=uf[s_][:]), r=["uf" + S], w=["ub" + S])
            else:
                c.op("dve", lambda e: e.tensor_copy(ub[s_][:], uf[s_][:]), r=["uf" + S], w=["ub" + S])
            for dc in range(8):
                c.op("pe", lambda e: e.transpose(tps[s_][:, dc * 128:(dc + 1) * 128], ub[s_][:, dc * 128:(dc + 1) * 128],
                                                 identb[:]), r=["ub" + S, "identb"], w=["tps" + S])
            if eng == "act":
                c.op("act", lambda e: e.copy(out=uT[s_][:], in_=tps[s_][:]), r=["tps" + S], w=["uT" + S])
            else:
                c.op("dve", lambda e: e.tensor_copy(uT[s_][:], tps[s_][:]), r=["tps" + S], w=["uT" + S])
            c.dma("sp", ut_o[l, ch], uT[s_][:].rearrange("p (dc e) -> p dc e", e=128), r=["uT" + S])
        for cc in range(8):
            s_ = k % NBUF0; k += 1
            S = str(s_)
            c.dma("pool", vb[s_][:], wo_d[l, cc * 128:(cc + 1) * 128, :], w=["vb" + S])
            c.dma("sp", wo_o[l, cc], vb[s_][:], r=["vb" + S])
        pqv = pq_d[l].rearrange("(dc p) j -> p dc j", p=128)
        for jc in range(16):
            s_ = k % NBUF0; k += 1
            S = str(s_)
            c.dma("pool", wb[s_][:], pqv[:, :, jc * 128:(jc + 1) * 128], w=["wb" + S])
            c.dma("sp", pq_o[l, jc], wb[s_][:], r=["wb" + S])


def build_L0():
    nc = bass.Bass("TRN2", target_bir_lowering=False)
    di = lambda name, shape: nc.dram_tensor(name, list(shape), F32, kind="ExternalInput").ap()
    do = lambda name, shape: nc.dram_tensor(name, list(shape), BF16, kind="ExternalOutput").ap()
    A = dict(u=di("u", [2, EPC, D]), v=di("v", [2, EPC, D]), w_out=di("w_out", [2, D, D]),
             peer_q=di("peer_q", [2, D, 2048]), ident=di("ident", [128, 128]),
             UT=do("UT", [2, EPC // 128, 128, 8, 128]), Vb=do("Vb", [2, EPC, D]),
             woutb=do("woutb", [2, 8, 128, D]), pqb=do("pqb", [2, 16, 128, 8, 128]))
    c = Ctx(nc)
    c.begin_phase("")
    emit_L0(c, A, range(2), EPC // 128)
    c.end_phase()
    c.finish()
    return nc


TS = 256
NST = TOK // TS
NEG = -1.0e30


def emit_L3(c, A, final):
    nc = c.nc
    x_d, c_d, adaw_d, adab_d, g_d, lnf_d = A["x"], A["c_fm"], A["ada_w"], A["ada_b_fm"], A["ln_g_fm"], A["lnf_fm"]
    wo_d, pq_d, keys_d, ut_d, vb_d, id_d, ones_d, xo_d = (A["woutb"], A["pqb"], A["keys"], A["UT"], A["Vb"],
                                                          A["ident"], A["ones"], A["xo"])
    ysel = "hsel" in A
    sb = c.sb
    c.adaw_sb = sb("adaw", [128, 8, 256])
    c_sb = sb("c_sb", [128, 8]); sig_c = sb("sig_c", [128, 8]); silu_c = sb("silu_c", [128, 8])
    adab = sb("adab", [128, 48]); lng = sb("lng", [128, 8]); lnf = sb("lnf", [128, 8])
    modT = sb("modT", [128, 32]); effs = sb("effs", [128, 8]); epsb = sb("epsb", [128, 1])
    identf = sb("identf", [128, 128]); identb = sb("identb", [128, 128], BF16); onesf = sb("onesf", [128, 128])
    diag = sb("diag", [128, 512])
    g1bc = sb("g1bc", [128, D]); g2bc = sb("g2bc", [128, D]); lnfbc = sb("lnfbc", [128, D])
    keysT = sb("keysT", [128, 16, 128], BF16)
    wst = [sb("wst%d" % i, [128, D], BF16) for i in range(3)]
    x1 = [sb("x1_%d" % i, [128, D]) for i in range(2)]
    yTb = sb("yTb", [128, 8, TS], BF16)
    if ysel:
        ysa = sb("ysa", [128, 8, TS], BF16); ysb = sb("ysb", [128, 8, TS], BF16); hsel = sb("hsel_sb", [128, 2])
    junk = sb("junk", [128, D], BF16); xn = sb("xn", [128, D], BF16)
    ssq = sb("ssq", [128, 4]); rstd = sb("rstd", [128, 4])
    h2T = sb("h2T", [128, 8, TS], BF16); qT = sb("qT", [128, 16, TS], BF16)
    ST = sb("ST", [128, 16, TS]); SC = sb("SC", [128, 2048])
    kf = SC[:].rearrange("p (a d) -> p a d", d=128)
    wk = [sb("wk%d" % i, [128, 128]) for i in range(2)]
    stop = sb("stop", [128, 16, 16]); candh = [sb("candh%d" % i, [128, 256]) for i in range(2)]
    cwk = [sb("cwk%d" % i, [128, 256]) for i in range(2)]; ctop = sb("ctop", [128, 8, 16])
    negm = sb("negm", [128, 8]); esub = sb("esub", [128, 128]); exs = sb("exs", [128, 128])
    Zs = sb("Zs", [128, 8]); invZ = sb("invZ", [128, 8])
    PACK = sb("PACK", [128, 4, 128]); PKT = sb("PKT", [128, 4, TS])
    rep = [sb("rep%d" % i, [128, 256]) for i in range(3)]
    E0 = [sb("E0_%d" % i, [128, 128], BF16) for i in range(3)]
    D1 = [sb("D1_%d" % i, [128, 128], BF16) for i in range(3)]
    ex1 = [sb("ex1_%d" % i, [128, 128]) for i in range(3)]
    gate = sb("gate_all", [128, 128, TS], BF16)
    ut4 = [sb("ut4_%d" % i, [128, 2, D], BF16) for i in range(3)]
    vt4 = [sb("vt4_%d" % i, [128, 2, D], BF16) for i in range(3)]
    NCH = 2
    ge = [sb("ge%d" % i, [128, TS], BF16) for i in range(2)]
    AT = [sb("AT%d" % i, [128, TS], BF16) for i in range(2)]
    tmpo = sb("tmpo", [128, 512])
    B = [c.ps("b%d" % i, [128, 512]) for i in range(8)]
    Bb = c.ps

    ld = lambda dst, src, key: c.dma("sp", dst, src, w=[key])
    ld(c_sb[:], c_d, "c_sb"); ld(adab[:], adab_d, "adab"); ld(lng[:], g_d, "lng"); ld(lnf[:], lnf_d, "lnf")
    ld(identf[:], id_d, "identf"); ld(onesf[:], ones_d, "onesf")
    if ysel:
        ld(hsel[:], A["hsel"], "hsel")
    ld(kf, keys_d.rearrange("h p k d -> k (h p) d"), "kf")
    c.op("dve", lambda e: e.tensor_copy(identb[:], identf[:]), r=["identf"], w=["identb"])
    c.op("dve", lambda e: e.memset(epsb[:], NORM_EPS), w=["epsb"])
    c.op("act", lambda e: e.activation(out=sig_c[:], in_=c_sb[:], func=AF.Sigmoid), r=["c_sb"], w=["sig_c"])
    c.op("dve", lambda e: e.tensor_tensor(out=silu_c[:], in0=c_sb[:], in1=sig_c[:], op=ALU.mult),
         r=["c_sb", "sig_c"], w=["silu_c"])
    emit_mod_fm(c, adaw_d, 0, 32, silu_c, adab, 16, modT[:, 0:32], B[0], "modT")
    c.op("dve", lambda e: e.scalar_tensor_tensor(out=effs[:], in0=modT[:, 16:24], scalar=1.0, in1=lng[:],
                                                  op0=ALU.add, op1=ALU.mult), r=["modT", "lng"], w=["effs"])

    def bcast_rows(vec8, vkey, out_tile, okey):
        for hf in range(2):
            for q in range(4):
                jc = hf * 4 + q
                c.op("dve", lambda e: e.tensor_scalar(out=diag[:, q * 128:(q + 1) * 128], in0=identf[:],
                                                      scalar1=vec8[:, jc:jc + 1], scalar2=None, op0=ALU.mult),
                     r=["identf", vkey], w=["diag%d" % q])
                c.op("pe", lambda e: e.matmul(B[1][:, q * 128:(q + 1) * 128], onesf[:], diag[:, q * 128:(q + 1) * 128],
                                              start=True, stop=True), r=["onesf", "diag%d" % q], w=["b1"])
            c.op("act", lambda e: e.copy(out=out_tile[:, hf * 512:(hf + 1) * 512], in_=B[1][:]), r=["b1"], w=[okey])

    bcast_rows(modT[:, 0:8], "modT", g1bc, "g1bc")
    bcast_rows(modT[:, 24:32], "modT", g2bc, "g2bc")
    if final:
        bcast_rows(lnf, "lnf", lnfbc, "lnfbc")
    for q4 in range(4):
        for q in range(4):
            hp16 = q4 * 4 + q
            c.op("pe", lambda e: e.transpose(B[2][:, q * 128:(q + 1) * 128], kf[:, hp16, :], identf[:]),
                 r=["kf", "identf"], w=["b2"])
        c.op("act", lambda e: e.copy(out=keysT[:, q4 * 4:(q4 + 1) * 4, :],
                                     in_=B[2][:].rearrange("p (q k) -> p q k", k=128)), r=["b2"], w=["keysT"])
    wsi = [0]

    def wstream(src_ap):
        i = wsi[0] % 3; wsi[0] += 1
        c.dma("sp", wst[i][:], src_ap, w=["wst%d" % i])
        return wst[i], "wst%d" % i

    def body():
        dbg(1)
        for st in range(DBG_NST or NST):
            tok0 = st * TS
            if not ysel:
                c.dma("pool", yTb[:], A["yT"].rearrange("(cc p) t -> p cc t", p=128)[:, :, tok0:tok0 + TS], w=["yTb"])
            else:
                c.dma("pool", ysa[:], A["yA"].rearrange("(cc p) t -> p cc t", p=128)[:, :, tok0:tok0 + TS], w=["ysa"])
                c.dma("pool", ysb[:], A["yB"].rearrange("(cc p) t -> p cc t", p=128)[:, :, tok0:tok0 + TS], w=["ysb"])
                c.op("dve", lambda e: e.tensor_scalar(out=ysa[:], in0=ysa[:], scalar1=hsel[:, 0:1], scalar2=None,
                                                      op0=ALU.mult), r=["ysa", "hsel"], w=["ysa"])
                c.op("dve", lambda e: e.scalar_tensor_tensor(out=yTb[:], in0=ysb[:], scalar=hsel[:, 1:2], in1=ysa[:],
                                                             op0=ALU.mult, op1=ALU.add),
                     r=["ysa", "ysb", "hsel"], w=["yTb"])
            wts = []
            for tt in range(2):
                X = x1[tt]; Xk = "x1_%d" % tt
                c.dma("sp", X[:], x_d[tok0 + tt * 128: tok0 + (tt + 1) * 128, :], w=[Xk])
                for cc in range(8):
                    wt, wkk = wstream(wo_d[cc])
                    for hf in range(2):
                        c.op("pe", lambda e: e.matmul(B[hf][:], yTb[:, cc, tt * 128:(tt + 1) * 128],
                                                      wt[:, hf * 512:(hf + 1) * 512], start=(cc == 0), stop=(cc == 7)),
                             r=["yTb", wkk], w=["b%d" % hf])
                for hf in range(2):
                    c.op("dve", lambda e: e.tensor_tensor(out=tmpo[:], in0=B[hf][:], in1=g1bc[:, hf * 512:(hf + 1) * 512],
                                                          op=ALU.mult), r=["b%d" % hf, "g1bc"], w=["tmpo"])
                    c.op("dve", lambda e: e.tensor_tensor(out=X[:, hf * 512:(hf + 1) * 512], in0=tmpo[:],
                                                          in1=X[:, hf * 512:(hf + 1) * 512], op=ALU.add),
                         r=["tmpo", Xk], w=[Xk])
                c.op("dve", lambda e: e.memset(ssq[:, tt:tt + 1], 0.0), w=["ssq"])
                c.op("act", lambda e: e.activation(out=junk[:], in_=X[:], func=AF.Square, accum_out=ssq[:, tt:tt + 1]),
                     r=[Xk, "ssq"], w=["junk", "ssq"])
                c.op("act", lambda e: e.activation(out=rstd[:, tt:tt + 1], in_=ssq[:, tt:tt + 1], func=AF.Sqrt,
                                                   bias=epsb[:, 0:1], scale=1.0 / D), r=["ssq", "epsb"], w=["rstd"])
                c.op("dve", lambda e: e.reciprocal(out=rstd[:, tt:tt + 1], in_=rstd[:, tt:tt + 1]), r=["rstd"], w=["rstd"])
                c.op("dve", lambda e: e.tensor_scalar(out=xn[:], in0=X[:], scalar1=rstd[:, tt:tt + 1], scalar2=None,
                                                      op0=ALU.mult), r=[Xk, "rstd"], w=["xn"])
                tpb = B[2 + tt][:].bitcast(BF16)
                for dc in range(8):
                    c.op("pe", lambda e: e.transpose(tpb[:, dc * 128:(dc + 1) * 128], xn[:, dc * 128:(dc + 1) * 128],
                                                     identb[:]), r=["xn", "identb"], w=["b%d" % (2 + tt)])
                for dc in range(8):
                    c.op("act", lambda e: e.activation(out=h2T[:, dc, tt * 128:(tt + 1) * 128],
                                                       in_=tpb[:, dc * 128:(dc + 1) * 128], func=AF.Identity,
                                                       bias=modT[:, 8 + dc: 9 + dc], scale=effs[:, dc:dc + 1]),
                         r=["b%d" % (2 + tt), "modT", "effs"], w=["h2T"])
            dbg(2)
            for hp16 in range(16):
                wt, wkk = wstream(pq_d[hp16].rearrange("p dc j -> p (dc j)"))
                bi = 4 + (hp16 // 2) % 2
                sub = hp16 % 2
                for dc in range(8):
                    c.op("pe", lambda e: e.matmul(B[bi][:, sub * TS:(sub + 1) * TS], wt[:, dc * 128:(dc + 1) * 128],
                                                  h2T[:, dc, :], start=(dc == 0), stop=(dc == 7)),
                         r=[wkk, "h2T"], w=["b%d" % bi])
                if sub == 1:
                    c.op("act", lambda e: e.copy(out=qT[:, hp16 - 1: hp16 + 1, :],
                                                 in_=B[bi][:].rearrange("p (s t) -> p s t", t=TS)),
                         r=["b%d" % bi], w=["qT"])
            for hp16 in range(16):
                bi = 6 + (hp16 // 2) % 2
                sub = hp16 % 2
                c.op("pe", lambda e: e.matmul(B[bi][:, sub * TS:(sub + 1) * TS], keysT[:, hp16, :], qT[:, hp16, :],
                                              start=True, stop=True), r=["keysT", "qT"], w=["b%d" % bi])
                if sub == 1:
                    c.op("dve", lambda e: e.tensor_copy(ST[:, hp16 - 1: hp16 + 1, :],
                                                        B[bi][:].rearrange("p (s t) -> p s t", t=TS)),
                         r=["b%d" % bi], w=["ST"])
            dbg(3)
            stop4 = stop[:].rearrange("p (h two) a -> p h two a", two=2)
            for tt in range(2):
                for q4 in range(4):
                    for q in range(4):
                        hp16 = q4 * 4 + q
                        c.op("pe", lambda e: e.transpose(B[q4][:, q * 128:(q + 1) * 128],
                                                         ST[:, hp16, tt * 128:(tt + 1) * 128], identf[:]),
                             r=["ST", "identf"], w=["b%d" % q4])
                    c.op("act", lambda e: e.copy(out=SC[:, q4 * 512:(q4 + 1) * 512], in_=B[q4][:]),
                         r=["b%d" % q4], w=["SC%d" % q4, "kf"])
                for hp16 in range(16):
                    w_ = wk[hp16 % 2]; wkk = "wk%d" % (hp16 % 2)
                    scv = SC[:, hp16 * 128:(hp16 + 1) * 128]; sck = "SC%d" % (hp16 // 4)
                    c.op("dve", lambda e: e.max(out=stop[:, hp16, 0:8], in_=scv), r=[sck], w=["stopA%d" % hp16])
                    c.op("dve", lambda e: e.match_replace(out=w_[:], in_to_replace=stop[:, hp16, 0:8], in_values=scv,
                                                          imm_value=NEG), r=[sck, "stopA%d" % hp16], w=[wkk])
                    c.op("dve", lambda e: e.max(out=stop[:, hp16, 8:16], in_=w_[:]), r=[wkk], w=["stopB%d" % hp16])
                stopkeys = ["stopA%d" % i for i in range(16)] + ["stopB%d" % i for i in range(16)]
                for h in range(8):
                    ch_ = candh[h % 2]; chk = "candh%d" % (h % 2)
                    cw_ = cwk[h % 2]; cwkk = "cwk%d" % (h % 2)
                    c.op("dve", lambda e: e.tensor_tensor(
                        out=ch_[:].rearrange("p (a b) -> p a b", b=16),
                        in0=stop4[:, h, 0, :].unsqueeze(2).broadcast_to([128, 16, 16]),
                        in1=stop4[:, h, 1, :].unsqueeze(1).broadcast_to([128, 16, 16]), op=ALU.add),
                        r=["stopA%d" % (2 * h), "stopB%d" % (2 * h), "stopA%d" % (2 * h + 1), "stopB%d" % (2 * h + 1)],
                        w=[chk])
                    c.op("dve", lambda e: e.max(out=ctop[:, h, 0:8], in_=ch_[:]), r=[chk], w=["ctopA%d" % h])
                    c.op("dve", lambda e: e.match_replace(out=cw_[:], in_to_replace=ctop[:, h, 0:8], in_values=ch_[:],
                                                          imm_value=NEG), r=[chk, "ctopA%d" % h], w=[cwkk])
                    c.op("dve", lambda e: e.max(out=ctop[:, h, 8:16], in_=cw_[:]), r=[cwkk], w=["ctopB%d" % h])
                ctk = ["ctopA%d" % h for h in range(8)] + ["ctopB%d" % h for h in range(8)]
                c.op("dve", lambda e: e.tensor_scalar(out=negm[:], in0=ctop[:, :, 0], scalar1=-1.0, scalar2=None,
                                                      op0=ALU.mult), r=ctk, w=["negm"])
                c.op("dve", lambda e: e.tensor_tensor(out=esub[:].rearrange("p (h a) -> p h a", a=16), in0=ctop[:],
                                                      in1=negm[:].unsqueeze(2).broadcast_to([128, 8, 16]), op=ALU.add),
                     r=ctk + ["negm"], w=["esub"])
                c.op("act", lambda e: e.activation(out=exs[:], in_=esub[:], func=AF.Exp), r=["esub"], w=["exs"])
                c.op("dve", lambda e: e.reduce_sum(out=Zs[:], in_=exs[:].rearrange("p (h a) -> p h a", a=16),
                                                   axis=mybir.AxisListType.X), r=["exs"], w=["Zs"])
                c.op("dve", lambda e: e.reciprocal(out=invZ[:], in_=Zs[:]), r=["Zs"], w=["invZ"])
                P3 = lambda j: PACK[:, j, :].rearrange("p (h a) -> p h a", a=16)
                c.op("dve", lambda e: e.tensor_copy(P3(0), stop4[:, :, 0, :]), r=stopkeys, w=["PACK0"])
                c.op("dve", lambda e: e.tensor_tensor(out=P3(1), in0=ctop[:, :, 15].unsqueeze(2).broadcast_to([128, 8, 16]),
                                                      in1=stop4[:, :, 0, :], op=ALU.subtract), r=stopkeys + ctk, w=["PACK1"])
                c.op("dve", lambda e: e.tensor_tensor(out=P3(2), in0=stop4[:, :, 0, :],
                                                      in1=negm[:].unsqueeze(2).broadcast_to([128, 8, 16]), op=ALU.add),
                     r=stopkeys + ["negm"], w=["PACK2"])
                c.op("dve", lambda e: e.tensor_copy(P3(3), invZ[:].unsqueeze(2).broadcast_to([128, 8, 16])),
                     r=["invZ"], w=["PACK3"])
                for j in range(4):
                    c.op("pe", lambda e: e.transpose(B[4][:, j * 128:(j + 1) * 128], PACK[:, j, :], identf[:]),
                         r=["PACK%d" % j, "identf"], w=["b4"])
                c.op("act", lambda e: e.copy(out=PKT[:, :, tt * 128:(tt + 1) * 128],
                                             in_=B[4][:].rearrange("p (j t) -> p j t", t=128)), r=["b4"], w=["PKT"])
            dbg(4)
            def s6A(t):
                s_ = t % 3; S = str(s_)
                c.op("pool", lambda e: e.tensor_copy(
                    rep[s_][:].rearrange("p (two h a) -> p two h a", two=2, a=16),
                    ST[:, :, t].rearrange("p (h two) -> p two h", two=2).unsqueeze(3).broadcast_to([128, 2, 8, 16])),
                    r=["ST"], w=["rep" + S])
                c.op("pe", lambda e: e.transpose(B[0 + s_][:, 0:128], rep[s_][:, 0:128], identf[:]),
                     r=["rep" + S, "identf"], w=["b%d" % (0 + s_)])
                c.op("pe", lambda e: e.transpose(B[3 + s_][:, 0:128], rep[s_][:, 128:256], identf[:]),
                     r=["rep" + S, "identf"], w=["b%d" % (3 + s_)])

            def s6B(t):
                s_ = t % 3; S = str(s_)
                c.op("dve", lambda e: e.tensor_scalar(out=E0[s_][:], in0=B[0 + s_][:, 0:128], scalar1=PKT[:, 0, t:t + 1],
                                                      scalar2=PKT[:, 3, t:t + 1], op0=ALU.is_equal, op1=ALU.mult),
                     r=["b%d" % (0 + s_), "PKT"], w=["E0_" + S])
                c.op("act", lambda e: e.activation(out=ex1[s_][:], in_=B[3 + s_][:, 0:128], func=AF.Exp,
                                                   bias=PKT[:, 2, t:t + 1]), r=["b%d" % (3 + s_), "PKT"], w=["ex1_" + S])
                c.op("dve", lambda e: e.scalar_tensor_tensor(out=D1[s_][:], in0=B[3 + s_][:, 0:128],
                                                             scalar=PKT[:, 1, t:t + 1], in1=ex1[s_][:],
                                                             op0=ALU.is_ge, op1=ALU.mult),
                     r=["b%d" % (3 + s_), "PKT", "ex1_" + S], w=["D1_" + S])

            def s6C(t):
                s_ = t % 3; S = str(s_)
                gs = (t // 4) % 2
                c.op("pe", lambda e: e.matmul(B[6 + gs][:, (t % 4) * 128:(t % 4 + 1) * 128], D1[s_][:], E0[s_][:],
                                              start=True, stop=True), r=["D1_" + S, "E0_" + S], w=["b%d" % (6 + gs)])
                if t % 4 == 3:
                    c.op("act", lambda e: e.copy(out=gate[:, :, t - 3:t + 1].rearrange("p i t -> p t i"),
                                                 in_=B[6 + gs][:].rearrange("p (t i) -> p t i", i=128)),
                         r=["b%d" % (6 + gs)], w=["gate"])

            s6A(0)
            s6A(1)
            for t in range(TS):
                if t + 2 < TS:
                    s6A(t + 2)
                s6B(t)
                s6C(t)
            dbg(5)
            def s7U(i0):
                g4, cix = i0 // NCH, i0 % NCH
                bf_ = g4 % 3; Bf = str(bf_)
                if cix == 0:
                    c.dma("sp", ut4[bf_][:], ut_d[g4 * NCH:(g4 + 1) * NCH].rearrange("c p dc e -> p c (dc e)"),
                          w=["ut4_" + Bf])
                    c.dma("sp", vt4[bf_][:],
                          vb_d[g4 * NCH * 128:(g4 + 1) * NCH * 128, :].rearrange("(c p) d -> p c d", p=128),
                          w=["vt4_" + Bf])
                pb = 4 + i0 % 4
                for dc in range(8):
                    c.op("pe", lambda e: e.matmul(B[pb][:, 0:TS], ut4[bf_][:, cix, dc * 128:(dc + 1) * 128],
                                                  h2T[:, dc, :], start=(dc == 0), stop=(dc == 7)),
                         r=["ut4_" + Bf, "h2T"], w=["b%d" % pb])

            def s7V(i0):
                g4, cix = i0 // NCH, i0 % NCH
                bf_ = g4 % 3; Bf = str(bf_)
                pb = 4 + i0 % 4
                gb = i0 % 2
                c.op("act", lambda e: e.activation(out=ge[gb][:], in_=B[pb][:, 0:TS], func=AF.Gelu),
                     r=["b%d" % pb], w=["ge%d" % gb])
                c.op("pool", lambda e: e.tensor_tensor(out=AT[gb][:], in0=ge[gb][:], in1=gate[:, i0, :], op=ALU.mult),
                     r=["ge%d" % gb, "gate"], w=["AT%d" % gb])
                for tt in range(2):
                    for hf in range(2):
                        c.op("pe", lambda e: e.matmul(B[tt * 2 + hf][:], AT[gb][:, tt * 128:(tt + 1) * 128],
                                                      vt4[bf_][:, cix, hf * 512:(hf + 1) * 512],
                                                      start=(i0 == 0), stop=(i0 == 127)),
                             r=["AT%d" % gb, "vt4_" + Bf], w=["b%d" % (tt * 2 + hf)])

            s7U(0)
            s7U(1)
            for i0 in range(128):
                if i0 + 2 < 128:
                    s7U(i0 + 2)
                s7V(i0)
            dbg(6)
            for tt in range(2):
                X = x1[tt]; Xk = "x1_%d" % tt
                for hf in range(2):
                    c.op("dve", lambda e: e.tensor_tensor(out=tmpo[:], in0=B[tt * 2 + hf][:],
                                                          in1=g2bc[:, hf * 512:(hf + 1) * 512], op=ALU.mult),
                         r=["b%d" % (tt * 2 + hf), "g2bc"], w=["tmpo"])
                    c.op("dve", lambda e: e.tensor_tensor(out=X[:, hf * 512:(hf + 1) * 512], in0=tmpo[:],
                                                          in1=X[:, hf * 512:(hf + 1) * 512], op=ALU.add),
                         r=["tmpo", Xk], w=[Xk])
                if final:
                    c.op("dve", lambda e: e.memset(ssq[:, 2 + tt:3 + tt], 0.0), w=["ssq"])
                    c.op("act", lambda e: e.activation(out=junk[:], in_=X[:], func=AF.Square,
                                                       accum_out=ssq[:, 2 + tt:3 + tt]), r=[Xk, "ssq"], w=["junk", "ssq"])
                    c.op("act", lambda e: e.activation(out=rstd[:, 2 + tt:3 + tt], in_=ssq[:, 2 + tt:3 + tt], func=AF.Sqrt,
                                                       bias=epsb[:, 0:1], scale=1.0 / D), r=["ssq", "epsb"], w=["rstd"])
                    c.op("dve", lambda e: e.reciprocal(out=rstd[:, 2 + tt:3 + tt], in_=rstd[:, 2 + tt:3 + tt]),
                         r=["rstd"], w=["rstd"])
                    c.op("dve", lambda e: e.scalar_tensor_tensor(out=X[:], in0=X[:], scalar=rstd[:, 2 + tt:3 + tt],
                                                                 in1=lnfbc[:], op0=ALU.mult, op1=ALU.mult),
                         r=[Xk, "rstd", "lnfbc"], w=[Xk])
                c.dma("sp", xo_d[tok0 + tt * 128: tok0 + (tt + 1) * 128, :], X[:], r=[Xk])

    try:
        body()
    except _Stop:
        pass


def build_L3(final):
    nc = bass.Bass("TRN2", target_bir_lowering=False)
    dt = lambda name, shape, d=F32, kind="ExternalInput": nc.dram_tensor(name, list(shape), d, kind=kind).ap()
    A = dict(x=dt("x", [TOK, D]), yT=dt("yT", [D, TOK]), c_fm=dt("c_fm", [128, 8]), ada_w=dt("ada_w", [D, 4 * D]),
             ada_b_fm=dt("ada_b_fm", [128, 48]), ln_g_fm=dt("ln_g_fm", [128, 8]), lnf_fm=dt("lnf_fm", [128, 8]),
             woutb=dt("woutb", [8, 128, D], BF16), pqb=dt("pqb", [16, 128, 8, 128], BF16),
             keys=dt("keys", [8, 2, 128, 128]), UT=dt("UT", [128, 128, 8, 128], BF16), Vb=dt("Vb", [128 * 128, D], BF16),
             ident=dt("ident", [128, 128]), ones=dt("ones", [128, 128]),
             xo=dt("xo", [TOK, D], F32, "ExternalOutput"))
    c = Ctx(nc)
    c.begin_phase("")
    emit_L3(c, A, final)
    c.end_phase()
    c.finish()
    return nc


NJC = 11
NPC = 1280


def build_fused():
    nc = bass.Bass("TRN2", target_bir_lowering=False)
    di = lambda name, shape, d=F32: nc.dram_tensor(name, list(shape), d, kind="ExternalInput").ap()
    it = lambda name, shape, d=F32: nc.dram_tensor(name, list(shape), d).ap()
    x_seq = di("x_seq", [SEQ, D]); x_mine = di("x_mine", [TOK, D]); c_fm = di("c_fm", [128, 8]); hsel = di("hsel", [128, 2])
    ada_w = di("ada_w", [2, D, 6 * D]); ada_b_fm = di("ada_b_fm", [2, 128, 48])
    ln1 = di("ln1_g_fm", [2, 128, 8]); ln2 = di("ln2_g_fm", [2, 128, 8]); lnf = di("lnf_fm", [128, 8])
    w_in = di("w_in", [2, D, NPC]); vres_down = di("vres_down", [D, 32])
    pvec = di("pvec", [2, 128, 32]); psel = di("psel", [128, 2, 4]); invdiv = di("invdiv", [128, 2, TB])
    w_up = di("w_up", [2, 64, 256]); a_up = di("a_up", [2, 64, 256]); g_up = di("g_up", [2, 128, 256])
    vres_up = di("vres_up", [32, 256]); pool_w = di("pool_w", [2, 2, 128, 128])
    cst = {k: di(k, v.shape) for k, v in l2_consts().items()}
    ones = di("ones", [128, 128])
    nex = 128 if DBG_SKIP0 else 128 * 128
    peer_u = di("peer_u", [2, nex, D]); peer_v = di("peer_v", [2, nex, D])
    w_out = di("w_out", [2, D, D]); peer_q = di("peer_q", [2, D, 2048]); keys = di("peer_keys", [2, 8, 2, 128, 128])
    xo = nc.dram_tensor("xo", [TOK, D], F32, kind="ExternalOutput").ap()
    UT = it("UT_s", [2, 128, 128, 8, 128], BF16); Vb = it("Vb_s", [2, 128 * 128, D], BF16)
    woutb = it("woutb_s", [2, 8, 128, D], BF16); pqb = it("pqb_s", [2, 16, 128, 8, 128], BF16)
    P = it("P_s", [NJC * 128, SEQ]); Yl = it("Yl_s", [512, SEQ]); Ya = it("Ya_s", [D, SEQ]); VF = it("VF_s", [256, SEQ])
    XO0 = it("XO0_s", [TOK, D]); X1 = it("X1_s", [SEQ, D])
    PAIRS = [[0, 1], [2, 3], [4, 5], [6, 7]]

    c = Ctx(nc)
    ph = [0]

    def phase(fn):
        if DBG_PHASES is not None and ph[0] >= DBG_PHASES:
            ph[0] += 1
            return
        c.begin_phase("p%d_" % ph[0]); ph[0] += 1
        fn()
        c.end_phase()

    if DBG_SKIP0:
        ph[0] += 1
    else:
        phase(lambda: emit_L0(c, dict(u=peer_u, v=peer_v, w_out=w_out, peer_q=peer_q, ident=cst["ident"],
                                       UT=UT, Vb=Vb, woutb=woutb, pqb=pqb), range(2), 128))
    for l in range(2):
        xsrc = x_seq if l == 0 else X1
        ncol = NPC if l == 0 else NPC + 32
        njc = (ncol + 127) // 128
        w_parts = [(w_in[l], 0, NPC)] + ([(vres_down, NPC, 32)] if l > 0 else [])
        for hf in range(2):
            xt_fn = {}
            if l > 0:
                xt_fn = dict(x_tile=lambda i, hf=hf: X1[((i // 4) * 2 + hf) * 512 + (i % 4) * 128:
                                                        ((i // 4) * 2 + hf) * 512 + (i % 4) * 128 + 128, :])
            phase(lambda: emit_L1(c, dict(xt_fn, x=xsrc[hf * TOK:(hf + 1) * TOK], c_fm=c_fm, ada_w=ada_w[l][:, 0:2 * D],
                                           ada_b_fm=ada_b_fm[l], ln_g_fm=ln1[l], w_parts=w_parts, ident=cst["ident"],
                                           out=P[0:njc * 128, hf * TOK:(hf + 1) * TOK], ncol=ncol)))
        A = dict(rT=P[0:256], kT=P[256:512], vT=P[512:768], waT=P[768:896], gdT=P[896:1024], poolT=P[1024:1280],
                 pvec=pvec[l], wup=w_up[l], aup=a_up[l], gup=g_up[l], poolw=pool_w[l], psel=psel, invdiv=invdiv,
                 yrw=Yl[0:256], ypl=Yl[256:512])
        A.update(cst)
        if l == 0:
            A["vout"] = VF
        else:
            A.update(vdT=P[1280:1312], vfT=VF, vup=vres_up)
        phase(lambda: emit_L2(c, A, l > 0))
        if DBG_PHASES is None or DBG_PHASES > ph[0]:
            for k in range(4):
                c.collective(lambda gq: gq.collective_compute(
                    "AllGather", ALU.bypass, replica_groups=PAIRS,
                    ins=[Yl[k * 128:(k + 1) * 128].opt()], outs=[Ya[k * 256:(k + 1) * 256].opt()]))
        phase(lambda: emit_L3(c, dict(x=(x_mine if l == 0 else XO0), yA=Ya[:, 0:TOK], yB=Ya[:, TOK:2 * TOK], hsel=hsel,
                                       c_fm=c_fm, ada_w=ada_w[l][:, 2 * D:6 * D], ada_b_fm=ada_b_fm[l], ln_g_fm=ln2[l],
                                       lnf_fm=lnf, woutb=woutb[l], pqb=pqb[l], keys=keys[l], UT=UT[l], Vb=Vb[l],
                                       ident=cst["ident"], ones=ones, xo=(XO0 if l == 0 else xo)), l == 1))
        if l == 0 and DBG_COLL and (DBG_PHASES is None or DBG_PHASES > ph[0]):
            for k in range(4):
                c.collective(lambda gq: gq.collective_compute(
                    "AllGather", ALU.bypass, replica_groups=PAIRS,
                    ins=[XO0[k * 512:(k + 1) * 512].opt()], outs=[X1[k * 1024:(k + 1) * 1024].opt()]))
    c.finish()
    return nc


def kernel_fused(inp):
    f = lambda a: np.ascontiguousarray(a, dtype=np.float32)
    cst = l2_consts()
    blocks = []
    for k in range(4):
        for r in range(2):
            blocks.append((r * 256 + k * 128) if k < 2 else (512 + r * 256 + (k - 2) * 128))
    perm = np.concatenate([np.arange(b0, b0 + 128) for b0 in blocks])
    shared = dict(ada_w=f(inp["ada_w"]), ada_b_fm=f(np.stack([_fm(inp["ada_b"][l]) for l in range(2)])),
                  ln1_g_fm=f(np.stack([_fm(inp["ln1_g"][l]) for l in range(2)])),
                  ln2_g_fm=f(np.stack([_fm(inp["ln2_g"][l]) for l in range(2)])), lnf_fm=_fm(inp["lnf_g"]),
                  vres_down=f(inp["vres_down"][0]),
                  ones=np.ones((128, 128), np.float32), peer_u=f(inp["peer_u"]), peer_v=f(inp["peer_v"]),
                  w_out=f(inp["w_out"][:, perm, :]), peer_q=f(inp["peer_q"]), peer_keys=f(inp["peer_keys"]))
    shared.update(cst)
    dummyT = np.zeros((2336, 1), np.float32)
    per_g = []
    for g in range(2):
        cs = slice(g * 256, (g + 1) * 256)
        cols = np.concatenate([np.arange(g * 256, (g + 1) * 256), 512 + np.arange(g * 256, (g + 1) * 256),
                               1024 + np.arange(g * 256, (g + 1) * 256), np.arange(1536, 1792),
                               1792 + np.arange(g * 256, (g + 1) * 256)])
        d0 = l2_inputs(inp, 0, g, dummyT, None)
        d1 = l2_inputs(inp, 1, g, dummyT, np.zeros((1, 1), np.float32))
        per_g.append(dict(w_in=f(inp["w_in"][:, :, cols]), pvec=f(np.stack([d0["pvec"], d1["pvec"]])),
                          psel=d0["psel"], invdiv=d0["invdiv"], w_up=f(inp["w_up"][:, :, cs]),
                          a_up=f(inp["a_up"][:, :, cs]), g_up=f(inp["g_up"][:, :, cs]),
                          vres_up=f(inp["vres_up"][0][:, cs]), pool_w=f(inp["pool_w"][:, 2 * g:2 * g + 2])))
    x = f(inp["x"])
    in_maps = []
    for core in range(NCORE):
        b, g = core // 2, core % 2
        hs = np.zeros((128, 2), np.float32); hs[:, g] = 1.0
        m = dict(shared)
        m.update(per_g[g])
        m.update(x_seq=f(x[b]), x_mine=f(x[b, g * TOK:(g + 1) * TOK]), c_fm=_fm(inp["c"][b]), hsel=hs)
        in_maps.append(m)
    if inp.get("_only_maps") is not None:
        return in_maps
    res = _run(_prog("fused", build_fused), in_maps)
    out = np.stack([np.asarray(res[core]["xo"]) for core in range(NCORE)], axis=0)
    return np.ascontiguousarray(out.reshape(NB, SEQ, D).astype(np.float32))


_PROGS = {}


def _prog(key, fn):
    if key not in _PROGS:
        _PROGS[key] = fn()
    return _PROGS[key]


def _run(nc, in_maps):
    res = run_bass_kernel_spmd(nc, in_maps, core_ids=list(range(NCORE)))
    return res.results


FUSED = True


def kernel(**inputs):
    inp = {k: np.asarray(v) for k, v in inputs.items()}
    if FUSED:
        return kernel_fused(inp)
    f = lambda a: np.ascontiguousarray(a, dtype=np.float32)
    ident = np.eye(128, dtype=np.float32); ones = np.ones((128, 128), np.float32)
    in_maps = []
    for core in range(NCORE):
        sl = slice(core * EPC, (core + 1) * EPC)
        in_maps.append(dict(u=f(inp["peer_u"][:, sl]), v=f(inp["peer_v"][:, sl]), w_out=f(inp["w_out"]),
                            peer_q=f(inp["peer_q"]), ident=ident))
    r0 = _run(_prog("L0", build_L0), in_maps)
    UT = [np.ascontiguousarray(np.concatenate([np.asarray(r0[c_]["UT"])[l] for c_ in range(NCORE)], axis=0)) for l in range(2)]
    Vb = [np.ascontiguousarray(np.concatenate([np.asarray(r0[c_]["Vb"])[l] for c_ in range(NCORE)], axis=0)) for l in range(2)]
    woutb = [np.ascontiguousarray(np.asarray(r0[0]["woutb"])[l]) for l in range(2)]
    pqb = [np.ascontiguousarray(np.asarray(r0[0]["pqb"])[l]) for l in range(2)]
    del r0
    x = f(inp["x"]).reshape(NCORE, TOK, D)
    vfirst = None
    for l in range(2):
        wfull = inp["w_in"][l] if l == 0 else np.concatenate([inp["w_in"][l], inp["vres_down"][l - 1]], axis=1)
        ncol = wfull.shape[1]
        adab_fm = _fm(inp["ada_b"][l])
        in_maps = []
        for core in range(NCORE):
            b = core // 2
            in_maps.append(dict(x=f(x[core]), c_fm=_fm(inp["c"][b]), ada_w=f(inp["ada_w"][l][:, 0:2 * D]),
                                ada_b_fm=adab_fm, ln_g_fm=_fm(inp["ln1_g"][l]), w_full=f(wfull), ident=ident))
        r1 = _run(_prog(("L1", ncol), lambda: build_L1(ncol)), in_maps)
        in_maps = []
        for core in range(NCORE):
            b, g = core // 2, core % 2
            projT = np.concatenate([r1[2 * b]["projT"], r1[2 * b + 1]["projT"]], axis=1)
            in_maps.append(l2_inputs(inp, l, g, projT, None if l == 0 else vfirst[core]))
        del r1
        r2 = _run(_prog(("L2", l > 0), lambda: build_L2(l > 0)), in_maps)
        if l == 0:
            vfirst = [np.asarray(r2[core]["vout"]) for core in range(NCORE)]
        in_maps = []
        for core in range(NCORE):
            b, hf = core // 2, core % 2
            ts = slice(hf * TOK, (hf + 1) * TOK)
            y0, y1 = r2[2 * b]["yT"], r2[2 * b + 1]["yT"]
            yT = np.concatenate([y0[0:256, ts], y1[0:256, ts], y0[256:512, ts], y1[256:512, ts]], axis=0)
            in_maps.append(dict(x=f(x[core]), yT=f(yT), c_fm=_fm(inp["c"][b]), ada_w=f(inp["ada_w"][l][:, 2 * D:6 * D]),
                                ada_b_fm=adab_fm, ln_g_fm=_fm(inp["ln2_g"][l]), lnf_fm=_fm(inp["lnf_g"]),
                                woutb=woutb[l], pqb=pqb[l], keys=f(inp["peer_keys"][l]), UT=UT[l], Vb=Vb[l],
                                ident=ident, ones=ones))
        del r2
        r3 = _run(_prog(("L3", l == 1), lambda: build_L3(l == 1)), in_maps)
        x = np.stack([np.asarray(r3[core]["xo"]) for core in range(NCORE)], axis=0)
        del r3
    return np.ascontiguousarray(x.reshape(NB, SEQ, D).astype(np.float32))
```

```python
from contextlib import ExitStack
import numpy as np
import ml_dtypes
import concourse.bass as bass
import concourse.mybir as mybir
from concourse.bass_utils import run_bass_kernel_spmd

F32 = mybir.dt.float32
BF16 = mybir.dt.bfloat16
AF = mybir.ActivationFunctionType
ALU = mybir.AluOpType

D = 1024
SEQ = 4096
NB = 4
NCORE = 8
TOK = 2048
NORM_EPS = 1e-6
GN_EPS = 64e-5
CH = 64


class Ctx:
    NDMA = 12

    def __init__(self, nc):
        self.nc = nc
        self.stack = ExitStack()
        self.E = dict(pe=nc.tensor, act=nc.scalar, dve=nc.vector, pool=nc.gpsimd, sp=nc.sync)
        self.sem = {}
        for e in ("pe", "act", "dve", "pool"):
            self.sem[e] = self.stack.enter_context(nc.semaphore("c_" + e))
        self.cnt = {e: 0 for e in self.sem}
        self.seen = {e: {} for e in self.E}
        self.dsem = {}
        self.dval = {}
        self.dnext = {}
        for q in ("sp", "pool", "act"):
            self.dsem[q] = [self.stack.enter_context(nc.semaphore("d_%s%d" % (q, i))) for i in range(self.NDMA)]
            self.dval[q] = [0] * self.NDMA
            self.dnext[q] = 0
        self.W = {}
        self.R = {}
        self.n_ins = 0
        self.nwait = {}
        self.ccsem = self.stack.enter_context(nc.semaphore("cc_sem"))
        self.ccval = 0

    scope = None
    pfx = ""

    def begin_phase(self, pfx):
        self.scope = ExitStack()
        self.pfx = pfx

    def end_phase(self):
        self.barrier()
        self.scope.close()
        self.scope = None
        self.W = {}
        self.R = {}

    def barrier(self):
        for eng in self.E:
            for e2 in self.cnt:
                if eng == "pe" and e2 == "pe":
                    continue
                self._wait(eng, e2, self.cnt[e2])
            for q in self.dsem:
                for i in range(self.NDMA):
                    self._wait(eng, (q, i), self.dval[q][i])

    def sb(self, name, shape, dt=F32):
        st = self.scope if self.scope is not None else self.stack
        return st.enter_context(self.nc.sbuf_tensor(self.pfx + name, list(shape), dt))

    def ps(self, name, shape, dt=F32):
        st = self.scope if self.scope is not None else self.stack
        return st.enter_context(self.nc.psum_tensor(self.pfx + name, list(shape), dt))

    def _semh(self, semid):
        if semid == "cc":
            return self.ccsem
        if isinstance(semid, str):
            return self.sem[semid]
        return self.dsem[semid[0]][semid[1]]

    def _wait(self, eng, semid, val):
        if val <= 0:
            return
        if self.seen[eng].get(semid, 0) >= val:
            return
        self.E[eng].wait_ge(self._semh(semid), val)
        self.seen[eng][semid] = val
        self.n_ins += 1
        self.nwait[eng] = self.nwait.get(eng, 0) + 1

    def _deps(self, r, w):
        deps = {}
        for k in r:
            for s, v in self.W.get(k, {}).items():
                deps[s] = max(deps.get(s, 0), v)
        for k in w:
            for s, v in self.W.get(k, {}).items():
                deps[s] = max(deps.get(s, 0), v)
            for s, v in self.R.get(k, {}).items():
                deps[s] = max(deps.get(s, 0), v)
        return deps

    def _record(self, semid, val, r, w):
        for k in w:
            self.W[k] = {semid: val}
            self.R[k] = {}
        for k in r:
            self.R.setdefault(k, {})[semid] = val

    def op(self, eng, fn, r=(), w=()):
        deps = self._deps(r, w)
        for s, v in deps.items():
            if eng == "pe" and s == "pe":
                continue
            self._wait(eng, s, v)
        ins = fn(self.E[eng])
        self.cnt[eng] += 1
        ins.then_inc(self.sem[eng], 1)
        self.n_ins += 1
        self._record(eng, self.cnt[eng], r, w)
        return ins

    def dma(self, q, out, in_, r=(), w=(), **kw):
        i = self.dnext[q]
        self.dnext[q] = (i + 1) % self.NDMA
        semid = (q, i)
        self._wait(q, semid, self.dval[q][i])
        deps = self._deps(r, w)
        for s, v in deps.items():
            self._wait(q, s, v)
        ins = self.E[q].dma_start(out=out, in_=in_, **kw)
        self.dval[q][i] += 16
        ins.then_inc(self.dsem[q][i], 16)
        self.n_ins += 1
        self._record(semid, self.dval[q][i], r, w)
        return ins

    def collective(self, fn):
        self.barrier()
        ins = fn(self.nc.gpsimd)
        self.ccval += 1
        ins.then_inc(self.ccsem)
        for eng in self.E:
            self._wait(eng, "cc", self.ccval)

    def finish(self):
        global LAST_CNT
        LAST_CNT = (dict(self.cnt), {q: list(v) for q, v in self.dval.items()}, self.n_ins, dict(self.nwait))
        for q in self.dsem:
            for i in range(self.NDMA):
                self._wait("sp", (q, i), self.dval[q][i])
        for e in self.cnt:
            self._wait("sp", e, self.cnt[e])
        self.stack.close()


def _fm(vec, n=None):
    v = np.ascontiguousarray(np.asarray(vec, dtype=np.float32).reshape(-1, 128).T)
    return v


def emit_mod_fm(c, adaw_dram, col0, ncolchunks, silu_c, adab_fm_sb, adab_col0, out_sb, ps_tile, tag):
    nc = c.nc
    wv = adaw_dram.rearrange("(dc p) j -> p dc j", p=128)
    cap = c.adaw_sb.shape[2] // 128
    for half in range(0, ncolchunks, cap):
        nch = min(cap, ncolchunks - half)
        wt = c.adaw_sb
        c.dma("sp", wt[:, :, 0:nch * 128], wv[:, :, col0 + half * 128: col0 + (half + nch) * 128],
              w=["adaw"])
        for jc in range(nch):
            for dc in range(8):
                c.op("pe", lambda e, jc=jc, dc=dc: e.matmul(
                    ps_tile[:, half + jc: half + jc + 1], wt[:, dc, jc * 128:(jc + 1) * 128],
                    silu_c[:, dc:dc + 1], start=(dc == 0), stop=(dc == 7)),
                    r=["adaw", "silu_c"], w=[tag + "_ps"])
    c.op("dve", lambda e: e.tensor_tensor(out=out_sb, in0=ps_tile[:, 0:ncolchunks],
                                           in1=adab_fm_sb[:, adab_col0: adab_col0 + ncolchunks], op=ALU.add),
         r=[tag + "_ps", "adab"], w=[tag])


DBG_STAGE = 99
DBG_PHASES = None
DBG_COLL = True
DBG_SKIP0 = False
LAST_CNT = None
DBG_NST = None


def emit_L1(c, A):
    ncol = A["ncol"]
    njc = (ncol + 127) // 128
    x_d, c_d, adaw_d, adab_d, g_d, id_d, out_d = (A["x"], A["c_fm"], A["ada_w"], A["ada_b_fm"], A["ln_g_fm"],
                                                   A["ident"], A["out"])
    c.adaw_sb = c.sb("adaw", [128, 8, 1024], F32)
    c_sb = c.sb("c_sb", [128, 8]); sig_c = c.sb("sig_c", [128, 8]); silu_c = c.sb("silu_c", [128, 8])
    adab = c.sb("adab", [128, 48]); lng = c.sb("lng", [128, 8])
    modT = c.sb("modT", [128, 16]); effs = c.sb("effs", [128, 8])
    identf = c.sb("identf", [128, 128]); identb = c.sb("identb", [128, 128], BF16)
    wbf = c.sb("wbf", [128, 8, njc * 128], BF16)
    hT = c.sb("hT", [128, 8, TOK], BF16)
    xt = [c.sb("xt%d" % i, [128, D]) for i in range(2)]
    junk = c.sb("junk", [128, D], BF16)
    xn = [c.sb("xn%d" % i, [128, D], BF16) for i in range(2)]
    ss = c.sb("ss", [128, 32]); rstd = c.sb("rstd", [128, 32])
    stg = [c.sb("stg%d" % i, [128, TOK]) for i in range(2)]
    mod_ps = c.ps("mod_ps", [128, 512])
    tp_ps = [c.ps("tp_ps%d" % i, [128, 1024], BF16) for i in range(2)]
    mm_ps = [c.ps("mm_ps%d" % i, [128, 512]) for i in range(4)]

    c.dma("sp", c_sb[:], c_d, w=["c_sb"])
    c.dma("sp", adab[:], adab_d, w=["adab"])
    c.dma("sp", lng[:], g_d, w=["lng"])
    c.dma("sp", identf[:], id_d, w=["identf"])
    c.op("dve", lambda e: e.tensor_copy(identb[:], identf[:]), r=["identf"], w=["identb"])
    if njc * 128 != ncol:
        c.op("pool", lambda e: e.memset(wbf[:, :, ncol:njc * 128], 0.0), w=["wbf"])
    for (w_ap, col0, ncols) in A["w_parts"]:
        wv = w_ap.rearrange("(dc p) j -> p dc j", p=128)
        for dc in range(8):
            c.dma("pool", wbf[:, dc, col0:col0 + ncols], wv[:, dc, :], w=["wbf"])
    c.op("act", lambda e: e.activation(out=sig_c[:], in_=c_sb[:], func=AF.Sigmoid), r=["c_sb"], w=["sig_c"])
    c.op("dve", lambda e: e.tensor_tensor(out=silu_c[:], in0=c_sb[:], in1=sig_c[:], op=ALU.mult),
         r=["c_sb", "sig_c"], w=["silu_c"])
    emit_mod_fm(c, adaw_d, 0, 16, silu_c, adab, 0, modT[:, 0:16], mod_ps, "modT")
    c.op("dve", lambda e: e.scalar_tensor_tensor(out=effs[:], in0=modT[:, 8:16], scalar=1.0, in1=lng[:],
                                                  op0=ALU.add, op1=ALU.mult), r=["modT", "lng"], w=["effs"])
    c.op("dve", lambda e: e.memset(ss[:], 0.0), w=["ss%d" % i for i in range(TOK // 128)])
    epsb = c.sb("epsb", [128, 1])
    c.op("dve", lambda e: e.memset(epsb[:], NORM_EPS), w=["epsb"])
    ntile = TOK // 128
    for i in range(ntile):
        s = i % 2
        c.dma("sp", xt[s][:], (A["x_tile"](i) if "x_tile" in A else x_d[i * 128:(i + 1) * 128, :]), w=["xt%d" % s])
        c.op("act", lambda e, s=s, i=i: e.activation(out=junk[:], in_=xt[s][:], func=AF.Square,
                                                     accum_out=ss[:, i:i + 1]),
             r=["xt%d" % s], w=["junk", "ss%d" % i])
        c.op("act", lambda e, i=i: e.activation(out=rstd[:, i:i + 1], in_=ss[:, i:i + 1], func=AF.Sqrt,
                                                bias=epsb[:, 0:1], scale=1.0 / D),
             r=["ss%d" % i, "epsb"], w=["rstd%d" % i])
        c.op("dve", lambda e, i=i: e.reciprocal(out=rstd[:, i:i + 1], in_=rstd[:, i:i + 1]),
             r=["rstd%d" % i], w=["rstd%d" % i])
        c.op("dve", lambda e, s=s, i=i: e.tensor_scalar(out=xn[s][:], in0=xt[s][:], scalar1=rstd[:, i:i + 1],
                                                        scalar2=None, op0=ALU.mult),
             r=["xt%d" % s, "rstd%d" % i], w=["xn%d" % s])
        for dc in range(8):
            c.op("pe", lambda e, s=s, dc=dc: e.transpose(tp_ps[s][:, dc * 128:(dc + 1) * 128],
                                                         xn[s][:, dc * 128:(dc + 1) * 128], identb[:]),
                 r=["xn%d" % s, "identb"], w=["tp%d" % s])
        for dc in range(8):
            if s == 0:
                c.op("act", lambda e, s=s, dc=dc, i=i: e.activation(
                    out=hT[:, dc, i * 128:(i + 1) * 128], in_=tp_ps[s][:, dc * 128:(dc + 1) * 128],
                    func=AF.Identity, bias=modT[:, dc:dc + 1], scale=effs[:, dc:dc + 1]),
                    r=["tp%d" % s, "modT", "effs"], w=["hT_%d_%d_%d" % (dc, i // 4, s)])
            else:
                c.op("dve", lambda e, s=s, dc=dc, i=i: e.tensor_scalar(
                    out=hT[:, dc, i * 128:(i + 1) * 128], in0=tp_ps[s][:, dc * 128:(dc + 1) * 128],
                    scalar1=effs[:, dc:dc + 1], scalar2=modT[:, dc:dc + 1], op0=ALU.mult, op1=ALU.add),
                    r=["tp%d" % s, "modT", "effs"], w=["hT_%d_%d_%d" % (dc, i // 4, s)])
    k = 0
    for jc in range(njc):
        st = stg[jc % 2]
        for tb in range(TOK // 512):
            pt = mm_ps[k % 4]; pk = "mm%d" % (k % 4); k += 1
            for dc in range(8):
                c.op("pe", lambda e, pt=pt, dc=dc, jc=jc, tb=tb: e.matmul(
                    pt[:], wbf[:, dc, jc * 128:(jc + 1) * 128], hT[:, dc, tb * 512:(tb + 1) * 512],
                    start=(dc == 0), stop=(dc == 7)),
                    r=["wbf", "hT_%d_%d_0" % (dc, tb), "hT_%d_%d_1" % (dc, tb)], w=[pk])
            eng = "act" if tb % 2 == 0 else "dve"
            if eng == "act":
                c.op("act", lambda e, pt=pt, st=st, tb=tb: e.copy(out=st[:, tb * 512:(tb + 1) * 512], in_=pt[:]),
                     r=[pk], w=["stg%d_%d" % (jc % 2, tb)])
            else:
                c.op("dve", lambda e, pt=pt, st=st, tb=tb: e.tensor_copy(st[:, tb * 512:(tb + 1) * 512], pt[:]),
                     r=[pk], w=["stg%d_%d" % (jc % 2, tb)])
        c.dma("sp", out_d[jc * 128:(jc + 1) * 128, :], st[:],
              r=["stg%d_%d" % (jc % 2, tb) for tb in range(4)])


def build_L1(ncol):
    nc = bass.Bass("TRN2", target_bir_lowering=False)
    njc = (ncol + 127) // 128
    di = lambda name, shape: nc.dram_tensor(name, list(shape), F32, kind="ExternalInput").ap()
    A = dict(x=di("x", [TOK, D]), c_fm=di("c_fm", [128, 8]), ada_w=di("ada_w", [D, 2 * D]),
             ada_b_fm=di("ada_b_fm", [128, 48]), ln_g_fm=di("ln_g_fm", [128, 8]), ident=di("ident", [128, 128]),
             ncol=ncol)
    A["w_parts"] = [(di("w_full", [D, ncol]), 0, ncol)]
    A["out"] = nc.dram_tensor("projT", [njc * 128, TOK], F32, kind="ExternalOutput").ap()
    c = Ctx(nc)
    c.begin_phase("")
    emit_L1(c, A)
    c.end_phase()
    c.finish()
    return nc


class _Stop(Exception):
    pass


def dbg(stage):
    if DBG_STAGE == stage:
        raise _Stop()


TB = 512
NBLK = SEQ // TB
CPB = TB // CH
C0 = float(np.exp(-0.5))
POOL_WINDOWS = (2, 4, 8, 16)


def l2_consts():
    s_idx = np.arange(64)[:, None]; t_idx = np.arange(64)[None, :]
    su = (t_idx > s_idx).astype(np.float32); iu = (t_idx >= s_idx).astype(np.float32)
    maskA = np.tile(np.concatenate([su, iu], axis=1), (1, 4))
    maskC = np.tile((t_idx < s_idx).astype(np.float32), (1, 4))
    identrep = np.tile(np.eye(64, dtype=np.float32), (1, 4))
    scanmask = np.ones((128, TB), np.float32); scanmask[:, ::CH] = 0.0
    blockones = np.kron(np.eye(2, dtype=np.float32), np.ones((64, 64), np.float32))
    return dict(maskA=maskA, maskC=maskC, identrep=identrep, scanmask=scanmask, blockones=blockones,
                ident=np.eye(128, dtype=np.float32))


def emit_L2(c, A, layer1):
    nc = c.nc
    rT, kT, vT, waT, gdT, poolT = A["rT"], A["kT"], A["vT"], A["waT"], A["gdT"], A["poolT"]
    if layer1:
        vdT, vfT, vup_d = A["vdT"], A["vfT"], A["vup"]
    pvec_d, wup_d, aup_d, gup_d, poolw_d = A["pvec"], A["wup"], A["aup"], A["gup"], A["poolw"]
    maskA_d, maskC_d, identrep_d = A["maskA"], A["maskC"], A["identrep"]
    scanmask_d, bo_d, id_d, invdiv_d, psel_d = A["scanmask"], A["blockones"], A["ident"], A["invdiv"], A["psel"]
    yrw, ypl = A["yrw"], A["ypl"]
    if not layer1:
        vout = A["vout"]
    sb = c.sb
    pvec = sb("pvec_sb", [128, 32]); omk = sb("omk", [128, 2])
    wup = sb("wup_sb", [128, 256], BF16); aup = sb("aup_sb", [128, 256], BF16); gup = sb("gup_sb", [128, 256], BF16)
    vup = sb("vup_sb", [32, 256], BF16); poolw = sb("poolw_sb", [128, 2, 128], BF16)
    maskA = sb("maskA_sb", [64, 512]); maskC = sb("maskC_sb", [64, 256]); identrep = sb("identrep_sb", [64, 256])
    scanmask = sb("scanmask_sb", [128, TB]); bo = sb("bo_sb", [128, 128]); identf = sb("identf", [128, 128])
    identb = sb("identb", [128, 128], BF16); invdiv = sb("invdiv_sb", [128, 2, TB])
    epsk = sb("epsk", [128, 1]); gneps = sb("gneps", [128, 1]); psel = sb("psel_sb", [128, 2, 4])
    Xwa = sb("Xwa", [128, TB + 1]); Xg = sb("Xg", [128, TB + 1]); Xvd = sb("Xvd", [32, TB + 1])
    dwa = sb("dwa", [128, TB]); swa = sb("swa", [128, TB]); th = sb("th", [64, TB], BF16); adb = sb("adb", [128, TB], BF16)
    dg = sb("dg", [128, TB]); sgd = sb("sgd", [128, TB]); sg = sb("sg", [128, TB], BF16)
    dvd = sb("dvd", [32, TB]); vdb = sb("vdb", [32, TB], BF16)
    P2 = range(2)
    Xr = [sb("Xr_sh", [128, TB + 1])] * 2; Xk = [sb("Xk_sh", [128, TB + 1])] * 2
    Xv = [sb("Xv_sh", [128, TB + 1])] * 2; Xvf = [sb("Xvf_sh", [128, TB])] * 2
    tmpd = [sb("tmpd_sh", [128, TB])] * 2
    r_s = [sb("r_s%d" % i, [128, TB]) for i in P2]; k_s = [sb("k_s%d" % i, [128, TB]) for i in P2]
    v_s = [sb("v_s%d" % i, [128, TB]) for i in P2]
    sigw = [sb("sigw%d" % i, [128, TB]) for i in P2]; cum = [sb("cum%d" % i, [128, TB]) for i in P2]
    cumx = [sb("cumx_sh", [128, TB])] * 2
    G = [sb("G%d" % i, [128, TB]) for i in P2]; Ginv = [sb("Ginv%d" % i, [128, TB]) for i in P2]
    Gex = [sb("Gex%d" % i, [128, TB]) for i in P2]
    a_ = [sb("a_%d" % i, [128, TB]) for i in P2]; gg = [sb("gg%d" % i, [128, TB]) for i in P2]
    vsig = [sb("vsig_sh", [128, TB])] * 2
    kkraw = [sb("kkraw_sh", [128, TB])] * 2; sq = [sb("sq_sh", [128, TB])] * 2
    rn = [sb("rn_sh", [128, TB])] * 2; kk = [sb("kk%d" % i, [128, TB]) for i in P2]
    fac = [sb("fac_sh", [128, TB])] * 2; kmod = [sb("kmod%d" % i, [128, TB]) for i in P2]
    rk2 = [sb("rk2_sh", [128, TB])] * 2; bonus = [sb("bonus%d" % i, [128, TB]) for i in P2]
    t1 = [sb("t1_sh", [128, TB])] * 2
    ARt = [sb("ARt%d" % i, [128, CPB * 128], BF16) for i in P2]
    Bt = [sb("Bt%d" % i, [128, TB], BF16) for i in P2]; Kt = [sb("Kt%d" % i, [128, TB], BF16) for i in P2]
    Vb = [sb("Vb%d" % i, [128, TB], BF16) for i in P2]
    yraw = [sb("yraw%d" % i, [128, TB]) for i in P2]; yc = [sb("yc%d" % i, [128, TB]) for i in P2]
    ysq = [sb("ysq_sh", [128, TB])] * 2; yrs = [sb("yrs_sh", [128, TB])] * 2
    yo = [sb("yo%d" % i, [128, TB]) for i in P2]
    M0b = [[sb("M0b%d_%d" % (i, j), [64, 64], BF16) for j in range(2)] for i in range(4)]
    ARo = [sb("ARo%d" % i, [64, CPB * 128], BF16) for i in P2]
    Bo = [sb("Bo%d" % i, [64, TB], BF16) for i in P2]; Ko = [sb("Ko%d" % i, [64, TB], BF16) for i in P2]
    GCs = [sb("GCs%d" % i, [128, CPB]) for i in P2]; GCo = [sb("GCo%d" % i, [64, CPB]) for i in P2]
    tokmaj = [sb("tokmaj%d" % j, [64, 768], BF16) for j in range(2)]
    SA = [sb("SA%d" % j, [64, 512], BF16) for j in range(2)]; SB_ = [sb("SB%d" % j, [64, 512], BF16) for j in range(2)]
    SC = [sb("SC%d" % j, [64, 256], BF16) for j in range(2)]
    TT = [sb("TT%d" % j, [64, 256], BF16) for j in range(2)]; PP = [sb("PP%d" % j, [64, 512], BF16) for j in range(2)]
    W1 = sb("W1", [64, 256], BF16); U = [sb("U%d" % j, [64, 256], BF16) for j in range(2)]
    TTFs = [sb("TTF%d" % j, [64, 256], BF16) for j in range(2)]
    Xp = [sb("Xp%d" % i, [128, TB + 15]) for i in P2]
    plv = [[sb("plv%d_%d" % (i, k), [128, TB + 15]) for k in range(4)] for i in P2]
    pacc = [sb("pacc%d" % i, [128, TB]) for i in P2]
    pd = [sb("pd%d" % i, [128, TB], BF16) for i in P2]; pm = [sb("pm%d" % i, [128, TB]) for i in P2]
    po = [sb("po%d" % i, [128, TB]) for i in P2]
    bk = {n: c.ps("bk_" + n, [128, 512]) for n in ("A", "B", "C", "D", "E", "FG", "HI")}
    trp = c.ps("bk_trp", [128, 1024], BF16)

    ld = lambda dst, src, key: c.dma("sp", dst, src, w=[key])
    ld(pvec[:], pvec_d, "pvec"); ld(maskA[:], maskA_d, "maskA"); ld(maskC[:], maskC_d, "maskC")
    ld(identrep[:], identrep_d, "identrep"); ld(scanmask[:], scanmask_d, "scanmask"); ld(bo[:], bo_d, "bo")
    ld(identf[:], id_d, "identf"); ld(invdiv[:], invdiv_d, "invdiv"); ld(psel[:], psel_d, "psel")
    c.dma("pool", wup[0:64, :], wup_d, w=["wup"]); c.dma("pool", aup[64:128, :], aup_d, w=["aup"])
    c.dma("pool", gup[:], gup_d, w=["gup"]); c.dma("pool", poolw[:], poolw_d.rearrange("g c d -> c g d"), w=["poolw"])
    if layer1:
        c.dma("pool", vup[0:32, :], vup_d, w=["vup"])
    c.op("dve", lambda e: e.tensor_copy(identb[:], identf[:]), r=["identf"], w=["identb"])
    c.op("dve", lambda e: e.memset(epsk[:], 1e-24), w=["epsk"])
    c.op("dve", lambda e: e.memset(gneps[:], GN_EPS), w=["gneps"])
    c.op("dve", lambda e: e.tensor_scalar(out=omk[:], in0=pvec[:, 12:14], scalar1=-1.0, scalar2=1.0,
                                          op0=ALU.mult, op1=ALU.add), r=["pvec"], w=["omk"])
    for i in range(4):
        for j in range(2):
            c.op("dve", lambda e, i=i, j=j: e.memset(M0b[i][j][:], 0.0), w=["M0b%d_%d" % (i, j)])
    for i in P2:
        for k_ in range(4):
            c.op("pool", lambda e, i=i, k_=k_: e.memset(plv[i][k_][:], 0.0), w=["plv%d_%d" % (i, k_)])
    pv = lambda col, hp=0: pvec[:, col + hp: col + hp + 1]

    def tshift(eng, X, d, out, mu, n, kX, kd, kout):
        c.op(eng, lambda e: e.tensor_tensor(out=d, in0=X[0:n, 0:TB], in1=X[0:n, 1:TB + 1], op=ALU.subtract),
             r=[kX], w=[kd])
        if eng == "dve":
            c.op(eng, lambda e: e.scalar_tensor_tensor(out=out, in0=d, scalar=mu, in1=X[0:n, 1:TB + 1],
                                                       op0=ALU.mult, op1=ALU.add), r=[kX, kd, "pvec"], w=[kout])
        else:
            c.op(eng, lambda e: e.tensor_scalar(out=d, in0=d, scalar1=mu, scalar2=None, op0=ALU.mult),
                 r=[kd, "pvec"], w=[kd])
            c.op(eng, lambda e: e.tensor_tensor(out=out, in0=d, in1=X[0:n, 1:TB + 1], op=ALU.add),
                 r=[kX, kd], w=[kout])

    def load_halo(X, src, rows, b, n, key, halo=1):
        t0 = b * TB
        if b == 0:
            c.op("pool", lambda e: e.memset(X[0:n, 0:halo], 0.0), w=[key])
            c.dma("sp", X[0:n, halo:halo + TB], src[rows, 0:TB], w=[key])
        else:
            c.dma("sp", X[0:n, :], src[rows, t0 - halo:t0 + TB], w=[key])

    chunk_counter = [0]

    def body():
        for b in range(NBLK):
            t0 = b * TB
            load_halo(Xwa, waT, slice(0, 128), b, 128, "Xwa")
            load_halo(Xg, gdT, slice(0, 128), b, 128, "Xg")
            tshift("pool", Xwa, dwa[:], swa[:], pv(25), 128, "Xwa", "dwa", "swa")
            c.op("act", lambda e: e.activation(out=th[:], in_=swa[0:64, :], func=AF.Tanh), r=["swa"], w=["th"])
            c.op("pool", lambda e: e.tensor_copy(adb[64:128, :], swa[64:128, :]), r=["swa"], w=["adb"])
            tshift("pool", Xg, dg[:], sgd[:], pv(24), 128, "Xg", "dg", "sgd")
            c.op("act", lambda e: e.activation(out=sg[:], in_=sgd[:], func=AF.Sigmoid), r=["sgd"], w=["sg"])
            if layer1:
                load_halo(Xvd, vdT, slice(0, 32), b, 32, "Xvd")
                tshift("pool", Xvd, dvd[:], vdb[:], pvec[0:32, 26:27], 32, "Xvd", "dvd", "vdb")
            dbg(1)
            for hp in P2:
                rows = slice(hp * 128, (hp + 1) * 128)
                H = str(hp)
                load_halo(Xr[hp], rT, rows, b, 128, "Xr")
                load_halo(Xk[hp], kT, rows, b, 128, "Xk")
                load_halo(Xv[hp], vT, rows, b, 128, "Xv")
                tshift("dve", Xr[hp], tmpd[hp][:], r_s[hp][:], pv(0, hp), 128, "Xr", "tmpd", "r_s" + H)
                tshift("dve", Xk[hp], tmpd[hp][:], k_s[hp][:], pv(2, hp), 128, "Xk", "tmpd", "k_s" + H)
                tshift("dve", Xv[hp], tmpd[hp][:], v_s[hp][:], pv(4, hp), 128, "Xv", "tmpd", "v_s" + H)
                cols = slice(hp * 128, (hp + 1) * 128)
                c.op("pe", lambda e: e.matmul(bk["A"][:], wup[0:64, cols], th[:], start=True, stop=True),
                     r=["wup", "th"], w=["bkA"])
                c.op("act", lambda e: e.activation(out=sigw[hp][:], in_=bk["A"][:], func=AF.Sigmoid, bias=pv(6, hp)),
                     r=["bkA", "pvec"], w=["sigw" + H])
                c.op("pe", lambda e: e.matmul(bk["B"][:], aup[64:128, cols], adb[64:128, :], start=True, stop=True),
                     r=["aup", "adb"], w=["bkB"])
                c.op("act", lambda e: e.activation(out=a_[hp][:], in_=bk["B"][:], func=AF.Sigmoid, bias=pv(8, hp)),
                     r=["bkB", "pvec"], w=["a_" + H])
                c.op("pe", lambda e: e.matmul(bk["C"][:], gup[:, cols], sg[:], start=True, stop=True),
                     r=["gup", "sg"], w=["bkC"])
                c.op("act", lambda e: e.copy(out=gg[hp][:], in_=bk["C"][:]), r=["bkC"], w=["gg" + H])
                if layer1:
                    c.dma("sp", Xvf[hp][:], vfT[rows, t0:t0 + TB], w=["Xvf"])
                    c.op("pe", lambda e: e.matmul(bk["D"][:], vup[0:32, cols], vdb[0:32, :], start=True, stop=True),
                         r=["vup", "vdb"], w=["bkD"])
                    c.op("act", lambda e: e.activation(out=vsig[hp][:], in_=bk["D"][:], func=AF.Sigmoid, bias=pv(27, hp)),
                         r=["bkD", "pvec"], w=["vsig"])
                    c.op("dve", lambda e: e.tensor_tensor(out=tmpd[hp][:], in0=Xvf[hp][:], in1=v_s[hp][:], op=ALU.subtract),
                         r=["Xvf", "v_s" + H], w=["tmpd"])
                    c.op("dve", lambda e: e.tensor_tensor(out=tmpd[hp][:], in0=tmpd[hp][:], in1=vsig[hp][:], op=ALU.mult),
                         r=["vsig", "tmpd"], w=["tmpd"])
                    c.op("dve", lambda e: e.tensor_tensor(out=v_s[hp][:], in0=v_s[hp][:], in1=tmpd[hp][:], op=ALU.add),
                         r=["v_s" + H, "tmpd"], w=["v_s" + H])
                else:
                    c.dma("sp", vout[rows, t0:t0 + TB], v_s[hp][:], r=["v_s" + H])
                c.op("dve", lambda e: e.tensor_scalar(out=kkraw[hp][:], in0=k_s[hp][:], scalar1=pv(10, hp), scalar2=None,
                                                      op0=ALU.mult), r=["k_s" + H, "pvec"], w=["kkraw"])
                c.op("pool", lambda e: e.tensor_tensor(out=sq[hp][:], in0=kkraw[hp][:], in1=kkraw[hp][:], op=ALU.mult),
                     r=["kkraw"], w=["sq"])
                c.op("pe", lambda e: e.matmul(bk["E"][:], bo[:], sq[hp][:], start=True, stop=True),
                     r=["bo", "sq"], w=["bkE"])
                c.op("act", lambda e: e.activation(out=rn[hp][:], in_=bk["E"][:], func=AF.Sqrt, bias=epsk[:, 0:1]),
                     r=["bkE", "epsk"], w=["rn"])
                c.op("dve", lambda e: e.reciprocal(out=rn[hp][:], in_=rn[hp][:]), r=["rn"], w=["rn"])
                c.op("dve", lambda e: e.tensor_tensor(out=kk[hp][:], in0=kkraw[hp][:], in1=rn[hp][:], op=ALU.mult),
                     r=["kkraw", "rn"], w=["kk" + H])
                c.op("pool", lambda e: e.tensor_scalar(out=fac[hp][:], in0=a_[hp][:], scalar1=pv(12, hp),
                                                       scalar2=omk[:, hp:hp + 1], op0=ALU.mult, op1=ALU.add),
                     r=["a_" + H, "pvec", "omk"], w=["fac"])
                c.op("pool", lambda e: e.tensor_tensor(out=kmod[hp][:], in0=k_s[hp][:], in1=fac[hp][:], op=ALU.mult),
                     r=["k_s" + H, "fac"], w=["kmod" + H])
                c.op("pool", lambda e: e.tensor_scalar(out=rk2[hp][:], in0=r_s[hp][:], scalar1=pv(16, hp), scalar2=None,
                                                       op0=ALU.mult), r=["r_s" + H, "pvec"], w=["rk2"])
                c.op("pool", lambda e: e.tensor_tensor(out=rk2[hp][:], in0=rk2[hp][:], in1=kmod[hp][:], op=ALU.mult),
                     r=["rk2", "kmod" + H], w=["rk2"])
                c.op("pe", lambda e: e.matmul(bk["FG"][:], bo[:], rk2[hp][:], start=True, stop=True),
                     r=["bo", "rk2"], w=["bkFG"])
                c.op("dve", lambda e: e.tensor_tensor(out=bonus[hp][:], in0=bk["FG"][:], in1=v_s[hp][:], op=ALU.mult),
                     r=["bkFG", "v_s" + H], w=["bonus" + H])
                c.op("dve", lambda e: e.tensor_tensor_scan(out=cum[hp][:], data0=scanmask[:], data1=sigw[hp][:],
                                                           initial=0.0, op0=ALU.mult, op1=ALU.add),
                     r=["scanmask", "sigw" + H], w=["cum" + H])
                c.op("pool", lambda e: e.tensor_tensor(out=cumx[hp][:], in0=cum[hp][:], in1=sigw[hp][:], op=ALU.subtract),
                     r=["cum" + H, "sigw" + H], w=["cumx"])
                c.op("act", lambda e: e.activation(out=G[hp][:], in_=cum[hp][:], func=AF.Exp, scale=-C0),
                     r=["cum" + H], w=["G" + H])
                c.op("act", lambda e: e.activation(out=Ginv[hp][:], in_=cum[hp][:], func=AF.Exp, scale=C0),
                     r=["cum" + H], w=["Ginv" + H])
                c.op("act", lambda e: e.activation(out=Gex[hp][:], in_=cumx[hp][:], func=AF.Exp, scale=-C0),
                     r=["cumx"], w=["Gex" + H])
                AR3 = ARt[hp][:].rearrange("p (c two t) -> p c two t", two=2, t=CH)
                v3 = lambda ap: ap.rearrange("p (c t) -> p c t", t=CH)
                c.op("dve", lambda e: e.tensor_tensor(out=AR3[:, :, 1, :], in0=v3(r_s[hp][:]), in1=v3(G[hp][:]), op=ALU.mult),
                     r=["r_s" + H, "G" + H], w=["ARt" + H])
                c.op("dve", lambda e: e.scalar_tensor_tensor(out=AR3[:, :, 0, :], in0=v3(kk[hp][:]), scalar=-1.0,
                                                             in1=v3(Gex[hp][:]), op0=ALU.mult, op1=ALU.mult),
                     r=["kk" + H, "Gex" + H], w=["ARt" + H])
                c.op("pool", lambda e: e.tensor_tensor(out=t1[hp][:], in0=kk[hp][:], in1=a_[hp][:], op=ALU.mult),
                     r=["kk" + H, "a_" + H], w=["t1"])
                c.op("dve", lambda e: e.tensor_tensor(out=Bt[hp][:], in0=t1[hp][:], in1=Ginv[hp][:], op=ALU.mult),
                     r=["t1", "Ginv" + H], w=["Bt" + H])
                c.op("pool", lambda e: e.tensor_tensor(out=Kt[hp][:], in0=kmod[hp][:], in1=Ginv[hp][:], op=ALU.mult),
                     r=["kmod" + H, "Ginv" + H], w=["Kt" + H])
                c.op("pool", lambda e: e.tensor_copy(Vb[hp][:], v_s[hp][:]), r=["v_s" + H], w=["Vb" + H])
                c.op("pool", lambda e: e.tensor_copy(GCs[hp][:], G[hp][:].rearrange("p (c t) -> p c t", t=CH)[:, :, CH - 1]),
                     r=["G" + H], w=["GCs" + H])
                c.dma("sp", ARo[hp][:], ARt[hp][64:128, :], r=["ARt" + H], w=["ARo" + H])
                c.dma("sp", Bo[hp][:], Bt[hp][64:128, :], r=["Bt" + H], w=["Bo" + H])
                c.dma("sp", Ko[hp][:], Kt[hp][64:128, :], r=["Kt" + H], w=["Ko" + H])
                c.dma("sp", GCo[hp][:], GCs[hp][64:128, :], r=["GCs" + H], w=["GCo" + H])

            dbg(2)
            for gi in P2:
                Gk = str(gi)
                load_halo(Xp[gi], poolT, slice(gi * 128, (gi + 1) * 128), b, 128, "Xp" + Gk, halo=15)
                cur = Xp[gi]; curk = "Xp" + Gk
                for lv in range(4):
                    sh = 1 << lv
                    dst = plv[gi][lv]; dk = "plv%d_%d" % (gi, lv)
                    c.op("pool", lambda e: e.tensor_tensor(out=dst[:, sh:15 + TB], in0=cur[:, sh:15 + TB],
                                                           in1=cur[:, 0:15 + TB - sh], op=ALU.add), r=[curk], w=[dk])
                    cur, curk = dst, dk
                c.op("dve", lambda e: e.tensor_scalar(out=pacc[gi][:], in0=plv[gi][0][:, 15:15 + TB],
                                                      scalar1=psel[:, gi, 0:1], scalar2=None, op0=ALU.mult),
                     r=["plv%d_0" % gi, "psel"], w=["pacc" + Gk])
                for lv in range(1, 4):
                    c.op("dve", lambda e: e.scalar_tensor_tensor(out=pacc[gi][:], in0=plv[gi][lv][:, 15:15 + TB],
                                                                 scalar=psel[:, gi, lv:lv + 1], in1=pacc[gi][:],
                                                                 op0=ALU.mult, op1=ALU.add),
                         r=["plv%d_%d" % (gi, lv), "psel", "pacc" + Gk], w=["pacc" + Gk])
                if b == 0:
                    c.op("dve", lambda e: e.tensor_tensor(out=pm[gi][:], in0=pacc[gi][:], in1=invdiv[:, gi, :],
                                                          op=ALU.mult), r=["pacc" + Gk, "invdiv"], w=["pm" + Gk])
                    c.op("dve", lambda e: e.tensor_tensor(out=pd[gi][:], in0=pm[gi][:], in1=Xp[gi][:, 15:15 + TB],
                                                          op=ALU.subtract), r=["pm" + Gk, "Xp" + Gk], w=["pd" + Gk])
                else:
                    c.op("dve", lambda e: e.scalar_tensor_tensor(out=pd[gi][:], in0=pacc[gi][:],
                                                                 scalar=pv(29, gi), in1=Xp[gi][:, 15:15 + TB],
                                                                 op0=ALU.mult, op1=ALU.subtract),
                         r=["pacc" + Gk, "Xp" + Gk, "pvec"], w=["pd" + Gk])
                c.op("pe", lambda e: e.matmul(bk["D"][:], poolw[:, gi, :], pd[gi][:], start=True, stop=True),
                     r=["poolw", "pd" + Gk], w=["bkD"])
                c.op("act", lambda e: e.activation(out=po[gi][:], in_=bk["D"][:], func=AF.Identity, scale=pv(22, gi)),
                     r=["bkD", "pvec"], w=["po" + Gk])
                c.dma("sp", ypl[gi * 128:(gi + 1) * 128, t0:t0 + TB], po[gi][:], r=["po" + Gk])

            dbg(3)
            def emit_chunk(ci, cc, part):
                pp = cc % 2
                cs = slice(ci * CH, (ci + 1) * CH)
                tm = tokmaj[pp]; sa = SA[pp]; sbb = SB_[pp]; sc = SC[pp]
                tmk, sak, sbk, sck = "tokmaj%d" % pp, "SA%d" % pp, "SB%d" % pp, "SC%d" % pp
                TTF = TTFs[pp]; TTFk = "TTF%d" % pp
                def ARv(h, lo, hi):
                    hp_ = h // 2
                    t_ = ARt[hp_] if h % 2 == 0 else ARo[hp_]
                    return t_[0:64, ci * 128 + lo: ci * 128 + hi]
                def Bv(h):
                    hp_ = h // 2
                    return (Bt[hp_] if h % 2 == 0 else Bo[hp_])[0:64, cs]
                def Kv(h):
                    hp_ = h // 2
                    return (Kt[hp_] if h % 2 == 0 else Ko[hp_])[0:64, cs]
                ARk = lambda h: ("ARt%d" if h % 2 == 0 else "ARo%d") % (h // 2)
                Bk = lambda h: ("Bt%d" if h % 2 == 0 else "Bo%d") % (h // 2)
                Kk = lambda h: ("Kt%d" if h % 2 == 0 else "Ko%d") % (h // 2)
                if part == "I":
                    for hp in P2:
                        H = str(hp)
                        for j, (src, sk) in enumerate(((Bt[hp], "Bt" + H), (Kt[hp], "Kt" + H), (Vb[hp], "Vb" + H))):
                            c.op("pe", lambda e, src=src, j=j, hp=hp: e.transpose(
                                trp[0:64, j * 256 + hp * 128: j * 256 + (hp + 1) * 128], src[:, cs], identb[:]),
                                r=[sk, "identb"], w=["bktrp"])
                    c.op("act", lambda e: e.copy(out=tm[:], in_=trp[0:64, 0:768]), r=["bktrp"], w=[tmk])
                    yield
                    dbg(4)
                    for h in range(4):
                        c.op("pe", lambda e: e.matmul(bk["A"][0:64, h * 128:(h + 1) * 128], Bv(h), ARv(h, 0, 128),
                                                      start=True, stop=True), r=[Bk(h), ARk(h)], w=["bkA"])
                        c.op("pe", lambda e: e.matmul(bk["B"][0:64, h * 128:(h + 1) * 128], Kv(h), ARv(h, 0, 128),
                                                      start=True, stop=True), r=[Kk(h), ARk(h)], w=["bkB"])
                        c.op("pe", lambda e: e.matmul(bk["C"][0:64, h * 64:(h + 1) * 64], ARv(h, 0, 64), Bv(h),
                                                      start=True, stop=True), r=[Bk(h), ARk(h)], w=["bkC"])
                    c.op("dve", lambda e: e.tensor_tensor(out=sa[:], in0=bk["A"][0:64, :], in1=maskA[:], op=ALU.mult),
                         r=["bkA", "maskA"], w=[sak])
                    c.op("dve", lambda e: e.tensor_tensor(out=sbb[:], in0=bk["B"][0:64, :], in1=maskA[:], op=ALU.mult),
                         r=["bkB", "maskA"], w=[sbk])
                    c.op("dve", lambda e: e.tensor_tensor(out=sc[:], in0=bk["C"][0:64, 0:256], in1=maskC[:], op=ALU.mult),
                         r=["bkC", "maskC"], w=[sck])
                    yield
                    Nv = lambda h: sa[:, h * 128: h * 128 + 64]
                    NTv = lambda h: sc[:, h * 64:(h + 1) * 64]
                    dbg(5)
                    c.op("pool", lambda e: e.tensor_tensor(
                        out=TT[0][:].rearrange("p (h t) -> p h t", t=64),
                        in0=sa[:].rearrange("p (h x) -> p h x", x=128)[:, :, 0:64],
                        in1=identrep[:].rearrange("p (h t) -> p h t", t=64), op=ALU.add),
                        r=[sak, "identrep"], w=["TT0"])
                    for h in range(4):
                        c.op("pe", lambda e: e.matmul(bk["D"][0:64, h * 64:(h + 1) * 64], NTv(h), Nv(h), start=True, stop=True),
                             r=[sak, sck], w=["bkD"])
                        c.op("pe", lambda e: e.matmul(bk["D"][0:64, 256 + h * 64: 256 + (h + 1) * 64], Nv(h), NTv(h),
                                                      start=True, stop=True), r=[sak, sck], w=["bkD"])
                    c.op("act", lambda e: e.copy(out=PP[0][:], in_=bk["D"][0:64, :]), r=["bkD"], w=["PP0"])
                    yield
                    for lv in range(1, 6):
                        pi = (lv - 1) % 2; po_ = lv % 2
                        Pv = lambda h: PP[pi][:, h * 64:(h + 1) * 64]
                        PTv = lambda h: PP[pi][:, 256 + h * 64: 256 + (h + 1) * 64]
                        TTv = lambda h: TT[pi][:, h * 64:(h + 1) * 64]
                        for h in range(4):
                            c.op("pe", lambda e: e.matmul(bk["E"][0:64, h * 64:(h + 1) * 64], identb[0:64, 0:64], TTv(h),
                                                          start=True, stop=False), r=["identb", "TT%d" % pi], w=["bkE"])
                            c.op("pe", lambda e: e.matmul(bk["E"][0:64, h * 64:(h + 1) * 64], PTv(h), TTv(h),
                                                          start=False, stop=True), r=["PP%d" % pi, "TT%d" % pi], w=["bkE"])
                        if lv < 5:
                            for h in range(4):
                                c.op("pe", lambda e: e.matmul(bk["D"][0:64, h * 64:(h + 1) * 64], PTv(h), Pv(h),
                                                              start=True, stop=True), r=["PP%d" % pi], w=["bkD"])
                                c.op("pe", lambda e: e.matmul(bk["D"][0:64, 256 + h * 64: 256 + (h + 1) * 64], Pv(h), PTv(h),
                                                              start=True, stop=True), r=["PP%d" % pi], w=["bkD"])
                        if lv == 5:
                            c.op("dve", lambda e: e.tensor_copy(TTF[:], bk["E"][0:64, 0:256]), r=["bkE"], w=[TTFk])
                        else:
                            c.op("dve", lambda e: e.tensor_copy(TT[po_][:], bk["E"][0:64, 0:256]), r=["bkE"], w=["TT%d" % po_])
                        if lv < 5:
                            c.op("act", lambda e: e.copy(out=PP[po_][:], in_=bk["D"][0:64, :]), r=["bkD"], w=["PP%d" % po_])
                        yield
                if part == "D":
                    TTf = TTF; TTk = TTFk
                    Mold = [M0b[h][pp] for h in range(4)]; Mnew = [M0b[h][1 - pp] for h in range(4)]
                    Mok = ["M0b%d_%d" % (h, pp) for h in range(4)]; Mnk = ["M0b%d_%d" % (h, 1 - pp) for h in range(4)]
                    Uc = U[pp]; Uk = "U%d" % pp
                    for h in range(4):
                        c.op("pe", lambda e: e.matmul(bk["FG"][0:64, h * 64:(h + 1) * 64], ARv(h, 0, 64), Mold[h][:],
                                                      start=True, stop=False), r=[ARk(h), Mok[h]], w=["bkFG"])
                        c.op("pe", lambda e: e.matmul(bk["FG"][0:64, h * 64:(h + 1) * 64], sbb[:, h * 128: h * 128 + 64],
                                                      tm[:, 512 + h * 64: 512 + (h + 1) * 64], start=False, stop=True),
                             r=[sbk, tmk], w=["bkFG"])
                    c.op("act", lambda e: e.copy(out=W1[:], in_=bk["FG"][0:64, 0:256]), r=["bkFG"], w=["W1"])
                    yield
                    for h in range(4):
                        c.op("pe", lambda e: e.matmul(bk["FG"][0:64, 256 + h * 64: 256 + (h + 1) * 64],
                                                      TTf[:, h * 64:(h + 1) * 64], W1[:, h * 64:(h + 1) * 64],
                                                      start=True, stop=True), r=[TTk, "W1"], w=["bkFG"])
                    c.op("act", lambda e: e.copy(out=Uc[:], in_=bk["FG"][0:64, 256:512]), r=["bkFG"], w=[Uk])
                    yield
                    dbg(7)
                    for h in range(4):
                        hp, base = h // 2, (h % 2) * 64
                        o = bk["HI"][base:base + 64, hp * 64:(hp + 1) * 64]
                        c.op("pe", lambda e: e.matmul(o, Mold[h][:], ARv(h, 64, 128), start=True, stop=False),
                             r=[ARk(h), Mok[h]], w=["bkHI"])
                        c.op("pe", lambda e: e.matmul(o, Uc[:, h * 64:(h + 1) * 64], sa[:, h * 128 + 64:(h + 1) * 128],
                                                      start=False, stop=False), r=[Uk, sak], w=["bkHI"])
                        c.op("pe", lambda e: e.matmul(o, tm[:, 512 + h * 64: 512 + (h + 1) * 64],
                                                      sbb[:, h * 128 + 64:(h + 1) * 128], start=False, stop=True),
                             r=[tmk, sbk], w=["bkHI"])
                    for h in range(4):
                        o = bk["HI"][0:64, 256 + h * 64: 256 + (h + 1) * 64]
                        c.op("pe", lambda e: e.matmul(o, tm[:, 256 + h * 64: 256 + (h + 1) * 64],
                                                      tm[:, 512 + h * 64: 512 + (h + 1) * 64], start=True, stop=False),
                             r=[tmk], w=["bkHI"])
                        c.op("pe", lambda e: e.matmul(o, tm[:, h * 64:(h + 1) * 64], Uc[:, h * 64:(h + 1) * 64],
                                                      start=False, stop=False), r=[tmk, Uk], w=["bkHI"])
                        c.op("pe", lambda e: e.matmul(o, identb[0:64, 0:64], Mold[h][:],
                                                      start=False, stop=True), r=["identb", Mok[h]], w=["bkHI"])
                    yield
                    for hp in P2:
                        H = str(hp)
                        c.op("act", lambda e: e.copy(out=yraw[hp][:, cs], in_=bk["HI"][:, hp * 64:(hp + 1) * 64]),
                             r=["bkHI"], w=["yraw" + H])
                    for h in range(4):
                        hp = h // 2
                        gsrc, gk = (GCs[hp], "GCs%d" % hp) if h % 2 == 0 else (GCo[hp], "GCo%d" % hp)
                        c.op("act", lambda e: e.activation(out=Mnew[h][:], in_=bk["HI"][0:64, 256 + h * 64: 256 + (h + 1) * 64],
                                                           func=AF.Identity, scale=gsrc[0:64, ci:ci + 1]),
                             r=["bkHI", gk], w=[Mnk[h]])

            cc0 = chunk_counter[0]; chunk_counter[0] += CPB

            def drive(gens):
                gens = list(gens)
                while gens:
                    for g_ in list(gens):
                        try:
                            next(g_)
                        except StopIteration:
                            gens.remove(g_)

            drive([emit_chunk(0, cc0, "I")])
            for ci in range(CPB):
                gens = [emit_chunk(ci, cc0 + ci, "D")]
                if ci + 1 < CPB:
                    gens.insert(0, emit_chunk(ci + 1, cc0 + ci + 1, "I"))
                drive(gens)
            dbg(8)
            for hp in P2:
                H = str(hp)
                rows = slice(hp * 128, (hp + 1) * 128)
                c.op("pe", lambda e: e.matmul(bk["A"][:], bo[:], yraw[hp][:], start=True, stop=True),
                     r=["bo", "yraw" + H], w=["bkA"])
                c.op("dve", lambda e: e.scalar_tensor_tensor(out=yc[hp][:], in0=bk["A"][:], scalar=-1.0 / 64,
                                                             in1=yraw[hp][:], op0=ALU.mult, op1=ALU.add),
                     r=["bkA", "yraw" + H], w=["yc" + H])
                c.op("pool", lambda e: e.tensor_tensor(out=ysq[hp][:], in0=yc[hp][:], in1=yc[hp][:], op=ALU.mult),
                     r=["yc" + H], w=["ysq"])
                c.op("pe", lambda e: e.matmul(bk["B"][:], bo[:], ysq[hp][:], start=True, stop=True),
                     r=["bo", "ysq"], w=["bkB"])
                c.op("act", lambda e: e.activation(out=yrs[hp][:], in_=bk["B"][:], func=AF.Sqrt, bias=gneps[:, 0:1],
                                                   scale=1.0 / 64), r=["bkB", "gneps"], w=["yrs"])
                c.op("dve", lambda e: e.reciprocal(out=yrs[hp][:], in_=yrs[hp][:]), r=["yrs"], w=["yrs"])
                c.op("dve", lambda e: e.tensor_tensor(out=yc[hp][:], in0=yc[hp][:], in1=yrs[hp][:], op=ALU.mult),
                     r=["yc" + H, "yrs"], w=["yc" + H])
                c.op("dve", lambda e: e.tensor_scalar(out=yc[hp][:], in0=yc[hp][:], scalar1=pv(18, hp), scalar2=pv(20, hp),
                                                      op0=ALU.mult, op1=ALU.add), r=["yc" + H, "pvec"], w=["yc" + H])
                c.op("pool", lambda e: e.tensor_tensor(out=yc[hp][:], in0=yc[hp][:], in1=bonus[hp][:], op=ALU.add),
                     r=["yc" + H, "bonus" + H], w=["yc" + H])
                c.op("pool", lambda e: e.tensor_tensor(out=yo[hp][:], in0=yc[hp][:], in1=gg[hp][:], op=ALU.mult),
                     r=["yc" + H, "gg" + H], w=["yo" + H])
                c.dma("sp", yrw[rows, t0:t0 + TB], yo[hp][:], r=["yo" + H])
    try:
        body()
    except _Stop:
        pass


def build_L2(layer1):
    nc = bass.Bass("TRN2", target_bir_lowering=False)
    dt = lambda name, shape, kind="ExternalInput": nc.dram_tensor(name, list(shape), F32, kind=kind).ap()
    A = dict(rT=dt("rT", [256, SEQ]), kT=dt("kT", [256, SEQ]), vT=dt("vT", [256, SEQ]),
             waT=dt("waT", [128, SEQ]), gdT=dt("gdT", [128, SEQ]), poolT=dt("poolT", [256, SEQ]))
    if layer1:
        A.update(vdT=dt("vdT", [32, SEQ]), vfT=dt("vfT", [256, SEQ]), vup=dt("vup", [32, 256]))
    A.update(pvec=dt("pvec", [128, 32]), wup=dt("wup", [64, 256]), aup=dt("aup", [64, 256]),
             gup=dt("gup", [128, 256]), poolw=dt("poolw", [2, 128, 128]),
             maskA=dt("maskA", [64, 512]), maskC=dt("maskC", [64, 256]), identrep=dt("identrep", [64, 256]),
             scanmask=dt("scanmask", [128, TB]), blockones=dt("blockones", [128, 128]), ident=dt("ident", [128, 128]),
             invdiv=dt("invdiv", [128, 2, TB]), psel=dt("psel", [128, 2, 4]))
    yT = dt("yT", [512, SEQ], "ExternalOutput")
    A["yrw"] = yT[0:256]; A["ypl"] = yT[256:512]
    if not layer1:
        A["vout"] = dt("vout", [256, SEQ], "ExternalOutput")
    c = Ctx(nc)
    c.begin_phase("")
    emit_L2(c, A, layer1)
    c.end_phase()
    c.finish()
    return nc


def l2_inputs(inp, l, g, projT, vfT):
    f = lambda a: np.ascontiguousarray(a, dtype=np.float32)
    mu = inp["mu_shift"][l]
    cs = slice(g * 256, (g + 1) * 256)
    pvec = np.zeros((128, 32), np.float32)
    def put(col, vec512):
        v = np.asarray(vec512)[cs].reshape(2, 128)
        pvec[:, col] = v[0]; pvec[:, col + 1] = v[1]
    put(0, mu[0:512]); put(2, mu[512:1024]); put(4, mu[1024:1536])
    put(6, inp["w0"][l]); put(8, inp["a0"][l]); put(10, inp["k_k"][l]); put(12, inp["k_a"][l])
    put(16, inp["r_k"][l].reshape(512)); put(18, inp["lnx_g"][l]); put(20, inp["lnx_b"][l])
    put(22, inp["pool_scale"][l])
    pvec[:, 24] = mu[1664:1792]; pvec[0:64, 25] = mu[1536:1600]; pvec[64:128, 25] = mu[1600:1664]
    d = dict(rT=f(projT[0:512][cs]), kT=f(projT[512:1024][cs]), vT=f(projT[1024:1536][cs]),
             waT=f(projT[1536:1664]), gdT=f(projT[1664:1792]), poolT=f(projT[1792:2304][cs]),
             wup=f(inp["w_up"][l][:, cs]), aup=f(inp["a_up"][l][:, cs]), gup=f(inp["g_up"][l][:, cs]),
             poolw=f(inp["pool_w"][l][2 * g:2 * g + 2]))
    if l > 0:
        pvec[0:32, 26] = inp["vres_mu"][l - 1]
        put(27, inp["vres_v0"][l - 1])
        d.update(vdT=f(projT[2304:2336]), vfT=f(vfT), vup=f(inp["vres_up"][l - 1][:, cs]))
    d["pvec"] = pvec
    pos = np.arange(1, TB + 1, dtype=np.float32)
    invdiv = np.stack([np.broadcast_to(1.0 / np.minimum(pos, float(POOL_WINDOWS[2 * g + gi])), (128, TB))
                       for gi in range(2)], axis=1)
    d["invdiv"] = f(invdiv)
    psel = np.zeros((128, 2, 4), np.float32)
    for gi in range(2):
        psel[:, gi, 2 * g + gi] = 1.0
        pvec[:, 29 + gi] = 1.0 / POOL_WINDOWS[2 * g + gi]
    d["psel"] = psel
    d.update(l2_consts())
    return d


EPC = 2048
def emit_L0(c, A, layers, nch):
    u_d, v_d, wo_d, pq_d, id_d = A["u"], A["v"], A["w_out"], A["peer_q"], A["ident"]
    ut_o, vb_o, wo_o, pq_o = A["UT"], A["Vb"], A["woutb"], A["pqb"]
    identf = c.sb("identf", [128, 128]); identb = c.sb("identb", [128, 128], BF16)
    NBUF0 = 4
    uf = [c.sb("uf%d" % i, [128, D]) for i in range(NBUF0)]
    ub = [c.sb("ub%d" % i, [128, D], BF16) for i in range(NBUF0)]
    uT = [c.sb("uT%d" % i, [128, D], BF16) for i in range(NBUF0)]
    vb = [c.sb("vb%d" % i, [128, D], BF16) for i in range(NBUF0)]
    wb = [c.sb("wb%d" % i, [128, 8, 128], BF16) for i in range(NBUF0)]
    tps = [c.ps("tps%d" % i, [128, 1024], BF16) for i in range(NBUF0)]
    c.dma("sp", identf[:], id_d, w=["identf"])
    c.op("dve", lambda e: e.tensor_copy(identb[:], identf[:]), r=["identf"], w=["identb"])
    k = 0
    for l in layers:
        for ch in range(nch):
            s_ = k % NBUF0; k += 1
            S = str(s_)
            rows = slice(ch * 128, (ch + 1) * 128)
            c.dma("sp", uf[s_][:], u_d[l, rows, :], w=["uf" + S])
            c.dma("pool", vb[s_][:], v_d[l, rows, :], w=["vb" + S])
            c.dma("sp", vb_o[l, rows, :], vb[s_][:], r=["vb" + S])
            eng = "act" if s_ % 2 == 0 else "dve"
            if eng == "act":
                c.op("act", lambda e: e.copy(out=ub[s_][:], in_=uf[s_][:]), r=["uf" + S], w=["ub" + S])
            else:
                c.op("dve", lambda e: e.tensor_copy(ub[s_][:], uf[s_][:]), r=["uf" + S], w=["ub" + S])
            for dc in range(8):
                c.op("pe", lambda e: e.transpose(tps[s_][:, dc * 128:(dc + 1) * 128], ub[s_][:, dc * 128:(dc + 1) * 128],
                                                 identb[:]), r=["ub" + S, "identb"], w=["tps" + S])
            if eng == "act":
                c.op("act", lambda e: e.copy(out=uT[s_][:], in_=tps[s_][:]), r=["tps" + S], w=["uT" + S])
            else:
                c.op("dve", lambda e: e.tensor_copy(uT[s_][:], tps[s_][:]), r=["tps" + S], w=["uT" + S])
            c.dma("sp", ut_o[l, ch], uT[s_][:].rearrange("p (dc e) -> p dc e", e=128), r=["uT" + S])
        for cc in range(8):
            s_ = k % NBUF0; k += 1
            S = str(s_)
            c.dma("pool", vb[s_][:], wo_d[l, cc * 128:(cc + 1) * 128, :], w=["vb" + S])
            c.dma("sp", wo_o[l, cc], vb[s_][:], r=["vb" + S])
        pqv = pq_d[l].rearrange("(dc p) j -> p dc j", p=128)
        for jc in range(16):
            s_ = k % NBUF0; k += 1
            S = str(s_)
            c.dma("pool", wb[s_][:], pqv[:, :, jc * 128:(jc + 1) * 128], w=["wb" + S])
            c.dma("sp", pq_o[l, jc], wb[s_][:], r=["wb" + S])


def build_L0():
    nc = bass.Bass("TRN2", target_bir_lowering=False)
    di = lambda name, shape: nc.dram_tensor(name, list(shape), F32, kind="ExternalInput").ap()
    do = lambda name, shape: nc.dram_tensor(name, list(shape), BF16, kind="ExternalOutput").ap()
    A = dict(u=di("u", [2, EPC, D]), v=di("v", [2, EPC, D]), w_out=di("w_out", [2, D, D]),
             peer_q=di("peer_q", [2, D, 2048]), ident=di("ident", [128, 128]),
             UT=do("UT", [2, EPC // 128, 128, 8, 128]), Vb=do("Vb", [2, EPC, D]),
             woutb=do("woutb", [2, 8, 128, D]), pqb=do("pqb", [2, 16, 128, 8, 128]))
    c = Ctx(nc)
    c.begin_phase("")
    emit_L0(c, A, range(2), EPC // 128)
    c.end_phase()
    c.finish()
    return nc


TS = 256
NST = TOK // TS
NEG = -1.0e30


def emit_L3(c, A, final):
    nc = c.nc
    x_d, c_d, adaw_d, adab_d, g_d, lnf_d = A["x"], A["c_fm"], A["ada_w"], A["ada_b_fm"], A["ln_g_fm"], A["lnf_fm"]
    wo_d, pq_d, keys_d, ut_d, vb_d, id_d, ones_d, xo_d = (A["woutb"], A["pqb"], A["keys"], A["UT"], A["Vb"],
                                                          A["ident"], A["ones"], A["xo"])
    ysel = "hsel" in A
    sb = c.sb
    c.adaw_sb = sb("adaw", [128, 8, 256])
    c_sb = sb("c_sb", [128, 8]); sig_c = sb("sig_c", [128, 8]); silu_c = sb("silu_c", [128, 8])
    adab = sb("adab", [128, 48]); lng = sb("lng", [128, 8]); lnf = sb("lnf", [128, 8])
    modT = sb("modT", [128, 32]); effs = sb("effs", [128, 8]); epsb = sb("epsb", [128, 1])
    identf = sb("identf", [128, 128]); identb = sb("identb", [128, 128], BF16); onesf = sb("onesf", [128, 128])
    diag = sb("diag", [128, 512])
    g1bc = sb("g1bc", [128, D]); g2bc = sb("g2bc", [128, D]); lnfbc = sb("lnfbc", [128, D])
    keysT = sb("keysT", [128, 16, 128], BF16)
    wst = [sb("wst%d" % i, [128, D], BF16) for i in range(3)]
    x1 = [sb("x1_%d" % i, [128, D]) for i in range(2)]
    yTb = sb("yTb", [128, 8, TS], BF16)
    if ysel:
        ysa = sb("ysa", [128, 8, TS], BF16); ysb = sb("ysb", [128, 8, TS], BF16); hsel = sb("hsel_sb", [128, 2])
    junk = sb("junk", [128, D], BF16); xn = sb("xn", [128, D], BF16)
    ssq = sb("ssq", [128, 4]); rstd = sb("rstd", [128, 4])
    h2T = sb("h2T", [128, 8, TS], BF16); qT = sb("qT", [128, 16, TS], BF16)
    ST = sb("ST", [128, 16, TS]); SC = sb("SC", [128, 2048])
    kf = SC[:].rearrange("p (a d) -> p a d", d=128)
    wk = [sb("wk%d" % i, [128, 128]) for i in range(2)]
    stop = sb("stop", [128, 16, 16]); candh = [sb("candh%d" % i, [128, 256]) for i in range(2)]
    cwk = [sb("cwk%d" % i, [128, 256]) for i in range(2)]; ctop = sb("ctop", [128, 8, 16])
    negm = sb("negm", [128, 8]); esub = sb("esub", [128, 128]); exs = sb("exs", [128, 128])
    Zs = sb("Zs", [128, 8]); invZ = sb("invZ", [128, 8])
    PACK = sb("PACK", [128, 4, 128]); PKT = sb("PKT", [128, 4, TS])
    rep = [sb("rep%d" % i, [128, 256]) for i in range(3)]
    E0 = [sb("E0_%d" % i, [128, 128], BF16) for i in range(3)]
    D1 = [sb("D1_%d" % i, [128, 128], BF16) for i in range(3)]
    ex1 = [sb("ex1_%d" % i, [128, 128]) for i in range(3)]
    gate = sb("gate_all", [128, 128, TS], BF16)
    ut4 = [sb("ut4_%d" % i, [128, 2, D], BF16) for i in range(3)]
    vt4 = [sb("vt4_%d" % i, [128, 2, D], BF16) for i in range(3)]
    NCH = 2
    ge = [sb("ge%d" % i, [128, TS], BF16) for i in range(2)]
    AT = [sb("AT%d" % i, [128, TS], BF16) for i in range(2)]
    tmpo = sb("tmpo", [128, 512])
    B = [c.ps("b%d" % i, [128, 512]) for i in range(8)]
    Bb = c.ps

    ld = lambda dst, src, key: c.dma("sp", dst, src, w=[key])
    ld(c_sb[:], c_d, "c_sb"); ld(adab[:], adab_d, "adab"); ld(lng[:], g_d, "lng"); ld(lnf[:], lnf_d, "lnf")
    ld(identf[:], id_d, "identf"); ld(onesf[:], ones_d, "onesf")
    if ysel:
        ld(hsel[:], A["hsel"], "hsel")
    ld(kf, keys_d.rearrange("h p k d -> k (h p) d"), "kf")
    c.op("dve", lambda e: e.tensor_copy(identb[:], identf[:]), r=["identf"], w=["identb"])
    c.op("dve", lambda e: e.memset(epsb[:], NORM_EPS), w=["epsb"])
    c.op("act", lambda e: e.activation(out=sig_c[:], in_=c_sb[:], func=AF.Sigmoid), r=["c_sb"], w=["sig_c"])
    c.op("dve", lambda e: e.tensor_tensor(out=silu_c[:], in0=c_sb[:], in1=sig_c[:], op=ALU.mult),
         r=["c_sb", "sig_c"], w=["silu_c"])
    emit_mod_fm(c, adaw_d, 0, 32, silu_c, adab, 16, modT[:, 0:32], B[0], "modT")
    c.op("dve", lambda e: e.scalar_tensor_tensor(out=effs[:], in0=modT[:, 16:24], scalar=1.0, in1=lng[:],
                                                  op0=ALU.add, op1=ALU.mult), r=["modT", "lng"], w=["effs"])

    def bcast_rows(vec8, vkey, out_tile, okey):
        for hf in range(2):
            for q in range(4):
                jc = hf * 4 + q
                c.op("dve", lambda e: e.tensor_scalar(out=diag[:, q * 128:(q + 1) * 128], in0=identf[:],
                                                      scalar1=vec8[:, jc:jc + 1], scalar2=None, op0=ALU.mult),
                     r=["identf", vkey], w=["diag%d" % q])
                c.op("pe", lambda e: e.matmul(B[1][:, q * 128:(q + 1) * 128], onesf[:], diag[:, q * 128:(q + 1) * 128],
                                              start=True, stop=True), r=["onesf", "diag%d" % q], w=["b1"])
            c.op("act", lambda e: e.copy(out=out_tile[:, hf * 512:(hf + 1) * 512], in_=B[1][:]), r=["b1"], w=[okey])

    bcast_rows(modT[:, 0:8], "modT", g1bc, "g1bc")
    bcast_rows(modT[:, 24:32], "modT", g2bc, "g2bc")
    if final:
        bcast_rows(lnf, "lnf", lnfbc, "lnfbc")
    for q4 in range(4):
        for q in range(4):
            hp16 = q4 * 4 + q
            c.op("pe", lambda e: e.transpose(B[2][:, q * 128:(q + 1) * 128], kf[:, hp16, :], identf[:]),
                 r=["kf", "identf"], w=["b2"])
        c.op("act", lambda e: e.copy(out=keysT[:, q4 * 4:(q4 + 1) * 4, :],
                                     in_=B[2][:].rearrange("p (q k) -> p q k", k=128)), r=["b2"], w=["keysT"])
    wsi = [0]

    def wstream(src_ap):
        i = wsi[0] % 3; wsi[0] += 1
        c.dma("sp", wst[i][:], src_ap, w=["wst%d" % i])
        return wst[i], "wst%d" % i

    def body():
        dbg(1)
        for st in range(DBG_NST or NST):
            tok0 = st * TS
            def load_y(tk0):
                if not ysel:
                    c.dma("pool", yTb[:], A["yT"].rearrange("(cc p) t -> p cc t", p=128)[:, :, tk0:tk0 + TS], w=["yTb"])
                else:
                    c.dma("pool", ysa[:], A["yA"].rearrange("(cc p) t -> p cc t", p=128)[:, :, tk0:tk0 + TS], w=["ysa"])
                    c.dma("pool", ysb[:], A["yB"].rearrange("(cc p) t -> p cc t", p=128)[:, :, tk0:tk0 + TS], w=["ysb"])
            if st == 0 or not ysel:
                load_y(tok0)
            if ysel:
                c.op("dve", lambda e: e.tensor_scalar(out=ysa[:], in0=ysa[:], scalar1=hsel[:, 0:1], scalar2=None,
                                                      op0=ALU.mult), r=["ysa", "hsel"], w=["ysa"])
                c.op("dve", lambda e: e.scalar_tensor_tensor(out=yTb[:], in0=ysb[:], scalar=hsel[:, 1:2], in1=ysa[:],
                                                             op0=ALU.mult, op1=ALU.add),
                     r=["ysa", "ysb", "hsel"], w=["yTb"])
                if st + 1 < (DBG_NST or NST):
                    load_y(tok0 + TS)
            for tt in range(2):
                c.dma("sp", x1[tt][:], x_d[tok0 + tt * 128: tok0 + (tt + 1) * 128, :], w=["x1_%d" % tt])
            for cc in range(8):
                wt, wkk = wstream(wo_d[cc])
                for tt in range(2):
                    for hf in range(2):
                        c.op("pe", lambda e: e.matmul(B[tt * 2 + hf][:], yTb[:, cc, tt * 128:(tt + 1) * 128],
                                                      wt[:, hf * 512:(hf + 1) * 512], start=(cc == 0), stop=(cc == 7)),
                             r=["yTb", wkk], w=["b%d" % (tt * 2 + hf)])
            for tt in range(2):
                X = x1[tt]; Xk = "x1_%d" % tt
                for hf in range(2):
                    c.op("dve", lambda e: e.tensor_tensor(out=tmpo[:], in0=B[tt * 2 + hf][:], in1=g1bc[:, hf * 512:(hf + 1) * 512],
                                                          op=ALU.mult), r=["b%d" % (tt * 2 + hf), "g1bc"], w=["tmpo"])
                    c.op("dve", lambda e: e.tensor_tensor(out=X[:, hf * 512:(hf + 1) * 512], in0=tmpo[:],
                                                          in1=X[:, hf * 512:(hf + 1) * 512], op=ALU.add),
                         r=["tmpo", Xk], w=[Xk])
                c.op("dve", lambda e: e.memset(ssq[:, tt:tt + 1], 0.0), w=["ssq"])
                c.op("act", lambda e: e.activation(out=junk[:], in_=X[:], func=AF.Square, accum_out=ssq[:, tt:tt + 1]),
                     r=[Xk, "ssq"], w=["junk", "ssq"])
                c.op("act", lambda e: e.activation(out=rstd[:, tt:tt + 1], in_=ssq[:, tt:tt + 1], func=AF.Sqrt,
                                                   bias=epsb[:, 0:1], scale=1.0 / D), r=["ssq", "epsb"], w=["rstd"])
                c.op("dve", lambda e: e.reciprocal(out=rstd[:, tt:tt + 1], in_=rstd[:, tt:tt + 1]), r=["rstd"], w=["rstd"])
                c.op("dve", lambda e: e.tensor_scalar(out=xn[:], in0=X[:], scalar1=rstd[:, tt:tt + 1], scalar2=None,
                                                      op0=ALU.mult), r=[Xk, "rstd"], w=["xn"])
                tpb = B[4 + tt][:].bitcast(BF16)
                for dc in range(8):
                    c.op("pe", lambda e: e.transpose(tpb[:, dc * 128:(dc + 1) * 128], xn[:, dc * 128:(dc + 1) * 128],
                                                     identb[:]), r=["xn", "identb"], w=["b%d" % (4 + tt)])
                for dc in range(8):
                    c.op("act", lambda e: e.activation(out=h2T[:, dc, tt * 128:(tt + 1) * 128],
                                                       in_=tpb[:, dc * 128:(dc + 1) * 128], func=AF.Identity,
                                                       bias=modT[:, 8 + dc: 9 + dc], scale=effs[:, dc:dc + 1]),
                         r=["b%d" % (4 + tt), "modT", "effs"], w=["h2T"])
            dbg(2)
            for hp16 in range(16):
                wt, wkk = wstream(pq_d[hp16].rearrange("p dc j -> p (dc j)"))
                bi = 4 + (hp16 // 2) % 2
                sub = hp16 % 2
                for dc in range(8):
                    c.op("pe", lambda e: e.matmul(B[bi][:, sub * TS:(sub + 1) * TS], wt[:, dc * 128:(dc + 1) * 128],
                                                  h2T[:, dc, :], start=(dc == 0), stop=(dc == 7)),
                         r=[wkk, "h2T"], w=["b%d" % bi])
                if sub == 1:
                    c.op("act", lambda e: e.copy(out=qT[:, hp16 - 1: hp16 + 1, :],
                                                 in_=B[bi][:].rearrange("p (s t) -> p s t", t=TS)),
                         r=["b%d" % bi], w=["qT"])
            for hp16 in range(16):
                bi = 6 + (hp16 // 2) % 2
                sub = hp16 % 2
                c.op("pe", lambda e: e.matmul(B[bi][:, sub * TS:(sub + 1) * TS], keysT[:, hp16, :], qT[:, hp16, :],
                                              start=True, stop=True), r=["keysT", "qT"], w=["b%d" % bi])
                if sub == 1:
                    c.op("dve", lambda e: e.tensor_copy(ST[:, hp16 - 1: hp16 + 1, :],
                                                        B[bi][:].rearrange("p (s t) -> p s t", t=TS)),
                         r=["b%d" % bi], w=["ST"])
            dbg(3)
            stop4 = stop[:].rearrange("p (h two) a -> p h two a", two=2)
            for tt in range(2):
                for q4 in range(4):
                    for q in range(4):
                        hp16 = q4 * 4 + q
                        c.op("pe", lambda e: e.transpose(B[q4][:, q * 128:(q + 1) * 128],
                                                         ST[:, hp16, tt * 128:(tt + 1) * 128], identf[:]),
                             r=["ST", "identf"], w=["b%d" % q4])
                    c.op("act", lambda e: e.copy(out=SC[:, q4 * 512:(q4 + 1) * 512], in_=B[q4][:]),
                         r=["b%d" % q4], w=["SC%d" % q4, "kf"])
                for hp16 in range(16):
                    w_ = wk[hp16 % 2]; wkk = "wk%d" % (hp16 % 2)
                    scv = SC[:, hp16 * 128:(hp16 + 1) * 128]; sck = "SC%d" % (hp16 // 4)
                    c.op("dve", lambda e: e.max(out=stop[:, hp16, 0:8], in_=scv), r=[sck], w=["stopA%d" % hp16])
                    c.op("dve", lambda e: e.match_replace(out=w_[:], in_to_replace=stop[:, hp16, 0:8], in_values=scv,
                                                          imm_value=NEG), r=[sck, "stopA%d" % hp16], w=[wkk])
                    c.op("dve", lambda e: e.max(out=stop[:, hp16, 8:16], in_=w_[:]), r=[wkk], w=["stopB%d" % hp16])
                stopkeys = ["stopA%d" % i for i in range(16)] + ["stopB%d" % i for i in range(16)]
                for h in range(8):
                    ch_ = candh[h % 2]; chk = "candh%d" % (h % 2)
                    cw_ = cwk[h % 2]; cwkk = "cwk%d" % (h % 2)
                    c.op("dve", lambda e: e.tensor_tensor(
                        out=ch_[:].rearrange("p (a b) -> p a b", b=16),
                        in0=stop4[:, h, 0, :].unsqueeze(2).broadcast_to([128, 16, 16]),
                        in1=stop4[:, h, 1, :].unsqueeze(1).broadcast_to([128, 16, 16]), op=ALU.add),
                        r=["stopA%d" % (2 * h), "stopB%d" % (2 * h), "stopA%d" % (2 * h + 1), "stopB%d" % (2 * h + 1)],
                        w=[chk])
                    c.op("dve", lambda e: e.max(out=ctop[:, h, 0:8], in_=ch_[:]), r=[chk], w=["ctopA%d" % h])
                    c.op("dve", lambda e: e.match_replace(out=cw_[:], in_to_replace=ctop[:, h, 0:8], in_values=ch_[:],
                                                          imm_value=NEG), r=[chk, "ctopA%d" % h], w=[cwkk])
                    c.op("dve", lambda e: e.max(out=ctop[:, h, 8:16], in_=cw_[:]), r=[cwkk], w=["ctopB%d" % h])
                ctk = ["ctopA%d" % h for h in range(8)] + ["ctopB%d" % h for h in range(8)]
                c.op("dve", lambda e: e.tensor_scalar(out=negm[:], in0=ctop[:, :, 0], scalar1=-1.0, scalar2=None,
                                                      op0=ALU.mult), r=ctk, w=["negm"])
                c.op("dve", lambda e: e.tensor_tensor(out=esub[:].rearrange("p (h a) -> p h a", a=16), in0=ctop[:],
                                                      in1=negm[:].unsqueeze(2).broadcast_to([128, 8, 16]), op=ALU.add),
                     r=ctk + ["negm"], w=["esub"])
                c.op("act", lambda e: e.activation(out=exs[:], in_=esub[:], func=AF.Exp), r=["esub"], w=["exs"])
                c.op("dve", lambda e: e.reduce_sum(out=Zs[:], in_=exs[:].rearrange("p (h a) -> p h a", a=16),
                                                   axis=mybir.AxisListType.X), r=["exs"], w=["Zs"])
                c.op("dve", lambda e: e.reciprocal(out=invZ[:], in_=Zs[:]), r=["Zs"], w=["invZ"])
                P3 = lambda j: PACK[:, j, :].rearrange("p (h a) -> p h a", a=16)
                c.op("dve", lambda e: e.tensor_copy(P3(0), stop4[:, :, 0, :]), r=stopkeys, w=["PACK0"])
                c.op("dve", lambda e: e.tensor_tensor(out=P3(1), in0=ctop[:, :, 15].unsqueeze(2).broadcast_to([128, 8, 16]),
                                                      in1=stop4[:, :, 0, :], op=ALU.subtract), r=stopkeys + ctk, w=["PACK1"])
                c.op("dve", lambda e: e.tensor_tensor(out=P3(2), in0=stop4[:, :, 0, :],
                                                      in1=negm[:].unsqueeze(2).broadcast_to([128, 8, 16]), op=ALU.add),
                     r=stopkeys + ["negm"], w=["PACK2"])
                c.op("dve", lambda e: e.tensor_copy(P3(3), invZ[:].unsqueeze(2).broadcast_to([128, 8, 16])),
                     r=["invZ"], w=["PACK3"])
                for j in range(4):
                    c.op("pe", lambda e: e.transpose(B[4][:, j * 128:(j + 1) * 128], PACK[:, j, :], identf[:]),
                         r=["PACK%d" % j, "identf"], w=["b4"])
                c.op("act", lambda e: e.copy(out=PKT[:, :, tt * 128:(tt + 1) * 128],
                                             in_=B[4][:].rearrange("p (j t) -> p j t", t=128)), r=["b4"], w=["PKT"])
            dbg(4)
            def s6A(t):
                s_ = t % 3; S = str(s_)
                c.op("pool", lambda e: e.tensor_copy(
                    rep[s_][:].rearrange("p (two h a) -> p two h a", two=2, a=16),
                    ST[:, :, t].rearrange("p (h two) -> p two h", two=2).unsqueeze(3).broadcast_to([128, 2, 8, 16])),
                    r=["ST"], w=["rep" + S])
                c.op("pe", lambda e: e.transpose(B[0 + s_][:, 0:128], rep[s_][:, 0:128], identf[:]),
                     r=["rep" + S, "identf"], w=["b%d" % (0 + s_)])
                c.op("pe", lambda e: e.transpose(B[3 + s_][:, 0:128], rep[s_][:, 128:256], identf[:]),
                     r=["rep" + S, "identf"], w=["b%d" % (3 + s_)])

            def s6B(t):
                s_ = t % 3; S = str(s_)
                c.op("dve", lambda e: e.tensor_scalar(out=E0[s_][:], in0=B[0 + s_][:, 0:128], scalar1=PKT[:, 0, t:t + 1],
                                                      scalar2=PKT[:, 3, t:t + 1], op0=ALU.is_equal, op1=ALU.mult),
                     r=["b%d" % (0 + s_), "PKT"], w=["E0_" + S])
                c.op("act", lambda e: e.activation(out=ex1[s_][:], in_=B[3 + s_][:, 0:128], func=AF.Exp,
                                                   bias=PKT[:, 2, t:t + 1]), r=["b%d" % (3 + s_), "PKT"], w=["ex1_" + S])
                c.op("dve", lambda e: e.scalar_tensor_tensor(out=D1[s_][:], in0=B[3 + s_][:, 0:128],
                                                             scalar=PKT[:, 1, t:t + 1], in1=ex1[s_][:],
                                                             op0=ALU.is_ge, op1=ALU.mult),
                     r=["b%d" % (3 + s_), "PKT", "ex1_" + S], w=["D1_" + S])

            def s6C(t):
                s_ = t % 3; S = str(s_)
                gs = (t // 4) % 2
                c.op("pe", lambda e: e.matmul(B[6 + gs][:, (t % 4) * 128:(t % 4 + 1) * 128], D1[s_][:], E0[s_][:],
                                              start=True, stop=True), r=["D1_" + S, "E0_" + S], w=["b%d" % (6 + gs)])
                if t % 4 == 3:
                    c.op("act", lambda e: e.copy(out=gate[:, :, t - 3:t + 1].rearrange("p i t -> p t i"),
                                                 in_=B[6 + gs][:].rearrange("p (t i) -> p t i", i=128)),
                         r=["b%d" % (6 + gs)], w=["gate"])

            s6A(0)
            s6A(1)
            for t in range(TS):
                if t + 2 < TS:
                    s6A(t + 2)
                s6B(t)
                s6C(t)
            dbg(5)
            def s7U(i0):
                g4, cix = i0 // NCH, i0 % NCH
                bf_ = g4 % 3; Bf = str(bf_)
                if cix == 0:
                    c.dma("sp", ut4[bf_][:], ut_d[g4 * NCH:(g4 + 1) * NCH].rearrange("c p dc e -> p c (dc e)"),
                          w=["ut4_" + Bf])
                    c.dma("sp", vt4[bf_][:],
                          vb_d[g4 * NCH * 128:(g4 + 1) * NCH * 128, :].rearrange("(c p) d -> p c d", p=128),
                          w=["vt4_" + Bf])
                pb = 4 + i0 % 4
                for dc in range(8):
                    c.op("pe", lambda e: e.matmul(B[pb][:, 0:TS], ut4[bf_][:, cix, dc * 128:(dc + 1) * 128],
                                                  h2T[:, dc, :], start=(dc == 0), stop=(dc == 7)),
                         r=["ut4_" + Bf, "h2T"], w=["b%d" % pb])

            def s7V(i0):
                g4, cix = i0 // NCH, i0 % NCH
                bf_ = g4 % 3; Bf = str(bf_)
                pb = 4 + i0 % 4
                gb = i0 % 2
                c.op("act", lambda e: e.activation(out=ge[gb][:], in_=B[pb][:, 0:TS], func=AF.Gelu),
                     r=["b%d" % pb], w=["ge%d" % gb])
                c.op("pool", lambda e: e.tensor_tensor(out=AT[gb][:], in0=ge[gb][:], in1=gate[:, i0, :], op=ALU.mult),
                     r=["ge%d" % gb, "gate"], w=["AT%d" % gb])
                for tt in range(2):
                    for hf in range(2):
                        c.op("pe", lambda e: e.matmul(B[tt * 2 + hf][:], AT[gb][:, tt * 128:(tt + 1) * 128],
                                                      vt4[bf_][:, cix, hf * 512:(hf + 1) * 512],
                                                      start=(i0 == 0), stop=(i0 == 127)),
                             r=["AT%d" % gb, "vt4_" + Bf], w=["b%d" % (tt * 2 + hf)])

            s7U(0)
            s7U(1)
            for i0 in range(128):
                if i0 + 2 < 128:
                    s7U(i0 + 2)
                s7V(i0)
            dbg(6)
            for tt in range(2):
                X = x1[tt]; Xk = "x1_%d" % tt
                for hf in range(2):
                    c.op("dve", lambda e: e.tensor_tensor(out=tmpo[:], in0=B[tt * 2 + hf][:],
                                                          in1=g2bc[:, hf * 512:(hf + 1) * 512], op=ALU.mult),
                         r=["b%d" % (tt * 2 + hf), "g2bc"], w=["tmpo"])
                    c.op("dve", lambda e: e.tensor_tensor(out=X[:, hf * 512:(hf + 1) * 512], in0=tmpo[:],
                                                          in1=X[:, hf * 512:(hf + 1) * 512], op=ALU.add),
                         r=["tmpo", Xk], w=[Xk])
                if final:
                    c.op("dve", lambda e: e.memset(ssq[:, 2 + tt:3 + tt], 0.0), w=["ssq"])
                    c.op("act", lambda e: e.activation(out=junk[:], in_=X[:], func=AF.Square,
                                                       accum_out=ssq[:, 2 + tt:3 + tt]), r=[Xk, "ssq"], w=["junk", "ssq"])
                    c.op("act", lambda e: e.activation(out=rstd[:, 2 + tt:3 + tt], in_=ssq[:, 2 + tt:3 + tt], func=AF.Sqrt,
                                                       bias=epsb[:, 0:1], scale=1.0 / D), r=["ssq", "epsb"], w=["rstd"])
                    c.op("dve", lambda e: e.reciprocal(out=rstd[:, 2 + tt:3 + tt], in_=rstd[:, 2 + tt:3 + tt]),
                         r=["rstd"], w=["rstd"])
                    c.op("dve", lambda e: e.scalar_tensor_tensor(out=X[:], in0=X[:], scalar=rstd[:, 2 + tt:3 + tt],
                                                                 in1=lnfbc[:], op0=ALU.mult, op1=ALU.mult),
                         r=[Xk, "rstd", "lnfbc"], w=[Xk])
                c.dma("sp", xo_d[tok0 + tt * 128: tok0 + (tt + 1) * 128, :], X[:], r=[Xk])

    try:
        body()
    except _Stop:
        pass


def build_L3(final):
    nc = bass.Bass("TRN2", target_bir_lowering=False)
    dt = lambda name, shape, d=F32, kind="ExternalInput": nc.dram_tensor(name, list(shape), d, kind=kind).ap()
    yT_ = dt("yT", [D, TOK])
    A = dict(x=dt("x", [TOK, D]), yA=yT_, yB=yT_, hsel=dt("hsel", [128, 2]), c_fm=dt("c_fm", [128, 8]), ada_w=dt("ada_w", [D, 4 * D]),
             ada_b_fm=dt("ada_b_fm", [128, 48]), ln_g_fm=dt("ln_g_fm", [128, 8]), lnf_fm=dt("lnf_fm", [128, 8]),
             woutb=dt("woutb", [8, 128, D], BF16), pqb=dt("pqb", [16, 128, 8, 128], BF16),
             keys=dt("keys", [8, 2, 128, 128]), UT=dt("UT", [128, 128, 8, 128], BF16), Vb=dt("Vb", [128 * 128, D], BF16),
             ident=dt("ident", [128, 128]), ones=dt("ones", [128, 128]),
             xo=dt("xo", [TOK, D], F32, "ExternalOutput"))
    c = Ctx(nc)
    c.begin_phase("")
    emit_L3(c, A, final)
    c.end_phase()
    c.finish()
    return nc


NJC = 11
NPC = 1280


def build_fused():
    nc = bass.Bass("TRN2", target_bir_lowering=False)
    di = lambda name, shape, d=F32: nc.dram_tensor(name, list(shape), d, kind="ExternalInput").ap()
    it = lambda name, shape, d=F32: nc.dram_tensor(name, list(shape), d).ap()
    x_seq = di("x_seq", [SEQ, D]); x_mine = di("x_mine", [TOK, D]); c_fm = di("c_fm", [128, 8]); hsel = di("hsel", [128, 2])
    ada_w = di("ada_w", [2, D, 6 * D]); ada_b_fm = di("ada_b_fm", [2, 128, 48])
    ln1 = di("ln1_g_fm", [2, 128, 8]); ln2 = di("ln2_g_fm", [2, 128, 8]); lnf = di("lnf_fm", [128, 8])
    w_in = di("w_in", [2, D, NPC]); vres_down = di("vres_down", [D, 32])
    pvec = di("pvec", [2, 128, 32]); psel = di("psel", [128, 2, 4]); invdiv = di("invdiv", [128, 2, TB])
    w_up = di("w_up", [2, 64, 256]); a_up = di("a_up", [2, 64, 256]); g_up = di("g_up", [2, 128, 256])
    vres_up = di("vres_up", [32, 256]); pool_w = di("pool_w", [2, 2, 128, 128])
    cst = {k: di(k, v.shape) for k, v in l2_consts().items()}
    ones = di("ones", [128, 128])
    nex = 128 if DBG_SKIP0 else 128 * 128
    peer_u = di("peer_u", [2, nex, D]); peer_v = di("peer_v", [2, nex, D])
    w_out = di("w_out", [2, D, D]); peer_q = di("peer_q", [2, D, 2048]); keys = di("peer_keys", [2, 8, 2, 128, 128])
    xo = nc.dram_tensor("xo", [TOK, D], F32, kind="ExternalOutput").ap()
    UT = it("UT_s", [2, 128, 128, 8, 128], BF16); Vb = it("Vb_s", [2, 128 * 128, D], BF16)
    woutb = it("woutb_s", [2, 8, 128, D], BF16); pqb = it("pqb_s", [2, 16, 128, 8, 128], BF16)
    P = it("P_s", [NJC * 128, SEQ]); Yl = it("Yl_s", [512, SEQ]); Ya = it("Ya_s", [D, SEQ]); VF = it("VF_s", [256, SEQ])
    XO0 = it("XO0_s", [TOK, D]); X1 = it("X1_s", [SEQ, D])
    PAIRS = [[0, 1], [2, 3], [4, 5], [6, 7]]

    c = Ctx(nc)
    ph = [0]

    def phase(fn):
        if DBG_PHASES is not None and ph[0] >= DBG_PHASES:
            ph[0] += 1
            return
        c.begin_phase("p%d_" % ph[0]); ph[0] += 1
        fn()
        c.end_phase()

    if DBG_SKIP0:
        ph[0] += 1
    else:
        phase(lambda: emit_L0(c, dict(u=peer_u, v=peer_v, w_out=w_out, peer_q=peer_q, ident=cst["ident"],
                                       UT=UT, Vb=Vb, woutb=woutb, pqb=pqb), range(2), 128))
    for l in range(2):
        xsrc = x_seq if l == 0 else X1
        ncol = NPC if l == 0 else NPC + 32
        njc = (ncol + 127) // 128
        w_parts = [(w_in[l], 0, NPC)] + ([(vres_down, NPC, 32)] if l > 0 else [])
        for hf in range(2):
            xt_fn = {}
            if l > 0:
                xt_fn = dict(x_tile=lambda i, hf=hf: X1[((i // 4) * 2 + hf) * 512 + (i % 4) * 128:
                                                        ((i // 4) * 2 + hf) * 512 + (i % 4) * 128 + 128, :])
            phase(lambda: emit_L1(c, dict(xt_fn, x=xsrc[hf * TOK:(hf + 1) * TOK], c_fm=c_fm, ada_w=ada_w[l][:, 0:2 * D],
                                           ada_b_fm=ada_b_fm[l], ln_g_fm=ln1[l], w_parts=w_parts, ident=cst["ident"],
                                           out=P[0:njc * 128, hf * TOK:(hf + 1) * TOK], ncol=ncol)))
        A = dict(rT=P[0:256], kT=P[256:512], vT=P[512:768], waT=P[768:896], gdT=P[896:1024], poolT=P[1024:1280],
                 pvec=pvec[l], wup=w_up[l], aup=a_up[l], gup=g_up[l], poolw=pool_w[l], psel=psel, invdiv=invdiv,
                 yrw=Yl[0:256], ypl=Yl[256:512])
        A.update(cst)
        if l == 0:
            A["vout"] = VF
        else:
            A.update(vdT=P[1280:1312], vfT=VF, vup=vres_up)
        phase(lambda: emit_L2(c, A, l > 0))
        if DBG_PHASES is None or DBG_PHASES > ph[0]:
            for k in range(4):
                c.collective(lambda gq: gq.collective_compute(
                    "AllGather", ALU.bypass, replica_groups=PAIRS,
                    ins=[Yl[k * 128:(k + 1) * 128].opt()], outs=[Ya[k * 256:(k + 1) * 256].opt()]))
        phase(lambda: emit_L3(c, dict(x=(x_mine if l == 0 else XO0), yA=Ya[:, 0:TOK], yB=Ya[:, TOK:2 * TOK], hsel=hsel,
                                       c_fm=c_fm, ada_w=ada_w[l][:, 2 * D:6 * D], ada_b_fm=ada_b_fm[l], ln_g_fm=ln2[l],
                                       lnf_fm=lnf, woutb=woutb[l], pqb=pqb[l], keys=keys[l], UT=UT[l], Vb=Vb[l],
                                       ident=cst["ident"], ones=ones, xo=(XO0 if l == 0 else xo)), l == 1))
        if l == 0 and DBG_COLL and (DBG_PHASES is None or DBG_PHASES > ph[0]):
            for k in range(4):
                c.collective(lambda gq: gq.collective_compute(
                    "AllGather", ALU.bypass, replica_groups=PAIRS,
                    ins=[XO0[k * 512:(k + 1) * 512].opt()], outs=[X1[k * 1024:(k + 1) * 1024].opt()]))
    c.finish()
    return nc


def kernel_fused(inp):
    f = lambda a: np.ascontiguousarray(a, dtype=np.float32)
    cst = l2_consts()
    blocks = []
    for k in range(4):
        for r in range(2):
            blocks.append((r * 256 + k * 128) if k < 2 else (512 + r * 256 + (k - 2) * 128))
    perm = np.concatenate([np.arange(b0, b0 + 128) for b0 in blocks])
    shared = dict(ada_w=f(inp["ada_w"]), ada_b_fm=f(np.stack([_fm(inp["ada_b"][l]) for l in range(2)])),
                  ln1_g_fm=f(np.stack([_fm(inp["ln1_g"][l]) for l in range(2)])),
                  ln2_g_fm=f(np.stack([_fm(inp["ln2_g"][l]) for l in range(2)])), lnf_fm=_fm(inp["lnf_g"]),
                  vres_down=f(inp["vres_down"][0]),
                  ones=np.ones((128, 128), np.float32), peer_u=f(inp["peer_u"]), peer_v=f(inp["peer_v"]),
                  w_out=f(inp["w_out"][:, perm, :]), peer_q=f(inp["peer_q"]), peer_keys=f(inp["peer_keys"]))
    shared.update(cst)
    dummyT = np.zeros((2336, 1), np.float32)
    per_g = []
    for g in range(2):
        cs = slice(g * 256, (g + 1) * 256)
        cols = np.concatenate([np.arange(g * 256, (g + 1) * 256), 512 + np.arange(g * 256, (g + 1) * 256),
                               1024 + np.arange(g * 256, (g + 1) * 256), np.arange(1536, 1792),
                               1792 + np.arange(g * 256, (g + 1) * 256)])
        d0 = l2_inputs(inp, 0, g, dummyT, None)
        d1 = l2_inputs(inp, 1, g, dummyT, np.zeros((1, 1), np.float32))
        per_g.append(dict(w_in=f(inp["w_in"][:, :, cols]), pvec=f(np.stack([d0["pvec"], d1["pvec"]])),
                          psel=d0["psel"], invdiv=d0["invdiv"], w_up=f(inp["w_up"][:, :, cs]),
                          a_up=f(inp["a_up"][:, :, cs]), g_up=f(inp["g_up"][:, :, cs]),
                          vres_up=f(inp["vres_up"][0][:, cs]), pool_w=f(inp["pool_w"][:, 2 * g:2 * g + 2])))
    x = f(inp["x"])
    in_maps = []
    for core in range(NCORE):
        b, g = core // 2, core % 2
        hs = np.zeros((128, 2), np.float32); hs[:, g] = 1.0
        m = dict(shared)
        m.update(per_g[g])
        m.update(x_seq=f(x[b]), x_mine=f(x[b, g * TOK:(g + 1) * TOK]), c_fm=_fm(inp["c"][b]), hsel=hs)
        in_maps.append(m)
    if inp.get("_only_maps") is not None:
        return in_maps
    res = _run(_prog("fused", build_fused), in_maps)
    out = np.stack([np.asarray(res[core]["xo"]) for core in range(NCORE)], axis=0)
    return np.ascontiguousarray(out.reshape(NB, SEQ, D).astype(np.float32))


_PROGS = {}


def _prog(key, fn):
    if key not in _PROGS:
        _PROGS[key] = fn()
    return _PROGS[key]


def _run(nc, in_maps):
    res = run_bass_kernel_spmd(nc, in_maps, core_ids=list(range(NCORE)))
    return res.results


FUSED = True


def kernel(**inputs):
    inp = {k: np.asarray(v) for k, v in inputs.items()}
    if FUSED:
        return kernel_fused(inp)
    f = lambda a: np.ascontiguousarray(a, dtype=np.float32)
    ident = np.eye(128, dtype=np.float32); ones = np.ones((128, 128), np.float32)
    in_maps = []
    for core in range(NCORE):
        sl = slice(core * EPC, (core + 1) * EPC)
        in_maps.append(dict(u=f(inp["peer_u"][:, sl]), v=f(inp["peer_v"][:, sl]), w_out=f(inp["w_out"]),
                            peer_q=f(inp["peer_q"]), ident=ident))
    r0 = _run(_prog("L0", build_L0), in_maps)
    UT = [np.ascontiguousarray(np.concatenate([np.asarray(r0[c_]["UT"])[l] for c_ in range(NCORE)], axis=0)) for l in range(2)]
    Vb = [np.ascontiguousarray(np.concatenate([np.asarray(r0[c_]["Vb"])[l] for c_ in range(NCORE)], axis=0)) for l in range(2)]
    woutb = [np.ascontiguousarray(np.asarray(r0[0]["woutb"])[l]) for l in range(2)]
    pqb = [np.ascontiguousarray(np.asarray(r0[0]["pqb"])[l]) for l in range(2)]
    del r0
    x = f(inp["x"]).reshape(NCORE, TOK, D)
    vfirst = None
    for l in range(2):
        wfull = inp["w_in"][l] if l == 0 else np.concatenate([inp["w_in"][l], inp["vres_down"][l - 1]], axis=1)
        ncol = wfull.shape[1]
        adab_fm = _fm(inp["ada_b"][l])
        in_maps = []
        for core in range(NCORE):
            b = core // 2
            in_maps.append(dict(x=f(x[core]), c_fm=_fm(inp["c"][b]), ada_w=f(inp["ada_w"][l][:, 0:2 * D]),
                                ada_b_fm=adab_fm, ln_g_fm=_fm(inp["ln1_g"][l]), w_full=f(wfull), ident=ident))
        r1 = _run(_prog(("L1", ncol), lambda: build_L1(ncol)), in_maps)
        in_maps = []
        for core in range(NCORE):
            b, g = core // 2, core % 2
            projT = np.concatenate([r1[2 * b]["projT"], r1[2 * b + 1]["projT"]], axis=1)
            in_maps.append(l2_inputs(inp, l, g, projT, None if l == 0 else vfirst[core]))
        del r1
        r2 = _run(_prog(("L2", l > 0), lambda: build_L2(l > 0)), in_maps)
        if l == 0:
            vfirst = [np.asarray(r2[core]["vout"]) for core in range(NCORE)]
        in_maps = []
        for core in range(NCORE):
            b, hf = core // 2, core % 2
            ts = slice(hf * TOK, (hf + 1) * TOK)
            y0, y1 = r2[2 * b]["yT"], r2[2 * b + 1]["yT"]
            yT = np.concatenate([y0[0:256, ts], y1[0:256, ts], y0[256:512, ts], y1[256:512, ts]], axis=0)
            in_maps.append(dict(x=f(x[core]), yT=f(yT), c_fm=_fm(inp["c"][b]), ada_w=f(inp["ada_w"][l][:, 2 * D:6 * D]),
                                ada_b_fm=adab_fm, ln_g_fm=_fm(inp["ln2_g"][l]), lnf_fm=_fm(inp["lnf_g"]),
                                woutb=woutb[l], pqb=pqb[l], keys=f(inp["peer_keys"][l]), UT=UT[l], Vb=Vb[l],
                                ident=ident, ones=ones))
        del r2
        r3 = _run(_prog(("L3", l == 1), lambda: build_L3(l == 1)), in_maps)
        x = np.stack([np.asarray(r3[core]["xo"]) for core in range(NCORE)], axis=0)
        del r3
    return np.ascontiguousarray(x.reshape(NB, SEQ, D).astype(np.float32))
```
